# Optimizing a Trainium2 kernel written in Bass

```python
import jax, jax.numpy as jnp
from jax import lax
import numpy as np

D_MODEL = 2048
BATCH = 4
SEQ = 4096
DEPTH = 2

CHUNK = 64
Q_BLOCK = 128
N_BRANCHES = 4
BRANCH_WIDTH = D_MODEL // N_BRANCHES
POOL_WINDOWS = (2, 4, 8, 16)
POOL_GROUP = BRANCH_WIDTH // len(POOL_WINDOWS)
DSA_HEADS = 4
DSA_HEAD_DIM = BRANCH_WIDTH // DSA_HEADS
IDX_HEADS = 16
IDX_DIM = 64
TOPK_MAX = 256
MLA_HEADS = 4
MLA_NOPE = 128
MLA_ROPE = 64
MLA_V = BRANCH_WIDTH // MLA_HEADS
Q_LORA = 384
KV_LORA = 256
ROPE_BASE = 10000.0
CONV_WIDTH = 31
CONV_CH = BRANCH_WIDTH
FFN_HIDDEN = -(-8 * D_MODEL // (3 * 256)) * 256

LN_EPS = 1e-5
DEEPNORM_ALPHA = (2 * DEPTH) ** 0.25
DEEPNORM_BETA = (8 * DEPTH) ** -0.25

IN_SIZES = (
    BRANCH_WIDTH,
    DSA_HEADS * DSA_HEAD_DIM,
    DSA_HEADS * DSA_HEAD_DIM,
    DSA_HEADS * DSA_HEAD_DIM,
    IDX_HEADS * IDX_DIM,
    IDX_DIM,
    IDX_HEADS,
    Q_LORA,
    KV_LORA,
    MLA_ROPE,
    2 * CONV_CH,
)
IN_WIDTH = sum(IN_SIZES)
IN_SPLITS = tuple(int(v) for v in np.cumsum(IN_SIZES)[:-1])

kernel_name = 'hybrid_streaming_encoder_block'


def layer_norm(x, g, b):
    xf = x.astype(jnp.float32)
    mu = xf.mean(-1, keepdims=True)
    var = jnp.square(xf - mu).mean(-1, keepdims=True)
    return ((xf - mu) * lax.rsqrt(var + LN_EPS) * g + b).astype(x.dtype)


def plain_norm(x):
    xf = x.astype(jnp.float32)
    mu = xf.mean(-1, keepdims=True)
    var = jnp.square(xf - mu).mean(-1, keepdims=True)
    return ((xf - mu) * lax.rsqrt(var + LN_EPS)).astype(x.dtype)


def rms_norm(x, g):
    xf = x.astype(jnp.float32)
    return (xf * lax.rsqrt(jnp.mean(xf * xf, -1, keepdims=True) + LN_EPS) * g).astype(x.dtype)


def apply_rope(x, cos, sin):
    x1, x2 = jnp.split(x.astype(jnp.float32), 2, axis=-1)
    return jnp.concatenate([x1 * cos - x2 * sin, x2 * cos + x1 * sin], axis=-1).astype(x.dtype)


def alibi_slopes(n_heads):
    return jnp.asarray([2.0 ** (-8.0 * (h + 1) / n_heads) for h in range(n_heads)], jnp.float32)


def to_blocks(a):
    b, s = a.shape[:2]
    return jnp.swapaxes(a.reshape((b, s // Q_BLOCK, Q_BLOCK) + a.shape[2:]), 0, 1)


def from_blocks(o):
    nb, b, qb = o.shape[:3]
    return jnp.swapaxes(o, 0, 1).reshape(b, nb * qb, -1)


def pool_mixer(h, w_pool, pool_scale):
    b, s, _ = h.shape
    hf = h.astype(jnp.float32).reshape(b, s, len(POOL_WINDOWS), POOL_GROUP)
    cs = jnp.concatenate([jnp.zeros((b, 1) + hf.shape[2:], jnp.float32),
                          jnp.cumsum(hf, axis=1)], axis=1)
    t = jnp.arange(s)
    outs = []
    for g, w in enumerate(POOL_WINDOWS):
        cs_g = cs[:, :, g]
        lo = jnp.maximum(t + 1 - w, 0)
        cnt = jnp.minimum(t + 1, w).astype(jnp.float32)[None, :, None]
        outs.append((cs_g[:, t + 1] - cs_g[:, lo]) / cnt - hf[:, :, g])
    pooled = jnp.stack(outs, axis=2).astype(h.dtype)
    mixed = jnp.einsum('bsgc,gcd->bsgd', pooled, w_pool)
    return mixed.reshape(b, s, BRANCH_WIDTH) * pool_scale


def dsa_mixer(q, k, v, iq, ik, iw):
    b, s, n_heads, dh = q.shape
    n_sel = min(TOPK_MAX, s // 4)
    slopes = alibi_slopes(n_heads)
    key_pos = jnp.arange(s)
    ik_f = ik.astype(jnp.float32)

    def block(args):
        qb, iqb, iwb, t = args
        limit = (t // CHUNK + 1) * CHUNK
        admissible = key_pos[None, :] < limit[:, None]
        dots = jnp.einsum('bqhd,bsd->bqhs', iqb.astype(jnp.float32), ik_f) * IDX_DIM ** -0.5
        score = jnp.einsum('bqh,bqhs->bqs', iwb.astype(jnp.float32) * IDX_HEADS ** -0.5,
                           jax.nn.relu(dots))
        score = jnp.where(admissible[None], score, -jnp.inf)
        _, idx = lax.top_k(score, n_sel)
        valid = idx < limit[None, :, None]
        k_sel = jax.vmap(lambda kk, ii: kk[ii])(k, idx)
        v_sel = jax.vmap(lambda vv, ii: vv[ii])(v, idx)
        logits = jnp.einsum('bqhd,bqkhd->bhqk', qb, k_sel).astype(jnp.float32) * dh ** -0.5
        dist = jnp.abs(t[None, :, None] - idx).astype(jnp.float32)
        logits = logits - slopes[None, :, None, None] * dist[:, None]
        logits = jnp.where(valid[:, None], logits, -jnp.inf)
        p = jax.nn.softmax(logits, axis=-1).astype(v.dtype)
        return jnp.einsum('bhqk,bqkhd->bqhd', p, v_sel)

    t_blocks = jnp.arange(s).reshape(s // Q_BLOCK, Q_BLOCK)
    out = lax.map(block, (to_blocks(q), to_blocks(iq), to_blocks(iw), t_blocks))
    return from_blocks(out)


def mla_mixer(cq, ckv, kr, q_norm, w_q_up, kv_norm, w_kv_up, cos, sin):
    b, s, _ = cq.shape
    q = (rms_norm(cq, q_norm) @ w_q_up).reshape(b, s, MLA_HEADS, MLA_NOPE + MLA_ROPE)
    q_nope = q[..., :MLA_NOPE]
    q_rope = apply_rope(q[..., MLA_NOPE:], cos[:, None], sin[:, None])
    kv = (rms_norm(ckv, kv_norm) @ w_kv_up).reshape(b, s, MLA_HEADS, MLA_NOPE + MLA_V)
    k_nope, v = kv[..., :MLA_NOPE], kv[..., MLA_NOPE:]
    k_rope = apply_rope(kr, cos, sin)
    scale = (MLA_NOPE + MLA_ROPE) ** -0.5
    key_chunk = jnp.arange(s) // CHUNK

    def block(args):
        qn, qr, t = args
        logits = (jnp.einsum('bqhd,bshd->bhqs', qn, k_nope).astype(jnp.float32)
                  + jnp.einsum('bqhr,bsr->bhqs', qr, k_rope).astype(jnp.float32)) * scale
        mask = key_chunk[None, :] <= (t // CHUNK)[:, None]
        logits = jnp.where(mask, logits, -jnp.inf)
        p = jax.nn.softmax(logits, axis=-1).astype(v.dtype)
        return jnp.einsum('bhqs,bshd->bqhd', p, v)

    t_blocks = jnp.arange(s).reshape(s // Q_BLOCK, Q_BLOCK)
    out = lax.map(block, (to_blocks(q_nope), to_blocks(q_rope), t_blocks))
    return from_blocks(out)


def conv_mixer(h, w_dw, b_dw, ln_g, ln_b):
    a, g = jnp.split(h, 2, axis=-1)
    z = a * jax.nn.sigmoid(g)
    z = lax.conv_general_dilated(z, w_dw[:, None, :], (1,), [(CONV_WIDTH - 1, 0)],
                                 dimension_numbers=('NWC', 'WIO', 'NWC'),
                                 feature_group_count=CONV_CH) + b_dw
    return jax.nn.silu(layer_norm(z, ln_g, ln_b))


def setup_inputs(seed: int = 0) -> dict:
    key = jax.random.key(seed)
    keys = list(jax.random.split(key, 32))
    L, D = DEPTH, D_MODEL

    def nrm(shape, fan_in, scale=1.0):
        return jax.random.normal(keys.pop(), shape, jnp.float32) * (scale * fan_in ** -0.5)

    def gain(shape):
        return 1.0 + 0.01 * jax.random.normal(keys.pop(), shape, jnp.float32)

    def bias(shape):
        return 0.01 * jax.random.normal(keys.pop(), shape, jnp.float32)

    return {
        'x': jax.random.normal(keys.pop(), (BATCH, SEQ, D), jnp.float32),
        'c': jax.random.normal(keys.pop(), (BATCH, D), jnp.float32),
        'w_ada': nrm((L, D, 6 * D), D, 0.2),
        'b_ada': bias((L, 6 * D)),
        'w_in': nrm((L, D, IN_WIDTH), D),
        'w_pool': nrm((L, len(POOL_WINDOWS), POOL_GROUP, POOL_GROUP), POOL_GROUP),
        'pool_scale': gain((L, BRANCH_WIDTH)),
        'q_norm': gain((L, Q_LORA)),
        'w_q_up': nrm((L, Q_LORA, MLA_HEADS * (MLA_NOPE + MLA_ROPE)), Q_LORA),
        'kv_norm': gain((L, KV_LORA)),
        'w_kv_up': nrm((L, KV_LORA, MLA_HEADS * (MLA_NOPE + MLA_V)), KV_LORA),
        'w_dw': nrm((L, CONV_WIDTH, CONV_CH), CONV_WIDTH),
        'b_dw': bias((L, CONV_CH)),
        'conv_ln_g': gain((L, CONV_CH)),
        'conv_ln_b': bias((L, CONV_CH)),
        'w_branch': nrm((L, N_BRANCHES, BRANCH_WIDTH, D), BRANCH_WIDTH, DEEPNORM_BETA),
        'w_gate': nrm((L, N_BRANCHES, D, D), D),
        'b_gate': bias((L, N_BRANCHES, D)),
        'w_o': nrm((L, D, D), D, DEEPNORM_BETA),
        'ln1_g': gain((L, D)),
        'ln1_b': bias((L, D)),
        'w_ffn_in': nrm((L, D, 2 * FFN_HIDDEN), D),
        'w_ffn_out': nrm((L, FFN_HIDDEN, D), FFN_HIDDEN, DEEPNORM_BETA),
        'ln2_g': gain((L, D)),
        'ln2_b': bias((L, D)),
    }


def reference(x, c, w_ada, b_ada, w_in, w_pool, pool_scale, q_norm, w_q_up, kv_norm,
              w_kv_up, w_dw, b_dw, conv_ln_g, conv_ln_b, w_branch, w_gate, b_gate, w_o,
              ln1_g, ln1_b, w_ffn_in, w_ffn_out, ln2_g, ln2_b):
    b, s, d = x.shape
    pos = jnp.arange(s, dtype=jnp.float32)
    inv_freq = ROPE_BASE ** (-jnp.arange(0, MLA_ROPE, 2, dtype=jnp.float32) / MLA_ROPE)
    ang = pos[:, None] * inv_freq[None, :]
    cos, sin = jnp.cos(ang), jnp.sin(ang)
    c_act = jax.nn.silu(c)

    for l in range(DEPTH):
        mod = (c_act @ w_ada[l] + b_ada[l]).reshape(b, 6, 1, d)
        sh1, sc1, g1, sh2, sc2, g2 = (mod[:, i] for i in range(6))

        u = plain_norm(x) * (1.0 + sc1) + sh1
        (h_pool, dq, dk, dv, iq, ik, iw, cq, ckv, kr, h_conv) = jnp.split(
            u @ w_in[l], IN_SPLITS, axis=-1)
        y_a = pool_mixer(h_pool, w_pool[l], pool_scale[l])
        y_b = dsa_mixer(dq.reshape(b, s, DSA_HEADS, DSA_HEAD_DIM),
                        dk.reshape(b, s, DSA_HEADS, DSA_HEAD_DIM),
                        dv.reshape(b, s, DSA_HEADS, DSA_HEAD_DIM),
                        iq.reshape(b, s, IDX_HEADS, IDX_DIM), ik, iw)
        y_c = mla_mixer(cq, ckv, kr, q_norm[l], w_q_up[l], kv_norm[l], w_kv_up[l], cos, sin)
        y_d = conv_mixer(h_conv, w_dw[l], b_dw[l], conv_ln_g[l], conv_ln_b[l])
        gated = [jax.nn.sigmoid(u @ w_gate[l, i] + b_gate[l, i]) * (y @ w_branch[l, i])
                 for i, y in enumerate((y_a, y_b, y_c, y_d))]
        merged = gated[0] + gated[1] + gated[2] + gated[3]
        x = layer_norm(DEEPNORM_ALPHA * x + (1.0 + g1) * (merged @ w_o[l]), ln1_g[l], ln1_b[l])

        u2 = plain_norm(x) * (1.0 + sc2) + sh2
        a, gt = jnp.split(u2 @ w_ffn_in[l], 2, axis=-1)
        ffn = (jax.nn.silu(a) * gt) @ w_ffn_out[l]
        x = layer_norm(DEEPNORM_ALPHA * x + (1.0 + g2) * ffn, ln2_g[l], ln2_b[l])
    return x
```

```python
import numpy as np
from contextlib import ExitStack
import concourse.bass as bass
import concourse.mybir as mybir
from concourse.bass_utils import run_bass_kernel_spmd

F32 = mybir.dt.float32
BF16 = mybir.dt.bfloat16
AF = mybir.ActivationFunctionType
ALU = mybir.AluOpType
AX = mybir.AxisListType

SAME_ENGINE_SYNC = True


class Buf:
    def __init__(self, handle, name):
        self.h = handle
        self.name = name
        self.w = {}
        self.r = {}

    def __getitem__(self, idx):
        return self.h[idx]


class BufV(Buf):
    def __init__(self, handle, name, off, width):
        super().__init__(handle, name)
        self.off = off
        self.width = width

    def __getitem__(self, idx):
        if not isinstance(idx, tuple):
            idx = (idx, slice(None))
        p, c = idx
        a = 0 if c.start is None else c.start
        b = self.width if c.stop is None else c.stop
        return self.h[p, self.off + a:self.off + b]


class BufK(Buf):
    def __init__(self, handle, name, k0, nk):
        super().__init__(handle, name)
        self.k0 = k0
        self.nk = nk

    def __getitem__(self, idx):
        if not isinstance(idx, tuple):
            return self.h[idx, self.k0:self.k0 + self.nk, :]
        p, k = idx[0], idx[1]
        n = idx[2] if len(idx) > 2 else slice(None)
        if isinstance(k, slice):
            a = 0 if k.start is None else k.start
            b = self.nk if k.stop is None else k.stop
            return self.h[p, self.k0 + a:self.k0 + b, n]
        return self.h[p, self.k0 + k, n]


class Eng:
    def __init__(self, kb, name, eng, ndma=0):
        self.kb = kb
        self.name = name
        self.e = eng
        self.sem = kb.newsem("c_" + name)
        self.count = 0
        self.seen = {}
        self.dsems = [kb.newsem(f"d_{name}{i}") for i in range(ndma)]
        self.dcount = 0

    def wait(self, sem, val):
        if val <= 0:
            return
        if sem is self.sem and not SAME_ENGINE_SYNC:
            return
        if self.seen.get(id(sem), 0) >= val:
            return
        self.e.wait_ge(sem, val)
        self.seen[id(sem)] = val

    def _deps(self, reads, writes):
        for b in reads:
            for sem, val in b.w.values():
                self.wait(sem, val)
        for b in writes:
            for sem, val in b.w.values():
                self.wait(sem, val)
            for sem, val in b.r.values():
                self.wait(sem, val)

    def _mark(self, reads, writes, ev):
        for b in reads:
            b.r[id(ev[0])] = ev
        for b in writes:
            b.w = {id(ev[0]): ev}
            b.r = {}

    def op(self, ins_fn, reads=(), writes=(), signal=True):
        signal = True
        self._deps(reads, writes)
        ins = ins_fn()
        if signal:
            self.count += 1
            ins.then_inc(self.sem, 1)
        ev = (self.sem, self.count if signal else self.count + 1)
        self._mark(reads, writes, ev)
        return ins

    def mark_only(self, reads, writes):
        ev = (self.sem, self.count + 1)
        self._mark(reads, writes, ev)

    def dma(self, out, in_, reads=(), writes=(), **kw):
        n = len(self.dsems)
        i = self.dcount
        j = i % n
        sem = self.dsems[j]
        if i >= n:
            self.wait(sem, 16 * (i // n))
        self._deps(reads, writes)
        ins = self.e.dma_start(out=out, in_=in_, **kw)
        ins.then_inc(sem, 16)
        self.dcount += 1
        ev = (sem, 16 * (i // n + 1))
        self._mark(reads, writes, ev)
        return ev


class KB:
    def __init__(self):
        self.nc = bass.Bass("TRN2", target_bir_lowering=False)
        self.es = ExitStack()
        self.sems = []
        nc = self.nc
        self.pe = Eng(self, "pe", nc.tensor)
        self.act = Eng(self, "act", nc.scalar, ndma=4)
        self.dve = Eng(self, "dve", nc.vector)
        self.pool = Eng(self, "pool", nc.gpsimd, ndma=4)
        self.sp = Eng(self, "sp", nc.sync, ndma=8)
        self.engs = [self.pe, self.act, self.dve, self.pool, self.sp]
        self.uid = 0

    def newsem(self, name):
        s = self.es.enter_context(self.nc.semaphore(name))
        self.sems.append(s)
        return s

    def dram(self, name, shape, dt, kind="Internal"):
        return self.nc.dram_tensor(name, list(shape), dt, kind=kind).ap()

    def sb(self, shape, dt, name=None, stack=None):
        self.uid += 1
        name = f"{name or 't'}_{self.uid}"
        h = (stack or self.es).enter_context(self.nc.sbuf_tensor(name, list(shape), dt))
        return Buf(h, name)

    def ps(self, shape, dt=F32, name=None, stack=None):
        self.uid += 1
        name = f"{name or 'p'}_{self.uid}"
        h = (stack or self.es).enter_context(self.nc.psum_tensor(name, list(shape), dt))
        return Buf(h, name)

    def barrier(self):
        evs = []
        for g in self.engs:
            if g.count > 0:
                evs.append((g.sem, g.count))
            n = len(g.dsems)
            for j in range(n):
                cnt = (g.dcount - j + n - 1) // n if g.dcount > j else 0
                if cnt > 0:
                    evs.append((g.dsems[j], 16 * cnt))
        for g in self.engs:
            for sem, val in evs:
                g.wait(sem, val)

    def finish(self):
        self.barrier()
        self.es.close()


class Ring:
    def __init__(self, bufs):
        self.bufs = bufs
        self.i = 0

    def next(self):
        b = self.bufs[self.i % len(self.bufs)]
        self.i += 1
        return b


D = 2048
KD = 16
SEQ = 4096
NB = 4
TOK = 2048
DEPTH = 2
FFN = 5632
EPS = 1e-5
ALPHA = (2 * DEPTH) ** 0.25
SEGS = [("hp", 512), ("dq", 512), ("dk", 512), ("dv", 512), ("iq", 1024), ("ik", 64), ("iw", 16),
        ("cq", 384), ("ckv", 256), ("kr", 64), ("hc", 1024)]
SEG_OFF = {}
_o = 0
for _n, _s in SEGS:
    SEG_OFF[_n] = _o
    _o += _s
FM_SEGS = ["hp", "dq", "dk", "iq", "ik", "cq", "ckv", "kr", "krs", "hc"]
FM_DT = {"hp": "f32", "dq": "bf", "dk": "bf", "iq": "bf", "ik": "bf", "cq": "f32", "ckv": "f32", "kr": "f32",
         "krs": "f32", "hc": "f32"}
FM_ROWS = {"hp": 512, "dq": 512, "dk": 512, "iq": 1024, "ik": 64, "cq": 384, "ckv": 256, "kr": 64, "krs": 64,
           "hc": 1024}
CTS = []
for _n in FM_SEGS:
    _r = FM_ROWS[_n]
    for _c in range(0, _r, 128):
        CTS.append((_n, _c, min(128, _r - _c)))
NCT = len(CTS)


def pk(v):
    v = np.asarray(v)
    return np.ascontiguousarray(v.reshape(-1, 128).T)


def dts(s):
    return F32 if s == "f32" else BF16


def build_mods():
    kb = KB()
    nc = kb.nc
    NCOL = 2 * 6 * D // 8
    NT = NCOL // 128
    wa = kb.dram("wa", [D, NCOL], F32, kind="ExternalInput")
    ba = kb.dram("ba", [128, NT], F32, kind="ExternalInput")
    cT = kb.dram("cT", [128, KD, NB], F32, kind="ExternalInput")
    modT = kb.dram("modT", [128, NT, NB], F32, kind="ExternalOutput")
    csb = kb.sb([128, KD, NB], F32, "csb")
    cact = kb.sb([128, KD, NB], F32, "cact")
    basb = kb.sb([128, NT], F32, "basb")
    msb = kb.sb([128, NT, NB], F32, "msb")
    wr = Ring([kb.sb([128, KD, 512], F32, "wst") for _ in range(2)])
    pr = Ring([kb.ps([128, 512], F32, "ps") for _ in range(2)])
    kb.sp.dma(csb[:], cT[:, :, :], writes=[csb])
    kb.sp.dma(basb[:], ba[:, :], writes=[basb])
    kb.act.op(lambda: nc.scalar.activation(out=cact[:], in_=csb[:], func=AF.Silu), reads=[csb], writes=[cact])
    wav = wa.rearrange("(k p) n -> p k n", p=128)
    for g in range(NCOL // 512):
        w = wr.next()
        kb.sp.dma(w[:], wav[:, :, g * 512:(g + 1) * 512], writes=[w])
        for j in range(4):
            t = g * 4 + j
            p = pr.next()
            for k in range(KD):
                kb.pe.op(lambda: nc.tensor.matmul(p[:, 0:NB], lhsT=w[:, k, j * 128:(j + 1) * 128], rhs=cact[:, k, :],
                                                  start=(k == 0), stop=(k == KD - 1)),
                         reads=[w, cact], writes=[p], signal=(k == KD - 1))
            kb.dve.op(lambda: nc.vector.tensor_scalar(out=msb[:, t, :], in0=p[:, 0:NB], scalar1=basb[:, t:t + 1],
                                                      scalar2=None, op0=ALU.add),
                      reads=[p, basb], writes=[msb])
    kb.sp.dma(modT[:, :, :], msb[:], reads=[msb])
    kb.finish()
    return nc


def run_mods(inputs):
    w_ada = inputs["w_ada"]
    b_ada = inputs["b_ada"]
    c = inputs["c"]
    wcat = np.concatenate([w_ada[0], w_ada[1]], axis=1)
    bcat = np.concatenate([b_ada[0], b_ada[1]], axis=0)
    cT = np.ascontiguousarray(c.reshape(NB, KD, 128).transpose(2, 1, 0))
    NCOL = 3072
    in_maps = []
    for core in range(8):
        sl = slice(core * NCOL, (core + 1) * NCOL)
        in_maps.append({"wa": np.ascontiguousarray(wcat[:, sl]), "ba": pk(bcat[sl]), "cT": cT})
    nc = build_mods()
    res = run_bass_kernel_spmd(nc, in_maps, core_ids=list(range(8)))
    mt = np.concatenate([r["modT"] for r in res.results], axis=1)
    return [np.ascontiguousarray(mt[:, :, b]) for b in range(NB)]


class Ctx:
    pass


def setup_common(kb, modT_d):
    nc = kb.nc
    cx = Ctx()
    cx.P = [kb.ps([128, 512], F32, f"P{i}") for i in range(6)]
    cx.PB = []
    for i in range(2):
        big = kb.ps([128, 1024], BF16, f"PBB{i}")
        cx.PB += [BufV(big.h, f"PB{2 * i}", 0, 512), BufV(big.h, f"PB{2 * i + 1}", 512, 512)]
    cx.ones_f = kb.sb([128, 128], F32, "ones_f")
    cx.ones_b = kb.sb([128, 128], BF16, "ones_b")
    cx.eps = kb.sb([128, 1], F32, "eps")
    cx.mod = kb.sb([128, DEPTH * 96], F32, "mod")
    cx.onep = kb.sb([128, DEPTH * 96], F32, "onep")
    kb.pool.op(lambda: nc.gpsimd.memset(cx.ones_f[:], 1.0), writes=[cx.ones_f])
    kb.pool.op(lambda: nc.gpsimd.memset(cx.ones_b[:], 1.0), writes=[cx.ones_b])
    kb.pool.op(lambda: nc.gpsimd.memset(cx.eps[:], EPS), writes=[cx.eps])
    if modT_d is not None:
        kb.sp.dma(cx.mod[:], modT_d[:, :], writes=[cx.mod])
        kb.dve.op(lambda: nc.vector.tensor_scalar(out=cx.onep[:], in0=cx.mod[:], scalar1=1.0, scalar2=None, op0=ALU.add),
                  reads=[cx.mod], writes=[cx.onep])
    return cx


def mcol(l, i, k):
    return (l * 6 + i) * 16 + k


def col_stats(kb, cx, chunks, nfeat, want_mean, tmp, N=512):
    nc = kb.nc
    ps_s, ps_q = cx.P[0], cx.P[1]
    n = len(chunks)
    for k, (b, ap) in enumerate(chunks):
        rows = ap.shape[0]
        sq = tmp["sq"].next()
        kb.act.op(lambda: nc.scalar.activation(out=sq[:rows, :N], in_=ap, func=AF.Square), reads=[b], writes=[sq])
        kb.pe.op(lambda: nc.tensor.matmul(ps_q[:, :N], lhsT=cx.ones_f[:rows, :], rhs=sq[:rows, :N], start=(k == 0),
                                          stop=(k == n - 1)), reads=[sq, cx.ones_f], writes=[ps_q])
        if want_mean:
            kb.pe.op(lambda: nc.tensor.matmul(ps_s[:, :N], lhsT=cx.ones_f[:rows, :], rhs=ap, start=(k == 0),
                                              stop=(k == n - 1)), reads=[b, cx.ones_f], writes=[ps_s])
    inv = 1.0 / nfeat
    var, rstd = tmp["var"], tmp["rstd"]
    if want_mean:
        mean, msq, nmr = tmp["mean"], tmp["msq"], tmp["nmr"]
        kb.dve.op(lambda: nc.vector.tensor_scalar(out=mean[:, :N], in0=ps_s[:, :N], scalar1=inv, scalar2=None,
                                                  op0=ALU.mult), reads=[ps_s], writes=[mean])
        kb.dve.op(lambda: nc.vector.tensor_tensor(out=msq[:, :N], in0=mean[:, :N], in1=mean[:, :N], op=ALU.mult),
                  reads=[mean], writes=[msq])
        kb.dve.op(lambda: nc.vector.scalar_tensor_tensor(out=var[:, :N], in0=ps_q[:, :N], scalar=inv, in1=msq[:, :N],
                                                         op0=ALU.mult, op1=ALU.subtract),
                  reads=[ps_q, msq], writes=[var])
        kb.act.op(lambda: nc.scalar.activation(out=var[:, :N], in_=var[:, :N], func=AF.Sqrt, bias=cx.eps[:, 0:1],
                                               scale=1.0), reads=[var, cx.eps], writes=[var])
    else:
        kb.act.op(lambda: nc.scalar.activation(out=var[:, :N], in_=ps_q[:, :N], func=AF.Sqrt, bias=cx.eps[:, 0:1],
                                               scale=inv), reads=[ps_q, cx.eps], writes=[var])
    kb.dve.op(lambda: nc.vector.reciprocal(out=rstd[:, :N], in_=var[:, :N]), reads=[var], writes=[rstd])
    if want_mean:
        kb.dve.op(lambda: nc.vector.scalar_tensor_tensor(out=nmr[:, :N], in0=mean[:, :N], scalar=-1.0,
                                                         in1=rstd[:, :N], op0=ALU.mult, op1=ALU.mult),
                  reads=[mean, rstd], writes=[nmr])
        return rstd, nmr
    return rstd, None


def stat_tmps(kb, st, N=512):
    t = {"sq": Ring([kb.sb([128, N], F32, "sq", st) for _ in range(2)])}
    for nm in ("mean", "msq", "var", "rstd", "nmr"):
        t[nm] = kb.sb([128, N], F32, nm, st)
    return t


def load_cast(kb, dst, dst_ap, src_ap, stg_ring, stg_view):
    nc = kb.nc
    s = stg_ring.next()
    kb.sp.dma(stg_view(s), src_ap, writes=[s])
    kb.pool.op(lambda: nc.gpsimd.tensor_copy(out=dst_ap, in_=stg_view(s)), reads=[s], writes=[dst])


def evac(kb, i, out_buf, out_ap, ps_buf, ps_ap):
    nc = kb.nc
    if i % 2 == 0:
        kb.act.op(lambda: nc.scalar.copy(out=out_ap, in_=ps_ap), reads=[ps_buf], writes=[out_buf])
    else:
        kb.dve.op(lambda: nc.vector.tensor_copy(out=out_ap, in_=ps_ap), reads=[ps_buf], writes=[out_buf])


def stage_A(kb, cx, l, xT, W, O, S):
    nc = kb.nc
    P = cx.P
    xTv = xT.rearrange("(k p) n -> p k n", p=128)
    with ExitStack() as st:
        uT = [kb.sb([128, KD, 512], BF16, f"uT{t}", st) for t in range(4)]
        with ExitStack() as s1:
            xr = Ring([kb.sb([128, KD, 512], F32, "xt", s1) for _ in range(2)])
            tmp = stat_tmps(kb, s1)
            t1r = Ring([kb.sb([128, 512], F32, "t1", s1) for _ in range(2)])
            t2r = Ring([kb.sb([128, 512], F32, "t2", s1) for _ in range(2)])
            for tt in range(4):
                xt = xr.next()
                kb.sp.dma(xt[:], xTv[:, :, tt * 512:(tt + 1) * 512], writes=[xt])
                rstd, nmr = col_stats(kb, cx, [(xt, xt[:, k, :]) for k in range(KD)], D, True, tmp)
                for k in range(KD):
                    t1 = t1r.next()
                    t2 = t2r.next()
                    kb.dve.op(lambda: nc.vector.tensor_tensor(out=t1[:], in0=xt[:, k, :], in1=rstd[:], op=ALU.mult),
                              reads=[xt, rstd], writes=[t1])
                    kb.pool.op(lambda: nc.gpsimd.tensor_tensor(out=t2[:], in0=t1[:], in1=nmr[:], op=ALU.add),
                               reads=[t1, nmr], writes=[t2])
                    kb.act.op(lambda: nc.scalar.activation(out=uT[tt][:, k, :], in_=t2[:], func=AF.Identity,
                                                           scale=cx.onep[:, mcol(l, 1, k):mcol(l, 1, k) + 1],
                                                           bias=cx.mod[:, mcol(l, 0, k):mcol(l, 0, k) + 1]),
                              reads=[t2, cx.onep, cx.mod], writes=[uT[tt]])
                kb.act.dma(O["uT"].rearrange("(k p) n -> p k n", p=128)[:, :, tt * 512:(tt + 1) * 512], uT[tt][:],
                           reads=[uT[tt]])
            kb.barrier()
        with ExitStack() as s2:
            wst = Ring([kb.sb([128, KD, 128], F32, "wst", s2) for _ in range(2)])
            wbf = Ring([kb.sb([128, KD, 128], BF16, "wbf", s2) for _ in range(2)])
            obf = Ring([kb.sb([128, TOK], BF16, "obf", s2) for _ in range(2)])
            of32 = Ring([kb.sb([128, TOK], F32, "of32", s2) for _ in range(2)])
            psr = Ring([P[2], P[3], P[4], P[5]])
            dest = {"hp": O["hpT"], "dq": O["dqT"], "dk": O["dkT"], "iq": O["iqT"], "ik": O["ikT"], "cq": S["cqT"],
                    "ckv": S["ckvT"], "kr": S["krraw"], "krs": S["krsraw"], "hc": S["hcT"]}
            ei = 0
            for ct, (seg, c0, ncols) in enumerate(CTS):
                wb = wbf.next()
                load_cast(kb, wb, wb[:], W["win_ct"][ct], wst, lambda s: s[:])
                ob = (of32 if FM_DT[seg] == "f32" else obf).next()
                for tt in range(4):
                    ps = psr.next()
                    for k in range(KD):
                        kb.pe.op(lambda: nc.tensor.matmul(ps[:], lhsT=wb[:, k, :], rhs=uT[tt][:, k, :],
                                                          start=(k == 0), stop=(k == KD - 1)),
                                 reads=[wb, uT[tt]], writes=[ps])
                    evac(kb, ei, ob, ob[:ncols, tt * 512:(tt + 1) * 512], ps, ps[:ncols, :])
                    ei += 1
                kb.act.dma(dest[seg][c0:c0 + ncols, :], ob[:ncols, :], reads=[ob])
            wv = kb.sb([128, KD, 512], BF16, "wv", s2)
            wiw = kb.sb([128, KD, 128], BF16, "wiw", s2)
            for j in range(4):
                load_cast(kb, wv, wv[:, :, j * 128:(j + 1) * 128], W["wdv_ct"][j], wst, lambda s: s[:])
            load_cast(kb, wiw, wiw[:], W["wiw_ct"][0], wst, lambda s: s[:])
            vob = Ring([kb.sb([128, 512], BF16, "vob", s2) for _ in range(2)])
            iwo = kb.sb([128, 16, 16], F32, "iwo", s2)
            for tb in range(16):
                tt, off = tb // 4, (tb % 4) * 128
                ps = psr.next()
                for k in range(KD):
                    kb.pe.op(lambda: nc.tensor.matmul(ps[:], lhsT=uT[tt][:, k, off:off + 128], rhs=wv[:, k, :],
                                                      start=(k == 0), stop=(k == KD - 1)),
                             reads=[wv, uT[tt]], writes=[ps])
                vo = vob.next()
                evac(kb, tb, vo, vo[:], ps, ps[:])
                kb.act.dma(O["dV"][tb * 128:(tb + 1) * 128, :], vo[:], reads=[vo])
                ps2 = psr.next()
                for k in range(KD):
                    kb.pe.op(lambda: nc.tensor.matmul(ps2[:, 0:16], lhsT=uT[tt][:, k, off:off + 128], rhs=wiw[:, k, 0:16],
                                                      start=(k == 0), stop=(k == KD - 1)),
                             reads=[wiw, uT[tt]], writes=[ps2])
                evac(kb, tb + 1, iwo, iwo[:, tb, :], ps2, ps2[:, 0:16])
            kb.act.dma(O["iw"].rearrange("(b p) h -> p b h", p=128), iwo[:], reads=[iwo])
            kb.barrier()
    with ExitStack() as s3:
        stg = Ring([kb.sb([128, 1024], F32, "stg", s3) for _ in range(2)])
        wq = kb.sb([128, 3, 1024], BF16, "wq", s3)
        wkv = kb.sb([128, 2, 1024], BF16, "wkv", s3)
        for kc in range(3):
            load_cast(kb, wq, wq[:, kc, :], W["wq_all"][kc * 128:(kc + 1) * 128, :], stg, lambda s: s[:])
        for kc in range(2):
            load_cast(kb, wkv, wkv[:, kc, :], W["wkv_all"][kc * 128:(kc + 1) * 128, :], stg, lambda s: s[:])
        vecs = kb.sb([128, 8], F32, "vecs", s3)
        kb.sp.dma(vecs[:, 0:3], W["q_norm"][:, :], writes=[vecs])
        kb.sp.dma(vecs[:, 3:5], W["kv_norm"][:, :], writes=[vecs])
        tmp = stat_tmps(kb, s3)
        ar = Ring([kb.sb([128, 8, 512], F32, "hc", s3) for _ in range(1)])
        sig = kb.sb([128, 4, 512], F32, "sig", s3)
        zb = kb.sb([128, 4, 512], BF16, "zb", s3)
        cqs = kb.sb([128, 3, 512], F32, "cqs", s3)
        cqn = kb.sb([128, 3, 512], BF16, "cqn", s3)
        cks = kb.sb([128, 2, 512], F32, "cks", s3)
        ckn = kb.sb([128, 2, 512], BF16, "ckn", s3)
        cc = kb.sb([64, 512], F32, "cc", s3)
        ss = kb.sb([64, 512], F32, "ss", s3)
        krr = kb.sb([64, 2, 512], F32, "krr", s3)
        t1r = Ring([kb.sb([128, 512], F32, "t1", s3) for _ in range(2)])
        t2r = Ring([kb.sb([128, 512], F32, "t2", s3) for _ in range(2)])
        obr = Ring([kb.sb([128, 512], BF16, "ob", s3) for _ in range(3)])
        psr = Ring([P[2], P[3], P[4], P[5]])
        ei = 0
        for tt in range(4):
            ts = slice(tt * 512, (tt + 1) * 512)
            hc = ar.next()
            kb.sp.dma(hc[:], S["hcT"].rearrange("(k p) n -> p k n", p=128)[:, :, ts], writes=[hc])
            kb.act.op(lambda: nc.scalar.activation(out=sig[:], in_=hc[:, 4:8, :], func=AF.Sigmoid), reads=[hc],
                      writes=[sig])
            kb.dve.op(lambda: nc.vector.tensor_tensor(out=zb[:], in0=hc[:, 0:4, :], in1=sig[:], op=ALU.mult),
                      reads=[hc, sig], writes=[zb])
            kb.act.dma(O["zT"].rearrange("(k p) n -> p k n", p=128)[:, :, ts], zb[:], reads=[zb])
            kb.sp.dma(cc[:], W["ropec"][:, ts], writes=[cc])
            kb.sp.dma(ss[:], W["ropes"][:, ts], writes=[ss])
            kb.sp.dma(cqs[:], S["cqT"].rearrange("(k p) n -> p k n", p=128)[:, :, ts], writes=[cqs])
            rstd, _ = col_stats(kb, cx, [(cqs, cqs[:, k, :]) for k in range(3)], 384, False, tmp)
            for k in range(3):
                t1 = t1r.next()
                kb.dve.op(lambda: nc.vector.tensor_tensor(out=t1[:], in0=cqs[:, k, :], in1=rstd[:], op=ALU.mult),
                          reads=[cqs, rstd], writes=[t1])
                kb.act.op(lambda: nc.scalar.activation(out=cqn[:, k, :], in_=t1[:], func=AF.Copy,
                                                       scale=vecs[:, k:k + 1]), reads=[t1, vecs], writes=[cqn])
            for h in range(4):
                ps = psr.next()
                for k in range(3):
                    kb.pe.op(lambda: nc.tensor.matmul(ps[:], lhsT=wq[:, k, h * 256:h * 256 + 128], rhs=cqn[:, k, :],
                                                      start=(k == 0), stop=(k == 2)), reads=[wq, cqn], writes=[ps])
                ob = obr.next()
                evac(kb, ei, ob, ob[:], ps, ps[:]); ei += 1
                kb.act.dma(O["qnT"][h * 128:(h + 1) * 128, ts], ob[:], reads=[ob])
                ps = psr.next()
                ps2 = psr.next()
                for k in range(3):
                    kb.pe.op(lambda: nc.tensor.matmul(ps[:64, :], lhsT=wq[:, k, h * 256 + 128:h * 256 + 192],
                                                      rhs=cqn[:, k, :], start=(k == 0), stop=(k == 2)),
                             reads=[wq, cqn], writes=[ps])
                for k in range(3):
                    kb.pe.op(lambda: nc.tensor.matmul(ps2[:64, :], lhsT=wq[:, k, h * 256 + 192:h * 256 + 256],
                                                      rhs=cqn[:, k, :], start=(k == 0), stop=(k == 2)),
                             reads=[wq, cqn], writes=[ps2])
                t1 = t1r.next(); t2 = t2r.next(); ob = obr.next()
                kb.dve.op(lambda: nc.vector.tensor_tensor(out=t1[:64, :], in0=ps[:64, :], in1=cc[:], op=ALU.mult),
                          reads=[ps, cc], writes=[t1])
                kb.dve.op(lambda: nc.vector.tensor_tensor(out=t2[:64, :], in0=ps2[:64, :], in1=ss[:], op=ALU.mult),
                          reads=[ps2, ss], writes=[t2])
                kb.pool.op(lambda: nc.gpsimd.tensor_tensor(out=ob[:64, :], in0=t1[:64, :], in1=t2[:64, :], op=ALU.add),
                           reads=[t1, t2], writes=[ob])
                kb.act.dma(O["qrT"][h * 64:(h + 1) * 64, ts], ob[:64, :], reads=[ob])
            kb.sp.dma(cks[:], S["ckvT"].rearrange("(k p) n -> p k n", p=128)[:, :, ts], writes=[cks])
            rstd, _ = col_stats(kb, cx, [(cks, cks[:, k, :]) for k in range(2)], 256, False, tmp)
            for k in range(2):
                t1 = t1r.next()
                kb.dve.op(lambda: nc.vector.tensor_tensor(out=t1[:], in0=cks[:, k, :], in1=rstd[:], op=ALU.mult),
                          reads=[cks, rstd], writes=[t1])
                kb.act.op(lambda: nc.scalar.activation(out=ckn[:, k, :], in_=t1[:], func=AF.Copy,
                                                       scale=vecs[:, 3 + k:4 + k]), reads=[t1, vecs], writes=[ckn])
            for h in range(4):
                ps = psr.next()
                for k in range(2):
                    kb.pe.op(lambda: nc.tensor.matmul(ps[:], lhsT=wkv[:, k, h * 128:(h + 1) * 128], rhs=ckn[:, k, :],
                                                      start=(k == 0), stop=(k == 1)), reads=[wkv, ckn], writes=[ps])
                ob = obr.next()
                evac(kb, ei, ob, ob[:], ps, ps[:]); ei += 1
                kb.act.dma(O["knT"][h * 128:(h + 1) * 128, ts], ob[:], reads=[ob])
            for tb in range(4):
                ps = psr.next()
                for k in range(2):
                    kb.pe.op(lambda: nc.tensor.matmul(ps[:], lhsT=ckn[:, k, tb * 128:(tb + 1) * 128],
                                                      rhs=wkv[:, k, 512:1024], start=(k == 0), stop=(k == 1)),
                             reads=[wkv, ckn], writes=[ps])
                ob = obr.next()
                evac(kb, ei, ob, ob[:], ps, ps[:]); ei += 1
                kb.act.dma(O["mV"][tt * 512 + tb * 128:tt * 512 + (tb + 1) * 128, :], ob[:], reads=[ob])
            kb.sp.dma(krr[:, 0, :], S["krraw"][:, ts], writes=[krr])
            kb.sp.dma(krr[:, 1, :], S["krsraw"][:, ts], writes=[krr])
            t1 = t1r.next(); t2 = t2r.next(); ob = obr.next()
            kb.dve.op(lambda: nc.vector.tensor_tensor(out=t1[:64, :], in0=krr[:, 0, :], in1=cc[:], op=ALU.mult),
                      reads=[krr, cc], writes=[t1])
            kb.dve.op(lambda: nc.vector.tensor_tensor(out=t2[:64, :], in0=krr[:, 1, :], in1=ss[:], op=ALU.mult),
                      reads=[krr, ss], writes=[t2])
            kb.pool.op(lambda: nc.gpsimd.tensor_tensor(out=ob[:64, :], in0=t1[:64, :], in1=t2[:64, :], op=ALU.add),
                       reads=[t1, t2], writes=[ob])
            kb.act.dma(O["krT"][:, ts], ob[:64, :], reads=[ob])
        kb.barrier()


A_OUT = {"uT": ([D, TOK], "bf"), "hpT": ([512, TOK], "f32"), "dqT": ([512, TOK], "bf"), "dkT": ([512, TOK], "bf"),
         "dV": ([TOK, 512], "bf"), "iqT": ([1024, TOK], "bf"), "ikT": ([64, TOK], "bf"), "iw": ([TOK, 16], "f32"),
         "qnT": ([512, TOK], "bf"), "qrT": ([256, TOK], "bf"), "knT": ([512, TOK], "bf"), "mV": ([TOK, 512], "bf"),
         "krT": ([64, TOK], "bf"), "zT": ([512, TOK], "bf")}
A_SCR = {"cqT": [384, TOK], "ckvT": [256, TOK], "krraw": [64, TOK], "krsraw": [64, TOK], "hcT": [1024, TOK]}
A_W = {"win_ct": [NCT, 128, KD, 128], "wdv_ct": [4, 128, KD, 128], "wiw_ct": [1, 128, KD, 128],
       "wq_all": [384, 1024], "wkv_all": [256, 1024], "q_norm": [128, 3], "kv_norm": [128, 2],
       "ropec": [64, TOK], "ropes": [64, TOK]}


def ct_layout(w, c0, ncols):
    out = np.zeros((128, KD, 128), np.float32)
    out[:, :, :ncols] = w[:, c0:c0 + ncols].reshape(KD, 128, ncols).transpose(1, 0, 2)
    return out


def host_A_weights(inputs, l, h):
    w_in = inputs["w_in"][l]
    cts = []
    for seg, c0, ncols in CTS:
        if seg == "krs":
            base = SEG_OFF["kr"]
            wsw = np.concatenate([w_in[:, base + 32:base + 64], w_in[:, base:base + 32]], axis=1)
            cts.append(ct_layout(wsw, 0, 64))
        else:
            cts.append(ct_layout(w_in, SEG_OFF[seg] + c0, ncols))
    out = {"win_ct": np.stack(cts)}
    out["wdv_ct"] = np.stack([ct_layout(w_in, SEG_OFF["dv"] + j * 128, 128) for j in range(4)])
    out["wiw_ct"] = np.stack([ct_layout(w_in, SEG_OFF["iw"], 16)])
    wq = inputs["w_q_up"][l]
    parts = []
    for hh in range(4):
        b = hh * 192
        parts += [wq[:, b:b + 128], wq[:, b + 128:b + 192], wq[:, b + 160:b + 192], wq[:, b + 128:b + 160]]
    out["wq_all"] = np.ascontiguousarray(np.concatenate(parts, axis=1))
    wkv = inputs["w_kv_up"][l]
    out["wkv_all"] = np.ascontiguousarray(np.concatenate(
        [wkv[:, hh * 256:hh * 256 + 128] for hh in range(4)] + [wkv[:, hh * 256 + 128:hh * 256 + 256] for hh in range(4)],
        axis=1))
    out["q_norm"] = pk(inputs["q_norm"][l])
    out["kv_norm"] = pk(inputs["kv_norm"][l])
    pos = np.arange(h * TOK, (h + 1) * TOK, dtype=np.float32)
    inv_freq = (np.float32(10000.0) ** (-np.arange(0, 64, 2, dtype=np.float32) / np.float32(64))).astype(np.float32)
    ang = pos[None, :] * inv_freq[:, None]
    cos, sin = np.cos(ang).astype(np.float32), np.sin(ang).astype(np.float32)
    out["ropec"] = np.ascontiguousarray(np.concatenate([cos, cos], axis=0))
    out["ropes"] = np.ascontiguousarray(np.concatenate([-sin, sin], axis=0))
    return out


def build_A(l):
    kb = KB()
    xT = kb.dram("xT", [D, TOK], F32, kind="ExternalInput")
    modT = kb.dram("modT", [128, DEPTH * 96], F32, kind="ExternalInput")
    W = {k: kb.dram(k, v, F32, kind="ExternalInput") for k, v in A_W.items()}
    O = {k: kb.dram(k, v[0], dts(v[1]), kind="ExternalOutput") for k, v in A_OUT.items()}
    S = {k: kb.dram(k, v, F32) for k, v in A_SCR.items()}
    cx = setup_common(kb, modT)
    stage_A(kb, cx, l, xT, W, O, S)
    kb.finish()
    return kb.nc


SLOPES = [2.0 ** (-8.0 * (h + 1) / 4) for h in range(4)]
NBIS = 16
NEGM = -30000.0


def key_tiles(i):
    tl = [(j * 512, 512, 0) for j in range(4)]
    for j in range(i // 4):
        tl.append((2048 + j * 512, 512, 1))
    tl.append((2048 + (i // 4) * 512, (i % 4 + 1) * 128, 2))
    return tl


def host_B_consts(h):
    c = {}
    q = np.arange(128)[:, None]
    s = np.arange(512)[None, :]
    c["AB"] = np.stack([SLOPES[hh] * (s - q) for hh in range(4)], axis=1).astype(np.float32)
    s1 = np.arange(128)[None, :]
    cm = np.where((s1 // 64) <= (q // 64), 0.0, 1.0)
    c["ABD"] = np.stack([-SLOPES[hh] * np.abs(q - s1) + SLOPES[hh] * 128.0 * wv for hh in range(4) for wv in range(4)],
                        axis=1).astype(np.float32)
    c["CM"] = (cm * NEGM).astype(np.float32)
    c["CMB"] = (cm * -1e6).astype(np.float32)
    c["ident"] = np.eye(128, dtype=np.float32)
    prevb = 0.0 if h == 1 else NEGM
    c["prevb"] = np.full((128, 2), prevb, np.float32)
    c["prevs"] = np.full((128, 2), 0.0 if h == 1 else -1e6, np.float32)
    cb = np.zeros((128, 16 * 4 * 8), np.float32)
    for i in range(16):
        tq0 = 2048 + 128 * i
        for hh in range(4):
            for ti, (s0, w, kind) in enumerate(key_tiles(i)):
                v = -SLOPES[hh] * (tq0 - s0)
                if kind == 0:
                    v += prevb
                cb[:, (i * 4 + hh) * 8 + ti] = v
    c["cb"] = cb
    c["pw"] = np.tile((2.0 ** -(np.arange(NBIS + 1) + 1.0))[None, :], (128, 1)).astype(np.float32)
    ic = np.zeros((128, 4, 16), np.float32)
    for g, w in enumerate((2, 4, 8, 16)):
        t = np.arange(16) + h * TOK
        ic[:, g, :] = 1.0 / np.minimum(t + 1, w)
    c["invc"] = ic
    return c


B_C = {"AB": [128, 4, 512], "ABD": [128, 16, 128], "CM": [128, 128], "CMB": [128, 128], "ident": [128, 128],
       "prevb": [128, 2], "prevs": [128, 2], "cb": [128, 512], "pw": [128, NBIS + 1], "invc": [128, 4, 16]}
B_W = {"w_pool": [4, 128, 128], "pool_scale": [128, 4], "w_dwT": [128, 4, 31], "b_dw": [128, 4],
       "cln_g": [128, 4], "cln_b": [128, 4]}
B_PREV = {"pdkT": ([512, TOK], "bf"), "pdV": ([TOK, 512], "bf"), "pikT": ([64, TOK], "bf"), "pknT": ([512, TOK], "bf"),
          "pmV": ([TOK, 512], "bf"), "pkrT": ([64, TOK], "bf"), "pz": ([512, 32], "bf"), "php": ([512, 16], "f32")}


def host_B_weights(inputs, l):
    out = {"w_pool": np.ascontiguousarray(inputs["w_pool"][l]), "pool_scale": pk(inputs["pool_scale"][l]),
           "b_dw": pk(inputs["b_dw"][l]), "cln_g": pk(inputs["conv_ln_g"][l]), "cln_b": pk(inputs["conv_ln_b"][l])}
    wd = inputs["w_dw"][l]
    out["w_dwT"] = np.ascontiguousarray(wd.reshape(31, 4, 128).transpose(2, 1, 0))
    return out


PARTS = {"B1", "B2", "B3", "B4"}
DBG = {"ni": 16, "tail": True, "fin": True, "nbis": NBIS, "att": True, "idx": True}


def sect(tag):
    if tag in PARTS:
        with ExitStack() as st:
            yield st


def attn_tail(kb, cx, T, Pb, W, s_chunk0, Vb, h, po, first, last):
    nc = kb.nc
    nb = W // 128
    pt = T["ptps"].next()
    for sb in range(nb):
        kb.pe.op(lambda: nc.tensor.transpose(out=pt[:, sb * 128:(sb + 1) * 128], in_=Pb[:, sb * 128:(sb + 1) * 128],
                                             identity=T["identb"][:]), reads=[Pb, T["identb"]], writes=[pt])
    pts = T["pts"].next()
    evac(kb, T["ei"][0], pts, pts[:, :W], pt, pt[:, :W])
    T["ei"][0] += 1
    for sb in range(nb):
        kb.pe.op(lambda: nc.tensor.matmul(po[:, 0:128], lhsT=pts[:, sb * 128:(sb + 1) * 128],
                                          rhs=Vb[:, s_chunk0 + sb, h * 128:(h + 1) * 128],
                                          start=(first and sb == 0), stop=(last and sb == nb - 1)),
                 reads=[pts, Vb], writes=[po])


def attn_finish(kb, cx, T, po, rs, npieces, yb, h, i):
    nc = kb.nc
    rsum, rinv, on = T["rsum"], T["rinv"], T["on"].next()
    kb.dve.op(lambda: nc.vector.tensor_reduce(out=rsum[:], in_=rs[:, 0:npieces], axis=AX.X, op=ALU.add), reads=[rs], writes=[rsum])
    kb.dve.op(lambda: nc.vector.reciprocal(out=rinv[:], in_=rsum[:]), reads=[rsum], writes=[rinv])
    kb.act.op(lambda: nc.scalar.activation(out=on[:], in_=po[:, 0:128], func=AF.Copy, scale=rinv[:, 0:1]),
              reads=[po, rinv], writes=[on])
    pt = T["ptps"].next()
    kb.pe.op(lambda: nc.tensor.transpose(out=pt[:, 0:128], in_=on[:], identity=T["identb"][:]),
             reads=[on, T["identb"]], writes=[pt])
    kb.dve.op(lambda: nc.vector.tensor_copy(out=yb[:, h, i * 128:(i + 1) * 128], in_=pt[:, 0:128]), reads=[pt],
              writes=[yb])


def stage_B(kb, cx, l, A, PV, W, C, yT):
    nc = kb.nc
    P, PB = cx.P, cx.PB
    yTv = yT.rearrange("(b k p) n -> b p k n", b=4, p=128)
    with ExitStack() as sc_:
        ident = kb.sb([128, 128], F32, "ident", sc_)
        identb = kb.sb([128, 128], BF16, "identb", sc_)
        kb.sp.dma(ident[:], C["ident"][:, :], writes=[ident])
        kb.pool.op(lambda: nc.gpsimd.tensor_copy(out=identb[:], in_=ident[:]), reads=[ident], writes=[identb])

        for st in sect("B1"):
            hh = kb.sb([128, 16 + TOK], F32, "hh", st)
            sA = kb.sb([128, 16 + TOK], F32, "sA", st)
            sB = kb.sb([128, 16 + TOK], F32, "sB", st)
            pl = kb.sb([128, TOK], BF16, "pl", st)
            t16 = kb.sb([128, 16], F32, "t16", st)
            invc = kb.sb([128, 4, 16], F32, "invc", st)
            wps = kb.sb([128, 4, 128], F32, "wps", st)
            wpb = kb.sb([128, 4, 128], BF16, "wpb", st)
            psc = kb.sb([128, 4], F32, "psc", st)
            ya = kb.sb([128, 4, TOK], BF16, "ya", st)
            kb.sp.dma(invc[:], C["invc"][:, :, :], writes=[invc])
            kb.sp.dma(wps[:], W["w_pool"].rearrange("g c d -> c g d"), writes=[wps])
            kb.pool.op(lambda: nc.gpsimd.tensor_copy(out=wpb[:], in_=wps[:]), reads=[wps], writes=[wpb])
            kb.sp.dma(psc[:], W["pool_scale"][:, :], writes=[psc])
            psr = Ring([P[2], P[3], P[4], P[5]])
            for g in range(4):
                w = 2 ** (g + 1)
                kb.sp.dma(hh[:, 0:16], PV["php"][g * 128:(g + 1) * 128, :], writes=[hh])
                kb.sp.dma(hh[:, 16:], A["hpT"][g * 128:(g + 1) * 128, :], writes=[hh])
                src, d, o = hh, 1, 1
                bufs = [sA, sB]
                for step in range(g + 1):
                    dst = bufs[step % 2]
                    kb.dve.op(lambda: nc.vector.tensor_tensor(out=dst[:, o:], in0=src[:, o:], in1=src[:, o - d:16 + TOK - d],
                                                              op=ALU.add), reads=[src], writes=[dst])
                    src = dst
                    d *= 2
                    o += d
                kb.dve.op(lambda: nc.vector.scalar_tensor_tensor(out=pl[:], in0=src[:, 16:], scalar=1.0 / w, in1=hh[:, 16:],
                                                                 op0=ALU.mult, op1=ALU.subtract),
                          reads=[src, hh], writes=[pl])
                kb.dve.op(lambda: nc.vector.tensor_tensor(out=t16[:], in0=src[:, 16:32], in1=invc[:, g, :], op=ALU.mult),
                          reads=[src, invc], writes=[t16])
                kb.dve.op(lambda: nc.vector.tensor_tensor(out=pl[:, 0:16], in0=t16[:], in1=hh[:, 16:32], op=ALU.subtract),
                          reads=[t16, hh], writes=[pl])
                for tt in range(4):
                    ps = psr.next()
                    kb.pe.op(lambda: nc.tensor.matmul(ps[:], lhsT=wpb[:, g, :], rhs=pl[:, tt * 512:(tt + 1) * 512],
                                                      start=True, stop=True), reads=[wpb, pl], writes=[ps])
                    kb.act.op(lambda: nc.scalar.activation(out=ya[:, g, tt * 512:(tt + 1) * 512], in_=ps[:], func=AF.Copy,
                                                           scale=psc[:, g:g + 1]), reads=[ps, psc], writes=[ya])
            kb.act.dma(yTv[0], ya[:], reads=[ya])
            kb.barrier()

        for st in sect("B2"):
            zb = kb.sb([128, 4, 32 + TOK], BF16, "zb", st)
            wdw = kb.sb([128, 4, 31], F32, "wdw", st)
            dg = kb.sb([128, 4 * 31, 128], BF16, "dg", st)
            vecs = kb.sb([128, 12], F32, "cvecs", st)
            v = kb.sb([128, 4, 512], F32, "cv", st)
            yd = kb.sb([128, 4, TOK], BF16, "yd", st)
            tmp = stat_tmps(kb, st)
            t1r = Ring([kb.sb([128, 512], F32, "t1", st) for _ in range(2)])
            t2r = Ring([kb.sb([128, 512], F32, "t2", st) for _ in range(2)])
            kb.sp.dma(zb[:, :, 0:32], PV["pz"].rearrange("(k p) n -> p k n", p=128), writes=[zb])
            kb.sp.dma(zb[:, :, 32:], A["zT"].rearrange("(k p) n -> p k n", p=128), writes=[zb])
            kb.sp.dma(wdw[:], W["w_dwT"][:, :, :], writes=[wdw])
            kb.sp.dma(vecs[:, 0:4], W["b_dw"][:, :], writes=[vecs])
            kb.sp.dma(vecs[:, 4:8], W["cln_g"][:, :], writes=[vecs])
            kb.sp.dma(vecs[:, 8:12], W["cln_b"][:, :], writes=[vecs])
            for c in range(4):
                for j in range(31):
                    kb.pool.op(lambda: nc.gpsimd.tensor_scalar(out=dg[:, c * 31 + j, :], in0=ident[:],
                                                               scalar1=wdw[:, c, j:j + 1], scalar2=None, op0=ALU.mult),
                               reads=[ident, wdw], writes=[dg])
            psr = Ring([P[2], P[3], P[4], P[5]])
            for tt in range(4):
                for c in range(4):
                    ps = psr.next()
                    for j in range(31):
                        o = 32 + tt * 512 - 30 + j
                        kb.pe.op(lambda: nc.tensor.matmul(ps[:], lhsT=dg[:, c * 31 + j, :], rhs=zb[:, c, o:o + 512],
                                                          start=(j == 0), stop=(j == 30)), reads=[dg, zb], writes=[ps])
                    kb.act.op(lambda: nc.scalar.activation(out=v[:, c, :], in_=ps[:], func=AF.Identity,
                                                           bias=vecs[:, c:c + 1], scale=1.0), reads=[ps, vecs], writes=[v])
                rstd, nmr = col_stats(kb, cx, [(v, v[:, c, :]) for c in range(4)], 512, True, tmp)
                for c in range(4):
                    t1 = t1r.next(); t2 = t2r.next()
                    kb.dve.op(lambda: nc.vector.tensor_tensor(out=t1[:], in0=v[:, c, :], in1=rstd[:], op=ALU.mult),
                              reads=[v, rstd], writes=[t1])
                    kb.pool.op(lambda: nc.gpsimd.tensor_tensor(out=t2[:], in0=t1[:], in1=nmr[:], op=ALU.add),
                               reads=[t1, nmr], writes=[t2])
                    kb.act.op(lambda: nc.scalar.activation(out=yd[:, c, tt * 512:(tt + 1) * 512], in_=t2[:], func=AF.Silu,
                                                           scale=vecs[:, 4 + c:5 + c], bias=vecs[:, 8 + c:9 + c]),
                              reads=[t2, vecs], writes=[yd])
            kb.act.dma(yTv[3], yd[:], reads=[yd])
            kb.barrier()

        def mk_T(st):
            T = {"identb": identb, "ei": [0]}
            T["ptps"] = Ring([PB[0], PB[1], PB[2], PB[3]])
            T["pts"] = Ring([kb.sb([128, 512], BF16, "pts", st) for _ in range(3)])
            T["Pb"] = Ring([kb.sb([128, 512], BF16, "Pb", st) for _ in range(3)])
            T["tmp"] = Ring([kb.sb([128, 512], F32, "atmp", st) for _ in range(3)])
            T["on"] = Ring([kb.sb([128, 128], BF16, "on", st) for _ in range(2)])
            T["rs"] = Ring([kb.sb([128, 16], F32, "rs", st) for _ in range(2)])
            T["rsum"] = kb.sb([128, 1], F32, "rsum", st)
            T["rinv"] = kb.sb([128, 1], F32, "rinv", st)
            return T

        def load_kv(st, K_own, K_prev, V_own, V_prev):
            Kb = kb.sb([128, 4, 2 * TOK], BF16, "Kb", st)
            Vb = kb.sb([128, 32, 512], BF16, "Vb", st)
            kb.sp.dma(Kb[:, :, 0:TOK], K_prev.rearrange("(k p) n -> p k n", p=128), writes=[Kb])
            kb.sp.dma(Kb[:, :, TOK:], K_own.rearrange("(k p) n -> p k n", p=128), writes=[Kb])
            kb.sp.dma(Vb[:, 0:16, :], V_prev.rearrange("(c p) d -> p c d", p=128), writes=[Vb])
            kb.sp.dma(Vb[:, 16:32, :], V_own.rearrange("(c p) d -> p c d", p=128), writes=[Vb])
            return Kb, Vb

        for st in sect("B3"):
            T = mk_T(st)
            Kb, Vb = load_kv(st, A["knT"], PV["pknT"], A["mV"], PV["pmV"])
            Qb = kb.sb([128, 4, TOK], BF16, "Qb", st)
            Qr = kb.sb([128, 4, TOK], BF16, "Qr", st)
            Kr = kb.sb([128, 2 * TOK], BF16, "Kr", st)
            kb.pool.op(lambda: nc.gpsimd.memset(Qr[:], 0.0), writes=[Qr])
            kb.pool.op(lambda: nc.gpsimd.memset(Kr[:], 0.0), writes=[Kr])
            CM = kb.sb([128, 128], F32, "CM", st)
            prevb = kb.sb([128, 2], F32, "prevb", st)
            yc = kb.sb([128, 4, TOK], BF16, "yc", st)
            kb.sp.dma(Qb[:], A["qnT"].rearrange("(k p) n -> p k n", p=128), writes=[Qb])
            kb.sp.dma(Qr[0:64], A["qrT"].rearrange("(k p) n -> p k n", p=64), writes=[Qr])
            kb.sp.dma(Kr[0:64, 0:TOK], PV["pkrT"][:, :], writes=[Kr])
            kb.sp.dma(Kr[0:64, TOK:], A["krT"][:, :], writes=[Kr])
            kb.sp.dma(CM[:], C["CM"][:, :], writes=[CM])
            kb.sp.dma(prevb[:], C["prevb"][:, :], writes=[prevb])
            scale = 192.0 ** -0.5
            psS = Ring([P[2], P[3]])
            psO = Ring([P[4], P[5]])
            for i in range(DBG["ni"]):
                qs = slice(i * 128, (i + 1) * 128)
                tl = key_tiles(i)
                for h in range(4):
                    po = psO.next()
                    rs = T["rs"].next()
                    npc = 0
                    for ti, (s0, Wd, kind) in enumerate(tl):
                        ps = psS.next()
                        kb.pe.op(lambda: nc.tensor.matmul(ps[:, :Wd], lhsT=Qb[:, h, qs], rhs=Kb[:, h, s0:s0 + Wd],
                                                          start=True, stop=False), reads=[Qb, Kb], writes=[ps])
                        kb.pe.op(lambda: nc.tensor.matmul(ps[:, :Wd], lhsT=Qr[:, h, qs], rhs=Kr[:, s0:s0 + Wd],
                                                          start=False, stop=True), reads=[Qr, Kr], writes=[ps])
                        Pb = T["Pb"].next()
                        if kind == 2:
                            tm = T["tmp"].next()
                            if Wd > 128:
                                kb.dve.op(lambda: nc.vector.tensor_scalar(out=tm[:, :Wd - 128], in0=ps[:, :Wd - 128],
                                                                          scalar1=scale, scalar2=None, op0=ALU.mult),
                                          reads=[ps], writes=[tm])
                            kb.dve.op(lambda: nc.vector.scalar_tensor_tensor(out=tm[:, Wd - 128:Wd], in0=ps[:, Wd - 128:Wd],
                                                                             scalar=scale, in1=CM[:], op0=ALU.mult,
                                                                             op1=ALU.add), reads=[ps, CM], writes=[tm])
                            kb.act.op(lambda: nc.scalar.activation(out=Pb[:, :Wd], in_=tm[:, :Wd], func=AF.Exp,
                                                                   accum_out=rs[:, npc:npc + 1]),
                                      reads=[tm], writes=[Pb, rs])
                            npc += 1
                        else:
                            if kind == 0:
                                kb.act.op(lambda: nc.scalar.activation(out=Pb[:, :Wd], in_=ps[:, :Wd], func=AF.Exp,
                                                                       scale=scale, bias=prevb[:, 0:1],
                                                                       accum_out=rs[:, npc:npc + 1]),
                                          reads=[ps, prevb], writes=[Pb, rs])
                            else:
                                kb.act.op(lambda: nc.scalar.activation(out=Pb[:, :Wd], in_=ps[:, :Wd], func=AF.Exp,
                                                                       scale=scale, accum_out=rs[:, npc:npc + 1]),
                                          reads=[ps], writes=[Pb, rs])
                            npc += 1
                        if DBG["tail"]:
                            attn_tail(kb, cx, T, Pb, Wd, s0 // 128, Vb, h, po, ti == 0, ti == len(tl) - 1)
                    if DBG["fin"]:
                        attn_finish(kb, cx, T, po, rs, npc, yc, h, i)
            kb.act.dma(yTv[2], yc[:], reads=[yc])
            kb.barrier()

        for st in sect("B4"):
            T = mk_T(st)
            Kb, Vb = load_kv(st, A["dkT"], PV["pdkT"], A["dV"], PV["pdV"])
            Qb = kb.sb([128, 4, TOK], BF16, "Qb", st)
            Ik = kb.sb([128, 2 * TOK], BF16, "Ik", st)
            iqr = Ring([kb.sb([128, 16, 128], BF16, "iq", st) for _ in range(2)])
            kb.pool.op(lambda: nc.gpsimd.memset(Ik[:], 0.0), writes=[Ik])
            for b_ in iqr.bufs:
                kb.pool.op(lambda: nc.gpsimd.memset(b_[:], 0.0), writes=[b_])
            iwr = Ring([kb.sb([128, 16], F32, "iwt", st) for _ in range(2)])
            iwa = kb.sb([128, 16], F32, "iwa", st)
            iws = kb.sb([128, 16], F32, "iws", st)
            dsg = kb.sb([128, 16, 128], BF16, "dsg", st)
            Rr = Ring([kb.sb([128, 512], BF16, "R", st) for _ in range(3)])
            scb = kb.sb([128, 2 * TOK], F32, "scb", st)
            junk = kb.sb([128, 2 * TOK], BF16, "junk", st)
            AB = kb.sb([128, 4, 512], F32, "AB", st)
            ABD = kb.sb([128, 16, 128], F32, "ABD", st)
            CMB = kb.sb([128, 128], F32, "CMB", st)
            prevs = kb.sb([128, 2], F32, "prevs", st)
            cbt = kb.sb([128, 512], F32, "cbt", st)
            pw = kb.sb([128, NBIS + 1], F32, "pw", st)
            am = kb.sb([128, 8], F32, "am", st)
            sm = {n: kb.sb([128, 1], F32, n, st) for n in ("M", "lo", "W0", "mid", "cnt", "stp")}
            wst_ = kb.sb([128, NBIS + 1], F32, "wsteps", st)
            yb = kb.sb([128, 4, TOK], BF16, "yb", st)
            kb.sp.dma(Qb[:], A["dqT"].rearrange("(k p) n -> p k n", p=128), writes=[Qb])
            kb.sp.dma(Ik[0:64, 0:TOK], PV["pikT"][:, :], writes=[Ik])
            kb.sp.dma(Ik[0:64, TOK:], A["ikT"][:, :], writes=[Ik])
            for nm, t_ in (("AB", AB), ("ABD", ABD)):
                kb.sp.dma(t_[:], C[nm][:, :, :], writes=[t_])
            for nm, t_ in (("CMB", CMB), ("prevs", prevs), ("cb", cbt), ("pw", pw)):
                kb.sp.dma(t_[:], C[nm][:, :], writes=[t_])
            scale = 128.0 ** -0.5
            psS = Ring([P[2], P[3]])
            psO = Ring([P[4], P[5]])
            iqv = A["iqT"].rearrange("(h d) n -> d h n", d=64)
            iwv = A["iw"].rearrange("(b p) h -> b p h", p=128)
            for i in range(DBG["ni"]):
                qs = slice(i * 128, (i + 1) * 128)
                tl = key_tiles(i)
                Ntot = tl[-1][0] + tl[-1][1]
                iq = iqr.next()
                iwt = iwr.next()
                kb.sp.dma(iq[0:64], iqv[:, :, qs], writes=[iq])
                kb.sp.dma(iwt[:], iwv[i], writes=[iwt])
                kb.act.op(lambda: nc.scalar.activation(out=iwa[:], in_=iwt[:], func=AF.Abs), reads=[iwt], writes=[iwa])
                kb.act.op(lambda: nc.scalar.activation(out=iws[:], in_=iwt[:], func=AF.Sign), reads=[iwt], writes=[iws])
                for hh in range(16):
                    kb.pool.op(lambda: nc.gpsimd.tensor_scalar(out=dsg[:, hh, :], in0=ident[:], scalar1=iws[:, hh:hh + 1],
                                                               scalar2=None, op0=ALU.mult),
                               reads=[ident, iws], writes=[dsg])
                for ti, (s0, Wd, kind) in enumerate(tl if DBG["idx"] else []):
                    pss = P[0] if ti % 2 == 0 else P[1]
                    Rs = {}
                    for hh in range(17):
                        if hh < 16:
                            ps = psS.next()
                            kb.pe.op(lambda: nc.tensor.matmul(ps[:, :Wd], lhsT=iq[:, hh, :], rhs=Ik[:, s0:s0 + Wd],
                                                              start=True, stop=True), reads=[iq, Ik], writes=[ps])
                            R = Rr.next()
                            kb.act.op(lambda: nc.scalar.activation(out=R[:, :Wd], in_=ps[:, :Wd], func=AF.Relu,
                                                                   scale=iwa[:, hh:hh + 1]), reads=[ps, iwa], writes=[R])
                            Rs[hh] = R
                        if hh >= 1 and DBG.get("acc", True):
                            g_ = hh - 1
                            R_ = Rs.pop(g_)
                            kb.pe.op(lambda: nc.tensor.matmul(pss[:, :Wd], lhsT=dsg[:, g_, :], rhs=R_[:, :Wd],
                                                              start=(g_ == 0), stop=(g_ == 15)), reads=[dsg, R_],
                                     writes=[pss])
                    if not DBG.get("post", True):
                        continue
                    kb.dve.op(lambda: nc.vector.tensor_reduce(out=am[:, ti:ti + 1], in_=pss[:, :Wd], axis=AX.X, op=ALU.max,
                                                              apply_absolute_value=True), reads=[pss], writes=[am])
                    if DBG.get("post", 2) == 1:
                        continue
                    if kind == 0:
                        kb.dve.op(lambda: nc.vector.tensor_scalar(out=scb[:, s0:s0 + Wd], in0=pss[:, :Wd],
                                                                  scalar1=prevs[:, 0:1], scalar2=None, op0=ALU.add),
                                  reads=[pss, prevs], writes=[scb])
                    else:
                        if kind == 1 or Wd > 128:
                            We = Wd if kind == 1 else Wd - 128
                            kb.dve.op(lambda: nc.vector.tensor_copy(out=scb[:, s0:s0 + We], in_=pss[:, :We]), reads=[pss],
                                      writes=[scb])
                        if kind == 2:
                            kb.dve.op(lambda: nc.vector.tensor_tensor(out=scb[:, s0 + Wd - 128:s0 + Wd],
                                                                      in0=pss[:, Wd - 128:Wd], in1=CMB[:], op=ALU.add),
                                      reads=[pss, CMB], writes=[scb])
                nt = len(tl)
                M, lo, W0, mid, cnt, stp = (sm[n] for n in ("M", "lo", "W0", "mid", "cnt", "stp"))
                kb.dve.op(lambda: nc.vector.tensor_reduce(out=M[:], in_=am[:, 0:nt], axis=AX.X, op=ALU.max), reads=[am], writes=[M])
                kb.dve.op(lambda: nc.vector.tensor_scalar(out=lo[:], in0=M[:], scalar1=-1.0, scalar2=-1.0, op0=ALU.mult,
                                                          op1=ALU.add), reads=[M], writes=[lo])
                kb.dve.op(lambda: nc.vector.tensor_scalar(out=W0[:], in0=M[:], scalar1=2.0, scalar2=2.0, op0=ALU.mult,
                                                          op1=ALU.add), reads=[M], writes=[W0])
                kb.dve.op(lambda: nc.vector.tensor_scalar(out=wst_[:], in0=pw[:], scalar1=W0[:, 0:1], scalar2=None,
                                                          op0=ALU.mult), reads=[pw, W0], writes=[wst_])
                kb.dve.op(lambda: nc.vector.tensor_tensor(out=mid[:], in0=lo[:], in1=wst_[:, 0:1], op=ALU.add),
                          reads=[lo, wst_], writes=[mid])
                for it in range(DBG["nbis"]):
                    kb.dve.op(lambda: nc.vector.tensor_scalar(out=junk[:, :Ntot], in0=scb[:, :Ntot], scalar1=mid[:, 0:1],
                                                              scalar2=None, op0=ALU.is_ge, op1=ALU.add, accum_out=cnt[:]),
                              reads=[scb, mid], writes=[junk, cnt])
                    kb.dve.op(lambda: nc.vector.tensor_scalar(out=stp[:], in0=cnt[:], scalar1=255.5,
                                                              scalar2=wst_[:, it:it + 1], op0=ALU.is_ge, op1=ALU.mult),
                              reads=[cnt, wst_], writes=[stp])
                    kb.dve.op(lambda: nc.vector.tensor_tensor(out=lo[:], in0=lo[:], in1=stp[:], op=ALU.add),
                              reads=[lo, stp], writes=[lo])
                    kb.dve.op(lambda: nc.vector.tensor_tensor(out=mid[:], in0=lo[:], in1=wst_[:, it + 1:it + 2],
                                                              op=ALU.add), reads=[lo, wst_], writes=[mid])
                kb.dve.op(lambda: nc.vector.tensor_scalar(out=junk[:, :Ntot], in0=scb[:, :Ntot], scalar1=lo[:, 0:1],
                                                          scalar2=NEGM, op0=ALU.is_lt, op1=ALU.mult),
                          reads=[scb, lo], writes=[junk])
                rss = [T["rs"].next() for _ in range(2)]
                for h in range(4 if DBG["att"] else 0):
                    po = psO.next()
                    rs = rss[h % 2]
                    npc = 0
                    for ti, (s0, Wd, kind) in enumerate(tl):
                        ps = psS.next()
                        kb.pe.op(lambda: nc.tensor.matmul(ps[:, :Wd], lhsT=Qb[:, h, qs], rhs=Kb[:, h, s0:s0 + Wd],
                                                          start=True, stop=False), reads=[Qb, Kb], writes=[ps])
                        kb.pe.op(lambda: nc.tensor.matmul(ps[:, :Wd], lhsT=identb[:], rhs=junk[:, s0:s0 + Wd], start=False,
                                                          stop=True), reads=[identb, junk], writes=[ps])
                        tm = T["tmp"].next()
                        Pb = T["Pb"].next()
                        cbc = (i * 4 + h) * 8 + ti
                        if kind == 2:
                            wv_ = Wd // 128 - 1
                            if Wd > 128:
                                kb.dve.op(lambda: nc.vector.scalar_tensor_tensor(out=tm[:, :Wd - 128], in0=ps[:, :Wd - 128],
                                                                                 scalar=scale, in1=AB[:, h, :Wd - 128],
                                                                                 op0=ALU.mult, op1=ALU.add),
                                          reads=[ps, AB], writes=[tm])
                            kb.dve.op(lambda: nc.vector.scalar_tensor_tensor(out=tm[:, Wd - 128:Wd], in0=ps[:, Wd - 128:Wd],
                                                                             scalar=scale, in1=ABD[:, h * 4 + wv_, :],
                                                                             op0=ALU.mult, op1=ALU.add),
                                      reads=[ps, ABD], writes=[tm])
                            kb.act.op(lambda: nc.scalar.activation(out=Pb[:, :Wd], in_=tm[:, :Wd], func=AF.Exp,
                                                                   bias=cbt[:, cbc:cbc + 1], accum_out=rs[:, npc:npc + 1]),
                                      reads=[tm, cbt], writes=[Pb, rs])
                            npc += 1
                        else:
                            kb.dve.op(lambda: nc.vector.scalar_tensor_tensor(out=tm[:, :Wd], in0=ps[:, :Wd], scalar=scale,
                                                                             in1=AB[:, h, :Wd], op0=ALU.mult, op1=ALU.add),
                                      reads=[ps, AB], writes=[tm])
                            kb.act.op(lambda: nc.scalar.activation(out=Pb[:, :Wd], in_=tm[:, :Wd], func=AF.Exp,
                                                                   bias=cbt[:, cbc:cbc + 1], accum_out=rs[:, npc:npc + 1]),
                                      reads=[tm, cbt], writes=[Pb, rs])
                            npc += 1
                        attn_tail(kb, cx, T, Pb, Wd, s0 // 128, Vb, h, po, ti == 0, ti == len(tl) - 1)
                    attn_finish(kb, cx, T, po, rs, npc, yb, h, i)
            kb.act.dma(yTv[1], yb[:], reads=[yb])
            kb.barrier()


C_W = {"wg_ct": [4 * 16, 128, KD, 128], "wb_ct": [4 * 16, 128, 4, 128], "wo_ct": [16, 128, KD, 128],
       "wfi_ct": [88, 128, KD, 128], "wfo_ct": [16, 128, 44, 128], "b_gate": [128, 64], "ln1_g": [128, 16],
       "ln1_b": [128, 16], "ln2_g": [128, 16], "ln2_b": [128, 16]}


def ctl(w, kchunks):
    K_, C_ = w.shape
    return np.ascontiguousarray(w.reshape(kchunks, 128, C_ // 128, 128).transpose(2, 1, 0, 3))


def host_C_weights(inputs, l):
    out = {}
    out["wg_ct"] = np.concatenate([ctl(inputs["w_gate"][l, i], KD) for i in range(4)], axis=0)
    out["wb_ct"] = np.concatenate([ctl(inputs["w_branch"][l, i], 4) for i in range(4)], axis=0)
    out["wo_ct"] = ctl(inputs["w_o"][l], KD)
    out["wfi_ct"] = ctl(inputs["w_ffn_in"][l], KD)
    out["wfo_ct"] = ctl(inputs["w_ffn_out"][l], 44)
    out["b_gate"] = np.ascontiguousarray(np.concatenate([pk(inputs["b_gate"][l, i]) for i in range(4)], axis=1))
    for n in ("ln1_g", "ln1_b", "ln2_g", "ln2_b"):
        out[n] = pk(inputs[n][l])
    return out


def stage_C(kb, cx, l, xT, uT_d, yT_d, W, S, xoT):
    nc = kb.nc
    P = cx.P
    v3 = lambda ap: ap.rearrange("(k p) n -> p k n", p=128)
    with ExitStack() as st:
        vec = kb.sb([128, 128], F32, "cvec", st)
        kb.sp.dma(vec[:, 0:64], W["b_gate"][:, :], writes=[vec])
        for j, n in enumerate(("ln1_g", "ln1_b", "ln2_g", "ln2_b")):
            kb.sp.dma(vec[:, 64 + 16 * j:80 + 16 * j], W[n][:, :], writes=[vec])
        wst = Ring([kb.sb([128, 16, 128], F32, "cwst", st) for _ in range(2)])
        wbf = Ring([kb.sb([128, 16, 128], BF16, "cwbf", st) for _ in range(3)])
        wfo = kb.sb([128, 44, 128], BF16, "cwfo", st)
        tmp = stat_tmps(kb, st)
        t1r = Ring([kb.sb([128, 512], F32, "t1", st) for _ in range(2)])
        t2r = Ring([kb.sb([128, 512], F32, "t2", st) for _ in range(2)])
        sgr = Ring([kb.sb([128, 512], F32, "sg", st) for _ in range(2)])
        u = kb.sb([128, KD, 512], BF16, "cu", st)
        z = kb.sb([128, KD, 512], F32, "cz", st)
        hb = kb.sb([128, 44, 512], BF16, "chb", st)
        mg = BufK(hb.h, "cmg", 0, 16)
        y = BufK(hb.h, "cy", 16, 16)
        acc = kb.sb([128, 512], F32, "cacc", st)
        xt = Ring([kb.sb([128, 512], F32, "cxt", st) for _ in range(2)])
        psr = Ring([P[2], P[3], P[4], P[5]])

        def wload(src, nk):
            wb = wbf.next()
            s = wst.next()
            kb.sp.dma(s[:, :nk, :], src, writes=[s])
            kb.pool.op(lambda: nc.gpsimd.tensor_copy(out=wb[:, :nk, :], in_=s[:, :nk, :]), reads=[s], writes=[wb])
            return wb

        def mm(ps, wb, nk, rhs_buf, rhs_fn):
            for k in range(nk):
                kb.pe.op(lambda: nc.tensor.matmul(ps[:], lhsT=wb[:, k, :], rhs=rhs_fn(k), start=(k == 0), stop=(k == nk - 1)),
                         reads=[wb, rhs_buf], writes=[ps])

        def layer_norm_to(src, gcol, bcol, dst_fn, dst_buf, also=None):
            rstd, nmr = col_stats(kb, cx, [(src, src[:, k, :]) for k in range(KD)], D, True, tmp)
            for k in range(KD):
                t1 = t1r.next(); t2 = t2r.next()
                kb.dve.op(lambda: nc.vector.tensor_tensor(out=t1[:], in0=src[:, k, :], in1=rstd[:], op=ALU.mult),
                          reads=[src, rstd], writes=[t1])
                kb.pool.op(lambda: nc.gpsimd.tensor_tensor(out=t2[:], in0=t1[:], in1=nmr[:], op=ALU.add),
                           reads=[t1, nmr], writes=[t2])
                kb.act.op(lambda: nc.scalar.activation(out=dst_fn(k), in_=t2[:], func=AF.Identity,
                                                       scale=vec[:, gcol + k:gcol + k + 1], bias=vec[:, bcol + k:bcol + k + 1]),
                          reads=[t2, vec], writes=[dst_buf])

        def adaln_to(src, l_, isc, ish, dst):
            rstd, nmr = col_stats(kb, cx, [(src, src[:, k, :]) for k in range(KD)], D, True, tmp)
            for k in range(KD):
                t1 = t1r.next(); t2 = t2r.next()
                kb.dve.op(lambda: nc.vector.tensor_tensor(out=t1[:], in0=src[:, k, :], in1=rstd[:], op=ALU.mult),
                          reads=[src, rstd], writes=[t1])
                kb.pool.op(lambda: nc.gpsimd.tensor_tensor(out=t2[:], in0=t1[:], in1=nmr[:], op=ALU.add),
                           reads=[t1, nmr], writes=[t2])
                kb.act.op(lambda: nc.scalar.activation(out=dst[:, k, :], in_=t2[:], func=AF.Identity,
                                                       scale=cx.onep[:, mcol(l_, isc, k):mcol(l_, isc, k) + 1],
                                                       bias=cx.mod[:, mcol(l_, ish, k):mcol(l_, ish, k) + 1]),
                          reads=[t2, cx.onep, cx.mod], writes=[dst])

        for tt in range(4):
            ts = slice(tt * 512, (tt + 1) * 512)
            kb.barrier()
            kb.sp.dma(u[:], v3(uT_d)[:, :, ts], writes=[u])
            kb.sp.dma(y[:], v3(yT_d)[:, :, ts], writes=[y])
            for j in range(16):
                for i in range(4):
                    wg = wload(W["wg_ct"][i * 16 + j], KD)
                    wbr = wload(W["wb_ct"][i * 16 + j], 4)
                    pg = psr.next(); pb = psr.next()
                    mm(pg, wg, KD, u, lambda k: u[:, k, :])
                    mm(pb, wbr, 4, y, lambda k: y[:, i * 4 + k, :])
                    sg = sgr.next()
                    kb.act.op(lambda: nc.scalar.activation(out=sg[:], in_=pg[:], func=AF.Sigmoid,
                                                           bias=vec[:, i * 16 + j:i * 16 + j + 1], scale=1.0),
                              reads=[pg, vec], writes=[sg])
                    if i == 0:
                        kb.dve.op(lambda: nc.vector.tensor_tensor(out=acc[:], in0=pb[:], in1=sg[:], op=ALU.mult),
                                  reads=[pb, sg], writes=[acc])
                    else:
                        t1 = t1r.next()
                        kb.dve.op(lambda: nc.vector.tensor_tensor(out=t1[:], in0=pb[:], in1=sg[:], op=ALU.mult),
                                  reads=[pb, sg], writes=[t1])
                        if i < 3:
                            kb.pool.op(lambda: nc.gpsimd.tensor_tensor(out=acc[:], in0=acc[:], in1=t1[:], op=ALU.add),
                                       reads=[acc, t1], writes=[acc])
                        else:
                            kb.pool.op(lambda: nc.gpsimd.tensor_tensor(out=mg[:, j, :], in0=acc[:], in1=t1[:], op=ALU.add),
                                       reads=[acc, t1], writes=[mg])
            for j in range(16):
                wo = wload(W["wo_ct"][j], KD)
                ps = psr.next()
                mm(ps, wo, KD, mg, lambda k: mg[:, k, :])
                x_ = xt.next()
                kb.sp.dma(x_[:], xT[j * 128:(j + 1) * 128, ts], writes=[x_])
                t1 = t1r.next()
                kb.act.op(lambda: nc.scalar.activation(out=t1[:], in_=ps[:], func=AF.Copy,
                                                       scale=cx.onep[:, mcol(l, 2, j):mcol(l, 2, j) + 1]),
                          reads=[ps, cx.onep], writes=[t1])
                kb.dve.op(lambda: nc.vector.scalar_tensor_tensor(out=z[:, j, :], in0=x_[:], scalar=ALPHA, in1=t1[:],
                                                                 op0=ALU.mult, op1=ALU.add), reads=[x_, t1], writes=[z])
            layer_norm_to(z, 64, 80, lambda k: z[:, k, :], z)
            adaln_to(z, l, 4, 3, u)
            for j in range(44):
                wa = wload(W["wfi_ct"][j], KD)
                wg_ = wload(W["wfi_ct"][44 + j], KD)
                pa = psr.next(); pg = psr.next()
                mm(pa, wa, KD, u, lambda k: u[:, k, :])
                mm(pg, wg_, KD, u, lambda k: u[:, k, :])
                sg = sgr.next()
                kb.act.op(lambda: nc.scalar.activation(out=sg[:], in_=pa[:], func=AF.Silu), reads=[pa], writes=[sg])
                kb.dve.op(lambda: nc.vector.tensor_tensor(out=hb[:, j, :], in0=pg[:], in1=sg[:], op=ALU.mult),
                          reads=[pg, sg], writes=[hb])
            for j in range(16):
                for (k0_, nk_) in ((0, 16), (16, 16), (32, 12)):
                    s_ = wst.next()
                    kb.sp.dma(s_[:, :nk_, :], W["wfo_ct"][j][:, k0_:k0_ + nk_, :], writes=[s_])
                    kb.pool.op(lambda: nc.gpsimd.tensor_copy(out=wfo[:, k0_:k0_ + nk_, :], in_=s_[:, :nk_, :]),
                               reads=[s_], writes=[wfo])
                ps = psr.next()
                mm(ps, wfo, 44, hb, lambda k: hb[:, k, :])
                t1 = t1r.next()
                kb.act.op(lambda: nc.scalar.activation(out=t1[:], in_=ps[:], func=AF.Copy,
                                                       scale=cx.onep[:, mcol(l, 5, j):mcol(l, 5, j) + 1]),
                          reads=[ps, cx.onep], writes=[t1])
                kb.dve.op(lambda: nc.vector.scalar_tensor_tensor(out=z[:, j, :], in0=z[:, j, :], scalar=ALPHA, in1=t1[:],
                                                                 op0=ALU.mult, op1=ALU.add), reads=[z, t1], writes=[z])
            layer_norm_to(z, 96, 112, lambda k: z[:, k, :], z)
            kb.act.dma(v3(xoT)[:, :, ts], z[:], reads=[z])
        kb.barrier()


def _launch(nc, in_maps):
    res = run_bass_kernel_spmd(nc, in_maps, core_ids=list(range(8)))
    return res.results


def build_B(l):
    kb = KB()
    modT = kb.dram("modT", [128, DEPTH * 96], F32, kind="ExternalInput")
    A = {k: kb.dram(k, v[0], dts(v[1]), kind="ExternalInput") for k, v in A_OUT.items() if k != "uT"}
    PV = {k: kb.dram(k, v[0], dts(v[1]), kind="ExternalInput") for k, v in B_PREV.items()}
    W = {k: kb.dram(k, v, F32, kind="ExternalInput") for k, v in B_W.items()}
    C = {k: kb.dram(k, v, F32, kind="ExternalInput") for k, v in B_C.items()}
    yT = kb.dram("yT", [2048, TOK], BF16, kind="ExternalOutput")
    cx = setup_common(kb, modT)
    stage_B(kb, cx, l, A, PV, W, C, yT)
    kb.finish()
    return kb.nc


def build_C(l):
    kb = KB()
    modT = kb.dram("modT", [128, DEPTH * 96], F32, kind="ExternalInput")
    xT = kb.dram("xT", [D, TOK], F32, kind="ExternalInput")
    uT = kb.dram("uT", [D, TOK], BF16, kind="ExternalInput")
    yT = kb.dram("yT", [2048, TOK], BF16, kind="ExternalInput")
    W = {k: kb.dram(k, v, F32, kind="ExternalInput") for k, v in C_W.items()}
    xo = kb.dram("xo", [D, TOK], F32, kind="ExternalOutput")
    cx = setup_common(kb, modT)
    stage_C(kb, cx, l, xT, uT, yT, W, {}, xo)
    kb.finish()
    return kb.nc


def kernel_unfused(**inputs):
    inputs = {k: np.asarray(v) for k, v in inputs.items()}
    x = inputs["x"]
    mods = run_mods(inputs)
    xT = []
    for core in range(8):
        b, h = core // 2, core % 2
        xT.append(np.ascontiguousarray(x[b, h * TOK:(h + 1) * TOK, :].T))
    for l in range(DEPTH):
        wA = [host_A_weights(inputs, l, h) for h in range(2)]
        in_maps = []
        for core in range(8):
            b, h = core // 2, core % 2
            m = {"xT": xT[core], "modT": mods[b]}
            m.update(wA[h])
            in_maps.append(m)
        ra = _launch(build_A(l), in_maps)
        wB = host_B_weights(inputs, l)
        cB = [host_B_consts(h) for h in range(2)]
        in_maps = []
        pm = {"pdkT": "dkT", "pdV": "dV", "pikT": "ikT", "pknT": "knT", "pmV": "mV", "pkrT": "krT"}
        for core in range(8):
            b, h = core // 2, core % 2
            own = ra[core]
            m = {"modT": mods[b]}
            for k in A_OUT:
                if k != "uT":
                    m[k] = np.asarray(own[k])
            if h == 1:
                prev = ra[core - 1]
                for k, v in pm.items():
                    m[k] = np.asarray(prev[v])
                m["pz"] = np.ascontiguousarray(np.asarray(prev["zT"])[:, -32:])
                m["php"] = np.ascontiguousarray(np.asarray(prev["hpT"])[:, -16:])
            else:
                for k, v in pm.items():
                    m[k] = np.zeros_like(np.asarray(own[v]))
                m["pz"] = np.zeros_like(np.asarray(own["zT"])[:, -32:])
                m["php"] = np.zeros((512, 16), np.float32)
            m.update(wB)
            m.update(cB[h])
            in_maps.append(m)
        rb = _launch(build_B(l), in_maps)
        wC = host_C_weights(inputs, l)
        in_maps = []
        for core in range(8):
            b = core // 2
            m = {"modT": mods[b], "xT": xT[core], "uT": np.asarray(ra[core]["uT"]), "yT": np.asarray(rb[core]["yT"])}
            m.update(wC)
            in_maps.append(m)
        rc = _launch(build_C(l), in_maps)
        xT = [np.asarray(rc[core]["xo"]) for core in range(8)]
    out = np.empty((NB, SEQ, D), np.float32)
    for core in range(8):
        b, h = core // 2, core % 2
        out[b, h * TOK:(h + 1) * TOK, :] = xT[core].T
    return out


def stage_M(kb, cx, cT, wadas, baT):
    nc = kb.nc
    with ExitStack() as st:
        csb = kb.sb([128, KD, 4], F32, "csb", st)
        cact = kb.sb([128, KD, 4], F32, "cact", st)
        basb = kb.sb([128, DEPTH * 96], F32, "basb", st)
        wr = Ring([kb.sb([128, KD, 512], F32, "wst", st) for _ in range(2)])
        pr = Ring([cx.P[2], cx.P[3]])
        kb.sp.dma(csb[:], cT[:, :, :], writes=[csb])
        kb.sp.dma(basb[:], baT[:, :], writes=[basb])
        kb.act.op(lambda: nc.scalar.activation(out=cact[:], in_=csb[:], func=AF.Silu), reads=[csb], writes=[cact])
        for l in range(DEPTH):
            wav = wadas[l].rearrange("(k p) n -> p k n", p=128)
            for g in range(24):
                w = wr.next()
                kb.sp.dma(w[:], wav[:, :, g * 512:(g + 1) * 512], writes=[w])
                for j in range(4):
                    t = l * 96 + g * 4 + j
                    p = pr.next()
                    for k in range(KD):
                        kb.pe.op(lambda: nc.tensor.matmul(p[:, 0:4], lhsT=w[:, k, j * 128:(j + 1) * 128], rhs=cact[:, k, :],
                                                          start=(k == 0), stop=(k == KD - 1)),
                                 reads=[w, cact], writes=[p])
                    kb.dve.op(lambda: nc.vector.tensor_scalar(out=cx.mod[:, t:t + 1], in0=p[:, 0:1],
                                                              scalar1=basb[:, t:t + 1], scalar2=None, op0=ALU.add),
                              reads=[p, basb], writes=[cx.mod])
        kb.dve.op(lambda: nc.vector.tensor_scalar(out=cx.onep[:], in0=cx.mod[:], scalar1=1.0, scalar2=None, op0=ALU.add),
                  reads=[cx.mod], writes=[cx.onep])
        kb.barrier()


A_WS = {k: v for k, v in A_W.items() if k not in ("ropec", "ropes")}
ROPE = {"ropec": [64, TOK], "ropes": [64, TOK]}


def build_fused():
    kb = KB()
    ext = lambda n, shp, dt=F32: kb.dram(n, shp, dt, kind="ExternalInput")
    xT = ext("xT", [D, SEQ])
    cT = ext("cT", [128, KD, 4])
    wadas = [ext(f"w_ada{l}", [D, 6 * D]) for l in range(DEPTH)]
    baT = ext("baT", [128, DEPTH * 96])
    z32 = ext("z32", [512, 32], BF16)
    z16 = ext("z16", [512, 16])
    rope = [{k: ext(f"{k}_h{h}", v) for k, v in ROPE.items()} for h in range(2)]
    BC = [{k: ext(f"{k}_h{h}", v) for k, v in B_C.items()} for h in range(2)]
    WA = [{k: ext(f"L{l}_{k}", v) for k, v in A_WS.items()} for l in range(DEPTH)]
    WB = [{k: ext(f"L{l}_{k}", v) for k, v in B_W.items()} for l in range(DEPTH)]
    WC = [{k: ext(f"L{l}_{k}", v) for k, v in C_W.items()} for l in range(DEPTH)]
    xo = kb.dram("xo", [D, SEQ], F32, kind="ExternalOutput")
    x1 = kb.dram("x1", [D, SEQ], F32)
    cx = setup_common(kb, None)
    stage_M(kb, cx, cT, wadas, baT)
    S = {k: kb.dram(f"S_{k}", v, F32) for k, v in A_SCR.items()}
    for l in range(DEPTH):
        xin = xT if l == 0 else x1
        xout = x1 if l == 0 else xo
        AO = [{k: kb.dram(f"A{l}{h}_{k}", v[0], dts(v[1])) for k, v in A_OUT.items()} for h in range(2)]
        yT = [kb.dram(f"y{l}{h}", [2048, TOK], BF16) for h in range(2)]
        for h in range(2):
            W = dict(WA[l])
            W.update(rope[h])
            stage_A(kb, cx, l, xin[:, h * TOK:(h + 1) * TOK], W, AO[h], S)
        for h in range(2):
            pm = {"pdkT": "dkT", "pdV": "dV", "pikT": "ikT", "pknT": "knT", "pmV": "mV", "pkrT": "krT"}
            PV = {k: AO[0][v] for k, v in pm.items()}
            if h == 1:
                PV["pz"] = AO[0]["zT"][:, TOK - 32:TOK]
                PV["php"] = AO[0]["hpT"][:, TOK - 16:TOK]
            else:
                PV["pz"] = z32
                PV["php"] = z16
            stage_B(kb, cx, l, AO[h], PV, WB[l], BC[h], yT[h])
        for h in range(2):
            stage_C(kb, cx, l, xin[:, h * TOK:(h + 1) * TOK], AO[h]["uT"], yT[h], WC[l], {},
                    xout[:, h * TOK:(h + 1) * TOK])
    kb.finish()
    return kb.nc


def kernel(**inputs):
    import ml_dtypes
    inputs = {k: np.asarray(v) for k, v in inputs.items()}
    x = inputs["x"]
    c = inputs["c"]
    shared = {"z32": np.zeros((512, 32), ml_dtypes.bfloat16), "z16": np.zeros((512, 16), np.float32)}
    for l in range(DEPTH):
        shared[f"w_ada{l}"] = np.ascontiguousarray(inputs["w_ada"][l])
    shared["baT"] = np.ascontiguousarray(np.concatenate([pk(inputs["b_ada"][l]) for l in range(DEPTH)], axis=1))
    for h in range(2):
        for k, v in host_B_consts(h).items():
            shared[f"{k}_h{h}"] = v
    for l in range(DEPTH):
        wa = [host_A_weights(inputs, l, h) for h in range(2)]
        for k in A_WS:
            shared[f"L{l}_{k}"] = wa[0][k]
        if l == 0:
            for h in range(2):
                for k in ROPE:
                    shared[f"{k}_h{h}"] = wa[h][k]
        for k, v in host_B_weights(inputs, l).items():
            shared[f"L{l}_{k}"] = v
        for k, v in host_C_weights(inputs, l).items():
            shared[f"L{l}_{k}"] = v
    in_maps = []
    for core in range(8):
        b = core // 2
        m = dict(shared)
        m["xT"] = np.ascontiguousarray(x[b].T)
        m["cT"] = np.ascontiguousarray(np.repeat(c[b].reshape(KD, 128).T[:, :, None], 4, axis=2))
        in_maps.append(m)
    res = run_bass_kernel_spmd(build_fused(), in_maps, core_ids=list(range(8)))
    out = np.empty((NB, SEQ, D), np.float32)
    for b in range(NB):
        out[b] = np.asarray(res.results[2 * b]["xo"]).T
    return out
```

```python
import numpy as np
from contextlib import ExitStack
import concourse.bass as bass
import concourse.mybir as mybir
from concourse.bass_utils import run_bass_kernel_spmd

F32 = mybir.dt.float32
BF16 = mybir.dt.bfloat16
AF = mybir.ActivationFunctionType
ALU = mybir.AluOpType
AX = mybir.AxisListType

SAME_ENGINE_SYNC = True


class Buf:
    def __init__(self, handle, name):
        self.h = handle
        self.name = name
        self.w = {}
        self.r = {}

    def __getitem__(self, idx):
        return self.h[idx]


class BufV(Buf):
    def __init__(self, handle, name, off, width):
        super().__init__(handle, name)
        self.off = off
        self.width = width

    def __getitem__(self, idx):
        if not isinstance(idx, tuple):
            idx = (idx, slice(None))
        p, c = idx
        a = 0 if c.start is None else c.start
        b = self.width if c.stop is None else c.stop
        return self.h[p, self.off + a:self.off + b]


class BufK(Buf):
    def __init__(self, handle, name, k0, nk):
        super().__init__(handle, name)
        self.k0 = k0
        self.nk = nk

    def __getitem__(self, idx):
        if not isinstance(idx, tuple):
            return self.h[idx, self.k0:self.k0 + self.nk, :]
        p, k = idx[0], idx[1]
        n = idx[2] if len(idx) > 2 else slice(None)
        if isinstance(k, slice):
            a = 0 if k.start is None else k.start
            b = self.nk if k.stop is None else k.stop
            return self.h[p, self.k0 + a:self.k0 + b, n]
        return self.h[p, self.k0 + k, n]


class Eng:
    def __init__(self, kb, name, eng, ndma=0):
        self.kb = kb
        self.name = name
        self.e = eng
        self.sem = kb.newsem("c_" + name)
        self.count = 0
        self.seen = {}
        self.dsems = [kb.newsem(f"d_{name}{i}") for i in range(ndma)]
        self.dcount = 0

    def wait(self, sem, val):
        if val <= 0:
            return
        if sem is self.sem and not SAME_ENGINE_SYNC:
            return
        if self.seen.get(id(sem), 0) >= val:
            return
        self.e.wait_ge(sem, val)
        self.seen[id(sem)] = val

    def _deps(self, reads, writes):
        for b in reads:
            for sem, val in b.w.values():
                self.wait(sem, val)
        for b in writes:
            for sem, val in b.w.values():
                self.wait(sem, val)
            for sem, val in b.r.values():
                self.wait(sem, val)

    def _mark(self, reads, writes, ev):
        for b in reads:
            b.r[id(ev[0])] = ev
        for b in writes:
            b.w = {id(ev[0]): ev}
            b.r = {}

    def op(self, ins_fn, reads=(), writes=(), signal=True):
        signal = True
        self._deps(reads, writes)
        ins = ins_fn()
        if signal:
            self.count += 1
            ins.then_inc(self.sem, 1)
        ev = (self.sem, self.count if signal else self.count + 1)
        self._mark(reads, writes, ev)
        return ins

    def mark_only(self, reads, writes):
        ev = (self.sem, self.count + 1)
        self._mark(reads, writes, ev)

    def dma(self, out, in_, reads=(), writes=(), **kw):
        n = len(self.dsems)
        i = self.dcount
        j = i % n
        sem = self.dsems[j]
        if i >= n:
            self.wait(sem, 16 * (i // n))
        self._deps(reads, writes)
        ins = self.e.dma_start(out=out, in_=in_, **kw)
        ins.then_inc(sem, 16)
        self.dcount += 1
        ev = (sem, 16 * (i // n + 1))
        self._mark(reads, writes, ev)
        return ev


class KB:
    def __init__(self):
        self.nc = bass.Bass("TRN2", target_bir_lowering=False)
        self.es = ExitStack()
        self.sems = []
        nc = self.nc
        self.pe = Eng(self, "pe", nc.tensor)
        self.act = Eng(self, "act", nc.scalar, ndma=4)
        self.dve = Eng(self, "dve", nc.vector)
        self.pool = Eng(self, "pool", nc.gpsimd, ndma=4)
        self.sp = Eng(self, "sp", nc.sync, ndma=8)
        self.engs = [self.pe, self.act, self.dve, self.pool, self.sp]
        self.uid = 0

    def newsem(self, name):
        s = self.es.enter_context(self.nc.semaphore(name))
        self.sems.append(s)
        return s

    def dram(self, name, shape, dt, kind="Internal"):
        return self.nc.dram_tensor(name, list(shape), dt, kind=kind).ap()

    def sb(self, shape, dt, name=None, stack=None):
        self.uid += 1
        name = f"{name or 't'}_{self.uid}"
        h = (stack or self.es).enter_context(self.nc.sbuf_tensor(name, list(shape), dt))
        return Buf(h, name)

    def ps(self, shape, dt=F32, name=None, stack=None):
        self.uid += 1
        name = f"{name or 'p'}_{self.uid}"
        h = (stack or self.es).enter_context(self.nc.psum_tensor(name, list(shape), dt))
        return Buf(h, name)

    def barrier(self):
        evs = []
        for g in self.engs:
            if g.count > 0:
                evs.append((g.sem, g.count))
            n = len(g.dsems)
            for j in range(n):
                cnt = (g.dcount - j + n - 1) // n if g.dcount > j else 0
                if cnt > 0:
                    evs.append((g.dsems[j], 16 * cnt))
        for g in self.engs:
            for sem, val in evs:
                g.wait(sem, val)

    def finish(self):
        self.barrier()
        self.es.close()


class Ring:
    def __init__(self, bufs):
        self.bufs = bufs
        self.i = 0

    def next(self):
        b = self.bufs[self.i % len(self.bufs)]
        self.i += 1
        return b


D = 2048
KD = 16
SEQ = 4096
NB = 4
TOK = 2048
DEPTH = 2
FFN = 5632
EPS = 1e-5
ALPHA = (2 * DEPTH) ** 0.25
SEGS = [("hp", 512), ("dq", 512), ("dk", 512), ("dv", 512), ("iq", 1024), ("ik", 64), ("iw", 16),
        ("cq", 384), ("ckv", 256), ("kr", 64), ("hc", 1024)]
SEG_OFF = {}
_o = 0
for _n, _s in SEGS:
    SEG_OFF[_n] = _o
    _o += _s
FM_SEGS = ["hp", "dq", "dk", "iq", "ik", "cq", "ckv", "kr", "krs", "hc"]
FM_DT = {"hp": "f32", "dq": "bf", "dk": "bf", "iq": "bf", "ik": "bf", "cq": "f32", "ckv": "f32", "kr": "f32",
         "krs": "f32", "hc": "f32"}
FM_ROWS = {"hp": 512, "dq": 512, "dk": 512, "iq": 1024, "ik": 64, "cq": 384, "ckv": 256, "kr": 64, "krs": 64,
           "hc": 1024}
CTS = []
for _n in FM_SEGS:
    _r = FM_ROWS[_n]
    for _c in range(0, _r, 128):
        CTS.append((_n, _c, min(128, _r - _c)))
NCT = len(CTS)


def pk(v):
    v = np.asarray(v)
    return np.ascontiguousarray(v.reshape(-1, 128).T)


def dts(s):
    return F32 if s == "f32" else BF16


def build_mods():
    kb = KB()
    nc = kb.nc
    NCOL = 2 * 6 * D // 8
    NT = NCOL // 128
    wa = kb.dram("wa", [D, NCOL], F32, kind="ExternalInput")
    ba = kb.dram("ba", [128, NT], F32, kind="ExternalInput")
    cT = kb.dram("cT", [128, KD, NB], F32, kind="ExternalInput")
    modT = kb.dram("modT", [128, NT, NB], F32, kind="ExternalOutput")
    csb = kb.sb([128, KD, NB], F32, "csb")
    cact = kb.sb([128, KD, NB], F32, "cact")
    basb = kb.sb([128, NT], F32, "basb")
    msb = kb.sb([128, NT, NB], F32, "msb")
    wr = Ring([kb.sb([128, KD, 512], F32, "wst") for _ in range(2)])
    pr = Ring([kb.ps([128, 512], F32, "ps") for _ in range(2)])
    kb.sp.dma(csb[:], cT[:, :, :], writes=[csb])
    kb.sp.dma(basb[:], ba[:, :], writes=[basb])
    kb.act.op(lambda: nc.scalar.activation(out=cact[:], in_=csb[:], func=AF.Silu), reads=[csb], writes=[cact])
    wav = wa.rearrange("(k p) n -> p k n", p=128)
    for g in range(NCOL // 512):
        w = wr.next()
        kb.sp.dma(w[:], wav[:, :, g * 512:(g + 1) * 512], writes=[w])
        for j in range(4):
            t = g * 4 + j
            p = pr.next()
            for k in range(KD):
                kb.pe.op(lambda: nc.tensor.matmul(p[:, 0:NB], lhsT=w[:, k, j * 128:(j + 1) * 128], rhs=cact[:, k, :],
                                                  start=(k == 0), stop=(k == KD - 1)),
                         reads=[w, cact], writes=[p], signal=(k == KD - 1))
            kb.dve.op(lambda: nc.vector.tensor_scalar(out=msb[:, t, :], in0=p[:, 0:NB], scalar1=basb[:, t:t + 1],
                                                      scalar2=None, op0=ALU.add),
                      reads=[p, basb], writes=[msb])
    kb.sp.dma(modT[:, :, :], msb[:], reads=[msb])
    kb.finish()
    return nc


def run_mods(inputs):
    w_ada = inputs["w_ada"]
    b_ada = inputs["b_ada"]
    c = inputs["c"]
    wcat = np.concatenate([w_ada[0], w_ada[1]], axis=1)
    bcat = np.concatenate([b_ada[0], b_ada[1]], axis=0)
    cT = np.ascontiguousarray(c.reshape(NB, KD, 128).transpose(2, 1, 0))
    NCOL = 3072
    in_maps = []
    for core in range(8):
        sl = slice(core * NCOL, (core + 1) * NCOL)
        in_maps.append({"wa": np.ascontiguousarray(wcat[:, sl]), "ba": pk(bcat[sl]), "cT": cT})
    nc = build_mods()
    res = run_bass_kernel_spmd(nc, in_maps, core_ids=list(range(8)))
    mt = np.concatenate([r["modT"] for r in res.results], axis=1)
    return [np.ascontiguousarray(mt[:, :, b]) for b in range(NB)]


class Ctx:
    pass


def setup_common(kb, modT_d):
    nc = kb.nc
    cx = Ctx()
    cx.P = [kb.ps([128, 512], F32, f"P{i}") for i in range(6)]
    cx.PB = []
    for i in range(2):
        big = kb.ps([128, 1024], BF16, f"PBB{i}")
        cx.PB += [BufV(big.h, f"PB{2 * i}", 0, 512), BufV(big.h, f"PB{2 * i + 1}", 512, 512)]
    cx.ones_f = kb.sb([128, 128], F32, "ones_f")
    cx.ones_b = kb.sb([128, 128], BF16, "ones_b")
    cx.eps = kb.sb([128, 1], F32, "eps")
    cx.mod = kb.sb([128, DEPTH * 96], F32, "mod")
    cx.onep = kb.sb([128, DEPTH * 96], F32, "onep")
    kb.pool.op(lambda: nc.gpsimd.memset(cx.ones_f[:], 1.0), writes=[cx.ones_f])
    kb.pool.op(lambda: nc.gpsimd.memset(cx.ones_b[:], 1.0), writes=[cx.ones_b])
    kb.pool.op(lambda: nc.gpsimd.memset(cx.eps[:], EPS), writes=[cx.eps])
    if modT_d is not None:
        kb.sp.dma(cx.mod[:], modT_d[:, :], writes=[cx.mod])
        kb.dve.op(lambda: nc.vector.tensor_scalar(out=cx.onep[:], in0=cx.mod[:], scalar1=1.0, scalar2=None, op0=ALU.add),
                  reads=[cx.mod], writes=[cx.onep])
    return cx


def mcol(l, i, k):
    return (l * 6 + i) * 16 + k


def col_stats(kb, cx, chunks, nfeat, want_mean, tmp, N=512):
    nc = kb.nc
    ps_s, ps_q = cx.P[0], cx.P[1]
    n = len(chunks)
    for k, (b, ap) in enumerate(chunks):
        rows = ap.shape[0]
        sq = tmp["sq"].next()
        kb.act.op(lambda: nc.scalar.activation(out=sq[:rows, :N], in_=ap, func=AF.Square), reads=[b], writes=[sq])
        kb.pe.op(lambda: nc.tensor.matmul(ps_q[:, :N], lhsT=cx.ones_f[:rows, :], rhs=sq[:rows, :N], start=(k == 0),
                                          stop=(k == n - 1)), reads=[sq, cx.ones_f], writes=[ps_q])
        if want_mean:
            kb.pe.op(lambda: nc.tensor.matmul(ps_s[:, :N], lhsT=cx.ones_f[:rows, :], rhs=ap, start=(k == 0),
                                              stop=(k == n - 1)), reads=[b, cx.ones_f], writes=[ps_s])
    inv = 1.0 / nfeat
    var, rstd = tmp["var"], tmp["rstd"]
    if want_mean:
        mean, msq, nmr = tmp["mean"], tmp["msq"], tmp["nmr"]
        kb.dve.op(lambda: nc.vector.tensor_scalar(out=mean[:, :N], in0=ps_s[:, :N], scalar1=inv, scalar2=None,
                                                  op0=ALU.mult), reads=[ps_s], writes=[mean])
        kb.dve.op(lambda: nc.vector.tensor_tensor(out=msq[:, :N], in0=mean[:, :N], in1=mean[:, :N], op=ALU.mult),
                  reads=[mean], writes=[msq])
        kb.dve.op(lambda: nc.vector.scalar_tensor_tensor(out=var[:, :N], in0=ps_q[:, :N], scalar=inv, in1=msq[:, :N],
                                                         op0=ALU.mult, op1=ALU.subtract),
                  reads=[ps_q, msq], writes=[var])
        kb.act.op(lambda: nc.scalar.activation(out=var[:, :N], in_=var[:, :N], func=AF.Sqrt, bias=cx.eps[:, 0:1],
                                               scale=1.0), reads=[var, cx.eps], writes=[var])
    else:
        kb.act.op(lambda: nc.scalar.activation(out=var[:, :N], in_=ps_q[:, :N], func=AF.Sqrt, bias=cx.eps[:, 0:1],
                                               scale=inv), reads=[ps_q, cx.eps], writes=[var])
    kb.dve.op(lambda: nc.vector.reciprocal(out=rstd[:, :N], in_=var[:, :N]), reads=[var], writes=[rstd])
    if want_mean:
        kb.dve.op(lambda: nc.vector.scalar_tensor_tensor(out=nmr[:, :N], in0=mean[:, :N], scalar=-1.0,
                                                         in1=rstd[:, :N], op0=ALU.mult, op1=ALU.mult),
                  reads=[mean, rstd], writes=[nmr])
        return rstd, nmr
    return rstd, None


def stat_tmps(kb, st, N=512):
    t = {"sq": Ring([kb.sb([128, N], F32, "sq", st) for _ in range(2)])}
    for nm in ("mean", "msq", "var", "rstd", "nmr"):
        t[nm] = kb.sb([128, N], F32, nm, st)
    return t


def load_cast(kb, dst, dst_ap, src_ap, stg_ring, stg_view):
    nc = kb.nc
    s = stg_ring.next()
    kb.sp.dma(stg_view(s), src_ap, writes=[s])
    kb.pool.op(lambda: nc.gpsimd.tensor_copy(out=dst_ap, in_=stg_view(s)), reads=[s], writes=[dst])


def evac(kb, i, out_buf, out_ap, ps_buf, ps_ap):
    nc = kb.nc
    if i % 2 == 0:
        kb.act.op(lambda: nc.scalar.copy(out=out_ap, in_=ps_ap), reads=[ps_buf], writes=[out_buf])
    else:
        kb.dve.op(lambda: nc.vector.tensor_copy(out=out_ap, in_=ps_ap), reads=[ps_buf], writes=[out_buf])


def stage_A(kb, cx, l, xT, W, O, S):
    nc = kb.nc
    P = cx.P
    xTv = xT.rearrange("(k p) n -> p k n", p=128)
    with ExitStack() as st:
        uT = [kb.sb([128, KD, 512], BF16, f"uT{t}", st) for t in range(4)]
        with ExitStack() as s1:
            xr = Ring([kb.sb([128, KD, 512], F32, "xt", s1) for _ in range(2)])
            tmp = stat_tmps(kb, s1)
            t1r = Ring([kb.sb([128, 512], F32, "t1", s1) for _ in range(2)])
            t2r = Ring([kb.sb([128, 512], F32, "t2", s1) for _ in range(2)])
            for tt in range(4):
                xt = xr.next()
                kb.sp.dma(xt[:], xTv[:, :, tt * 512:(tt + 1) * 512], writes=[xt])
                rstd, nmr = col_stats(kb, cx, [(xt, xt[:, k, :]) for k in range(KD)], D, True, tmp)
                for k in range(KD):
                    t1 = t1r.next()
                    t2 = t2r.next()
                    kb.dve.op(lambda: nc.vector.tensor_tensor(out=t1[:], in0=xt[:, k, :], in1=rstd[:], op=ALU.mult),
                              reads=[xt, rstd], writes=[t1])
                    kb.pool.op(lambda: nc.gpsimd.tensor_tensor(out=t2[:], in0=t1[:], in1=nmr[:], op=ALU.add),
                               reads=[t1, nmr], writes=[t2])
                    kb.act.op(lambda: nc.scalar.activation(out=uT[tt][:, k, :], in_=t2[:], func=AF.Identity,
                                                           scale=cx.onep[:, mcol(l, 1, k):mcol(l, 1, k) + 1],
                                                           bias=cx.mod[:, mcol(l, 0, k):mcol(l, 0, k) + 1]),
                              reads=[t2, cx.onep, cx.mod], writes=[uT[tt]])
                kb.act.dma(O["uT"].rearrange("(k p) n -> p k n", p=128)[:, :, tt * 512:(tt + 1) * 512], uT[tt][:],
                           reads=[uT[tt]])
            kb.barrier()
        with ExitStack() as s2:
            wst = Ring([kb.sb([128, KD, 128], F32, "wst", s2) for _ in range(2)])
            wbf = Ring([kb.sb([128, KD, 128], BF16, "wbf", s2) for _ in range(2)])
            obf = Ring([kb.sb([128, TOK], BF16, "obf", s2) for _ in range(2)])
            of32 = Ring([kb.sb([128, TOK], F32, "of32", s2) for _ in range(2)])
            psr = Ring([P[2], P[3], P[4], P[5]])
            dest = {"hp": O["hpT"], "dq": O["dqT"], "dk": O["dkT"], "iq": O["iqT"], "ik": O["ikT"], "cq": S["cqT"],
                    "ckv": S["ckvT"], "kr": S["krraw"], "krs": S["krsraw"], "hc": S["hcT"]}
            ei = 0
            for ct, (seg, c0, ncols) in enumerate(CTS):
                wb = wbf.next()
                load_cast(kb, wb, wb[:], W["win_ct"][ct], wst, lambda s: s[:])
                ob = (of32 if FM_DT[seg] == "f32" else obf).next()
                for tt in range(4):
                    ps = psr.next()
                    for k in range(KD):
                        kb.pe.op(lambda: nc.tensor.matmul(ps[:], lhsT=wb[:, k, :], rhs=uT[tt][:, k, :],
                                                          start=(k == 0), stop=(k == KD - 1)),
                                 reads=[wb, uT[tt]], writes=[ps])
                    evac(kb, ei, ob, ob[:ncols, tt * 512:(tt + 1) * 512], ps, ps[:ncols, :])
                    ei += 1
                kb.act.dma(dest[seg][c0:c0 + ncols, :], ob[:ncols, :], reads=[ob])
            wv = kb.sb([128, KD, 512], BF16, "wv", s2)
            wiw = kb.sb([128, KD, 128], BF16, "wiw", s2)
            for j in range(4):
                load_cast(kb, wv, wv[:, :, j * 128:(j + 1) * 128], W["wdv_ct"][j], wst, lambda s: s[:])
            load_cast(kb, wiw, wiw[:], W["wiw_ct"][0], wst, lambda s: s[:])
            vob = Ring([kb.sb([128, 512], BF16, "vob", s2) for _ in range(2)])
            iwo = kb.sb([128, 16, 16], F32, "iwo", s2)
            for tb in range(16):
                tt, off = tb // 4, (tb % 4) * 128
                ps = psr.next()
                for k in range(KD):
                    kb.pe.op(lambda: nc.tensor.matmul(ps[:], lhsT=uT[tt][:, k, off:off + 128], rhs=wv[:, k, :],
                                                      start=(k == 0), stop=(k == KD - 1)),
                             reads=[wv, uT[tt]], writes=[ps])
                vo = vob.next()
                evac(kb, tb, vo, vo[:], ps, ps[:])
                kb.act.dma(O["dV"][tb * 128:(tb + 1) * 128, :], vo[:], reads=[vo])
                ps2 = psr.next()
                for k in range(KD):
                    kb.pe.op(lambda: nc.tensor.matmul(ps2[:, 0:16], lhsT=uT[tt][:, k, off:off + 128], rhs=wiw[:, k, 0:16],
                                                      start=(k == 0), stop=(k == KD - 1)),
                             reads=[wiw, uT[tt]], writes=[ps2])
                evac(kb, tb + 1, iwo, iwo[:, tb, :], ps2, ps2[:, 0:16])
            kb.act.dma(O["iw"].rearrange("(b p) h -> p b h", p=128), iwo[:], reads=[iwo])
            kb.barrier()
    with ExitStack() as s3:
        stg = Ring([kb.sb([128, 1024], F32, "stg", s3) for _ in range(2)])
        wq = kb.sb([128, 3, 1024], BF16, "wq", s3)
        wkv = kb.sb([128, 2, 1024], BF16, "wkv", s3)
        for kc in range(3):
            load_cast(kb, wq, wq[:, kc, :], W["wq_all"][kc * 128:(kc + 1) * 128, :], stg, lambda s: s[:])
        for kc in range(2):
            load_cast(kb, wkv, wkv[:, kc, :], W["wkv_all"][kc * 128:(kc + 1) * 128, :], stg, lambda s: s[:])
        vecs = kb.sb([128, 8], F32, "vecs", s3)
        kb.sp.dma(vecs[:, 0:3], W["q_norm"][:, :], writes=[vecs])
        kb.sp.dma(vecs[:, 3:5], W["kv_norm"][:, :], writes=[vecs])
        tmp = stat_tmps(kb, s3)
        ar = Ring([kb.sb([128, 8, 512], F32, "hc", s3) for _ in range(1)])
        sig = kb.sb([128, 4, 512], F32, "sig", s3)
        zb = kb.sb([128, 4, 512], BF16, "zb", s3)
        cqs = kb.sb([128, 3, 512], F32, "cqs", s3)
        cqn = kb.sb([128, 3, 512], BF16, "cqn", s3)
        cks = kb.sb([128, 2, 512], F32, "cks", s3)
        ckn = kb.sb([128, 2, 512], BF16, "ckn", s3)
        cc = kb.sb([64, 512], F32, "cc", s3)
        ss = kb.sb([64, 512], F32, "ss", s3)
        krr = kb.sb([64, 2, 512], F32, "krr", s3)
        t1r = Ring([kb.sb([128, 512], F32, "t1", s3) for _ in range(2)])
        t2r = Ring([kb.sb([128, 512], F32, "t2", s3) for _ in range(2)])
        obr = Ring([kb.sb([128, 512], BF16, "ob", s3) for _ in range(3)])
        psr = Ring([P[2], P[3], P[4], P[5]])
        ei = 0
        for tt in range(4):
            ts = slice(tt * 512, (tt + 1) * 512)
            hc = ar.next()
            kb.sp.dma(hc[:], S["hcT"].rearrange("(k p) n -> p k n", p=128)[:, :, ts], writes=[hc])
            kb.act.op(lambda: nc.scalar.activation(out=sig[:], in_=hc[:, 4:8, :], func=AF.Sigmoid), reads=[hc],
                      writes=[sig])
            kb.dve.op(lambda: nc.vector.tensor_tensor(out=zb[:], in0=hc[:, 0:4, :], in1=sig[:], op=ALU.mult),
                      reads=[hc, sig], writes=[zb])
            kb.act.dma(O["zT"].rearrange("(k p) n -> p k n", p=128)[:, :, ts], zb[:], reads=[zb])
            kb.sp.dma(cc[:], W["ropec"][:, ts], writes=[cc])
            kb.sp.dma(ss[:], W["ropes"][:, ts], writes=[ss])
            kb.sp.dma(cqs[:], S["cqT"].rearrange("(k p) n -> p k n", p=128)[:, :, ts], writes=[cqs])
            rstd, _ = col_stats(kb, cx, [(cqs, cqs[:, k, :]) for k in range(3)], 384, False, tmp)
            for k in range(3):
                t1 = t1r.next()
                kb.dve.op(lambda: nc.vector.tensor_tensor(out=t1[:], in0=cqs[:, k, :], in1=rstd[:], op=ALU.mult),
                          reads=[cqs, rstd], writes=[t1])
                kb.act.op(lambda: nc.scalar.activation(out=cqn[:, k, :], in_=t1[:], func=AF.Copy,
                                                       scale=vecs[:, k:k + 1]), reads=[t1, vecs], writes=[cqn])
            for h in range(4):
                ps = psr.next()
                for k in range(3):
                    kb.pe.op(lambda: nc.tensor.matmul(ps[:], lhsT=wq[:, k, h * 256:h * 256 + 128], rhs=cqn[:, k, :],
                                                      start=(k == 0), stop=(k == 2)), reads=[wq, cqn], writes=[ps])
                ob = obr.next()
                evac(kb, ei, ob, ob[:], ps, ps[:]); ei += 1
                kb.act.dma(O["qnT"][h * 128:(h + 1) * 128, ts], ob[:], reads=[ob])
                ps = psr.next()
                ps2 = psr.next()
                for k in range(3):
                    kb.pe.op(lambda: nc.tensor.matmul(ps[:64, :], lhsT=wq[:, k, h * 256 + 128:h * 256 + 192],
                                                      rhs=cqn[:, k, :], start=(k == 0), stop=(k == 2)),
                             reads=[wq, cqn], writes=[ps])
                for k in range(3):
                    kb.pe.op(lambda: nc.tensor.matmul(ps2[:64, :], lhsT=wq[:, k, h * 256 + 192:h * 256 + 256],
                                                      rhs=cqn[:, k, :], start=(k == 0), stop=(k == 2)),
                             reads=[wq, cqn], writes=[ps2])
                t1 = t1r.next(); t2 = t2r.next(); ob = obr.next()
                kb.dve.op(lambda: nc.vector.tensor_tensor(out=t1[:64, :], in0=ps[:64, :], in1=cc[:], op=ALU.mult),
                          reads=[ps, cc], writes=[t1])
                kb.dve.op(lambda: nc.vector.tensor_tensor(out=t2[:64, :], in0=ps2[:64, :], in1=ss[:], op=ALU.mult),
                          reads=[ps2, ss], writes=[t2])
                kb.pool.op(lambda: nc.gpsimd.tensor_tensor(out=ob[:64, :], in0=t1[:64, :], in1=t2[:64, :], op=ALU.add),
                           reads=[t1, t2], writes=[ob])
                kb.act.dma(O["qrT"][h * 64:(h + 1) * 64, ts], ob[:64, :], reads=[ob])
            kb.sp.dma(cks[:], S["ckvT"].rearrange("(k p) n -> p k n", p=128)[:, :, ts], writes=[cks])
            rstd, _ = col_stats(kb, cx, [(cks, cks[:, k, :]) for k in range(2)], 256, False, tmp)
            for k in range(2):
                t1 = t1r.next()
                kb.dve.op(lambda: nc.vector.tensor_tensor(out=t1[:], in0=cks[:, k, :], in1=rstd[:], op=ALU.mult),
                          reads=[cks, rstd], writes=[t1])
                kb.act.op(lambda: nc.scalar.activation(out=ckn[:, k, :], in_=t1[:], func=AF.Copy,
                                                       scale=vecs[:, 3 + k:4 + k]), reads=[t1, vecs], writes=[ckn])
            for h in range(4):
                ps = psr.next()
                for k in range(2):
                    kb.pe.op(lambda: nc.tensor.matmul(ps[:], lhsT=wkv[:, k, h * 128:(h + 1) * 128], rhs=ckn[:, k, :],
                                                      start=(k == 0), stop=(k == 1)), reads=[wkv, ckn], writes=[ps])
                ob = obr.next()
                evac(kb, ei, ob, ob[:], ps, ps[:]); ei += 1
                kb.act.dma(O["knT"][h * 128:(h + 1) * 128, ts], ob[:], reads=[ob])
            for tb in range(4):
                ps = psr.next()
                for k in range(2):
                    kb.pe.op(lambda: nc.tensor.matmul(ps[:], lhsT=ckn[:, k, tb * 128:(tb + 1) * 128],
                                                      rhs=wkv[:, k, 512:1024], start=(k == 0), stop=(k == 1)),
                             reads=[wkv, ckn], writes=[ps])
                ob = obr.next()
                evac(kb, ei, ob, ob[:], ps, ps[:]); ei += 1
                kb.act.dma(O["mV"][tt * 512 + tb * 128:tt * 512 + (tb + 1) * 128, :], ob[:], reads=[ob])
            kb.sp.dma(krr[:, 0, :], S["krraw"][:, ts], writes=[krr])
            kb.sp.dma(krr[:, 1, :], S["krsraw"][:, ts], writes=[krr])
            t1 = t1r.next(); t2 = t2r.next(); ob = obr.next()
            kb.dve.op(lambda: nc.vector.tensor_tensor(out=t1[:64, :], in0=krr[:, 0, :], in1=cc[:], op=ALU.mult),
                      reads=[krr, cc], writes=[t1])
            kb.dve.op(lambda: nc.vector.tensor_tensor(out=t2[:64, :], in0=krr[:, 1, :], in1=ss[:], op=ALU.mult),
                      reads=[krr, ss], writes=[t2])
            kb.pool.op(lambda: nc.gpsimd.tensor_tensor(out=ob[:64, :], in0=t1[:64, :], in1=t2[:64, :], op=ALU.add),
                       reads=[t1, t2], writes=[ob])
            kb.act.dma(O["krT"][:, ts], ob[:64, :], reads=[ob])
        kb.barrier()


A_OUT = {"uT": ([D, TOK], "bf"), "hpT": ([512, TOK], "f32"), "dqT": ([512, TOK], "bf"), "dkT": ([512, TOK], "bf"),
         "dV": ([TOK, 512], "bf"), "iqT": ([1024, TOK], "bf"), "ikT": ([64, TOK], "bf"), "iw": ([TOK, 16], "f32"),
         "qnT": ([512, TOK], "bf"), "qrT": ([256, TOK], "bf"), "knT": ([512, TOK], "bf"), "mV": ([TOK, 512], "bf"),
         "krT": ([64, TOK], "bf"), "zT": ([512, TOK], "bf")}
A_SCR = {"cqT": [384, TOK], "ckvT": [256, TOK], "krraw": [64, TOK], "krsraw": [64, TOK], "hcT": [1024, TOK]}
A_W = {"win_ct": [NCT, 128, KD, 128], "wdv_ct": [4, 128, KD, 128], "wiw_ct": [1, 128, KD, 128],
       "wq_all": [384, 1024], "wkv_all": [256, 1024], "q_norm": [128, 3], "kv_norm": [128, 2],
       "ropec": [64, TOK], "ropes": [64, TOK]}


def ct_layout(w, c0, ncols):
    out = np.zeros((128, KD, 128), np.float32)
    out[:, :, :ncols] = w[:, c0:c0 + ncols].reshape(KD, 128, ncols).transpose(1, 0, 2)
    return out


def host_A_weights(inputs, l, h):
    w_in = inputs["w_in"][l]
    cts = []
    for seg, c0, ncols in CTS:
        if seg == "krs":
            base = SEG_OFF["kr"]
            wsw = np.concatenate([w_in[:, base + 32:base + 64], w_in[:, base:base + 32]], axis=1)
            cts.append(ct_layout(wsw, 0, 64))
        else:
            cts.append(ct_layout(w_in, SEG_OFF[seg] + c0, ncols))
    out = {"win_ct": np.stack(cts)}
    out["wdv_ct"] = np.stack([ct_layout(w_in, SEG_OFF["dv"] + j * 128, 128) for j in range(4)])
    out["wiw_ct"] = np.stack([ct_layout(w_in, SEG_OFF["iw"], 16)])
    wq = inputs["w_q_up"][l]
    parts = []
    for hh in range(4):
        b = hh * 192
        parts += [wq[:, b:b + 128], wq[:, b + 128:b + 192], wq[:, b + 160:b + 192], wq[:, b + 128:b + 160]]
    out["wq_all"] = np.ascontiguousarray(np.concatenate(parts, axis=1))
    wkv = inputs["w_kv_up"][l]
    out["wkv_all"] = np.ascontiguousarray(np.concatenate(
        [wkv[:, hh * 256:hh * 256 + 128] for hh in range(4)] + [wkv[:, hh * 256 + 128:hh * 256 + 256] for hh in range(4)],
        axis=1))
    out["q_norm"] = pk(inputs["q_norm"][l])
    out["kv_norm"] = pk(inputs["kv_norm"][l])
    pos = np.arange(h * TOK, (h + 1) * TOK, dtype=np.float32)
    inv_freq = (np.float32(10000.0) ** (-np.arange(0, 64, 2, dtype=np.float32) / np.float32(64))).astype(np.float32)
    ang = pos[None, :] * inv_freq[:, None]
    cos, sin = np.cos(ang).astype(np.float32), np.sin(ang).astype(np.float32)
    out["ropec"] = np.ascontiguousarray(np.concatenate([cos, cos], axis=0))
    out["ropes"] = np.ascontiguousarray(np.concatenate([-sin, sin], axis=0))
    return out


def build_A(l):
    kb = KB()
    xT = kb.dram("xT", [D, TOK], F32, kind="ExternalInput")
    modT = kb.dram("modT", [128, DEPTH * 96], F32, kind="ExternalInput")
    W = {k: kb.dram(k, v, F32, kind="ExternalInput") for k, v in A_W.items()}
    O = {k: kb.dram(k, v[0], dts(v[1]), kind="ExternalOutput") for k, v in A_OUT.items()}
    S = {k: kb.dram(k, v, F32) for k, v in A_SCR.items()}
    cx = setup_common(kb, modT)
    stage_A(kb, cx, l, xT, W, O, S)
    kb.finish()
    return kb.nc


SLOPES = [2.0 ** (-8.0 * (h + 1) / 4) for h in range(4)]
NBIS = 16
NEGM = -30000.0


def key_tiles(i):
    tl = [(j * 512, 512, 0) for j in range(4)]
    for j in range(i // 4):
        tl.append((2048 + j * 512, 512, 1))
    tl.append((2048 + (i // 4) * 512, (i % 4 + 1) * 128, 2))
    return tl


def host_B_consts(h):
    c = {}
    q = np.arange(128)[:, None]
    s = np.arange(512)[None, :]
    c["AB"] = np.stack([SLOPES[hh] * (s - q) for hh in range(4)], axis=1).astype(np.float32)
    s1 = np.arange(128)[None, :]
    cm = np.where((s1 // 64) <= (q // 64), 0.0, 1.0)
    c["ABD"] = np.stack([-SLOPES[hh] * np.abs(q - s1) + SLOPES[hh] * 128.0 * wv for hh in range(4) for wv in range(4)],
                        axis=1).astype(np.float32)
    c["CM"] = (cm * NEGM).astype(np.float32)
    c["CMB"] = (cm * -1e6).astype(np.float32)
    c["ident"] = np.eye(128, dtype=np.float32)
    prevb = 0.0 if h == 1 else NEGM
    c["prevb"] = np.full((128, 2), prevb, np.float32)
    c["prevs"] = np.full((128, 2), 0.0 if h == 1 else -1e6, np.float32)
    cb = np.zeros((128, 16 * 4 * 8), np.float32)
    for i in range(16):
        tq0 = 2048 + 128 * i
        for hh in range(4):
            for ti, (s0, w, kind) in enumerate(key_tiles(i)):
                v = -SLOPES[hh] * (tq0 - s0)
                if kind == 0:
                    v += prevb
                cb[:, (i * 4 + hh) * 8 + ti] = v
    c["cb"] = cb
    c["pw"] = np.tile((2.0 ** -(np.arange(NBIS + 1) + 1.0))[None, :], (128, 1)).astype(np.float32)
    ic = np.zeros((128, 4, 16), np.float32)
    for g, w in enumerate((2, 4, 8, 16)):
        t = np.arange(16) + h * TOK
        ic[:, g, :] = 1.0 / np.minimum(t + 1, w)
    c["invc"] = ic
    return c


B_C = {"AB": [128, 4, 512], "ABD": [128, 16, 128], "CM": [128, 128], "CMB": [128, 128], "ident": [128, 128],
       "prevb": [128, 2], "prevs": [128, 2], "cb": [128, 512], "pw": [128, NBIS + 1], "invc": [128, 4, 16]}
B_W = {"w_pool": [4, 128, 128], "pool_scale": [128, 4], "w_dwT": [128, 4, 31], "b_dw": [128, 4],
       "cln_g": [128, 4], "cln_b": [128, 4]}
B_PREV = {"pdkT": ([512, TOK], "bf"), "pdV": ([TOK, 512], "bf"), "pikT": ([64, TOK], "bf"), "pknT": ([512, TOK], "bf"),
          "pmV": ([TOK, 512], "bf"), "pkrT": ([64, TOK], "bf"), "pz": ([512, 32], "bf"), "php": ([512, 16], "f32")}


def host_B_weights(inputs, l):
    out = {"w_pool": np.ascontiguousarray(inputs["w_pool"][l]), "pool_scale": pk(inputs["pool_scale"][l]),
           "b_dw": pk(inputs["b_dw"][l]), "cln_g": pk(inputs["conv_ln_g"][l]), "cln_b": pk(inputs["conv_ln_b"][l])}
    wd = inputs["w_dw"][l]
    out["w_dwT"] = np.ascontiguousarray(wd.reshape(31, 4, 128).transpose(2, 1, 0))
    return out


PARTS = {"B1", "B2", "B3", "B4"}
DBG = {"ni": 16, "tail": True, "fin": True, "nbis": NBIS, "att": True, "idx": True}


def sect(tag):
    if tag in PARTS:
        with ExitStack() as st:
            yield st


def attn_tail(kb, cx, T, Pb, W, s_chunk0, Vb, h, po, first, last):
    nc = kb.nc
    nb = W // 128
    pt = T["ptps"].next()
    for sb in range(nb):
        kb.pe.op(lambda: nc.tensor.transpose(out=pt[:, sb * 128:(sb + 1) * 128], in_=Pb[:, sb * 128:(sb + 1) * 128],
                                             identity=T["identb"][:]), reads=[Pb, T["identb"]], writes=[pt])
    pts = T["pts"].next()
    evac(kb, T["ei"][0], pts, pts[:, :W], pt, pt[:, :W])
    T["ei"][0] += 1
    for sb in range(nb):
        kb.pe.op(lambda: nc.tensor.matmul(po[:, 0:128], lhsT=pts[:, sb * 128:(sb + 1) * 128],
                                          rhs=Vb[:, s_chunk0 + sb, h * 128:(h + 1) * 128],
                                          start=(first and sb == 0), stop=(last and sb == nb - 1)),
                 reads=[pts, Vb], writes=[po])


def attn_finish(kb, cx, T, po, rs, npieces, yb, h, i):
    nc = kb.nc
    rsum, rinv, on = T["rsum"], T["rinv"], T["on"].next()
    kb.dve.op(lambda: nc.vector.tensor_reduce(out=rsum[:], in_=rs[:, 0:npieces], axis=AX.X, op=ALU.add), reads=[rs], writes=[rsum])
    kb.dve.op(lambda: nc.vector.reciprocal(out=rinv[:], in_=rsum[:]), reads=[rsum], writes=[rinv])
    kb.act.op(lambda: nc.scalar.activation(out=on[:], in_=po[:, 0:128], func=AF.Copy, scale=rinv[:, 0:1]),
              reads=[po, rinv], writes=[on])
    pt = T["ptps"].next()
    kb.pe.op(lambda: nc.tensor.transpose(out=pt[:, 0:128], in_=on[:], identity=T["identb"][:]),
             reads=[on, T["identb"]], writes=[pt])
    kb.dve.op(lambda: nc.vector.tensor_copy(out=yb[:, h, i * 128:(i + 1) * 128], in_=pt[:, 0:128]), reads=[pt],
              writes=[yb])


def stage_B(kb, cx, l, A, PV, W, C, yT):
    nc = kb.nc
    P, PB = cx.P, cx.PB
    yTv = yT.rearrange("(b k p) n -> b p k n", b=4, p=128)
    with ExitStack() as sc_:
        ident = kb.sb([128, 128], F32, "ident", sc_)
        identb = kb.sb([128, 128], BF16, "identb", sc_)
        kb.sp.dma(ident[:], C["ident"][:, :], writes=[ident])
        kb.pool.op(lambda: nc.gpsimd.tensor_copy(out=identb[:], in_=ident[:]), reads=[ident], writes=[identb])

        for st in sect("B1"):
            hh = kb.sb([128, 16 + TOK], F32, "hh", st)
            sA = kb.sb([128, 16 + TOK], F32, "sA", st)
            sB = kb.sb([128, 16 + TOK], F32, "sB", st)
            pl = kb.sb([128, TOK], BF16, "pl", st)
            t16 = kb.sb([128, 16], F32, "t16", st)
            invc = kb.sb([128, 4, 16], F32, "invc", st)
            wps = kb.sb([128, 4, 128], F32, "wps", st)
            wpb = kb.sb([128, 4, 128], BF16, "wpb", st)
            psc = kb.sb([128, 4], F32, "psc", st)
            ya = kb.sb([128, 4, TOK], BF16, "ya", st)
            kb.sp.dma(invc[:], C["invc"][:, :, :], writes=[invc])
            kb.sp.dma(wps[:], W["w_pool"].rearrange("g c d -> c g d"), writes=[wps])
            kb.pool.op(lambda: nc.gpsimd.tensor_copy(out=wpb[:], in_=wps[:]), reads=[wps], writes=[wpb])
            kb.sp.dma(psc[:], W["pool_scale"][:, :], writes=[psc])
            psr = Ring([P[2], P[3], P[4], P[5]])
            for g in range(4):
                w = 2 ** (g + 1)
                kb.sp.dma(hh[:, 0:16], PV["php"][g * 128:(g + 1) * 128, :], writes=[hh])
                kb.sp.dma(hh[:, 16:], A["hpT"][g * 128:(g + 1) * 128, :], writes=[hh])
                src, d, o = hh, 1, 1
                bufs = [sA, sB]
                for step in range(g + 1):
                    dst = bufs[step % 2]
                    kb.dve.op(lambda: nc.vector.tensor_tensor(out=dst[:, o:], in0=src[:, o:], in1=src[:, o - d:16 + TOK - d],
                                                              op=ALU.add), reads=[src], writes=[dst])
                    src = dst
                    d *= 2
                    o += d
                kb.dve.op(lambda: nc.vector.scalar_tensor_tensor(out=pl[:], in0=src[:, 16:], scalar=1.0 / w, in1=hh[:, 16:],
                                                                 op0=ALU.mult, op1=ALU.subtract),
                          reads=[src, hh], writes=[pl])
                kb.dve.op(lambda: nc.vector.tensor_tensor(out=t16[:], in0=src[:, 16:32], in1=invc[:, g, :], op=ALU.mult),
                          reads=[src, invc], writes=[t16])
                kb.dve.op(lambda: nc.vector.tensor_tensor(out=pl[:, 0:16], in0=t16[:], in1=hh[:, 16:32], op=ALU.subtract),
                          reads=[t16, hh], writes=[pl])
                for tt in range(4):
                    ps = psr.next()
                    kb.pe.op(lambda: nc.tensor.matmul(ps[:], lhsT=wpb[:, g, :], rhs=pl[:, tt * 512:(tt + 1) * 512],
                                                      start=True, stop=True), reads=[wpb, pl], writes=[ps])
                    kb.act.op(lambda: nc.scalar.activation(out=ya[:, g, tt * 512:(tt + 1) * 512], in_=ps[:], func=AF.Copy,
                                                           scale=psc[:, g:g + 1]), reads=[ps, psc], writes=[ya])
            kb.act.dma(yTv[0], ya[:], reads=[ya])
            kb.barrier()

        for st in sect("B2"):
            zb = kb.sb([128, 4, 32 + TOK], BF16, "zb", st)
            wdw = kb.sb([128, 4, 31], F32, "wdw", st)
            dg = kb.sb([128, 4 * 31, 128], BF16, "dg", st)
            vecs = kb.sb([128, 12], F32, "cvecs", st)
            v = kb.sb([128, 4, 512], F32, "cv", st)
            yd = kb.sb([128, 4, TOK], BF16, "yd", st)
            tmp = stat_tmps(kb, st)
            t1r = Ring([kb.sb([128, 512], F32, "t1", st) for _ in range(2)])
            t2r = Ring([kb.sb([128, 512], F32, "t2", st) for _ in range(2)])
            kb.sp.dma(zb[:, :, 0:32], PV["pz"].rearrange("(k p) n -> p k n", p=128), writes=[zb])
            kb.sp.dma(zb[:, :, 32:], A["zT"].rearrange("(k p) n -> p k n", p=128), writes=[zb])
            kb.sp.dma(wdw[:], W["w_dwT"][:, :, :], writes=[wdw])
            kb.sp.dma(vecs[:, 0:4], W["b_dw"][:, :], writes=[vecs])
            kb.sp.dma(vecs[:, 4:8], W["cln_g"][:, :], writes=[vecs])
            kb.sp.dma(vecs[:, 8:12], W["cln_b"][:, :], writes=[vecs])
            for c in range(4):
                for j in range(31):
                    kb.pool.op(lambda: nc.gpsimd.tensor_scalar(out=dg[:, c * 31 + j, :], in0=ident[:],
                                                               scalar1=wdw[:, c, j:j + 1], scalar2=None, op0=ALU.mult),
                               reads=[ident, wdw], writes=[dg])
            psr = Ring([P[2], P[3], P[4], P[5]])
            for tt in range(4):
                for c in range(4):
                    ps = psr.next()
                    for j in range(31):
                        o = 32 + tt * 512 - 30 + j
                        kb.pe.op(lambda: nc.tensor.matmul(ps[:], lhsT=dg[:, c * 31 + j, :], rhs=zb[:, c, o:o + 512],
                                                          start=(j == 0), stop=(j == 30)), reads=[dg, zb], writes=[ps])
                    kb.act.op(lambda: nc.scalar.activation(out=v[:, c, :], in_=ps[:], func=AF.Identity,
                                                           bias=vecs[:, c:c + 1], scale=1.0), reads=[ps, vecs], writes=[v])
                rstd, nmr = col_stats(kb, cx, [(v, v[:, c, :]) for c in range(4)], 512, True, tmp)
                for c in range(4):
                    t1 = t1r.next(); t2 = t2r.next()
                    kb.dve.op(lambda: nc.vector.tensor_tensor(out=t1[:], in0=v[:, c, :], in1=rstd[:], op=ALU.mult),
                              reads=[v, rstd], writes=[t1])
                    kb.pool.op(lambda: nc.gpsimd.tensor_tensor(out=t2[:], in0=t1[:], in1=nmr[:], op=ALU.add),
                               reads=[t1, nmr], writes=[t2])
                    kb.act.op(lambda: nc.scalar.activation(out=yd[:, c, tt * 512:(tt + 1) * 512], in_=t2[:], func=AF.Silu,
                                                           scale=vecs[:, 4 + c:5 + c], bias=vecs[:, 8 + c:9 + c]),
                              reads=[t2, vecs], writes=[yd])
            kb.act.dma(yTv[3], yd[:], reads=[yd])
            kb.barrier()

        def mk_T(st):
            T = {"identb": identb, "ei": [0]}
            T["ptps"] = Ring([PB[0], PB[1], PB[2], PB[3]])
            T["pts"] = Ring([kb.sb([128, 512], BF16, "pts", st) for _ in range(3)])
            T["Pb"] = Ring([kb.sb([128, 512], BF16, "Pb", st) for _ in range(3)])
            T["tmp"] = Ring([kb.sb([128, 512], F32, "atmp", st) for _ in range(3)])
            T["on"] = Ring([kb.sb([128, 128], BF16, "on", st) for _ in range(2)])
            T["rs"] = Ring([kb.sb([128, 16], F32, "rs", st) for _ in range(2)])
            T["rsum"] = kb.sb([128, 1], F32, "rsum", st)
            T["rinv"] = kb.sb([128, 1], F32, "rinv", st)
            return T

        def load_kv(st, K_own, K_prev, V_own, V_prev):
            Kb = kb.sb([128, 4, 2 * TOK], BF16, "Kb", st)
            Vb = kb.sb([128, 32, 512], BF16, "Vb", st)
            kb.sp.dma(Kb[:, :, 0:TOK], K_prev.rearrange("(k p) n -> p k n", p=128), writes=[Kb])
            kb.sp.dma(Kb[:, :, TOK:], K_own.rearrange("(k p) n -> p k n", p=128), writes=[Kb])
            kb.sp.dma(Vb[:, 0:16, :], V_prev.rearrange("(c p) d -> p c d", p=128), writes=[Vb])
            kb.sp.dma(Vb[:, 16:32, :], V_own.rearrange("(c p) d -> p c d", p=128), writes=[Vb])
            return Kb, Vb

        for st in sect("B3"):
            T = mk_T(st)
            Kb, Vb = load_kv(st, A["knT"], PV["pknT"], A["mV"], PV["pmV"])
            Qb = kb.sb([128, 4, TOK], BF16, "Qb", st)
            Qr = kb.sb([128, 4, TOK], BF16, "Qr", st)
            Kr = kb.sb([128, 2 * TOK], BF16, "Kr", st)
            kb.pool.op(lambda: nc.gpsimd.memset(Qr[:], 0.0), writes=[Qr])
            kb.pool.op(lambda: nc.gpsimd.memset(Kr[:], 0.0), writes=[Kr])
            CM = kb.sb([128, 128], F32, "CM", st)
            prevb = kb.sb([128, 2], F32, "prevb", st)
            yc = kb.sb([128, 4, TOK], BF16, "yc", st)
            kb.sp.dma(Qb[:], A["qnT"].rearrange("(k p) n -> p k n", p=128), writes=[Qb])
            kb.sp.dma(Qr[0:64], A["qrT"].rearrange("(k p) n -> p k n", p=64), writes=[Qr])
            kb.sp.dma(Kr[0:64, 0:TOK], PV["pkrT"][:, :], writes=[Kr])
            kb.sp.dma(Kr[0:64, TOK:], A["krT"][:, :], writes=[Kr])
            kb.sp.dma(CM[:], C["CM"][:, :], writes=[CM])
            kb.sp.dma(prevb[:], C["prevb"][:, :], writes=[prevb])
            scale = 192.0 ** -0.5
            psS = Ring([P[2], P[3]])
            psO = Ring([P[4], P[5]])
            for i in range(DBG["ni"]):
                qs = slice(i * 128, (i + 1) * 128)
                tl = key_tiles(i)
                for h in range(4):
                    po = psO.next()
                    rs = T["rs"].next()
                    npc = 0
                    for ti, (s0, Wd, kind) in enumerate(tl):
                        ps = psS.next()
                        kb.pe.op(lambda: nc.tensor.matmul(ps[:, :Wd], lhsT=Qb[:, h, qs], rhs=Kb[:, h, s0:s0 + Wd],
                                                          start=True, stop=False), reads=[Qb, Kb], writes=[ps])
                        kb.pe.op(lambda: nc.tensor.matmul(ps[:, :Wd], lhsT=Qr[:, h, qs], rhs=Kr[:, s0:s0 + Wd],
                                                          start=False, stop=True), reads=[Qr, Kr], writes=[ps])
                        Pb = T["Pb"].next()
                        if kind == 2:
                            tm = T["tmp"].next()
                            if Wd > 128:
                                kb.dve.op(lambda: nc.vector.tensor_scalar(out=tm[:, :Wd - 128], in0=ps[:, :Wd - 128],
                                                                          scalar1=scale, scalar2=None, op0=ALU.mult),
                                          reads=[ps], writes=[tm])
                            kb.dve.op(lambda: nc.vector.scalar_tensor_tensor(out=tm[:, Wd - 128:Wd], in0=ps[:, Wd - 128:Wd],
                                                                             scalar=scale, in1=CM[:], op0=ALU.mult,
                                                                             op1=ALU.add), reads=[ps, CM], writes=[tm])
                            kb.act.op(lambda: nc.scalar.activation(out=Pb[:, :Wd], in_=tm[:, :Wd], func=AF.Exp,
                                                                   accum_out=rs[:, npc:npc + 1]),
                                      reads=[tm], writes=[Pb, rs])
                            npc += 1
                        else:
                            if kind == 0:
                                kb.act.op(lambda: nc.scalar.activation(out=Pb[:, :Wd], in_=ps[:, :Wd], func=AF.Exp,
                                                                       scale=scale, bias=prevb[:, 0:1],
                                                                       accum_out=rs[:, npc:npc + 1]),
                                          reads=[ps, prevb], writes=[Pb, rs])
                            else:
                                kb.act.op(lambda: nc.scalar.activation(out=Pb[:, :Wd], in_=ps[:, :Wd], func=AF.Exp,
                                                                       scale=scale, accum_out=rs[:, npc:npc + 1]),
                                          reads=[ps], writes=[Pb, rs])
                            npc += 1
                        if DBG["tail"]:
                            attn_tail(kb, cx, T, Pb, Wd, s0 // 128, Vb, h, po, ti == 0, ti == len(tl) - 1)
                    if DBG["fin"]:
                        attn_finish(kb, cx, T, po, rs, npc, yc, h, i)
            kb.act.dma(yTv[2], yc[:], reads=[yc])
            kb.barrier()

        for st in sect("B4"):
            T = mk_T(st)
            Kb, Vb = load_kv(st, A["dkT"], PV["pdkT"], A["dV"], PV["pdV"])
            Qb = kb.sb([128, 4, TOK], BF16, "Qb", st)
            Ik = kb.sb([128, 2 * TOK], BF16, "Ik", st)
            iqr = Ring([kb.sb([128, 16, 128], BF16, "iq", st) for _ in range(2)])
            kb.pool.op(lambda: nc.gpsimd.memset(Ik[:], 0.0), writes=[Ik])
            for b_ in iqr.bufs:
                kb.pool.op(lambda: nc.gpsimd.memset(b_[:], 0.0), writes=[b_])
            iwr = Ring([kb.sb([128, 16], F32, "iwt", st) for _ in range(2)])
            iwa = kb.sb([128, 16], F32, "iwa", st)
            iws = kb.sb([128, 16], F32, "iws", st)
            dsg = kb.sb([128, 16, 128], BF16, "dsg", st)
            Rr = Ring([kb.sb([128, 512], BF16, "R", st) for _ in range(3)])
            scb = kb.sb([128, 2 * TOK], F32, "scb", st)
            junk = kb.sb([128, 2 * TOK], BF16, "junk", st)
            AB = kb.sb([128, 4, 512], F32, "AB", st)
            ABD = kb.sb([128, 16, 128], F32, "ABD", st)
            CMB = kb.sb([128, 128], F32, "CMB", st)
            prevs = kb.sb([128, 2], F32, "prevs", st)
            cbt = kb.sb([128, 512], F32, "cbt", st)
            pw = kb.sb([128, NBIS + 1], F32, "pw", st)
            am = kb.sb([128, 8], F32, "am", st)
            sm = {n: kb.sb([128, 1], F32, n, st) for n in ("M", "lo", "W0", "mid", "cnt", "stp")}
            wst_ = kb.sb([128, NBIS + 1], F32, "wsteps", st)
            yb = kb.sb([128, 4, TOK], BF16, "yb", st)
            kb.sp.dma(Qb[:], A["dqT"].rearrange("(k p) n -> p k n", p=128), writes=[Qb])
            kb.sp.dma(Ik[0:64, 0:TOK], PV["pikT"][:, :], writes=[Ik])
            kb.sp.dma(Ik[0:64, TOK:], A["ikT"][:, :], writes=[Ik])
            for nm, t_ in (("AB", AB), ("ABD", ABD)):
                kb.sp.dma(t_[:], C[nm][:, :, :], writes=[t_])
            for nm, t_ in (("CMB", CMB), ("prevs", prevs), ("cb", cbt), ("pw", pw)):
                kb.sp.dma(t_[:], C[nm][:, :], writes=[t_])
            scale = 128.0 ** -0.5
            psS = Ring([P[2], P[3]])
            psO = Ring([P[4], P[5]])
            iqv = A["iqT"].rearrange("(h d) n -> d h n", d=64)
            iwv = A["iw"].rearrange("(b p) h -> b p h", p=128)
            for i in range(DBG["ni"]):
                qs = slice(i * 128, (i + 1) * 128)
                tl = key_tiles(i)
                Ntot = tl[-1][0] + tl[-1][1]
                iq = iqr.next()
                iwt = iwr.next()
                kb.sp.dma(iq[0:64], iqv[:, :, qs], writes=[iq])
                kb.sp.dma(iwt[:], iwv[i], writes=[iwt])
                kb.act.op(lambda: nc.scalar.activation(out=iwa[:], in_=iwt[:], func=AF.Abs), reads=[iwt], writes=[iwa])
                kb.act.op(lambda: nc.scalar.activation(out=iws[:], in_=iwt[:], func=AF.Sign), reads=[iwt], writes=[iws])
                for hh in range(16):
                    kb.pool.op(lambda: nc.gpsimd.tensor_scalar(out=dsg[:, hh, :], in0=ident[:], scalar1=iws[:, hh:hh + 1],
                                                               scalar2=None, op0=ALU.mult),
                               reads=[ident, iws], writes=[dsg])
                for ti, (s0, Wd, kind) in enumerate(tl if DBG["idx"] else []):
                    pss = P[0] if ti % 2 == 0 else P[1]
                    Rs = {}
                    for hh in range(17):
                        if hh < 16:
                            ps = psS.next()
                            kb.pe.op(lambda: nc.tensor.matmul(ps[:, :Wd], lhsT=iq[:, hh, :], rhs=Ik[:, s0:s0 + Wd],
                                                              start=True, stop=True), reads=[iq, Ik], writes=[ps])
                            R = Rr.next()
                            kb.act.op(lambda: nc.scalar.activation(out=R[:, :Wd], in_=ps[:, :Wd], func=AF.Relu,
                                                                   scale=iwa[:, hh:hh + 1]), reads=[ps, iwa], writes=[R])
                            Rs[hh] = R
                        if hh >= 1 and DBG.get("acc", True):
                            g_ = hh - 1
                            R_ = Rs.pop(g_)
                            kb.pe.op(lambda: nc.tensor.matmul(pss[:, :Wd], lhsT=dsg[:, g_, :], rhs=R_[:, :Wd],
                                                              start=(g_ == 0), stop=(g_ == 15)), reads=[dsg, R_],
                                     writes=[pss])
                    if not DBG.get("post", True):
                        continue
                    kb.dve.op(lambda: nc.vector.tensor_reduce(out=am[:, ti:ti + 1], in_=pss[:, :Wd], axis=AX.X, op=ALU.max,
                                                              apply_absolute_value=True), reads=[pss], writes=[am])
                    if DBG.get("post", 2) == 1:
                        continue
                    if kind == 0:
                        kb.dve.op(lambda: nc.vector.tensor_scalar(out=scb[:, s0:s0 + Wd], in0=pss[:, :Wd],
                                                                  scalar1=prevs[:, 0:1], scalar2=None, op0=ALU.add),
                                  reads=[pss, prevs], writes=[scb])
                    else:
                        if kind == 1 or Wd > 128:
                            We = Wd if kind == 1 else Wd - 128
                            kb.dve.op(lambda: nc.vector.tensor_copy(out=scb[:, s0:s0 + We], in_=pss[:, :We]), reads=[pss],
                                      writes=[scb])
                        if kind == 2:
                            kb.dve.op(lambda: nc.vector.tensor_tensor(out=scb[:, s0 + Wd - 128:s0 + Wd],
                                                                      in0=pss[:, Wd - 128:Wd], in1=CMB[:], op=ALU.add),
                                      reads=[pss, CMB], writes=[scb])
                nt = len(tl)
                M, lo, W0, mid, cnt, stp = (sm[n] for n in ("M", "lo", "W0", "mid", "cnt", "stp"))
                kb.dve.op(lambda: nc.vector.tensor_reduce(out=M[:], in_=am[:, 0:nt], axis=AX.X, op=ALU.max), reads=[am], writes=[M])
                kb.dve.op(lambda: nc.vector.tensor_scalar(out=lo[:], in0=M[:], scalar1=-1.0, scalar2=-1.0, op0=ALU.mult,
                                                          op1=ALU.add), reads=[M], writes=[lo])
                kb.dve.op(lambda: nc.vector.tensor_scalar(out=W0[:], in0=M[:], scalar1=2.0, scalar2=2.0, op0=ALU.mult,
                                                          op1=ALU.add), reads=[M], writes=[W0])
                kb.dve.op(lambda: nc.vector.tensor_scalar(out=wst_[:], in0=pw[:], scalar1=W0[:, 0:1], scalar2=None,
                                                          op0=ALU.mult), reads=[pw, W0], writes=[wst_])
                kb.dve.op(lambda: nc.vector.tensor_tensor(out=mid[:], in0=lo[:], in1=wst_[:, 0:1], op=ALU.add),
                          reads=[lo, wst_], writes=[mid])
                for it in range(DBG["nbis"]):
                    kb.dve.op(lambda: nc.vector.tensor_scalar(out=junk[:, :Ntot], in0=scb[:, :Ntot], scalar1=mid[:, 0:1],
                                                              scalar2=None, op0=ALU.is_ge, op1=ALU.add, accum_out=cnt[:]),
                              reads=[scb, mid], writes=[junk, cnt])
                    kb.dve.op(lambda: nc.vector.tensor_scalar(out=stp[:], in0=cnt[:], scalar1=255.5,
                                                              scalar2=wst_[:, it:it + 1], op0=ALU.is_ge, op1=ALU.mult),
                              reads=[cnt, wst_], writes=[stp])
                    kb.dve.op(lambda: nc.vector.tensor_tensor(out=lo[:], in0=lo[:], in1=stp[:], op=ALU.add),
                              reads=[lo, stp], writes=[lo])
                    kb.dve.op(lambda: nc.vector.tensor_tensor(out=mid[:], in0=lo[:], in1=wst_[:, it + 1:it + 2],
                                                              op=ALU.add), reads=[lo, wst_], writes=[mid])
                kb.dve.op(lambda: nc.vector.tensor_scalar(out=junk[:, :Ntot], in0=scb[:, :Ntot], scalar1=lo[:, 0:1],
                                                          scalar2=NEGM, op0=ALU.is_lt, op1=ALU.mult),
                          reads=[scb, lo], writes=[junk])
                rss = [T["rs"].next() for _ in range(2)]
                for h in range(4 if DBG["att"] else 0):
                    po = psO.next()
                    rs = rss[h % 2]
                    npc = 0
                    for ti, (s0, Wd, kind) in enumerate(tl):
                        ps = psS.next()
                        kb.pe.op(lambda: nc.tensor.matmul(ps[:, :Wd], lhsT=Qb[:, h, qs], rhs=Kb[:, h, s0:s0 + Wd],
                                                          start=True, stop=False), reads=[Qb, Kb], writes=[ps])
                        kb.pe.op(lambda: nc.tensor.matmul(ps[:, :Wd], lhsT=identb[:], rhs=junk[:, s0:s0 + Wd], start=False,
                                                          stop=True), reads=[identb, junk], writes=[ps])
                        tm = T["tmp"].next()
                        Pb = T["Pb"].next()
                        cbc = (i * 4 + h) * 8 + ti
                        if kind == 2:
                            wv_ = Wd // 128 - 1
                            if Wd > 128:
                                kb.dve.op(lambda: nc.vector.scalar_tensor_tensor(out=tm[:, :Wd - 128], in0=ps[:, :Wd - 128],
                                                                                 scalar=scale, in1=AB[:, h, :Wd - 128],
                                                                                 op0=ALU.mult, op1=ALU.add),
                                          reads=[ps, AB], writes=[tm])
                            kb.dve.op(lambda: nc.vector.scalar_tensor_tensor(out=tm[:, Wd - 128:Wd], in0=ps[:, Wd - 128:Wd],
                                                                             scalar=scale, in1=ABD[:, h * 4 + wv_, :],
                                                                             op0=ALU.mult, op1=ALU.add),
                                      reads=[ps, ABD], writes=[tm])
                            kb.act.op(lambda: nc.scalar.activation(out=Pb[:, :Wd], in_=tm[:, :Wd], func=AF.Exp,
                                                                   bias=cbt[:, cbc:cbc + 1], accum_out=rs[:, npc:npc + 1]),
                                      reads=[tm, cbt], writes=[Pb, rs])
                            npc += 1
                        else:
                            kb.dve.op(lambda: nc.vector.scalar_tensor_tensor(out=tm[:, :Wd], in0=ps[:, :Wd], scalar=scale,
                                                                             in1=AB[:, h, :Wd], op0=ALU.mult, op1=ALU.add),
                                      reads=[ps, AB], writes=[tm])
                            kb.act.op(lambda: nc.scalar.activation(out=Pb[:, :Wd], in_=tm[:, :Wd], func=AF.Exp,
                                                                   bias=cbt[:, cbc:cbc + 1], accum_out=rs[:, npc:npc + 1]),
                                      reads=[tm, cbt], writes=[Pb, rs])
                            npc += 1
                        attn_tail(kb, cx, T, Pb, Wd, s0 // 128, Vb, h, po, ti == 0, ti == len(tl) - 1)
                    attn_finish(kb, cx, T, po, rs, npc, yb, h, i)
            kb.act.dma(yTv[1], yb[:], reads=[yb])
            kb.barrier()


C_W = {"wg_ct": [4 * 16, 128, KD, 128], "wb_ct": [4 * 16, 128, 4, 128], "wo_ct": [16, 128, KD, 128],
       "wfi_ct": [88, 128, KD, 128], "wfo_ct": [16, 128, 44, 128], "b_gate": [128, 64], "ln1_g": [128, 16],
       "ln1_b": [128, 16], "ln2_g": [128, 16], "ln2_b": [128, 16]}


def ctl(w, kchunks):
    K_, C_ = w.shape
    return np.ascontiguousarray(w.reshape(kchunks, 128, C_ // 128, 128).transpose(2, 1, 0, 3))


def host_C_weights(inputs, l):
    out = {}
    out["wg_ct"] = np.concatenate([ctl(inputs["w_gate"][l, i], KD) for i in range(4)], axis=0)
    out["wb_ct"] = np.concatenate([ctl(inputs["w_branch"][l, i], 4) for i in range(4)], axis=0)
    out["wo_ct"] = ctl(inputs["w_o"][l], KD)
    out["wfi_ct"] = ctl(inputs["w_ffn_in"][l], KD)
    out["wfo_ct"] = ctl(inputs["w_ffn_out"][l], 44)
    out["b_gate"] = np.ascontiguousarray(np.concatenate([pk(inputs["b_gate"][l, i]) for i in range(4)], axis=1))
    for n in ("ln1_g", "ln1_b", "ln2_g", "ln2_b"):
        out[n] = pk(inputs[n][l])
    return out


PRECAST = False


def precast_weights(kb, cx, W, tag):
    nc = kb.nc
    out = dict(W)
    with ExitStack() as st:
        stg = Ring([kb.sb([128, 16, 128], F32, "pcs", st) for _ in range(3)])
        bfr = Ring([kb.sb([128, 16, 128], BF16, "pcb", st) for _ in range(3)])
        cnt = 0
        for name, n, nk in (("wg_ct", 64, 16), ("wb_ct", 64, 4), ("wo_ct", 16, 16), ("wfi_ct", 88, 16), ("wfo_ct", 16, 44)):
            dst = kb.dram(f"bf_{tag}_{name}", [n, 128, nk, 128], BF16)
            out[name] = dst
            for t in range(n):
                for k0 in range(0, nk, 16):
                    kk = min(16, nk - k0)
                    s_ = stg.next()
                    b_ = bfr.next()
                    kb.sp.dma(s_[:, :kk, :], W[name][t][:, k0:k0 + kk, :], writes=[s_])
                    if cnt % 2 == 0:
                        kb.pool.op(lambda: nc.gpsimd.tensor_copy(out=b_[:, :kk, :], in_=s_[:, :kk, :]), reads=[s_],
                                   writes=[b_])
                    else:
                        kb.dve.op(lambda: nc.vector.tensor_copy(out=b_[:, :kk, :], in_=s_[:, :kk, :]), reads=[s_],
                                  writes=[b_])
                    cnt += 1
                    kb.act.dma(dst[t][:, k0:k0 + kk, :], b_[:, :kk, :], reads=[b_])
        kb.barrier()
    return out


def stage_C(kb, cx, l, xT, uT_d, yT_d, W, S, xoT):
    nc = kb.nc
    P = cx.P
    v3 = lambda ap: ap.rearrange("(k p) n -> p k n", p=128)
    with ExitStack() as st:
        vec = kb.sb([128, 128], F32, "cvec", st)
        kb.sp.dma(vec[:, 0:64], W["b_gate"][:, :], writes=[vec])
        for j, n in enumerate(("ln1_g", "ln1_b", "ln2_g", "ln2_b")):
            kb.sp.dma(vec[:, 64 + 16 * j:80 + 16 * j], W[n][:, :], writes=[vec])
        wst = Ring([kb.sb([128, 16, 128], F32, "cwst", st) for _ in range(2)])
        wbf = Ring([kb.sb([128, 16, 128], BF16, "cwbf", st) for _ in range(3)])
        wfo = kb.sb([128, 44, 128], BF16, "cwfo", st)
        tmp = stat_tmps(kb, st)
        t1r = Ring([kb.sb([128, 512], F32, "t1", st) for _ in range(2)])
        t2r = Ring([kb.sb([128, 512], F32, "t2", st) for _ in range(2)])
        sgr = Ring([kb.sb([128, 512], F32, "sg", st) for _ in range(2)])
        u = kb.sb([128, KD, 512], BF16, "cu", st)
        z = kb.sb([128, KD, 512], F32, "cz", st)
        hb = kb.sb([128, 44, 512], BF16, "chb", st)
        mg = BufK(hb.h, "cmg", 0, 16)
        y = BufK(hb.h, "cy", 16, 16)
        acc = kb.sb([128, 512], F32, "cacc", st)
        xt = Ring([kb.sb([128, 512], F32, "cxt", st) for _ in range(2)])
        psr = Ring([P[2], P[3], P[4], P[5]])

        def wload(src, nk):
            wb = wbf.next()
            if PRECAST:
                kb.sp.dma(wb[:, :nk, :], src, writes=[wb])
                return wb
            s = wst.next()
            kb.sp.dma(s[:, :nk, :], src, writes=[s])
            kb.pool.op(lambda: nc.gpsimd.tensor_copy(out=wb[:, :nk, :], in_=s[:, :nk, :]), reads=[s], writes=[wb])
            return wb

        def mm(ps, wb, nk, rhs_buf, rhs_fn):
            for k in range(nk):
                kb.pe.op(lambda: nc.tensor.matmul(ps[:], lhsT=wb[:, k, :], rhs=rhs_fn(k), start=(k == 0), stop=(k == nk - 1)),
                         reads=[wb, rhs_buf], writes=[ps])

        def layer_norm_to(src, gcol, bcol, dst_fn, dst_buf, also=None):
            rstd, nmr = col_stats(kb, cx, [(src, src[:, k, :]) for k in range(KD)], D, True, tmp)
            for k in range(KD):
                t1 = t1r.next(); t2 = t2r.next()
                kb.dve.op(lambda: nc.vector.tensor_tensor(out=t1[:], in0=src[:, k, :], in1=rstd[:], op=ALU.mult),
                          reads=[src, rstd], writes=[t1])
                kb.pool.op(lambda: nc.gpsimd.tensor_tensor(out=t2[:], in0=t1[:], in1=nmr[:], op=ALU.add),
                           reads=[t1, nmr], writes=[t2])
                kb.act.op(lambda: nc.scalar.activation(out=dst_fn(k), in_=t2[:], func=AF.Identity,
                                                       scale=vec[:, gcol + k:gcol + k + 1], bias=vec[:, bcol + k:bcol + k + 1]),
                          reads=[t2, vec], writes=[dst_buf])

        def adaln_to(src, l_, isc, ish, dst):
            rstd, nmr = col_stats(kb, cx, [(src, src[:, k, :]) for k in range(KD)], D, True, tmp)
            for k in range(KD):
                t1 = t1r.next(); t2 = t2r.next()
                kb.dve.op(lambda: nc.vector.tensor_tensor(out=t1[:], in0=src[:, k, :], in1=rstd[:], op=ALU.mult),
                          reads=[src, rstd], writes=[t1])
                kb.pool.op(lambda: nc.gpsimd.tensor_tensor(out=t2[:], in0=t1[:], in1=nmr[:], op=ALU.add),
                           reads=[t1, nmr], writes=[t2])
                kb.act.op(lambda: nc.scalar.activation(out=dst[:, k, :], in_=t2[:], func=AF.Identity,
                                                       scale=cx.onep[:, mcol(l_, isc, k):mcol(l_, isc, k) + 1],
                                                       bias=cx.mod[:, mcol(l_, ish, k):mcol(l_, ish, k) + 1]),
                          reads=[t2, cx.onep, cx.mod], writes=[dst])

        for tt in range(4):
            ts = slice(tt * 512, (tt + 1) * 512)
            kb.barrier()
            kb.sp.dma(u[:], v3(uT_d)[:, :, ts], writes=[u])
            kb.sp.dma(y[:], v3(yT_d)[:, :, ts], writes=[y])
            for j in range(16):
                for i in range(4):
                    wg = wload(W["wg_ct"][i * 16 + j], KD)
                    wbr = wload(W["wb_ct"][i * 16 + j], 4)
                    pg = psr.next(); pb = psr.next()
                    mm(pg, wg, KD, u, lambda k: u[:, k, :])
                    mm(pb, wbr, 4, y, lambda k: y[:, i * 4 + k, :])
                    sg = sgr.next()
                    kb.act.op(lambda: nc.scalar.activation(out=sg[:], in_=pg[:], func=AF.Sigmoid,
                                                           bias=vec[:, i * 16 + j:i * 16 + j + 1], scale=1.0),
                              reads=[pg, vec], writes=[sg])
                    if i == 0:
                        kb.dve.op(lambda: nc.vector.tensor_tensor(out=acc[:], in0=pb[:], in1=sg[:], op=ALU.mult),
                                  reads=[pb, sg], writes=[acc])
                    else:
                        t1 = t1r.next()
                        kb.dve.op(lambda: nc.vector.tensor_tensor(out=t1[:], in0=pb[:], in1=sg[:], op=ALU.mult),
                                  reads=[pb, sg], writes=[t1])
                        if i < 3:
                            kb.pool.op(lambda: nc.gpsimd.tensor_tensor(out=acc[:], in0=acc[:], in1=t1[:], op=ALU.add),
                                       reads=[acc, t1], writes=[acc])
                        else:
                            kb.pool.op(lambda: nc.gpsimd.tensor_tensor(out=mg[:, j, :], in0=acc[:], in1=t1[:], op=ALU.add),
                                       reads=[acc, t1], writes=[mg])
            for j in range(16):
                wo = wload(W["wo_ct"][j], KD)
                ps = psr.next()
                mm(ps, wo, KD, mg, lambda k: mg[:, k, :])
                x_ = xt.next()
                kb.sp.dma(x_[:], xT[j * 128:(j + 1) * 128, ts], writes=[x_])
                t1 = t1r.next()
                kb.act.op(lambda: nc.scalar.activation(out=t1[:], in_=ps[:], func=AF.Copy,
                                                       scale=cx.onep[:, mcol(l, 2, j):mcol(l, 2, j) + 1]),
                          reads=[ps, cx.onep], writes=[t1])
                kb.dve.op(lambda: nc.vector.scalar_tensor_tensor(out=z[:, j, :], in0=x_[:], scalar=ALPHA, in1=t1[:],
                                                                 op0=ALU.mult, op1=ALU.add), reads=[x_, t1], writes=[z])
            layer_norm_to(z, 64, 80, lambda k: z[:, k, :], z)
            adaln_to(z, l, 4, 3, u)
            for j in range(44):
                wa = wload(W["wfi_ct"][j], KD)
                wg_ = wload(W["wfi_ct"][44 + j], KD)
                pa = psr.next(); pg = psr.next()
                mm(pa, wa, KD, u, lambda k: u[:, k, :])
                mm(pg, wg_, KD, u, lambda k: u[:, k, :])
                sg = sgr.next()
                kb.act.op(lambda: nc.scalar.activation(out=sg[:], in_=pa[:], func=AF.Silu), reads=[pa], writes=[sg])
                kb.dve.op(lambda: nc.vector.tensor_tensor(out=hb[:, j, :], in0=pg[:], in1=sg[:], op=ALU.mult),
                          reads=[pg, sg], writes=[hb])
            for j in range(16):
                if PRECAST:
                    kb.sp.dma(wfo[:], W["wfo_ct"][j], writes=[wfo])
                for (k0_, nk_) in (() if PRECAST else ((0, 16), (16, 16), (32, 12))):
                    s_ = wst.next()
                    kb.sp.dma(s_[:, :nk_, :], W["wfo_ct"][j][:, k0_:k0_ + nk_, :], writes=[s_])
                    kb.pool.op(lambda: nc.gpsimd.tensor_copy(out=wfo[:, k0_:k0_ + nk_, :], in_=s_[:, :nk_, :]),
                               reads=[s_], writes=[wfo])
                ps = psr.next()
                mm(ps, wfo, 44, hb, lambda k: hb[:, k, :])
                t1 = t1r.next()
                kb.act.op(lambda: nc.scalar.activation(out=t1[:], in_=ps[:], func=AF.Copy,
                                                       scale=cx.onep[:, mcol(l, 5, j):mcol(l, 5, j) + 1]),
                          reads=[ps, cx.onep], writes=[t1])
                kb.dve.op(lambda: nc.vector.scalar_tensor_tensor(out=z[:, j, :], in0=z[:, j, :], scalar=ALPHA, in1=t1[:],
                                                                 op0=ALU.mult, op1=ALU.add), reads=[z, t1], writes=[z])
            layer_norm_to(z, 96, 112, lambda k: z[:, k, :], z)
            kb.act.dma(v3(xoT)[:, :, ts], z[:], reads=[z])
        kb.barrier()


def _launch(nc, in_maps):
    res = run_bass_kernel_spmd(nc, in_maps, core_ids=list(range(8)))
    return res.results


def build_B(l):
    kb = KB()
    modT = kb.dram("modT", [128, DEPTH * 96], F32, kind="ExternalInput")
    A = {k: kb.dram(k, v[0], dts(v[1]), kind="ExternalInput") for k, v in A_OUT.items() if k != "uT"}
    PV = {k: kb.dram(k, v[0], dts(v[1]), kind="ExternalInput") for k, v in B_PREV.items()}
    W = {k: kb.dram(k, v, F32, kind="ExternalInput") for k, v in B_W.items()}
    C = {k: kb.dram(k, v, F32, kind="ExternalInput") for k, v in B_C.items()}
    yT = kb.dram("yT", [2048, TOK], BF16, kind="ExternalOutput")
    cx = setup_common(kb, modT)
    stage_B(kb, cx, l, A, PV, W, C, yT)
    kb.finish()
    return kb.nc


def build_C(l):
    kb = KB()
    modT = kb.dram("modT", [128, DEPTH * 96], F32, kind="ExternalInput")
    xT = kb.dram("xT", [D, TOK], F32, kind="ExternalInput")
    uT = kb.dram("uT", [D, TOK], BF16, kind="ExternalInput")
    yT = kb.dram("yT", [2048, TOK], BF16, kind="ExternalInput")
    W = {k: kb.dram(k, v, F32, kind="ExternalInput") for k, v in C_W.items()}
    xo = kb.dram("xo", [D, TOK], F32, kind="ExternalOutput")
    cx = setup_common(kb, modT)
    stage_C(kb, cx, l, xT, uT, yT, W, {}, xo)
    kb.finish()
    return kb.nc


def kernel_unfused(**inputs):
    inputs = {k: np.asarray(v) for k, v in inputs.items()}
    x = inputs["x"]
    mods = run_mods(inputs)
    xT = []
    for core in range(8):
        b, h = core // 2, core % 2
        xT.append(np.ascontiguousarray(x[b, h * TOK:(h + 1) * TOK, :].T))
    for l in range(DEPTH):
        wA = [host_A_weights(inputs, l, h) for h in range(2)]
        in_maps = []
        for core in range(8):
            b, h = core // 2, core % 2
            m = {"xT": xT[core], "modT": mods[b]}
            m.update(wA[h])
            in_maps.append(m)
        ra = _launch(build_A(l), in_maps)
        wB = host_B_weights(inputs, l)
        cB = [host_B_consts(h) for h in range(2)]
        in_maps = []
        pm = {"pdkT": "dkT", "pdV": "dV", "pikT": "ikT", "pknT": "knT", "pmV": "mV", "pkrT": "krT"}
        for core in range(8):
            b, h = core // 2, core % 2
            own = ra[core]
            m = {"modT": mods[b]}
            for k in A_OUT:
                if k != "uT":
                    m[k] = np.asarray(own[k])
            if h == 1:
                prev = ra[core - 1]
                for k, v in pm.items():
                    m[k] = np.asarray(prev[v])
                m["pz"] = np.ascontiguousarray(np.asarray(prev["zT"])[:, -32:])
                m["php"] = np.ascontiguousarray(np.asarray(prev["hpT"])[:, -16:])
            else:
                for k, v in pm.items():
                    m[k] = np.zeros_like(np.asarray(own[v]))
                m["pz"] = np.zeros_like(np.asarray(own["zT"])[:, -32:])
                m["php"] = np.zeros((512, 16), np.float32)
            m.update(wB)
            m.update(cB[h])
            in_maps.append(m)
        rb = _launch(build_B(l), in_maps)
        wC = host_C_weights(inputs, l)
        in_maps = []
        for core in range(8):
            b = core // 2
            m = {"modT": mods[b], "xT": xT[core], "uT": np.asarray(ra[core]["uT"]), "yT": np.asarray(rb[core]["yT"])}
            m.update(wC)
            in_maps.append(m)
        rc = _launch(build_C(l), in_maps)
        xT = [np.asarray(rc[core]["xo"]) for core in range(8)]
    out = np.empty((NB, SEQ, D), np.float32)
    for core in range(8):
        b, h = core // 2, core % 2
        out[b, h * TOK:(h + 1) * TOK, :] = xT[core].T
    return out


def stage_M(kb, cx, cT, wadas, baT):
    nc = kb.nc
    with ExitStack() as st:
        csb = kb.sb([128, KD, 4], F32, "csb", st)
        cact = kb.sb([128, KD, 4], F32, "cact", st)
        basb = kb.sb([128, DEPTH * 96], F32, "basb", st)
        wr = Ring([kb.sb([128, KD, 512], F32, "wst", st) for _ in range(2)])
        pr = Ring([cx.P[2], cx.P[3]])
        kb.sp.dma(csb[:], cT[:, :, :], writes=[csb])
        kb.sp.dma(basb[:], baT[:, :], writes=[basb])
        kb.act.op(lambda: nc.scalar.activation(out=cact[:], in_=csb[:], func=AF.Silu), reads=[csb], writes=[cact])
        for l in range(DEPTH):
            wav = wadas[l].rearrange("(k p) n -> p k n", p=128)
            for g in range(24):
                w = wr.next()
                kb.sp.dma(w[:], wav[:, :, g * 512:(g + 1) * 512], writes=[w])
                for j in range(4):
                    t = l * 96 + g * 4 + j
                    p = pr.next()
                    for k in range(KD):
                        kb.pe.op(lambda: nc.tensor.matmul(p[:, 0:4], lhsT=w[:, k, j * 128:(j + 1) * 128], rhs=cact[:, k, :],
                                                          start=(k == 0), stop=(k == KD - 1)),
                                 reads=[w, cact], writes=[p])
                    kb.dve.op(lambda: nc.vector.tensor_scalar(out=cx.mod[:, t:t + 1], in0=p[:, 0:1],
                                                              scalar1=basb[:, t:t + 1], scalar2=None, op0=ALU.add),
                              reads=[p, basb], writes=[cx.mod])
        kb.dve.op(lambda: nc.vector.tensor_scalar(out=cx.onep[:], in0=cx.mod[:], scalar1=1.0, scalar2=None, op0=ALU.add),
                  reads=[cx.mod], writes=[cx.onep])
        kb.barrier()


A_WS = {k: v for k, v in A_W.items() if k not in ("ropec", "ropes")}
ROPE = {"ropec": [64, TOK], "ropes": [64, TOK]}


def build_fused():
    global PRECAST
    PRECAST = True
    kb = KB()
    ext = lambda n, shp, dt=F32: kb.dram(n, shp, dt, kind="ExternalInput")
    xT = ext("xT", [D, SEQ])
    cT = ext("cT", [128, KD, 4])
    wadas = [ext(f"w_ada{l}", [D, 6 * D]) for l in range(DEPTH)]
    baT = ext("baT", [128, DEPTH * 96])
    z32 = ext("z32", [512, 32], BF16)
    z16 = ext("z16", [512, 16])
    rope = [{k: ext(f"{k}_h{h}", v) for k, v in ROPE.items()} for h in range(2)]
    BC = [{k: ext(f"{k}_h{h}", v) for k, v in B_C.items()} for h in range(2)]
    WA = [{k: ext(f"L{l}_{k}", v) for k, v in A_WS.items()} for l in range(DEPTH)]
    WB = [{k: ext(f"L{l}_{k}", v) for k, v in B_W.items()} for l in range(DEPTH)]
    WC = [{k: ext(f"L{l}_{k}", v) for k, v in C_W.items()} for l in range(DEPTH)]
    xo = kb.dram("xo", [D, SEQ], F32, kind="ExternalOutput")
    x1 = kb.dram("x1", [D, SEQ], F32)
    cx = setup_common(kb, None)
    stage_M(kb, cx, cT, wadas, baT)
    S = {k: kb.dram(f"S_{k}", v, F32) for k, v in A_SCR.items()}
    for l in range(DEPTH):
        xin = xT if l == 0 else x1
        xout = x1 if l == 0 else xo
        AO = [{k: kb.dram(f"A{l}{h}_{k}", v[0], dts(v[1])) for k, v in A_OUT.items()} for h in range(2)]
        yT = [kb.dram(f"y{l}{h}", [2048, TOK], BF16) for h in range(2)]
        for h in range(2):
            W = dict(WA[l])
            W.update(rope[h])
            stage_A(kb, cx, l, xin[:, h * TOK:(h + 1) * TOK], W, AO[h], S)
        for h in range(2):
            pm = {"pdkT": "dkT", "pdV": "dV", "pikT": "ikT", "pknT": "knT", "pmV": "mV", "pkrT": "krT"}
            PV = {k: AO[0][v] for k, v in pm.items()}
            if h == 1:
                PV["pz"] = AO[0]["zT"][:, TOK - 32:TOK]
                PV["php"] = AO[0]["hpT"][:, TOK - 16:TOK]
            else:
                PV["pz"] = z32
                PV["php"] = z16
            stage_B(kb, cx, l, AO[h], PV, WB[l], BC[h], yT[h])
        WCb = precast_weights(kb, cx, WC[l], f"L{l}")
        for h in range(2):
            stage_C(kb, cx, l, xin[:, h * TOK:(h + 1) * TOK], AO[h]["uT"], yT[h], WCb, {},
                    xout[:, h * TOK:(h + 1) * TOK])
    kb.finish()
    return kb.nc


def kernel(**inputs):
    import ml_dtypes
    inputs = {k: np.asarray(v) for k, v in inputs.items()}
    x = inputs["x"]
    c = inputs["c"]
    shared = {"z32": np.zeros((512, 32), ml_dtypes.bfloat16), "z16": np.zeros((512, 16), np.float32)}
    for l in range(DEPTH):
        shared[f"w_ada{l}"] = np.ascontiguousarray(inputs["w_ada"][l])
    shared["baT"] = np.ascontiguousarray(np.concatenate([pk(inputs["b_ada"][l]) for l in range(DEPTH)], axis=1))
    for h in range(2):
        for k, v in host_B_consts(h).items():
            shared[f"{k}_h{h}"] = v
    for l in range(DEPTH):
        wa = [host_A_weights(inputs, l, h) for h in range(2)]
        for k in A_WS:
            shared[f"L{l}_{k}"] = wa[0][k]
        if l == 0:
            for h in range(2):
                for k in ROPE:
                    shared[f"{k}_h{h}"] = wa[h][k]
        for k, v in host_B_weights(inputs, l).items():
            shared[f"L{l}_{k}"] = v
        for k, v in host_C_weights(inputs, l).items():
            shared[f"L{l}_{k}"] = v
    in_maps = []
    for core in range(8):
        b = core // 2
        m = dict(shared)
        m["xT"] = np.ascontiguousarray(x[b].T)
        m["cT"] = np.ascontiguousarray(np.repeat(c[b].reshape(KD, 128).T[:, :, None], 4, axis=2))
        in_maps.append(m)
    res = run_bass_kernel_spmd(build_fused(), in_maps, core_ids=list(range(8)))
    out = np.empty((NB, SEQ, D), np.float32)
    for b in range(NB):
        out[b] = np.asarray(res.results[2 * b]["xo"]).T
    return out
```

```python
import numpy as np
from contextlib import ExitStack
import concourse.bass as bass
import concourse.mybir as mybir
from concourse.bass_utils import run_bass_kernel_spmd

F32 = mybir.dt.float32
BF16 = mybir.dt.bfloat16
AF = mybir.ActivationFunctionType
ALU = mybir.AluOpType
AX = mybir.AxisListType

SAME_ENGINE_SYNC = True


class Buf:
    def __init__(self, handle, name):
        self.h = handle
        self.name = name
        self.w = {}
        self.r = {}

    def __getitem__(self, idx):
        return self.h[idx]


class BufV(Buf):
    def __init__(self, handle, name, off, width):
        super().__init__(handle, name)
        self.off = off
        self.width = width

    def __getitem__(self, idx):
        if not isinstance(idx, tuple):
            idx = (idx, slice(None))
        p, c = idx
        a = 0 if c.start is None else c.start
        b = self.width if c.stop is None else c.stop
        return self.h[p, self.off + a:self.off + b]


class BufK(Buf):
    def __init__(self, handle, name, k0, nk):
        super().__init__(handle, name)
        self.k0 = k0
        self.nk = nk

    def __getitem__(self, idx):
        if not isinstance(idx, tuple):
            return self.h[idx, self.k0:self.k0 + self.nk, :]
        p, k = idx[0], idx[1]
        n = idx[2] if len(idx) > 2 else slice(None)
        if isinstance(k, slice):
            a = 0 if k.start is None else k.start
            b = self.nk if k.stop is None else k.stop
            return self.h[p, self.k0 + a:self.k0 + b, n]
        return self.h[p, self.k0 + k, n]


class Eng:
    def __init__(self, kb, name, eng, ndma=0):
        self.kb = kb
        self.name = name
        self.e = eng
        self.sem = kb.newsem("c_" + name)
        self.count = 0
        self.seen = {}
        self.dsems = [kb.newsem(f"d_{name}{i}") for i in range(ndma)]
        self.dcount = 0

    def wait(self, sem, val):
        if val <= 0:
            return
        if sem is self.sem and not SAME_ENGINE_SYNC:
            return
        if self.seen.get(id(sem), 0) >= val:
            return
        self.e.wait_ge(sem, val)
        self.seen[id(sem)] = val

    def _deps(self, reads, writes):
        for b in reads:
            for sem, val in b.w.values():
                self.wait(sem, val)
        for b in writes:
            for sem, val in b.w.values():
                self.wait(sem, val)
            for sem, val in b.r.values():
                self.wait(sem, val)

    def _mark(self, reads, writes, ev):
        for b in reads:
            b.r[id(ev[0])] = ev
        for b in writes:
            b.w = {id(ev[0]): ev}
            b.r = {}

    def op(self, ins_fn, reads=(), writes=(), signal=True):
        signal = True
        self._deps(reads, writes)
        ins = ins_fn()
        if signal:
            self.count += 1
            ins.then_inc(self.sem, 1)
        ev = (self.sem, self.count if signal else self.count + 1)
        self._mark(reads, writes, ev)
        return ins

    def mark_only(self, reads, writes):
        ev = (self.sem, self.count + 1)
        self._mark(reads, writes, ev)

    def dma(self, out, in_, reads=(), writes=(), **kw):
        n = len(self.dsems)
        i = self.dcount
        j = i % n
        sem = self.dsems[j]
        if i >= n:
            self.wait(sem, 16 * (i // n))
        self._deps(reads, writes)
        ins = self.e.dma_start(out=out, in_=in_, **kw)
        ins.then_inc(sem, 16)
        self.dcount += 1
        ev = (sem, 16 * (i // n + 1))
        self._mark(reads, writes, ev)
        return ev


class KB:
    def __init__(self):
        self.nc = bass.Bass("TRN2", target_bir_lowering=False)
        self.es = ExitStack()
        self.sems = []
        nc = self.nc
        self.pe = Eng(self, "pe", nc.tensor)
        self.act = Eng(self, "act", nc.scalar, ndma=4)
        self.dve = Eng(self, "dve", nc.vector)
        self.pool = Eng(self, "pool", nc.gpsimd, ndma=4)
        self.sp = Eng(self, "sp", nc.sync, ndma=8)
        self.engs = [self.pe, self.act, self.dve, self.pool, self.sp]
        self.uid = 0

    def newsem(self, name):
        s = self.es.enter_context(self.nc.semaphore(name))
        self.sems.append(s)
        return s

    def dram(self, name, shape, dt, kind="Internal"):
        return self.nc.dram_tensor(name, list(shape), dt, kind=kind).ap()

    def sb(self, shape, dt, name=None, stack=None):
        self.uid += 1
        name = f"{name or 't'}_{self.uid}"
        h = (stack or self.es).enter_context(self.nc.sbuf_tensor(name, list(shape), dt))
        return Buf(h, name)

    def ps(self, shape, dt=F32, name=None, stack=None):
        self.uid += 1
        name = f"{name or 'p'}_{self.uid}"
        h = (stack or self.es).enter_context(self.nc.psum_tensor(name, list(shape), dt))
        return Buf(h, name)

    def barrier(self):
        evs = []
        for g in self.engs:
            if g.count > 0:
                evs.append((g.sem, g.count))
            n = len(g.dsems)
            for j in range(n):
                cnt = (g.dcount - j + n - 1) // n if g.dcount > j else 0
                if cnt > 0:
                    evs.append((g.dsems[j], 16 * cnt))
        for g in self.engs:
            for sem, val in evs:
                g.wait(sem, val)

    def finish(self):
        self.barrier()
        self.es.close()


class Ring:
    def __init__(self, bufs):
        self.bufs = bufs
        self.i = 0

    def next(self):
        b = self.bufs[self.i % len(self.bufs)]
        self.i += 1
        return b


D = 2048
KD = 16
SEQ = 4096
NB = 4
TOK = 2048
DEPTH = 2
FFN = 5632
EPS = 1e-5
ALPHA = (2 * DEPTH) ** 0.25
SEGS = [("hp", 512), ("dq", 512), ("dk", 512), ("dv", 512), ("iq", 1024), ("ik", 64), ("iw", 16),
        ("cq", 384), ("ckv", 256), ("kr", 64), ("hc", 1024)]
SEG_OFF = {}
_o = 0
for _n, _s in SEGS:
    SEG_OFF[_n] = _o
    _o += _s
FM_SEGS = ["hp", "dq", "dk", "iq", "ik", "cq", "ckv", "kr", "krs", "hc"]
FM_DT = {"hp": "f32", "dq": "bf", "dk": "bf", "iq": "bf", "ik": "bf", "cq": "f32", "ckv": "f32", "kr": "f32",
         "krs": "f32", "hc": "f32"}
FM_ROWS = {"hp": 512, "dq": 512, "dk": 512, "iq": 1024, "ik": 64, "cq": 384, "ckv": 256, "kr": 64, "krs": 64,
           "hc": 1024}
CTS = []
for _n in FM_SEGS:
    _r = FM_ROWS[_n]
    for _c in range(0, _r, 128):
        CTS.append((_n, _c, min(128, _r - _c)))
NCT = len(CTS)


def pk(v):
    v = np.asarray(v)
    return np.ascontiguousarray(v.reshape(-1, 128).T)


def dts(s):
    return F32 if s == "f32" else BF16


def build_mods():
    kb = KB()
    nc = kb.nc
    NCOL = 2 * 6 * D // 8
    NT = NCOL // 128
    wa = kb.dram("wa", [D, NCOL], F32, kind="ExternalInput")
    ba = kb.dram("ba", [128, NT], F32, kind="ExternalInput")
    cT = kb.dram("cT", [128, KD, NB], F32, kind="ExternalInput")
    modT = kb.dram("modT", [128, NT, NB], F32, kind="ExternalOutput")
    csb = kb.sb([128, KD, NB], F32, "csb")
    cact = kb.sb([128, KD, NB], F32, "cact")
    basb = kb.sb([128, NT], F32, "basb")
    msb = kb.sb([128, NT, NB], F32, "msb")
    wr = Ring([kb.sb([128, KD, 512], F32, "wst") for _ in range(2)])
    pr = Ring([kb.ps([128, 512], F32, "ps") for _ in range(2)])
    kb.sp.dma(csb[:], cT[:, :, :], writes=[csb])
    kb.sp.dma(basb[:], ba[:, :], writes=[basb])
    kb.act.op(lambda: nc.scalar.activation(out=cact[:], in_=csb[:], func=AF.Silu), reads=[csb], writes=[cact])
    wav = wa.rearrange("(k p) n -> p k n", p=128)
    for g in range(NCOL // 512):
        w = wr.next()
        kb.sp.dma(w[:], wav[:, :, g * 512:(g + 1) * 512], writes=[w])
        for j in range(4):
            t = g * 4 + j
            p = pr.next()
            for k in range(KD):
                kb.pe.op(lambda: nc.tensor.matmul(p[:, 0:NB], lhsT=w[:, k, j * 128:(j + 1) * 128], rhs=cact[:, k, :],
                                                  start=(k == 0), stop=(k == KD - 1)),
                         reads=[w, cact], writes=[p], signal=(k == KD - 1))
            kb.dve.op(lambda: nc.vector.tensor_scalar(out=msb[:, t, :], in0=p[:, 0:NB], scalar1=basb[:, t:t + 1],
                                                      scalar2=None, op0=ALU.add),
                      reads=[p, basb], writes=[msb])
    kb.sp.dma(modT[:, :, :], msb[:], reads=[msb])
    kb.finish()
    return nc


def run_mods(inputs):
    w_ada = inputs["w_ada"]
    b_ada = inputs["b_ada"]
    c = inputs["c"]
    wcat = np.concatenate([w_ada[0], w_ada[1]], axis=1)
    bcat = np.concatenate([b_ada[0], b_ada[1]], axis=0)
    cT = np.ascontiguousarray(c.reshape(NB, KD, 128).transpose(2, 1, 0))
    NCOL = 3072
    in_maps = []
    for core in range(8):
        sl = slice(core * NCOL, (core + 1) * NCOL)
        in_maps.append({"wa": np.ascontiguousarray(wcat[:, sl]), "ba": pk(bcat[sl]), "cT": cT})
    nc = build_mods()
    res = run_bass_kernel_spmd(nc, in_maps, core_ids=list(range(8)))
    mt = np.concatenate([r["modT"] for r in res.results], axis=1)
    return [np.ascontiguousarray(mt[:, :, b]) for b in range(NB)]


class Ctx:
    pass


def setup_common(kb, modT_d):
    nc = kb.nc
    cx = Ctx()
    cx.P = [kb.ps([128, 512], F32, f"P{i}") for i in range(6)]
    cx.PB = []
    for i in range(2):
        big = kb.ps([128, 1024], BF16, f"PBB{i}")
        cx.PB += [BufV(big.h, f"PB{2 * i}", 0, 512), BufV(big.h, f"PB{2 * i + 1}", 512, 512)]
    cx.ones_f = kb.sb([128, 128], F32, "ones_f")
    cx.ones_b = kb.sb([128, 128], BF16, "ones_b")
    cx.eps = kb.sb([128, 1], F32, "eps")
    cx.mod = kb.sb([128, DEPTH * 96], F32, "mod")
    cx.onep = kb.sb([128, DEPTH * 96], F32, "onep")
    kb.pool.op(lambda: nc.gpsimd.memset(cx.ones_f[:], 1.0), writes=[cx.ones_f])
    kb.pool.op(lambda: nc.gpsimd.memset(cx.ones_b[:], 1.0), writes=[cx.ones_b])
    kb.pool.op(lambda: nc.gpsimd.memset(cx.eps[:], EPS), writes=[cx.eps])
    if modT_d is not None:
        kb.sp.dma(cx.mod[:], modT_d[:, :], writes=[cx.mod])
        kb.dve.op(lambda: nc.vector.tensor_scalar(out=cx.onep[:], in0=cx.mod[:], scalar1=1.0, scalar2=None, op0=ALU.add),
                  reads=[cx.mod], writes=[cx.onep])
    return cx


def mcol(l, i, k):
    return (l * 6 + i) * 16 + k


def col_stats(kb, cx, chunks, nfeat, want_mean, tmp, N=512):
    nc = kb.nc
    ps_s, ps_q = cx.P[0], cx.P[1]
    n = len(chunks)
    for k, (b, ap) in enumerate(chunks):
        rows = ap.shape[0]
        sq = tmp["sq"].next()
        kb.act.op(lambda: nc.scalar.activation(out=sq[:rows, :N], in_=ap, func=AF.Square), reads=[b], writes=[sq])
        kb.pe.op(lambda: nc.tensor.matmul(ps_q[:, :N], lhsT=cx.ones_f[:rows, :], rhs=sq[:rows, :N], start=(k == 0),
                                          stop=(k == n - 1)), reads=[sq, cx.ones_f], writes=[ps_q])
        if want_mean:
            kb.pe.op(lambda: nc.tensor.matmul(ps_s[:, :N], lhsT=cx.ones_f[:rows, :], rhs=ap, start=(k == 0),
                                              stop=(k == n - 1)), reads=[b, cx.ones_f], writes=[ps_s])
    inv = 1.0 / nfeat
    var, rstd = tmp["var"], tmp["rstd"]
    if want_mean:
        mean, msq, nmr = tmp["mean"], tmp["msq"], tmp["nmr"]
        kb.dve.op(lambda: nc.vector.tensor_scalar(out=mean[:, :N], in0=ps_s[:, :N], scalar1=inv, scalar2=None,
                                                  op0=ALU.mult), reads=[ps_s], writes=[mean])
        kb.dve.op(lambda: nc.vector.tensor_tensor(out=msq[:, :N], in0=mean[:, :N], in1=mean[:, :N], op=ALU.mult),
                  reads=[mean], writes=[msq])
        kb.dve.op(lambda: nc.vector.scalar_tensor_tensor(out=var[:, :N], in0=ps_q[:, :N], scalar=inv, in1=msq[:, :N],
                                                         op0=ALU.mult, op1=ALU.subtract),
                  reads=[ps_q, msq], writes=[var])
        kb.act.op(lambda: nc.scalar.activation(out=var[:, :N], in_=var[:, :N], func=AF.Sqrt, bias=cx.eps[:, 0:1],
                                               scale=1.0), reads=[var, cx.eps], writes=[var])
    else:
        kb.act.op(lambda: nc.scalar.activation(out=var[:, :N], in_=ps_q[:, :N], func=AF.Sqrt, bias=cx.eps[:, 0:1],
                                               scale=inv), reads=[ps_q, cx.eps], writes=[var])
    kb.dve.op(lambda: nc.vector.reciprocal(out=rstd[:, :N], in_=var[:, :N]), reads=[var], writes=[rstd])
    if want_mean:
        kb.dve.op(lambda: nc.vector.scalar_tensor_tensor(out=nmr[:, :N], in0=mean[:, :N], scalar=-1.0,
                                                         in1=rstd[:, :N], op0=ALU.mult, op1=ALU.mult),
                  reads=[mean, rstd], writes=[nmr])
        return rstd, nmr
    return rstd, None


def stat_tmps(kb, st, N=512):
    t = {"sq": Ring([kb.sb([128, N], F32, "sq", st) for _ in range(2)])}
    for nm in ("mean", "msq", "var", "rstd", "nmr"):
        t[nm] = kb.sb([128, N], F32, nm, st)
    return t


def load_cast(kb, dst, dst_ap, src_ap, stg_ring, stg_view):
    nc = kb.nc
    s = stg_ring.next()
    kb.sp.dma(stg_view(s), src_ap, writes=[s])
    kb.pool.op(lambda: nc.gpsimd.tensor_copy(out=dst_ap, in_=stg_view(s)), reads=[s], writes=[dst])


def evac(kb, i, out_buf, out_ap, ps_buf, ps_ap):
    nc = kb.nc
    if i % 2 == 0:
        kb.act.op(lambda: nc.scalar.copy(out=out_ap, in_=ps_ap), reads=[ps_buf], writes=[out_buf])
    else:
        kb.dve.op(lambda: nc.vector.tensor_copy(out=out_ap, in_=ps_ap), reads=[ps_buf], writes=[out_buf])


def stage_A(kb, cx, l, xT, W, O, S):
    nc = kb.nc
    P = cx.P
    xTv = xT.rearrange("(k p) n -> p k n", p=128)
    with ExitStack() as st:
        uT = [kb.sb([128, KD, 512], BF16, f"uT{t}", st) for t in range(4)]
        with ExitStack() as s1:
            xr = Ring([kb.sb([128, KD, 512], F32, "xt", s1) for _ in range(2)])
            tmp = stat_tmps(kb, s1)
            t1r = Ring([kb.sb([128, 512], F32, "t1", s1) for _ in range(2)])
            t2r = Ring([kb.sb([128, 512], F32, "t2", s1) for _ in range(2)])
            for tt in range(4):
                xt = xr.next()
                kb.sp.dma(xt[:], xTv[:, :, tt * 512:(tt + 1) * 512], writes=[xt])
                rstd, nmr = col_stats(kb, cx, [(xt, xt[:, k, :]) for k in range(KD)], D, True, tmp)
                for k in range(KD):
                    t1 = t1r.next()
                    t2 = t2r.next()
                    kb.dve.op(lambda: nc.vector.tensor_tensor(out=t1[:], in0=xt[:, k, :], in1=rstd[:], op=ALU.mult),
                              reads=[xt, rstd], writes=[t1])
                    kb.pool.op(lambda: nc.gpsimd.tensor_tensor(out=t2[:], in0=t1[:], in1=nmr[:], op=ALU.add),
                               reads=[t1, nmr], writes=[t2])
                    kb.act.op(lambda: nc.scalar.activation(out=uT[tt][:, k, :], in_=t2[:], func=AF.Identity,
                                                           scale=cx.onep[:, mcol(l, 1, k):mcol(l, 1, k) + 1],
                                                           bias=cx.mod[:, mcol(l, 0, k):mcol(l, 0, k) + 1]),
                              reads=[t2, cx.onep, cx.mod], writes=[uT[tt]])
                kb.act.dma(O["uT"].rearrange("(k p) n -> p k n", p=128)[:, :, tt * 512:(tt + 1) * 512], uT[tt][:],
                           reads=[uT[tt]])
            kb.barrier()
        with ExitStack() as s2:
            wst = Ring([kb.sb([128, KD, 128], F32, "wst", s2) for _ in range(2)])
            wbf = Ring([kb.sb([128, KD, 128], BF16, "wbf", s2) for _ in range(2)])
            obf = Ring([kb.sb([128, TOK], BF16, "obf", s2) for _ in range(2)])
            of32 = Ring([kb.sb([128, TOK], F32, "of32", s2) for _ in range(2)])
            psr = Ring([P[2], P[3], P[4], P[5]])
            dest = {"hp": O["hpT"], "dq": O["dqT"], "dk": O["dkT"], "iq": O["iqT"], "ik": O["ikT"], "cq": S["cqT"],
                    "ckv": S["ckvT"], "kr": S["krraw"], "krs": S["krsraw"], "hc": S["hcT"]}
            ei = 0
            for ct, (seg, c0, ncols) in enumerate(CTS):
                wb = wbf.next()
                load_cast(kb, wb, wb[:], W["win_ct"][ct], wst, lambda s: s[:])
                ob = (of32 if FM_DT[seg] == "f32" else obf).next()
                for tt in range(4):
                    ps = psr.next()
                    for k in range(KD):
                        kb.pe.op(lambda: nc.tensor.matmul(ps[:], lhsT=wb[:, k, :], rhs=uT[tt][:, k, :],
                                                          start=(k == 0), stop=(k == KD - 1)),
                                 reads=[wb, uT[tt]], writes=[ps])
                    evac(kb, ei, ob, ob[:ncols, tt * 512:(tt + 1) * 512], ps, ps[:ncols, :])
                    ei += 1
                kb.act.dma(dest[seg][c0:c0 + ncols, :], ob[:ncols, :], reads=[ob])
            wv = kb.sb([128, KD, 512], BF16, "wv", s2)
            wiw = kb.sb([128, KD, 128], BF16, "wiw", s2)
            for j in range(4):
                load_cast(kb, wv, wv[:, :, j * 128:(j + 1) * 128], W["wdv_ct"][j], wst, lambda s: s[:])
            load_cast(kb, wiw, wiw[:], W["wiw_ct"][0], wst, lambda s: s[:])
            vob = Ring([kb.sb([128, 512], BF16, "vob", s2) for _ in range(2)])
            iwo = kb.sb([128, 16, 16], F32, "iwo", s2)
            for tb in range(16):
                tt, off = tb // 4, (tb % 4) * 128
                ps = psr.next()
                for k in range(KD):
                    kb.pe.op(lambda: nc.tensor.matmul(ps[:], lhsT=uT[tt][:, k, off:off + 128], rhs=wv[:, k, :],
                                                      start=(k == 0), stop=(k == KD - 1)),
                             reads=[wv, uT[tt]], writes=[ps])
                vo = vob.next()
                evac(kb, tb, vo, vo[:], ps, ps[:])
                kb.act.dma(O["dV"][tb * 128:(tb + 1) * 128, :], vo[:], reads=[vo])
                ps2 = psr.next()
                for k in range(KD):
                    kb.pe.op(lambda: nc.tensor.matmul(ps2[:, 0:16], lhsT=uT[tt][:, k, off:off + 128], rhs=wiw[:, k, 0:16],
                                                      start=(k == 0), stop=(k == KD - 1)),
                             reads=[wiw, uT[tt]], writes=[ps2])
                evac(kb, tb + 1, iwo, iwo[:, tb, :], ps2, ps2[:, 0:16])
            kb.act.dma(O["iw"].rearrange("(b p) h -> p b h", p=128), iwo[:], reads=[iwo])
            kb.barrier()
    with ExitStack() as s3:
        stg = Ring([kb.sb([128, 1024], F32, "stg", s3) for _ in range(2)])
        wq = kb.sb([128, 3, 1024], BF16, "wq", s3)
        wkv = kb.sb([128, 2, 1024], BF16, "wkv", s3)
        for kc in range(3):
            load_cast(kb, wq, wq[:, kc, :], W["wq_all"][kc * 128:(kc + 1) * 128, :], stg, lambda s: s[:])
        for kc in range(2):
            load_cast(kb, wkv, wkv[:, kc, :], W["wkv_all"][kc * 128:(kc + 1) * 128, :], stg, lambda s: s[:])
        vecs = kb.sb([128, 8], F32, "vecs", s3)
        kb.sp.dma(vecs[:, 0:3], W["q_norm"][:, :], writes=[vecs])
        kb.sp.dma(vecs[:, 3:5], W["kv_norm"][:, :], writes=[vecs])
        tmp = stat_tmps(kb, s3)
        ar = Ring([kb.sb([128, 8, 512], F32, "hc", s3) for _ in range(1)])
        sig = kb.sb([128, 4, 512], F32, "sig", s3)
        zb = kb.sb([128, 4, 512], BF16, "zb", s3)
        cqs = kb.sb([128, 3, 512], F32, "cqs", s3)
        cqn = kb.sb([128, 3, 512], BF16, "cqn", s3)
        cks = kb.sb([128, 2, 512], F32, "cks", s3)
        ckn = kb.sb([128, 2, 512], BF16, "ckn", s3)
        cc = kb.sb([64, 512], F32, "cc", s3)
        ss = kb.sb([64, 512], F32, "ss", s3)
        krr = kb.sb([64, 2, 512], F32, "krr", s3)
        t1r = Ring([kb.sb([128, 512], F32, "t1", s3) for _ in range(2)])
        t2r = Ring([kb.sb([128, 512], F32, "t2", s3) for _ in range(2)])
        obr = Ring([kb.sb([128, 512], BF16, "ob", s3) for _ in range(3)])
        psr = Ring([P[2], P[3], P[4], P[5]])
        ei = 0
        for tt in range(4):
            ts = slice(tt * 512, (tt + 1) * 512)
            hc = ar.next()
            kb.sp.dma(hc[:], S["hcT"].rearrange("(k p) n -> p k n", p=128)[:, :, ts], writes=[hc])
            kb.act.op(lambda: nc.scalar.activation(out=sig[:], in_=hc[:, 4:8, :], func=AF.Sigmoid), reads=[hc],
                      writes=[sig])
            kb.dve.op(lambda: nc.vector.tensor_tensor(out=zb[:], in0=hc[:, 0:4, :], in1=sig[:], op=ALU.mult),
                      reads=[hc, sig], writes=[zb])
            kb.act.dma(O["zT"].rearrange("(k p) n -> p k n", p=128)[:, :, ts], zb[:], reads=[zb])
            kb.sp.dma(cc[:], W["ropec"][:, ts], writes=[cc])
            kb.sp.dma(ss[:], W["ropes"][:, ts], writes=[ss])
            kb.sp.dma(cqs[:], S["cqT"].rearrange("(k p) n -> p k n", p=128)[:, :, ts], writes=[cqs])
            rstd, _ = col_stats(kb, cx, [(cqs, cqs[:, k, :]) for k in range(3)], 384, False, tmp)
            for k in range(3):
                t1 = t1r.next()
                kb.dve.op(lambda: nc.vector.tensor_tensor(out=t1[:], in0=cqs[:, k, :], in1=rstd[:], op=ALU.mult),
                          reads=[cqs, rstd], writes=[t1])
                kb.act.op(lambda: nc.scalar.activation(out=cqn[:, k, :], in_=t1[:], func=AF.Copy,
                                                       scale=vecs[:, k:k + 1]), reads=[t1, vecs], writes=[cqn])
            for h in range(4):
                ps = psr.next()
                for k in range(3):
                    kb.pe.op(lambda: nc.tensor.matmul(ps[:], lhsT=wq[:, k, h * 256:h * 256 + 128], rhs=cqn[:, k, :],
                                                      start=(k == 0), stop=(k == 2)), reads=[wq, cqn], writes=[ps])
                ob = obr.next()
                evac(kb, ei, ob, ob[:], ps, ps[:]); ei += 1
                kb.act.dma(O["qnT"][h * 128:(h + 1) * 128, ts], ob[:], reads=[ob])
                ps = psr.next()
                ps2 = psr.next()
                for k in range(3):
                    kb.pe.op(lambda: nc.tensor.matmul(ps[:64, :], lhsT=wq[:, k, h * 256 + 128:h * 256 + 192],
                                                      rhs=cqn[:, k, :], start=(k == 0), stop=(k == 2)),
                             reads=[wq, cqn], writes=[ps])
                for k in range(3):
                    kb.pe.op(lambda: nc.tensor.matmul(ps2[:64, :], lhsT=wq[:, k, h * 256 + 192:h * 256 + 256],
                                                      rhs=cqn[:, k, :], start=(k == 0), stop=(k == 2)),
                             reads=[wq, cqn], writes=[ps2])
                t1 = t1r.next(); t2 = t2r.next(); ob = obr.next()
                kb.dve.op(lambda: nc.vector.tensor_tensor(out=t1[:64, :], in0=ps[:64, :], in1=cc[:], op=ALU.mult),
                          reads=[ps, cc], writes=[t1])
                kb.dve.op(lambda: nc.vector.tensor_tensor(out=t2[:64, :], in0=ps2[:64, :], in1=ss[:], op=ALU.mult),
                          reads=[ps2, ss], writes=[t2])
                kb.pool.op(lambda: nc.gpsimd.tensor_tensor(out=ob[:64, :], in0=t1[:64, :], in1=t2[:64, :], op=ALU.add),
                           reads=[t1, t2], writes=[ob])
                kb.act.dma(O["qrT"][h * 64:(h + 1) * 64, ts], ob[:64, :], reads=[ob])
            kb.sp.dma(cks[:], S["ckvT"].rearrange("(k p) n -> p k n", p=128)[:, :, ts], writes=[cks])
            rstd, _ = col_stats(kb, cx, [(cks, cks[:, k, :]) for k in range(2)], 256, False, tmp)
            for k in range(2):
                t1 = t1r.next()
                kb.dve.op(lambda: nc.vector.tensor_tensor(out=t1[:], in0=cks[:, k, :], in1=rstd[:], op=ALU.mult),
                          reads=[cks, rstd], writes=[t1])
                kb.act.op(lambda: nc.scalar.activation(out=ckn[:, k, :], in_=t1[:], func=AF.Copy,
                                                       scale=vecs[:, 3 + k:4 + k]), reads=[t1, vecs], writes=[ckn])
            for h in range(4):
                ps = psr.next()
                for k in range(2):
                    kb.pe.op(lambda: nc.tensor.matmul(ps[:], lhsT=wkv[:, k, h * 128:(h + 1) * 128], rhs=ckn[:, k, :],
                                                      start=(k == 0), stop=(k == 1)), reads=[wkv, ckn], writes=[ps])
                ob = obr.next()
                evac(kb, ei, ob, ob[:], ps, ps[:]); ei += 1
                kb.act.dma(O["knT"][h * 128:(h + 1) * 128, ts], ob[:], reads=[ob])
            for tb in range(4):
                ps = psr.next()
                for k in range(2):
                    kb.pe.op(lambda: nc.tensor.matmul(ps[:], lhsT=ckn[:, k, tb * 128:(tb + 1) * 128],
                                                      rhs=wkv[:, k, 512:1024], start=(k == 0), stop=(k == 1)),
                             reads=[wkv, ckn], writes=[ps])
                ob = obr.next()
                evac(kb, ei, ob, ob[:], ps, ps[:]); ei += 1
                kb.act.dma(O["mV"][tt * 512 + tb * 128:tt * 512 + (tb + 1) * 128, :], ob[:], reads=[ob])
            kb.sp.dma(krr[:, 0, :], S["krraw"][:, ts], writes=[krr])
            kb.sp.dma(krr[:, 1, :], S["krsraw"][:, ts], writes=[krr])
            t1 = t1r.next(); t2 = t2r.next(); ob = obr.next()
            kb.dve.op(lambda: nc.vector.tensor_tensor(out=t1[:64, :], in0=krr[:, 0, :], in1=cc[:], op=ALU.mult),
                      reads=[krr, cc], writes=[t1])
            kb.dve.op(lambda: nc.vector.tensor_tensor(out=t2[:64, :], in0=krr[:, 1, :], in1=ss[:], op=ALU.mult),
                      reads=[krr, ss], writes=[t2])
            kb.pool.op(lambda: nc.gpsimd.tensor_tensor(out=ob[:64, :], in0=t1[:64, :], in1=t2[:64, :], op=ALU.add),
                       reads=[t1, t2], writes=[ob])
            kb.act.dma(O["krT"][:, ts], ob[:64, :], reads=[ob])
        kb.barrier()


A_OUT = {"uT": ([D, TOK], "bf"), "hpT": ([512, TOK], "f32"), "dqT": ([512, TOK], "bf"), "dkT": ([512, TOK], "bf"),
         "dV": ([TOK, 512], "bf"), "iqT": ([1024, TOK], "bf"), "ikT": ([64, TOK], "bf"), "iw": ([TOK, 16], "f32"),
         "qnT": ([512, TOK], "bf"), "qrT": ([256, TOK], "bf"), "knT": ([512, TOK], "bf"), "mV": ([TOK, 512], "bf"),
         "krT": ([64, TOK], "bf"), "zT": ([512, TOK], "bf")}
A_SCR = {"cqT": [384, TOK], "ckvT": [256, TOK], "krraw": [64, TOK], "krsraw": [64, TOK], "hcT": [1024, TOK]}
A_W = {"win_ct": [NCT, 128, KD, 128], "wdv_ct": [4, 128, KD, 128], "wiw_ct": [1, 128, KD, 128],
       "wq_all": [384, 1024], "wkv_all": [256, 1024], "q_norm": [128, 3], "kv_norm": [128, 2],
       "ropec": [64, TOK], "ropes": [64, TOK]}


def ct_layout(w, c0, ncols):
    out = np.zeros((128, KD, 128), np.float32)
    out[:, :, :ncols] = w[:, c0:c0 + ncols].reshape(KD, 128, ncols).transpose(1, 0, 2)
    return out


def host_A_weights(inputs, l, h):
    w_in = inputs["w_in"][l]
    cts = []
    for seg, c0, ncols in CTS:
        if seg == "krs":
            base = SEG_OFF["kr"]
            wsw = np.concatenate([w_in[:, base + 32:base + 64], w_in[:, base:base + 32]], axis=1)
            cts.append(ct_layout(wsw, 0, 64))
        else:
            cts.append(ct_layout(w_in, SEG_OFF[seg] + c0, ncols))
    out = {"win_ct": np.stack(cts)}
    out["wdv_ct"] = np.stack([ct_layout(w_in, SEG_OFF["dv"] + j * 128, 128) for j in range(4)])
    out["wiw_ct"] = np.stack([ct_layout(w_in, SEG_OFF["iw"], 16)])
    wq = inputs["w_q_up"][l]
    parts = []
    for hh in range(4):
        b = hh * 192
        parts += [wq[:, b:b + 128], wq[:, b + 128:b + 192], wq[:, b + 160:b + 192], wq[:, b + 128:b + 160]]
    out["wq_all"] = np.ascontiguousarray(np.concatenate(parts, axis=1))
    wkv = inputs["w_kv_up"][l]
    out["wkv_all"] = np.ascontiguousarray(np.concatenate(
        [wkv[:, hh * 256:hh * 256 + 128] for hh in range(4)] + [wkv[:, hh * 256 + 128:hh * 256 + 256] for hh in range(4)],
        axis=1))
    out["q_norm"] = pk(inputs["q_norm"][l])
    out["kv_norm"] = pk(inputs["kv_norm"][l])
    pos = np.arange(h * TOK, (h + 1) * TOK, dtype=np.float32)
    inv_freq = (np.float32(10000.0) ** (-np.arange(0, 64, 2, dtype=np.float32) / np.float32(64))).astype(np.float32)
    ang = pos[None, :] * inv_freq[:, None]
    cos, sin = np.cos(ang).astype(np.float32), np.sin(ang).astype(np.float32)
    out["ropec"] = np.ascontiguousarray(np.concatenate([cos, cos], axis=0))
    out["ropes"] = np.ascontiguousarray(np.concatenate([-sin, sin], axis=0))
    return out


def build_A(l):
    kb = KB()
    xT = kb.dram("xT", [D, TOK], F32, kind="ExternalInput")
    modT = kb.dram("modT", [128, DEPTH * 96], F32, kind="ExternalInput")
    W = {k: kb.dram(k, v, F32, kind="ExternalInput") for k, v in A_W.items()}
    O = {k: kb.dram(k, v[0], dts(v[1]), kind="ExternalOutput") for k, v in A_OUT.items()}
    S = {k: kb.dram(k, v, F32) for k, v in A_SCR.items()}
    cx = setup_common(kb, modT)
    stage_A(kb, cx, l, xT, W, O, S)
    kb.finish()
    return kb.nc


SLOPES = [2.0 ** (-8.0 * (h + 1) / 4) for h in range(4)]
NBIS = 16
NEGM = -30000.0


def key_tiles(i, with_prev=True):
    tl = [(j * 512, 512, 0) for j in range(4)] if with_prev else []
    for j in range(i // 4):
        tl.append((2048 + j * 512, 512, 1))
    tl.append((2048 + (i // 4) * 512, (i % 4 + 1) * 128, 2))
    return tl


def host_B_consts(h, skip_prev=False):
    c = {}
    q = np.arange(128)[:, None]
    s = np.arange(512)[None, :]
    c["AB"] = np.stack([SLOPES[hh] * (s - q) for hh in range(4)], axis=1).astype(np.float32)
    s1 = np.arange(128)[None, :]
    cm = np.where((s1 // 64) <= (q // 64), 0.0, 1.0)
    c["ABD"] = np.stack([-SLOPES[hh] * np.abs(q - s1) + SLOPES[hh] * 128.0 * wv for hh in range(4) for wv in range(4)],
                        axis=1).astype(np.float32)
    c["CM"] = (cm * NEGM).astype(np.float32)
    c["CMB"] = (cm * -1e6).astype(np.float32)
    c["ident"] = np.eye(128, dtype=np.float32)
    prevb = 0.0 if h == 1 else NEGM
    c["prevb"] = np.full((128, 2), prevb, np.float32)
    c["prevs"] = np.full((128, 2), 0.0 if h == 1 else -1e6, np.float32)
    cb = np.zeros((128, 16 * 4 * 8), np.float32)
    for i in range(16):
        tq0 = 2048 + 128 * i
        for hh in range(4):
            for ti, (s0, w, kind) in enumerate(key_tiles(i, not (skip_prev and h == 0))):
                v = -SLOPES[hh] * (tq0 - s0)
                if kind == 0:
                    v += prevb
                cb[:, (i * 4 + hh) * 8 + ti] = v
    c["cb"] = cb
    c["pw"] = np.tile((2.0 ** -(np.arange(NBIS + 1) + 1.0))[None, :], (128, 1)).astype(np.float32)
    ic = np.zeros((128, 4, 16), np.float32)
    for g, w in enumerate((2, 4, 8, 16)):
        t = np.arange(16) + h * TOK
        ic[:, g, :] = 1.0 / np.minimum(t + 1, w)
    c["invc"] = ic
    return c


B_C = {"AB": [128, 4, 512], "ABD": [128, 16, 128], "CM": [128, 128], "CMB": [128, 128], "ident": [128, 128],
       "prevb": [128, 2], "prevs": [128, 2], "cb": [128, 512], "pw": [128, NBIS + 1], "invc": [128, 4, 16]}
B_W = {"w_pool": [4, 128, 128], "pool_scale": [128, 4], "w_dwT": [128, 4, 31], "b_dw": [128, 4],
       "cln_g": [128, 4], "cln_b": [128, 4]}
B_PREV = {"pdkT": ([512, TOK], "bf"), "pdV": ([TOK, 512], "bf"), "pikT": ([64, TOK], "bf"), "pknT": ([512, TOK], "bf"),
          "pmV": ([TOK, 512], "bf"), "pkrT": ([64, TOK], "bf"), "pz": ([512, 32], "bf"), "php": ([512, 16], "f32")}


def host_B_weights(inputs, l):
    out = {"w_pool": np.ascontiguousarray(inputs["w_pool"][l]), "pool_scale": pk(inputs["pool_scale"][l]),
           "b_dw": pk(inputs["b_dw"][l]), "cln_g": pk(inputs["conv_ln_g"][l]), "cln_b": pk(inputs["conv_ln_b"][l])}
    wd = inputs["w_dw"][l]
    out["w_dwT"] = np.ascontiguousarray(wd.reshape(31, 4, 128).transpose(2, 1, 0))
    return out


PARTS = {"B1", "B2", "B3", "B4"}
DBG = {"ni": 16, "tail": True, "fin": True, "nbis": NBIS, "att": True, "idx": True}


def sect(tag):
    if tag in PARTS:
        with ExitStack() as st:
            yield st


def attn_tail(kb, cx, T, Pb, W, s_chunk0, Vb, h, po, first, last):
    nc = kb.nc
    nb = W // 128
    pt = T["ptps"].next()
    for sb in range(nb):
        kb.pe.op(lambda: nc.tensor.transpose(out=pt[:, sb * 128:(sb + 1) * 128], in_=Pb[:, sb * 128:(sb + 1) * 128],
                                             identity=T["identb"][:]), reads=[Pb, T["identb"]], writes=[pt])
    pts = T["pts"].next()
    evac(kb, T["ei"][0], pts, pts[:, :W], pt, pt[:, :W])
    T["ei"][0] += 1
    for sb in range(nb):
        kb.pe.op(lambda: nc.tensor.matmul(po[:, 0:128], lhsT=pts[:, sb * 128:(sb + 1) * 128],
                                          rhs=Vb[:, s_chunk0 + sb, h * 128:(h + 1) * 128],
                                          start=(first and sb == 0), stop=(last and sb == nb - 1)),
                 reads=[pts, Vb], writes=[po])


def attn_finish(kb, cx, T, po, rs, npieces, yb, h, i):
    nc = kb.nc
    rsum, rinv, on = T["rsum"], T["rinv"], T["on"].next()
    kb.dve.op(lambda: nc.vector.tensor_reduce(out=rsum[:], in_=rs[:, 0:npieces], axis=AX.X, op=ALU.add), reads=[rs], writes=[rsum])
    kb.dve.op(lambda: nc.vector.reciprocal(out=rinv[:], in_=rsum[:]), reads=[rsum], writes=[rinv])
    kb.act.op(lambda: nc.scalar.activation(out=on[:], in_=po[:, 0:128], func=AF.Copy, scale=rinv[:, 0:1]),
              reads=[po, rinv], writes=[on])
    pt = T["ptps"].next()
    kb.pe.op(lambda: nc.tensor.transpose(out=pt[:, 0:128], in_=on[:], identity=T["identb"][:]),
             reads=[on, T["identb"]], writes=[pt])
    kb.dve.op(lambda: nc.vector.tensor_copy(out=yb[:, h, i * 128:(i + 1) * 128], in_=pt[:, 0:128]), reads=[pt],
              writes=[yb])


def stage_B(kb, cx, l, A, PV, W, C, yT, with_prev=True):
    nc = kb.nc
    P, PB = cx.P, cx.PB
    yTv = yT.rearrange("(b k p) n -> b p k n", b=4, p=128)
    with ExitStack() as sc_:
        ident = kb.sb([128, 128], F32, "ident", sc_)
        identb = kb.sb([128, 128], BF16, "identb", sc_)
        kb.sp.dma(ident[:], C["ident"][:, :], writes=[ident])
        kb.pool.op(lambda: nc.gpsimd.tensor_copy(out=identb[:], in_=ident[:]), reads=[ident], writes=[identb])

        for st in sect("B1"):
            hh = kb.sb([128, 16 + TOK], F32, "hh", st)
            sA = kb.sb([128, 16 + TOK], F32, "sA", st)
            sB = kb.sb([128, 16 + TOK], F32, "sB", st)
            pl = kb.sb([128, TOK], BF16, "pl", st)
            t16 = kb.sb([128, 16], F32, "t16", st)
            invc = kb.sb([128, 4, 16], F32, "invc", st)
            wps = kb.sb([128, 4, 128], F32, "wps", st)
            wpb = kb.sb([128, 4, 128], BF16, "wpb", st)
            psc = kb.sb([128, 4], F32, "psc", st)
            ya = kb.sb([128, 4, TOK], BF16, "ya", st)
            kb.sp.dma(invc[:], C["invc"][:, :, :], writes=[invc])
            kb.sp.dma(wps[:], W["w_pool"].rearrange("g c d -> c g d"), writes=[wps])
            kb.pool.op(lambda: nc.gpsimd.tensor_copy(out=wpb[:], in_=wps[:]), reads=[wps], writes=[wpb])
            kb.sp.dma(psc[:], W["pool_scale"][:, :], writes=[psc])
            psr = Ring([P[2], P[3], P[4], P[5]])
            for g in range(4):
                w = 2 ** (g + 1)
                kb.sp.dma(hh[:, 0:16], PV["php"][g * 128:(g + 1) * 128, :], writes=[hh])
                kb.sp.dma(hh[:, 16:], A["hpT"][g * 128:(g + 1) * 128, :], writes=[hh])
                src, d, o = hh, 1, 1
                bufs = [sA, sB]
                for step in range(g + 1):
                    dst = bufs[step % 2]
                    kb.dve.op(lambda: nc.vector.tensor_tensor(out=dst[:, o:], in0=src[:, o:], in1=src[:, o - d:16 + TOK - d],
                                                              op=ALU.add), reads=[src], writes=[dst])
                    src = dst
                    d *= 2
                    o += d
                kb.dve.op(lambda: nc.vector.scalar_tensor_tensor(out=pl[:], in0=src[:, 16:], scalar=1.0 / w, in1=hh[:, 16:],
                                                                 op0=ALU.mult, op1=ALU.subtract),
                          reads=[src, hh], writes=[pl])
                kb.dve.op(lambda: nc.vector.tensor_tensor(out=t16[:], in0=src[:, 16:32], in1=invc[:, g, :], op=ALU.mult),
                          reads=[src, invc], writes=[t16])
                kb.dve.op(lambda: nc.vector.tensor_tensor(out=pl[:, 0:16], in0=t16[:], in1=hh[:, 16:32], op=ALU.subtract),
                          reads=[t16, hh], writes=[pl])
                for tt in range(4):
                    ps = psr.next()
                    kb.pe.op(lambda: nc.tensor.matmul(ps[:], lhsT=wpb[:, g, :], rhs=pl[:, tt * 512:(tt + 1) * 512],
                                                      start=True, stop=True), reads=[wpb, pl], writes=[ps])
                    kb.act.op(lambda: nc.scalar.activation(out=ya[:, g, tt * 512:(tt + 1) * 512], in_=ps[:], func=AF.Copy,
                                                           scale=psc[:, g:g + 1]), reads=[ps, psc], writes=[ya])
            kb.act.dma(yTv[0], ya[:], reads=[ya])
            kb.barrier()

        for st in sect("B2"):
            zb = kb.sb([128, 4, 32 + TOK], BF16, "zb", st)
            wdw = kb.sb([128, 4, 31], F32, "wdw", st)
            dg = kb.sb([128, 4 * 31, 128], BF16, "dg", st)
            vecs = kb.sb([128, 12], F32, "cvecs", st)
            v = kb.sb([128, 4, 512], F32, "cv", st)
            yd = kb.sb([128, 4, TOK], BF16, "yd", st)
            tmp = stat_tmps(kb, st)
            t1r = Ring([kb.sb([128, 512], F32, "t1", st) for _ in range(2)])
            t2r = Ring([kb.sb([128, 512], F32, "t2", st) for _ in range(2)])
            kb.sp.dma(zb[:, :, 0:32], PV["pz"].rearrange("(k p) n -> p k n", p=128), writes=[zb])
            kb.sp.dma(zb[:, :, 32:], A["zT"].rearrange("(k p) n -> p k n", p=128), writes=[zb])
            kb.sp.dma(wdw[:], W["w_dwT"][:, :, :], writes=[wdw])
            kb.sp.dma(vecs[:, 0:4], W["b_dw"][:, :], writes=[vecs])
            kb.sp.dma(vecs[:, 4:8], W["cln_g"][:, :], writes=[vecs])
            kb.sp.dma(vecs[:, 8:12], W["cln_b"][:, :], writes=[vecs])
            for c in range(4):
                for j in range(31):
                    kb.pool.op(lambda: nc.gpsimd.tensor_scalar(out=dg[:, c * 31 + j, :], in0=ident[:],
                                                               scalar1=wdw[:, c, j:j + 1], scalar2=None, op0=ALU.mult),
                               reads=[ident, wdw], writes=[dg])
            psr = Ring([P[2], P[3], P[4], P[5]])
            for tt in range(4):
                for c in range(4):
                    ps = psr.next()
                    for j in range(31):
                        o = 32 + tt * 512 - 30 + j
                        kb.pe.op(lambda: nc.tensor.matmul(ps[:], lhsT=dg[:, c * 31 + j, :], rhs=zb[:, c, o:o + 512],
                                                          start=(j == 0), stop=(j == 30)), reads=[dg, zb], writes=[ps])
                    kb.act.op(lambda: nc.scalar.activation(out=v[:, c, :], in_=ps[:], func=AF.Identity,
                                                           bias=vecs[:, c:c + 1], scale=1.0), reads=[ps, vecs], writes=[v])
                rstd, nmr = col_stats(kb, cx, [(v, v[:, c, :]) for c in range(4)], 512, True, tmp)
                for c in range(4):
                    t1 = t1r.next(); t2 = t2r.next()
                    kb.dve.op(lambda: nc.vector.tensor_tensor(out=t1[:], in0=v[:, c, :], in1=rstd[:], op=ALU.mult),
                              reads=[v, rstd], writes=[t1])
                    kb.pool.op(lambda: nc.gpsimd.tensor_tensor(out=t2[:], in0=t1[:], in1=nmr[:], op=ALU.add),
                               reads=[t1, nmr], writes=[t2])
                    kb.act.op(lambda: nc.scalar.activation(out=yd[:, c, tt * 512:(tt + 1) * 512], in_=t2[:], func=AF.Silu,
                                                           scale=vecs[:, 4 + c:5 + c], bias=vecs[:, 8 + c:9 + c]),
                              reads=[t2, vecs], writes=[yd])
            kb.act.dma(yTv[3], yd[:], reads=[yd])
            kb.barrier()

        def mk_T(st):
            T = {"identb": identb, "ei": [0]}
            T["ptps"] = Ring([PB[0], PB[1], PB[2], PB[3]])
            T["pts"] = Ring([kb.sb([128, 512], BF16, "pts", st) for _ in range(3)])
            T["Pb"] = Ring([kb.sb([128, 512], BF16, "Pb", st) for _ in range(3)])
            T["tmp"] = Ring([kb.sb([128, 512], F32, "atmp", st) for _ in range(3)])
            T["on"] = Ring([kb.sb([128, 128], BF16, "on", st) for _ in range(2)])
            T["rs"] = Ring([kb.sb([128, 16], F32, "rs", st) for _ in range(2)])
            T["rsum"] = kb.sb([128, 1], F32, "rsum", st)
            T["rinv"] = kb.sb([128, 1], F32, "rinv", st)
            return T

        def load_kv(st, K_own, K_prev, V_own, V_prev):
            Kb = kb.sb([128, 4, 2 * TOK], BF16, "Kb", st)
            Vb = kb.sb([128, 32, 512], BF16, "Vb", st)
            if with_prev:
                kb.sp.dma(Kb[:, :, 0:TOK], K_prev.rearrange("(k p) n -> p k n", p=128), writes=[Kb])
            kb.sp.dma(Kb[:, :, TOK:], K_own.rearrange("(k p) n -> p k n", p=128), writes=[Kb])
            if with_prev:
                kb.sp.dma(Vb[:, 0:16, :], V_prev.rearrange("(c p) d -> p c d", p=128), writes=[Vb])
            kb.sp.dma(Vb[:, 16:32, :], V_own.rearrange("(c p) d -> p c d", p=128), writes=[Vb])
            return Kb, Vb

        for st in sect("B3"):
            T = mk_T(st)
            Kb, Vb = load_kv(st, A["knT"], PV["pknT"], A["mV"], PV["pmV"])
            Qb = kb.sb([128, 4, TOK], BF16, "Qb", st)
            Qr = kb.sb([128, 4, TOK], BF16, "Qr", st)
            Kr = kb.sb([128, 2 * TOK], BF16, "Kr", st)
            kb.pool.op(lambda: nc.gpsimd.memset(Qr[:], 0.0), writes=[Qr])
            kb.pool.op(lambda: nc.gpsimd.memset(Kr[:], 0.0), writes=[Kr])
            CM = kb.sb([128, 128], F32, "CM", st)
            prevb = kb.sb([128, 2], F32, "prevb", st)
            yc = kb.sb([128, 4, TOK], BF16, "yc", st)
            kb.sp.dma(Qb[:], A["qnT"].rearrange("(k p) n -> p k n", p=128), writes=[Qb])
            kb.sp.dma(Qr[0:64], A["qrT"].rearrange("(k p) n -> p k n", p=64), writes=[Qr])
            kb.sp.dma(Kr[0:64, 0:TOK], PV["pkrT"][:, :], writes=[Kr])
            kb.sp.dma(Kr[0:64, TOK:], A["krT"][:, :], writes=[Kr])
            kb.sp.dma(CM[:], C["CM"][:, :], writes=[CM])
            kb.sp.dma(prevb[:], C["prevb"][:, :], writes=[prevb])
            scale = 192.0 ** -0.5
            psS = Ring([P[2], P[3]])
            psO = Ring([P[4], P[5]])
            for i in range(DBG["ni"]):
                qs = slice(i * 128, (i + 1) * 128)
                tl = key_tiles(i, with_prev)
                for h in range(4):
                    po = psO.next()
                    rs = T["rs"].next()
                    npc = 0
                    for ti, (s0, Wd, kind) in enumerate(tl):
                        ps = psS.next()
                        kb.pe.op(lambda: nc.tensor.matmul(ps[:, :Wd], lhsT=Qb[:, h, qs], rhs=Kb[:, h, s0:s0 + Wd],
                                                          start=True, stop=False), reads=[Qb, Kb], writes=[ps])
                        kb.pe.op(lambda: nc.tensor.matmul(ps[:, :Wd], lhsT=Qr[:, h, qs], rhs=Kr[:, s0:s0 + Wd],
                                                          start=False, stop=True), reads=[Qr, Kr], writes=[ps])
                        Pb = T["Pb"].next()
                        if kind == 2:
                            tm = T["tmp"].next()
                            if Wd > 128:
                                kb.dve.op(lambda: nc.vector.tensor_scalar(out=tm[:, :Wd - 128], in0=ps[:, :Wd - 128],
                                                                          scalar1=scale, scalar2=None, op0=ALU.mult),
                                          reads=[ps], writes=[tm])
                            kb.dve.op(lambda: nc.vector.scalar_tensor_tensor(out=tm[:, Wd - 128:Wd], in0=ps[:, Wd - 128:Wd],
                                                                             scalar=scale, in1=CM[:], op0=ALU.mult,
                                                                             op1=ALU.add), reads=[ps, CM], writes=[tm])
                            kb.act.op(lambda: nc.scalar.activation(out=Pb[:, :Wd], in_=tm[:, :Wd], func=AF.Exp,
                                                                   accum_out=rs[:, npc:npc + 1]),
                                      reads=[tm], writes=[Pb, rs])
                            npc += 1
                        else:
                            if kind == 0:
                                kb.act.op(lambda: nc.scalar.activation(out=Pb[:, :Wd], in_=ps[:, :Wd], func=AF.Exp,
                                                                       scale=scale, bias=prevb[:, 0:1],
                                                                       accum_out=rs[:, npc:npc + 1]),
                                          reads=[ps, prevb], writes=[Pb, rs])
                            else:
                                kb.act.op(lambda: nc.scalar.activation(out=Pb[:, :Wd], in_=ps[:, :Wd], func=AF.Exp,
                                                                       scale=scale, accum_out=rs[:, npc:npc + 1]),
                                          reads=[ps], writes=[Pb, rs])
                            npc += 1
                        if DBG["tail"]:
                            attn_tail(kb, cx, T, Pb, Wd, s0 // 128, Vb, h, po, ti == 0, ti == len(tl) - 1)
                    if DBG["fin"]:
                        attn_finish(kb, cx, T, po, rs, npc, yc, h, i)
            kb.act.dma(yTv[2], yc[:], reads=[yc])
            kb.barrier()

        for st in sect("B4"):
            T = mk_T(st)
            Kb, Vb = load_kv(st, A["dkT"], PV["pdkT"], A["dV"], PV["pdV"])
            Qb = kb.sb([128, 4, TOK], BF16, "Qb", st)
            Ik = kb.sb([128, 2 * TOK], BF16, "Ik", st)
            iqr = Ring([kb.sb([128, 16, 128], BF16, "iq", st) for _ in range(2)])
            kb.pool.op(lambda: nc.gpsimd.memset(Ik[:], 0.0), writes=[Ik])
            for b_ in iqr.bufs:
                kb.pool.op(lambda: nc.gpsimd.memset(b_[:], 0.0), writes=[b_])
            iwr = Ring([kb.sb([128, 16], F32, "iwt", st) for _ in range(2)])
            iwa = kb.sb([128, 16], F32, "iwa", st)
            iws = kb.sb([128, 16], F32, "iws", st)
            dsg = kb.sb([128, 16, 128], BF16, "dsg", st)
            Rr = Ring([kb.sb([128, 512], BF16, "R", st) for _ in range(3)])
            scb = kb.sb([128, 2 * TOK], F32, "scb", st)
            junk = kb.sb([128, 2 * TOK], BF16, "junk", st)
            AB = kb.sb([128, 4, 512], F32, "AB", st)
            ABD = kb.sb([128, 16, 128], F32, "ABD", st)
            CMB = kb.sb([128, 128], F32, "CMB", st)
            prevs = kb.sb([128, 2], F32, "prevs", st)
            cbt = kb.sb([128, 512], F32, "cbt", st)
            pw = kb.sb([128, NBIS + 1], F32, "pw", st)
            am = kb.sb([128, 8], F32, "am", st)
            sm = {n: kb.sb([128, 1], F32, n, st) for n in ("M", "lo", "W0", "mid", "cnt", "stp")}
            wst_ = kb.sb([128, NBIS + 1], F32, "wsteps", st)
            yb = kb.sb([128, 4, TOK], BF16, "yb", st)
            kb.sp.dma(Qb[:], A["dqT"].rearrange("(k p) n -> p k n", p=128), writes=[Qb])
            kb.sp.dma(Ik[0:64, 0:TOK], PV["pikT"][:, :], writes=[Ik])
            kb.sp.dma(Ik[0:64, TOK:], A["ikT"][:, :], writes=[Ik])
            for nm, t_ in (("AB", AB), ("ABD", ABD)):
                kb.sp.dma(t_[:], C[nm][:, :, :], writes=[t_])
            for nm, t_ in (("CMB", CMB), ("prevs", prevs), ("cb", cbt), ("pw", pw)):
                kb.sp.dma(t_[:], C[nm][:, :], writes=[t_])
            scale = 128.0 ** -0.5
            psS = Ring([P[2], P[3]])
            psO = Ring([P[4], P[5]])
            iqv = A["iqT"].rearrange("(h d) n -> d h n", d=64)
            iwv = A["iw"].rearrange("(b p) h -> b p h", p=128)
            for i in range(DBG["ni"]):
                qs = slice(i * 128, (i + 1) * 128)
                tl = key_tiles(i, with_prev)
                Ntot = tl[-1][0] + tl[-1][1]
                N0 = tl[0][0]
                iq = iqr.next()
                iwt = iwr.next()
                kb.sp.dma(iq[0:64], iqv[:, :, qs], writes=[iq])
                kb.sp.dma(iwt[:], iwv[i], writes=[iwt])
                kb.act.op(lambda: nc.scalar.activation(out=iwa[:], in_=iwt[:], func=AF.Abs), reads=[iwt], writes=[iwa])
                kb.act.op(lambda: nc.scalar.activation(out=iws[:], in_=iwt[:], func=AF.Sign), reads=[iwt], writes=[iws])
                for hh in range(16):
                    kb.pool.op(lambda: nc.gpsimd.tensor_scalar(out=dsg[:, hh, :], in0=ident[:], scalar1=iws[:, hh:hh + 1],
                                                               scalar2=None, op0=ALU.mult),
                               reads=[ident, iws], writes=[dsg])
                for ti, (s0, Wd, kind) in enumerate(tl if DBG["idx"] else []):
                    pss = P[0] if ti % 2 == 0 else P[1]
                    Rs = {}
                    for hh in range(17):
                        if hh < 16:
                            ps = psS.next()
                            kb.pe.op(lambda: nc.tensor.matmul(ps[:, :Wd], lhsT=iq[:, hh, :], rhs=Ik[:, s0:s0 + Wd],
                                                              start=True, stop=True), reads=[iq, Ik], writes=[ps])
                            R = Rr.next()
                            kb.act.op(lambda: nc.scalar.activation(out=R[:, :Wd], in_=ps[:, :Wd], func=AF.Relu,
                                                                   scale=iwa[:, hh:hh + 1]), reads=[ps, iwa], writes=[R])
                            Rs[hh] = R
                        if hh >= 1 and DBG.get("acc", True):
                            g_ = hh - 1
                            R_ = Rs.pop(g_)
                            kb.pe.op(lambda: nc.tensor.matmul(pss[:, :Wd], lhsT=dsg[:, g_, :], rhs=R_[:, :Wd],
                                                              start=(g_ == 0), stop=(g_ == 15)), reads=[dsg, R_],
                                     writes=[pss])
                    if not DBG.get("post", True):
                        continue
                    kb.dve.op(lambda: nc.vector.tensor_reduce(out=am[:, ti:ti + 1], in_=pss[:, :Wd], axis=AX.X, op=ALU.max,
                                                              apply_absolute_value=True), reads=[pss], writes=[am])
                    if DBG.get("post", 2) == 1:
                        continue
                    if kind == 0:
                        kb.dve.op(lambda: nc.vector.tensor_scalar(out=scb[:, s0:s0 + Wd], in0=pss[:, :Wd],
                                                                  scalar1=prevs[:, 0:1], scalar2=None, op0=ALU.add),
                                  reads=[pss, prevs], writes=[scb])
                    else:
                        if kind == 1 or Wd > 128:
                            We = Wd if kind == 1 else Wd - 128
                            kb.dve.op(lambda: nc.vector.tensor_copy(out=scb[:, s0:s0 + We], in_=pss[:, :We]), reads=[pss],
                                      writes=[scb])
                        if kind == 2:
                            kb.dve.op(lambda: nc.vector.tensor_tensor(out=scb[:, s0 + Wd - 128:s0 + Wd],
                                                                      in0=pss[:, Wd - 128:Wd], in1=CMB[:], op=ALU.add),
                                      reads=[pss, CMB], writes=[scb])
                nt = len(tl)
                M, lo, W0, mid, cnt, stp = (sm[n] for n in ("M", "lo", "W0", "mid", "cnt", "stp"))
                kb.dve.op(lambda: nc.vector.tensor_reduce(out=M[:], in_=am[:, 0:nt], axis=AX.X, op=ALU.max), reads=[am], writes=[M])
                kb.dve.op(lambda: nc.vector.tensor_scalar(out=lo[:], in0=M[:], scalar1=-1.0, scalar2=-1.0, op0=ALU.mult,
                                                          op1=ALU.add), reads=[M], writes=[lo])
                kb.dve.op(lambda: nc.vector.tensor_scalar(out=W0[:], in0=M[:], scalar1=2.0, scalar2=2.0, op0=ALU.mult,
                                                          op1=ALU.add), reads=[M], writes=[W0])
                kb.dve.op(lambda: nc.vector.tensor_scalar(out=wst_[:], in0=pw[:], scalar1=W0[:, 0:1], scalar2=None,
                                                          op0=ALU.mult), reads=[pw, W0], writes=[wst_])
                kb.dve.op(lambda: nc.vector.tensor_tensor(out=mid[:], in0=lo[:], in1=wst_[:, 0:1], op=ALU.add),
                          reads=[lo, wst_], writes=[mid])
                for it in range(DBG["nbis"]):
                    kb.dve.op(lambda: nc.vector.tensor_scalar(out=junk[:, N0:Ntot], in0=scb[:, N0:Ntot], scalar1=mid[:, 0:1],
                                                              scalar2=None, op0=ALU.is_ge, op1=ALU.add, accum_out=cnt[:]),
                              reads=[scb, mid], writes=[junk, cnt])
                    kb.dve.op(lambda: nc.vector.tensor_scalar(out=stp[:], in0=cnt[:], scalar1=255.5,
                                                              scalar2=wst_[:, it:it + 1], op0=ALU.is_ge, op1=ALU.mult),
                              reads=[cnt, wst_], writes=[stp])
                    kb.dve.op(lambda: nc.vector.tensor_tensor(out=lo[:], in0=lo[:], in1=stp[:], op=ALU.add),
                              reads=[lo, stp], writes=[lo])
                    kb.dve.op(lambda: nc.vector.tensor_tensor(out=mid[:], in0=lo[:], in1=wst_[:, it + 1:it + 2],
                                                              op=ALU.add), reads=[lo, wst_], writes=[mid])
                kb.dve.op(lambda: nc.vector.tensor_scalar(out=junk[:, N0:Ntot], in0=scb[:, N0:Ntot], scalar1=lo[:, 0:1],
                                                          scalar2=NEGM, op0=ALU.is_lt, op1=ALU.mult),
                          reads=[scb, lo], writes=[junk])
                rss = [T["rs"].next() for _ in range(2)]
                for h in range(4 if DBG["att"] else 0):
                    po = psO.next()
                    rs = rss[h % 2]
                    npc = 0
                    for ti, (s0, Wd, kind) in enumerate(tl):
                        ps = psS.next()
                        kb.pe.op(lambda: nc.tensor.matmul(ps[:, :Wd], lhsT=Qb[:, h, qs], rhs=Kb[:, h, s0:s0 + Wd],
                                                          start=True, stop=False), reads=[Qb, Kb], writes=[ps])
                        kb.pe.op(lambda: nc.tensor.matmul(ps[:, :Wd], lhsT=identb[:], rhs=junk[:, s0:s0 + Wd], start=False,
                                                          stop=True), reads=[identb, junk], writes=[ps])
                        tm = T["tmp"].next()
                        Pb = T["Pb"].next()
                        cbc = (i * 4 + h) * 8 + ti
                        if kind == 2:
                            wv_ = Wd // 128 - 1
                            if Wd > 128:
                                kb.dve.op(lambda: nc.vector.scalar_tensor_tensor(out=tm[:, :Wd - 128], in0=ps[:, :Wd - 128],
                                                                                 scalar=scale, in1=AB[:, h, :Wd - 128],
                                                                                 op0=ALU.mult, op1=ALU.add),
                                          reads=[ps, AB], writes=[tm])
                            kb.dve.op(lambda: nc.vector.scalar_tensor_tensor(out=tm[:, Wd - 128:Wd], in0=ps[:, Wd - 128:Wd],
                                                                             scalar=scale, in1=ABD[:, h * 4 + wv_, :],
                                                                             op0=ALU.mult, op1=ALU.add),
                                      reads=[ps, ABD], writes=[tm])
                            kb.act.op(lambda: nc.scalar.activation(out=Pb[:, :Wd], in_=tm[:, :Wd], func=AF.Exp,
                                                                   bias=cbt[:, cbc:cbc + 1], accum_out=rs[:, npc:npc + 1]),
                                      reads=[tm, cbt], writes=[Pb, rs])
                            npc += 1
                        else:
                            kb.dve.op(lambda: nc.vector.scalar_tensor_tensor(out=tm[:, :Wd], in0=ps[:, :Wd], scalar=scale,
                                                                             in1=AB[:, h, :Wd], op0=ALU.mult, op1=ALU.add),
                                      reads=[ps, AB], writes=[tm])
                            kb.act.op(lambda: nc.scalar.activation(out=Pb[:, :Wd], in_=tm[:, :Wd], func=AF.Exp,
                                                                   bias=cbt[:, cbc:cbc + 1], accum_out=rs[:, npc:npc + 1]),
                                      reads=[tm, cbt], writes=[Pb, rs])
                            npc += 1
                        attn_tail(kb, cx, T, Pb, Wd, s0 // 128, Vb, h, po, ti == 0, ti == len(tl) - 1)
                    attn_finish(kb, cx, T, po, rs, npc, yb, h, i)
            kb.act.dma(yTv[1], yb[:], reads=[yb])
            kb.barrier()


C_W = {"wg_ct": [4 * 16, 128, KD, 128], "wb_ct": [4 * 16, 128, 4, 128], "wo_ct": [16, 128, KD, 128],
       "wfi_ct": [88, 128, KD, 128], "wfo_ct": [16, 128, 44, 128], "b_gate": [128, 64], "ln1_g": [128, 16],
       "ln1_b": [128, 16], "ln2_g": [128, 16], "ln2_b": [128, 16]}


def ctl(w, kchunks):
    K_, C_ = w.shape
    return np.ascontiguousarray(w.reshape(kchunks, 128, C_ // 128, 128).transpose(2, 1, 0, 3))


def host_C_weights(inputs, l):
    out = {}
    out["wg_ct"] = np.concatenate([ctl(inputs["w_gate"][l, i], KD) for i in range(4)], axis=0)
    out["wb_ct"] = np.concatenate([ctl(inputs["w_branch"][l, i], 4) for i in range(4)], axis=0)
    out["wo_ct"] = ctl(inputs["w_o"][l], KD)
    out["wfi_ct"] = ctl(inputs["w_ffn_in"][l], KD)
    out["wfo_ct"] = ctl(inputs["w_ffn_out"][l], 44)
    out["b_gate"] = np.ascontiguousarray(np.concatenate([pk(inputs["b_gate"][l, i]) for i in range(4)], axis=1))
    for n in ("ln1_g", "ln1_b", "ln2_g", "ln2_b"):
        out[n] = pk(inputs[n][l])
    return out


PRECAST = False


def precast_weights(kb, cx, W, tag):
    nc = kb.nc
    out = dict(W)
    with ExitStack() as st:
        stg = Ring([kb.sb([128, 16, 128], F32, "pcs", st) for _ in range(3)])
        bfr = Ring([kb.sb([128, 16, 128], BF16, "pcb", st) for _ in range(3)])
        cnt = 0
        for name, n, nk in (("wg_ct", 64, 16), ("wb_ct", 64, 4), ("wo_ct", 16, 16), ("wfi_ct", 88, 16), ("wfo_ct", 16, 44)):
            dst = kb.dram(f"bf_{tag}_{name}", [n, 128, nk, 128], BF16)
            out[name] = dst
            for t in range(n):
                for k0 in range(0, nk, 16):
                    kk = min(16, nk - k0)
                    s_ = stg.next()
                    b_ = bfr.next()
                    kb.sp.dma(s_[:, :kk, :], W[name][t][:, k0:k0 + kk, :], writes=[s_])
                    if cnt % 2 == 0:
                        kb.pool.op(lambda: nc.gpsimd.tensor_copy(out=b_[:, :kk, :], in_=s_[:, :kk, :]), reads=[s_],
                                   writes=[b_])
                    else:
                        kb.dve.op(lambda: nc.vector.tensor_copy(out=b_[:, :kk, :], in_=s_[:, :kk, :]), reads=[s_],
                                  writes=[b_])
                    cnt += 1
                    kb.act.dma(dst[t][:, k0:k0 + kk, :], b_[:, :kk, :], reads=[b_])
        kb.barrier()
    return out


def stage_C(kb, cx, l, xT, uT_d, yT_d, W, S, xoT):
    nc = kb.nc
    P = cx.P
    v3 = lambda ap: ap.rearrange("(k p) n -> p k n", p=128)
    with ExitStack() as st:
        vec = kb.sb([128, 128], F32, "cvec", st)
        kb.sp.dma(vec[:, 0:64], W["b_gate"][:, :], writes=[vec])
        for j, n in enumerate(("ln1_g", "ln1_b", "ln2_g", "ln2_b")):
            kb.sp.dma(vec[:, 64 + 16 * j:80 + 16 * j], W[n][:, :], writes=[vec])
        wst = Ring([kb.sb([128, 16, 128], F32, "cwst", st) for _ in range(2)])
        wbf = Ring([kb.sb([128, 16, 128], BF16, "cwbf", st) for _ in range(3)])
        wfo = kb.sb([128, 44, 128], BF16, "cwfo", st)
        tmp = stat_tmps(kb, st)
        t1r = Ring([kb.sb([128, 512], F32, "t1", st) for _ in range(2)])
        t2r = Ring([kb.sb([128, 512], F32, "t2", st) for _ in range(2)])
        sgr = Ring([kb.sb([128, 512], F32, "sg", st) for _ in range(2)])
        u = kb.sb([128, KD, 512], BF16, "cu", st)
        z = kb.sb([128, KD, 512], F32, "cz", st)
        hb = kb.sb([128, 44, 512], BF16, "chb", st)
        mg = BufK(hb.h, "cmg", 0, 16)
        y = BufK(hb.h, "cy", 16, 16)
        acc = kb.sb([128, 512], F32, "cacc", st)
        xt = Ring([kb.sb([128, 512], F32, "cxt", st) for _ in range(2)])
        psr = Ring([P[2], P[3], P[4], P[5]])

        def wload(src, nk):
            wb = wbf.next()
            if PRECAST:
                kb.sp.dma(wb[:, :nk, :], src, writes=[wb])
                return wb
            s = wst.next()
            kb.sp.dma(s[:, :nk, :], src, writes=[s])
            kb.pool.op(lambda: nc.gpsimd.tensor_copy(out=wb[:, :nk, :], in_=s[:, :nk, :]), reads=[s], writes=[wb])
            return wb

        def mm(ps, wb, nk, rhs_buf, rhs_fn):
            for k in range(nk):
                kb.pe.op(lambda: nc.tensor.matmul(ps[:], lhsT=wb[:, k, :], rhs=rhs_fn(k), start=(k == 0), stop=(k == nk - 1)),
                         reads=[wb, rhs_buf], writes=[ps])

        def layer_norm_to(src, gcol, bcol, dst_fn, dst_buf, also=None):
            rstd, nmr = col_stats(kb, cx, [(src, src[:, k, :]) for k in range(KD)], D, True, tmp)
            for k in range(KD):
                t1 = t1r.next(); t2 = t2r.next()
                kb.dve.op(lambda: nc.vector.tensor_tensor(out=t1[:], in0=src[:, k, :], in1=rstd[:], op=ALU.mult),
                          reads=[src, rstd], writes=[t1])
                kb.pool.op(lambda: nc.gpsimd.tensor_tensor(out=t2[:], in0=t1[:], in1=nmr[:], op=ALU.add),
                           reads=[t1, nmr], writes=[t2])
                kb.act.op(lambda: nc.scalar.activation(out=dst_fn(k), in_=t2[:], func=AF.Identity,
                                                       scale=vec[:, gcol + k:gcol + k + 1], bias=vec[:, bcol + k:bcol + k + 1]),
                          reads=[t2, vec], writes=[dst_buf])

        def adaln_to(src, l_, isc, ish, dst):
            rstd, nmr = col_stats(kb, cx, [(src, src[:, k, :]) for k in range(KD)], D, True, tmp)
            for k in range(KD):
                t1 = t1r.next(); t2 = t2r.next()
                kb.dve.op(lambda: nc.vector.tensor_tensor(out=t1[:], in0=src[:, k, :], in1=rstd[:], op=ALU.mult),
                          reads=[src, rstd], writes=[t1])
                kb.pool.op(lambda: nc.gpsimd.tensor_tensor(out=t2[:], in0=t1[:], in1=nmr[:], op=ALU.add),
                           reads=[t1, nmr], writes=[t2])
                kb.act.op(lambda: nc.scalar.activation(out=dst[:, k, :], in_=t2[:], func=AF.Identity,
                                                       scale=cx.onep[:, mcol(l_, isc, k):mcol(l_, isc, k) + 1],
                                                       bias=cx.mod[:, mcol(l_, ish, k):mcol(l_, ish, k) + 1]),
                          reads=[t2, cx.onep, cx.mod], writes=[dst])

        for tt in range(4):
            ts = slice(tt * 512, (tt + 1) * 512)
            kb.barrier()
            kb.sp.dma(u[:], v3(uT_d)[:, :, ts], writes=[u])
            kb.sp.dma(y[:], v3(yT_d)[:, :, ts], writes=[y])
            for j in range(16):
                for i in range(4):
                    wg = wload(W["wg_ct"][i * 16 + j], KD)
                    wbr = wload(W["wb_ct"][i * 16 + j], 4)
                    pg = psr.next(); pb = psr.next()
                    mm(pg, wg, KD, u, lambda k: u[:, k, :])
                    mm(pb, wbr, 4, y, lambda k: y[:, i * 4 + k, :])
                    sg = sgr.next()
                    kb.act.op(lambda: nc.scalar.activation(out=sg[:], in_=pg[:], func=AF.Sigmoid,
                                                           bias=vec[:, i * 16 + j:i * 16 + j + 1], scale=1.0),
                              reads=[pg, vec], writes=[sg])
                    if i == 0:
                        kb.dve.op(lambda: nc.vector.tensor_tensor(out=acc[:], in0=pb[:], in1=sg[:], op=ALU.mult),
                                  reads=[pb, sg], writes=[acc])
                    else:
                        t1 = t1r.next()
                        kb.dve.op(lambda: nc.vector.tensor_tensor(out=t1[:], in0=pb[:], in1=sg[:], op=ALU.mult),
                                  reads=[pb, sg], writes=[t1])
                        if i < 3:
                            kb.pool.op(lambda: nc.gpsimd.tensor_tensor(out=acc[:], in0=acc[:], in1=t1[:], op=ALU.add),
                                       reads=[acc, t1], writes=[acc])
                        else:
                            kb.pool.op(lambda: nc.gpsimd.tensor_tensor(out=mg[:, j, :], in0=acc[:], in1=t1[:], op=ALU.add),
                                       reads=[acc, t1], writes=[mg])
            for j in range(16):
                wo = wload(W["wo_ct"][j], KD)
                ps = psr.next()
                mm(ps, wo, KD, mg, lambda k: mg[:, k, :])
                x_ = xt.next()
                kb.sp.dma(x_[:], xT[j * 128:(j + 1) * 128, ts], writes=[x_])
                t1 = t1r.next()
                kb.act.op(lambda: nc.scalar.activation(out=t1[:], in_=ps[:], func=AF.Copy,
                                                       scale=cx.onep[:, mcol(l, 2, j):mcol(l, 2, j) + 1]),
                          reads=[ps, cx.onep], writes=[t1])
                kb.dve.op(lambda: nc.vector.scalar_tensor_tensor(out=z[:, j, :], in0=x_[:], scalar=ALPHA, in1=t1[:],
                                                                 op0=ALU.mult, op1=ALU.add), reads=[x_, t1], writes=[z])
            layer_norm_to(z, 64, 80, lambda k: z[:, k, :], z)
            adaln_to(z, l, 4, 3, u)
            for j in range(44):
                wa = wload(W["wfi_ct"][j], KD)
                wg_ = wload(W["wfi_ct"][44 + j], KD)
                pa = psr.next(); pg = psr.next()
                mm(pa, wa, KD, u, lambda k: u[:, k, :])
                mm(pg, wg_, KD, u, lambda k: u[:, k, :])
                sg = sgr.next()
                kb.act.op(lambda: nc.scalar.activation(out=sg[:], in_=pa[:], func=AF.Silu), reads=[pa], writes=[sg])
                kb.dve.op(lambda: nc.vector.tensor_tensor(out=hb[:, j, :], in0=pg[:], in1=sg[:], op=ALU.mult),
                          reads=[pg, sg], writes=[hb])
            for j in range(16):
                if PRECAST:
                    kb.sp.dma(wfo[:], W["wfo_ct"][j], writes=[wfo])
                for (k0_, nk_) in (() if PRECAST else ((0, 16), (16, 16), (32, 12))):
                    s_ = wst.next()
                    kb.sp.dma(s_[:, :nk_, :], W["wfo_ct"][j][:, k0_:k0_ + nk_, :], writes=[s_])
                    kb.pool.op(lambda: nc.gpsimd.tensor_copy(out=wfo[:, k0_:k0_ + nk_, :], in_=s_[:, :nk_, :]),
                               reads=[s_], writes=[wfo])
                ps = psr.next()
                mm(ps, wfo, 44, hb, lambda k: hb[:, k, :])
                t1 = t1r.next()
                kb.act.op(lambda: nc.scalar.activation(out=t1[:], in_=ps[:], func=AF.Copy,
                                                       scale=cx.onep[:, mcol(l, 5, j):mcol(l, 5, j) + 1]),
                          reads=[ps, cx.onep], writes=[t1])
                kb.dve.op(lambda: nc.vector.scalar_tensor_tensor(out=z[:, j, :], in0=z[:, j, :], scalar=ALPHA, in1=t1[:],
                                                                 op0=ALU.mult, op1=ALU.add), reads=[z, t1], writes=[z])
            layer_norm_to(z, 96, 112, lambda k: z[:, k, :], z)
            kb.act.dma(v3(xoT)[:, :, ts], z[:], reads=[z])
        kb.barrier()


def _launch(nc, in_maps):
    res = run_bass_kernel_spmd(nc, in_maps, core_ids=list(range(8)))
    return res.results


def build_B(l):
    kb = KB()
    modT = kb.dram("modT", [128, DEPTH * 96], F32, kind="ExternalInput")
    A = {k: kb.dram(k, v[0], dts(v[1]), kind="ExternalInput") for k, v in A_OUT.items() if k != "uT"}
    PV = {k: kb.dram(k, v[0], dts(v[1]), kind="ExternalInput") for k, v in B_PREV.items()}
    W = {k: kb.dram(k, v, F32, kind="ExternalInput") for k, v in B_W.items()}
    C = {k: kb.dram(k, v, F32, kind="ExternalInput") for k, v in B_C.items()}
    yT = kb.dram("yT", [2048, TOK], BF16, kind="ExternalOutput")
    cx = setup_common(kb, modT)
    stage_B(kb, cx, l, A, PV, W, C, yT)
    kb.finish()
    return kb.nc


def build_C(l):
    kb = KB()
    modT = kb.dram("modT", [128, DEPTH * 96], F32, kind="ExternalInput")
    xT = kb.dram("xT", [D, TOK], F32, kind="ExternalInput")
    uT = kb.dram("uT", [D, TOK], BF16, kind="ExternalInput")
    yT = kb.dram("yT", [2048, TOK], BF16, kind="ExternalInput")
    W = {k: kb.dram(k, v, F32, kind="ExternalInput") for k, v in C_W.items()}
    xo = kb.dram("xo", [D, TOK], F32, kind="ExternalOutput")
    cx = setup_common(kb, modT)
    stage_C(kb, cx, l, xT, uT, yT, W, {}, xo)
    kb.finish()
    return kb.nc


def kernel_unfused(**inputs):
    inputs = {k: np.asarray(v) for k, v in inputs.items()}
    x = inputs["x"]
    mods = run_mods(inputs)
    xT = []
    for core in range(8):
        b, h = core // 2, core % 2
        xT.append(np.ascontiguousarray(x[b, h * TOK:(h + 1) * TOK, :].T))
    for l in range(DEPTH):
        wA = [host_A_weights(inputs, l, h) for h in range(2)]
        in_maps = []
        for core in range(8):
            b, h = core // 2, core % 2
            m = {"xT": xT[core], "modT": mods[b]}
            m.update(wA[h])
            in_maps.append(m)
        ra = _launch(build_A(l), in_maps)
        wB = host_B_weights(inputs, l)
        cB = [host_B_consts(h) for h in range(2)]
        in_maps = []
        pm = {"pdkT": "dkT", "pdV": "dV", "pikT": "ikT", "pknT": "knT", "pmV": "mV", "pkrT": "krT"}
        for core in range(8):
            b, h = core // 2, core % 2
            own = ra[core]
            m = {"modT": mods[b]}
            for k in A_OUT:
                if k != "uT":
                    m[k] = np.asarray(own[k])
            if h == 1:
                prev = ra[core - 1]
                for k, v in pm.items():
                    m[k] = np.asarray(prev[v])
                m["pz"] = np.ascontiguousarray(np.asarray(prev["zT"])[:, -32:])
                m["php"] = np.ascontiguousarray(np.asarray(prev["hpT"])[:, -16:])
            else:
                for k, v in pm.items():
                    m[k] = np.zeros_like(np.asarray(own[v]))
                m["pz"] = np.zeros_like(np.asarray(own["zT"])[:, -32:])
                m["php"] = np.zeros((512, 16), np.float32)
            m.update(wB)
            m.update(cB[h])
            in_maps.append(m)
        rb = _launch(build_B(l), in_maps)
        wC = host_C_weights(inputs, l)
        in_maps = []
        for core in range(8):
            b = core // 2
            m = {"modT": mods[b], "xT": xT[core], "uT": np.asarray(ra[core]["uT"]), "yT": np.asarray(rb[core]["yT"])}
            m.update(wC)
            in_maps.append(m)
        rc = _launch(build_C(l), in_maps)
        xT = [np.asarray(rc[core]["xo"]) for core in range(8)]
    out = np.empty((NB, SEQ, D), np.float32)
    for core in range(8):
        b, h = core // 2, core % 2
        out[b, h * TOK:(h + 1) * TOK, :] = xT[core].T
    return out


def stage_M(kb, cx, cT, wadas, baT):
    nc = kb.nc
    with ExitStack() as st:
        csb = kb.sb([128, KD, 4], F32, "csb", st)
        cact = kb.sb([128, KD, 4], F32, "cact", st)
        basb = kb.sb([128, DEPTH * 96], F32, "basb", st)
        wr = Ring([kb.sb([128, KD, 512], F32, "wst", st) for _ in range(2)])
        pr = Ring([cx.P[2], cx.P[3]])
        kb.sp.dma(csb[:], cT[:, :, :], writes=[csb])
        kb.sp.dma(basb[:], baT[:, :], writes=[basb])
        kb.act.op(lambda: nc.scalar.activation(out=cact[:], in_=csb[:], func=AF.Silu), reads=[csb], writes=[cact])
        for l in range(DEPTH):
            wav = wadas[l].rearrange("(k p) n -> p k n", p=128)
            for g in range(24):
                w = wr.next()
                kb.sp.dma(w[:], wav[:, :, g * 512:(g + 1) * 512], writes=[w])
                for j in range(4):
                    t = l * 96 + g * 4 + j
                    p = pr.next()
                    for k in range(KD):
                        kb.pe.op(lambda: nc.tensor.matmul(p[:, 0:4], lhsT=w[:, k, j * 128:(j + 1) * 128], rhs=cact[:, k, :],
                                                          start=(k == 0), stop=(k == KD - 1)),
                                 reads=[w, cact], writes=[p])
                    kb.dve.op(lambda: nc.vector.tensor_scalar(out=cx.mod[:, t:t + 1], in0=p[:, 0:1],
                                                              scalar1=basb[:, t:t + 1], scalar2=None, op0=ALU.add),
                              reads=[p, basb], writes=[cx.mod])
        kb.dve.op(lambda: nc.vector.tensor_scalar(out=cx.onep[:], in0=cx.mod[:], scalar1=1.0, scalar2=None, op0=ALU.add),
                  reads=[cx.mod], writes=[cx.onep])
        kb.barrier()


A_WS = {k: v for k, v in A_W.items() if k not in ("ropec", "ropes")}
ROPE = {"ropec": [64, TOK], "ropes": [64, TOK]}


def build_fused():
    global PRECAST
    PRECAST = True
    kb = KB()
    ext = lambda n, shp, dt=F32: kb.dram(n, shp, dt, kind="ExternalInput")
    xT = ext("xT", [D, SEQ])
    cT = ext("cT", [128, KD, 4])
    wadas = [ext(f"w_ada{l}", [D, 6 * D]) for l in range(DEPTH)]
    baT = ext("baT", [128, DEPTH * 96])
    z32 = ext("z32", [512, 32], BF16)
    z16 = ext("z16", [512, 16])
    rope = [{k: ext(f"{k}_h{h}", v) for k, v in ROPE.items()} for h in range(2)]
    BC = [{k: ext(f"{k}_h{h}", v) for k, v in B_C.items()} for h in range(2)]
    WA = [{k: ext(f"L{l}_{k}", v) for k, v in A_WS.items()} for l in range(DEPTH)]
    WB = [{k: ext(f"L{l}_{k}", v) for k, v in B_W.items()} for l in range(DEPTH)]
    WC = [{k: ext(f"L{l}_{k}", v) for k, v in C_W.items()} for l in range(DEPTH)]
    xo = kb.dram("xo", [D, SEQ], F32, kind="ExternalOutput")
    x1 = kb.dram("x1", [D, SEQ], F32)
    cx = setup_common(kb, None)
    stage_M(kb, cx, cT, wadas, baT)
    S = {k: kb.dram(f"S_{k}", v, F32) for k, v in A_SCR.items()}
    for l in range(DEPTH):
        xin = xT if l == 0 else x1
        xout = x1 if l == 0 else xo
        AO = [{k: kb.dram(f"A{l}{h}_{k}", v[0], dts(v[1])) for k, v in A_OUT.items()} for h in range(2)]
        yT = [kb.dram(f"y{l}{h}", [2048, TOK], BF16) for h in range(2)]
        for h in range(2):
            W = dict(WA[l])
            W.update(rope[h])
            stage_A(kb, cx, l, xin[:, h * TOK:(h + 1) * TOK], W, AO[h], S)
        for h in range(2):
            pm = {"pdkT": "dkT", "pdV": "dV", "pikT": "ikT", "pknT": "knT", "pmV": "mV", "pkrT": "krT"}
            PV = {k: AO[0][v] for k, v in pm.items()}
            if h == 1:
                PV["pz"] = AO[0]["zT"][:, TOK - 32:TOK]
                PV["php"] = AO[0]["hpT"][:, TOK - 16:TOK]
            else:
                PV["pz"] = z32
                PV["php"] = z16
            stage_B(kb, cx, l, AO[h], PV, WB[l], BC[h], yT[h], with_prev=(h == 1))
        WCb = precast_weights(kb, cx, WC[l], f"L{l}")
        for h in range(2):
            stage_C(kb, cx, l, xin[:, h * TOK:(h + 1) * TOK], AO[h]["uT"], yT[h], WCb, {},
                    xout[:, h * TOK:(h + 1) * TOK])
    kb.finish()
    return kb.nc


def kernel(**inputs):
    import ml_dtypes
    inputs = {k: np.asarray(v) for k, v in inputs.items()}
    x = inputs["x"]
    c = inputs["c"]
    shared = {"z32": np.zeros((512, 32), ml_dtypes.bfloat16), "z16": np.zeros((512, 16), np.float32)}
    for l in range(DEPTH):
        shared[f"w_ada{l}"] = np.ascontiguousarray(inputs["w_ada"][l])
    shared["baT"] = np.ascontiguousarray(np.concatenate([pk(inputs["b_ada"][l]) for l in range(DEPTH)], axis=1))
    for h in range(2):
        for k, v in host_B_consts(h, skip_prev=True).items():
            shared[f"{k}_h{h}"] = v
    for l in range(DEPTH):
        wa = [host_A_weights(inputs, l, h) for h in range(2)]
        for k in A_WS:
            shared[f"L{l}_{k}"] = wa[0][k]
        if l == 0:
            for h in range(2):
                for k in ROPE:
                    shared[f"{k}_h{h}"] = wa[h][k]
        for k, v in host_B_weights(inputs, l).items():
            shared[f"L{l}_{k}"] = v
        for k, v in host_C_weights(inputs, l).items():
            shared[f"L{l}_{k}"] = v
    in_maps = []
    for core in range(8):
        b = core // 2
        m = dict(shared)
        m["xT"] = np.ascontiguousarray(x[b].T)
        m["cT"] = np.ascontiguousarray(np.repeat(c[b].reshape(KD, 128).T[:, :, None], 4, axis=2))
        in_maps.append(m)
    res = run_bass_kernel_spmd(build_fused(), in_maps, core_ids=list(range(8)))
    out = np.empty((NB, SEQ, D), np.float32)
    for b in range(NB):
        out[b] = np.asarray(res.results[2 * b]["xo"]).T
    return out
```

```python
import numpy as np
from contextlib import ExitStack
import concourse.bass as bass
import concourse.mybir as mybir
from concourse.bass_utils import run_bass_kernel_spmd

F32 = mybir.dt.float32
BF16 = mybir.dt.bfloat16
AF = mybir.ActivationFunctionType
ALU = mybir.AluOpType
AX = mybir.AxisListType

SAME_ENGINE_SYNC = True


class Buf:
    def __init__(self, handle, name):
        self.h = handle
        self.name = name
        self.w = {}
        self.r = {}

    def __getitem__(self, idx):
        return self.h[idx]


class BufV(Buf):
    def __init__(self, handle, name, off, width):
        super().__init__(handle, name)
        self.off = off
        self.width = width

    def __getitem__(self, idx):
        if not isinstance(idx, tuple):
            idx = (idx, slice(None))
        p, c = idx
        a = 0 if c.start is None else c.start
        b = self.width if c.stop is None else c.stop
        return self.h[p, self.off + a:self.off + b]


class BufK(Buf):
    def __init__(self, handle, name, k0, nk):
        super().__init__(handle, name)
        self.k0 = k0
        self.nk = nk

    def __getitem__(self, idx):
        if not isinstance(idx, tuple):
            return self.h[idx, self.k0:self.k0 + self.nk, :]
        p, k = idx[0], idx[1]
        n = idx[2] if len(idx) > 2 else slice(None)
        if isinstance(k, slice):
            a = 0 if k.start is None else k.start
            b = self.nk if k.stop is None else k.stop
            return self.h[p, self.k0 + a:self.k0 + b, n]
        return self.h[p, self.k0 + k, n]


class Eng:
    def __init__(self, kb, name, eng, ndma=0):
        self.kb = kb
        self.name = name
        self.e = eng
        self.sem = kb.newsem("c_" + name)
        self.count = 0
        self.seen = {}
        self.dsems = [kb.newsem(f"d_{name}{i}") for i in range(ndma)]
        self.dcount = 0

    def wait(self, sem, val):
        if val <= 0:
            return
        if sem is self.sem and (not SAME_ENGINE_SYNC or self.name == "pe"):
            return
        if self.seen.get(id(sem), 0) >= val:
            return
        self.e.wait_ge(sem, val)
        self.seen[id(sem)] = val

    def _deps(self, reads, writes):
        for b in reads:
            for sem, val in b.w.values():
                self.wait(sem, val)
        for b in writes:
            for sem, val in b.w.values():
                self.wait(sem, val)
            for sem, val in b.r.values():
                self.wait(sem, val)

    def _mark(self, reads, writes, ev):
        for b in reads:
            b.r[id(ev[0])] = ev
        for b in writes:
            b.w = {id(ev[0]): ev}
            b.r = {}

    def op(self, ins_fn, reads=(), writes=(), signal=True):
        signal = True
        self._deps(reads, writes)
        ins = ins_fn()
        if signal:
            self.count += 1
            ins.then_inc(self.sem, 1)
        ev = (self.sem, self.count if signal else self.count + 1)
        self._mark(reads, writes, ev)
        return ins

    def mark_only(self, reads, writes):
        ev = (self.sem, self.count + 1)
        self._mark(reads, writes, ev)

    def dma(self, out, in_, reads=(), writes=(), **kw):
        n = len(self.dsems)
        i = self.dcount
        j = i % n
        sem = self.dsems[j]
        if i >= n:
            self.wait(sem, 16 * (i // n))
        self._deps(reads, writes)
        ins = self.e.dma_start(out=out, in_=in_, **kw)
        ins.then_inc(sem, 16)
        self.dcount += 1
        ev = (sem, 16 * (i // n + 1))
        self._mark(reads, writes, ev)
        return ev


class KB:
    def __init__(self):
        self.nc = bass.Bass("TRN2", target_bir_lowering=False)
        self.es = ExitStack()
        self.sems = []
        nc = self.nc
        self.pe = Eng(self, "pe", nc.tensor)
        self.act = Eng(self, "act", nc.scalar, ndma=4)
        self.dve = Eng(self, "dve", nc.vector)
        self.pool = Eng(self, "pool", nc.gpsimd, ndma=4)
        self.sp = Eng(self, "sp", nc.sync, ndma=8)
        self.engs = [self.pe, self.act, self.dve, self.pool, self.sp]
        self.uid = 0

    def newsem(self, name):
        s = self.es.enter_context(self.nc.semaphore(name))
        self.sems.append(s)
        return s

    def dram(self, name, shape, dt, kind="Internal"):
        return self.nc.dram_tensor(name, list(shape), dt, kind=kind).ap()

    def sb(self, shape, dt, name=None, stack=None):
        self.uid += 1
        name = f"{name or 't'}_{self.uid}"
        h = (stack or self.es).enter_context(self.nc.sbuf_tensor(name, list(shape), dt))
        return Buf(h, name)

    def ps(self, shape, dt=F32, name=None, stack=None):
        self.uid += 1
        name = f"{name or 'p'}_{self.uid}"
        h = (stack or self.es).enter_context(self.nc.psum_tensor(name, list(shape), dt))
        return Buf(h, name)

    def barrier(self):
        evs = []
        for g in self.engs:
            if g.count > 0:
                evs.append((g.sem, g.count))
            n = len(g.dsems)
            for j in range(n):
                cnt = (g.dcount - j + n - 1) // n if g.dcount > j else 0
                if cnt > 0:
                    evs.append((g.dsems[j], 16 * cnt))
        for g in self.engs:
            for sem, val in evs:
                g.wait(sem, val)

    def finish(self):
        self.barrier()
        self.es.close()


class Ring:
    def __init__(self, bufs):
        self.bufs = bufs
        self.i = 0

    def next(self):
        b = self.bufs[self.i % len(self.bufs)]
        self.i += 1
        return b


D = 2048
KD = 16
SEQ = 4096
NB = 4
TOK = 2048
DEPTH = 2
FFN = 5632
EPS = 1e-5
ALPHA = (2 * DEPTH) ** 0.25
SEGS = [("hp", 512), ("dq", 512), ("dk", 512), ("dv", 512), ("iq", 1024), ("ik", 64), ("iw", 16),
        ("cq", 384), ("ckv", 256), ("kr", 64), ("hc", 1024)]
SEG_OFF = {}
_o = 0
for _n, _s in SEGS:
    SEG_OFF[_n] = _o
    _o += _s
FM_SEGS = ["hp", "dq", "dk", "iq", "ik", "cq", "ckv", "kr", "krs", "hc"]
FM_DT = {"hp": "f32", "dq": "bf", "dk": "bf", "iq": "bf", "ik": "bf", "cq": "f32", "ckv": "f32", "kr": "f32",
         "krs": "f32", "hc": "f32"}
FM_ROWS = {"hp": 512, "dq": 512, "dk": 512, "iq": 1024, "ik": 64, "cq": 384, "ckv": 256, "kr": 64, "krs": 64,
           "hc": 1024}
CTS = []
for _n in FM_SEGS:
    _r = FM_ROWS[_n]
    for _c in range(0, _r, 128):
        CTS.append((_n, _c, min(128, _r - _c)))
NCT = len(CTS)


def pk(v):
    v = np.asarray(v)
    return np.ascontiguousarray(v.reshape(-1, 128).T)


def dts(s):
    return F32 if s == "f32" else BF16


def build_mods():
    kb = KB()
    nc = kb.nc
    NCOL = 2 * 6 * D // 8
    NT = NCOL // 128
    wa = kb.dram("wa", [D, NCOL], F32, kind="ExternalInput")
    ba = kb.dram("ba", [128, NT], F32, kind="ExternalInput")
    cT = kb.dram("cT", [128, KD, NB], F32, kind="ExternalInput")
    modT = kb.dram("modT", [128, NT, NB], F32, kind="ExternalOutput")
    csb = kb.sb([128, KD, NB], F32, "csb")
    cact = kb.sb([128, KD, NB], F32, "cact")
    basb = kb.sb([128, NT], F32, "basb")
    msb = kb.sb([128, NT, NB], F32, "msb")
    wr = Ring([kb.sb([128, KD, 512], F32, "wst") for _ in range(2)])
    pr = Ring([kb.ps([128, 512], F32, "ps") for _ in range(2)])
    kb.sp.dma(csb[:], cT[:, :, :], writes=[csb])
    kb.sp.dma(basb[:], ba[:, :], writes=[basb])
    kb.act.op(lambda: nc.scalar.activation(out=cact[:], in_=csb[:], func=AF.Silu), reads=[csb], writes=[cact])
    wav = wa.rearrange("(k p) n -> p k n", p=128)
    for g in range(NCOL // 512):
        w = wr.next()
        kb.sp.dma(w[:], wav[:, :, g * 512:(g + 1) * 512], writes=[w])
        for j in range(4):
            t = g * 4 + j
            p = pr.next()
            for k in range(KD):
                kb.pe.op(lambda: nc.tensor.matmul(p[:, 0:NB], lhsT=w[:, k, j * 128:(j + 1) * 128], rhs=cact[:, k, :],
                                                  start=(k == 0), stop=(k == KD - 1)),
                         reads=[w, cact], writes=[p], signal=(k == KD - 1))
            kb.dve.op(lambda: nc.vector.tensor_scalar(out=msb[:, t, :], in0=p[:, 0:NB], scalar1=basb[:, t:t + 1],
                                                      scalar2=None, op0=ALU.add),
                      reads=[p, basb], writes=[msb])
    kb.sp.dma(modT[:, :, :], msb[:], reads=[msb])
    kb.finish()
    return nc


def run_mods(inputs):
    w_ada = inputs["w_ada"]
    b_ada = inputs["b_ada"]
    c = inputs["c"]
    wcat = np.concatenate([w_ada[0], w_ada[1]], axis=1)
    bcat = np.concatenate([b_ada[0], b_ada[1]], axis=0)
    cT = np.ascontiguousarray(c.reshape(NB, KD, 128).transpose(2, 1, 0))
    NCOL = 3072
    in_maps = []
    for core in range(8):
        sl = slice(core * NCOL, (core + 1) * NCOL)
        in_maps.append({"wa": np.ascontiguousarray(wcat[:, sl]), "ba": pk(bcat[sl]), "cT": cT})
    nc = build_mods()
    res = run_bass_kernel_spmd(nc, in_maps, core_ids=list(range(8)))
    mt = np.concatenate([r["modT"] for r in res.results], axis=1)
    return [np.ascontiguousarray(mt[:, :, b]) for b in range(NB)]


class Ctx:
    pass


def setup_common(kb, modT_d):
    nc = kb.nc
    cx = Ctx()
    cx.P = [kb.ps([128, 512], F32, f"P{i}") for i in range(6)]
    cx.PB = []
    for i in range(2):
        big = kb.ps([128, 1024], BF16, f"PBB{i}")
        cx.PB += [BufV(big.h, f"PB{2 * i}", 0, 512), BufV(big.h, f"PB{2 * i + 1}", 512, 512)]
    cx.ones_f = kb.sb([128, 128], F32, "ones_f")
    cx.ones_b = kb.sb([128, 128], BF16, "ones_b")
    cx.eps = kb.sb([128, 1], F32, "eps")
    cx.mod = kb.sb([128, DEPTH * 96], F32, "mod")
    cx.onep = kb.sb([128, DEPTH * 96], F32, "onep")
    kb.pool.op(lambda: nc.gpsimd.memset(cx.ones_f[:], 1.0), writes=[cx.ones_f])
    kb.pool.op(lambda: nc.gpsimd.memset(cx.ones_b[:], 1.0), writes=[cx.ones_b])
    kb.pool.op(lambda: nc.gpsimd.memset(cx.eps[:], EPS), writes=[cx.eps])
    if modT_d is not None:
        kb.sp.dma(cx.mod[:], modT_d[:, :], writes=[cx.mod])
        kb.dve.op(lambda: nc.vector.tensor_scalar(out=cx.onep[:], in0=cx.mod[:], scalar1=1.0, scalar2=None, op0=ALU.add),
                  reads=[cx.mod], writes=[cx.onep])
    return cx


def mcol(l, i, k):
    return (l * 6 + i) * 16 + k


def col_stats(kb, cx, chunks, nfeat, want_mean, tmp, N=512):
    nc = kb.nc
    ps_s, ps_q = cx.P[0], cx.P[1]
    n = len(chunks)
    for k, (b, ap) in enumerate(chunks):
        rows = ap.shape[0]
        sq = tmp["sq"].next()
        kb.act.op(lambda: nc.scalar.activation(out=sq[:rows, :N], in_=ap, func=AF.Square), reads=[b], writes=[sq])
        kb.pe.op(lambda: nc.tensor.matmul(ps_q[:, :N], lhsT=cx.ones_f[:rows, :], rhs=sq[:rows, :N], start=(k == 0),
                                          stop=(k == n - 1)), reads=[sq, cx.ones_f], writes=[ps_q])
        if want_mean:
            kb.pe.op(lambda: nc.tensor.matmul(ps_s[:, :N], lhsT=cx.ones_f[:rows, :], rhs=ap, start=(k == 0),
                                              stop=(k == n - 1)), reads=[b, cx.ones_f], writes=[ps_s])
    inv = 1.0 / nfeat
    var, rstd = tmp["var"], tmp["rstd"]
    if want_mean:
        mean, msq, nmr = tmp["mean"], tmp["msq"], tmp["nmr"]
        kb.dve.op(lambda: nc.vector.tensor_scalar(out=mean[:, :N], in0=ps_s[:, :N], scalar1=inv, scalar2=None,
                                                  op0=ALU.mult), reads=[ps_s], writes=[mean])
        kb.dve.op(lambda: nc.vector.tensor_tensor(out=msq[:, :N], in0=mean[:, :N], in1=mean[:, :N], op=ALU.mult),
                  reads=[mean], writes=[msq])
        kb.dve.op(lambda: nc.vector.scalar_tensor_tensor(out=var[:, :N], in0=ps_q[:, :N], scalar=inv, in1=msq[:, :N],
                                                         op0=ALU.mult, op1=ALU.subtract),
                  reads=[ps_q, msq], writes=[var])
        kb.act.op(lambda: nc.scalar.activation(out=var[:, :N], in_=var[:, :N], func=AF.Sqrt, bias=cx.eps[:, 0:1],
                                               scale=1.0), reads=[var, cx.eps], writes=[var])
    else:
        kb.act.op(lambda: nc.scalar.activation(out=var[:, :N], in_=ps_q[:, :N], func=AF.Sqrt, bias=cx.eps[:, 0:1],
                                               scale=inv), reads=[ps_q, cx.eps], writes=[var])
    kb.dve.op(lambda: nc.vector.reciprocal(out=rstd[:, :N], in_=var[:, :N]), reads=[var], writes=[rstd])
    if want_mean:
        kb.dve.op(lambda: nc.vector.scalar_tensor_tensor(out=nmr[:, :N], in0=mean[:, :N], scalar=-1.0,
                                                         in1=rstd[:, :N], op0=ALU.mult, op1=ALU.mult),
                  reads=[mean, rstd], writes=[nmr])
        return rstd, nmr
    return rstd, None


def stat_tmps(kb, st, N=512):
    t = {"sq": Ring([kb.sb([128, N], F32, "sq", st) for _ in range(2)])}
    for nm in ("mean", "msq", "var", "rstd", "nmr"):
        t[nm] = kb.sb([128, N], F32, nm, st)
    return t


def load_cast(kb, dst, dst_ap, src_ap, stg_ring, stg_view):
    nc = kb.nc
    s = stg_ring.next()
    kb.sp.dma(stg_view(s), src_ap, writes=[s])
    kb.pool.op(lambda: nc.gpsimd.tensor_copy(out=dst_ap, in_=stg_view(s)), reads=[s], writes=[dst])


def evac(kb, i, out_buf, out_ap, ps_buf, ps_ap):
    nc = kb.nc
    if i % 2 == 0:
        kb.act.op(lambda: nc.scalar.copy(out=out_ap, in_=ps_ap), reads=[ps_buf], writes=[out_buf])
    else:
        kb.dve.op(lambda: nc.vector.tensor_copy(out=out_ap, in_=ps_ap), reads=[ps_buf], writes=[out_buf])


def stage_A(kb, cx, l, xT, W, O, S):
    nc = kb.nc
    P = cx.P
    xTv = xT.rearrange("(k p) n -> p k n", p=128)
    with ExitStack() as st:
        uT = [kb.sb([128, KD, 512], BF16, f"uT{t}", st) for t in range(4)]
        with ExitStack() as s1:
            xr = Ring([kb.sb([128, KD, 512], F32, "xt", s1) for _ in range(2)])
            tmp = stat_tmps(kb, s1)
            t1r = Ring([kb.sb([128, 512], F32, "t1", s1) for _ in range(2)])
            t2r = Ring([kb.sb([128, 512], F32, "t2", s1) for _ in range(2)])
            for tt in range(4):
                xt = xr.next()
                kb.sp.dma(xt[:], xTv[:, :, tt * 512:(tt + 1) * 512], writes=[xt])
                rstd, nmr = col_stats(kb, cx, [(xt, xt[:, k, :]) for k in range(KD)], D, True, tmp)
                for k in range(KD):
                    t1 = t1r.next()
                    t2 = t2r.next()
                    kb.dve.op(lambda: nc.vector.tensor_tensor(out=t1[:], in0=xt[:, k, :], in1=rstd[:], op=ALU.mult),
                              reads=[xt, rstd], writes=[t1])
                    kb.pool.op(lambda: nc.gpsimd.tensor_tensor(out=t2[:], in0=t1[:], in1=nmr[:], op=ALU.add),
                               reads=[t1, nmr], writes=[t2])
                    kb.act.op(lambda: nc.scalar.activation(out=uT[tt][:, k, :], in_=t2[:], func=AF.Identity,
                                                           scale=cx.onep[:, mcol(l, 1, k):mcol(l, 1, k) + 1],
                                                           bias=cx.mod[:, mcol(l, 0, k):mcol(l, 0, k) + 1]),
                              reads=[t2, cx.onep, cx.mod], writes=[uT[tt]])
                kb.act.dma(O["uT"].rearrange("(k p) n -> p k n", p=128)[:, :, tt * 512:(tt + 1) * 512], uT[tt][:],
                           reads=[uT[tt]])
            kb.barrier()
        with ExitStack() as s2:
            wst = Ring([kb.sb([128, KD, 128], F32, "wst", s2) for _ in range(2)])
            wbf = Ring([kb.sb([128, KD, 128], BF16, "wbf", s2) for _ in range(2)])
            obf = Ring([kb.sb([128, TOK], BF16, "obf", s2) for _ in range(2)])
            of32 = Ring([kb.sb([128, TOK], F32, "of32", s2) for _ in range(2)])
            psr = Ring([P[2], P[3], P[4], P[5]])
            dest = {"hp": O["hpT"], "dq": O["dqT"], "dk": O["dkT"], "iq": O["iqT"], "ik": O["ikT"], "cq": S["cqT"],
                    "ckv": S["ckvT"], "kr": S["krraw"], "krs": S["krsraw"], "hc": S["hcT"]}
            ei = 0
            for ct, (seg, c0, ncols) in enumerate(CTS):
                wb = wbf.next()
                load_cast(kb, wb, wb[:], W["win_ct"][ct], wst, lambda s: s[:])
                ob = (of32 if FM_DT[seg] == "f32" else obf).next()
                for tt in range(4):
                    ps = psr.next()
                    for k in range(KD):
                        kb.pe.op(lambda: nc.tensor.matmul(ps[:], lhsT=wb[:, k, :], rhs=uT[tt][:, k, :],
                                                          start=(k == 0), stop=(k == KD - 1)),
                                 reads=[wb, uT[tt]], writes=[ps])
                    evac(kb, ei, ob, ob[:ncols, tt * 512:(tt + 1) * 512], ps, ps[:ncols, :])
                    ei += 1
                kb.act.dma(dest[seg][c0:c0 + ncols, :], ob[:ncols, :], reads=[ob])
            wv = kb.sb([128, KD, 512], BF16, "wv", s2)
            wiw = kb.sb([128, KD, 128], BF16, "wiw", s2)
            for j in range(4):
                load_cast(kb, wv, wv[:, :, j * 128:(j + 1) * 128], W["wdv_ct"][j], wst, lambda s: s[:])
            load_cast(kb, wiw, wiw[:], W["wiw_ct"][0], wst, lambda s: s[:])
            vob = Ring([kb.sb([128, 512], BF16, "vob", s2) for _ in range(2)])
            iwo = kb.sb([128, 16, 16], F32, "iwo", s2)
            for tb in range(16):
                tt, off = tb // 4, (tb % 4) * 128
                ps = psr.next()
                for k in range(KD):
                    kb.pe.op(lambda: nc.tensor.matmul(ps[:], lhsT=uT[tt][:, k, off:off + 128], rhs=wv[:, k, :],
                                                      start=(k == 0), stop=(k == KD - 1)),
                             reads=[wv, uT[tt]], writes=[ps])
                vo = vob.next()
                evac(kb, tb, vo, vo[:], ps, ps[:])
                kb.act.dma(O["dV"][tb * 128:(tb + 1) * 128, :], vo[:], reads=[vo])
                ps2 = psr.next()
                for k in range(KD):
                    kb.pe.op(lambda: nc.tensor.matmul(ps2[:, 0:16], lhsT=uT[tt][:, k, off:off + 128], rhs=wiw[:, k, 0:16],
                                                      start=(k == 0), stop=(k == KD - 1)),
                             reads=[wiw, uT[tt]], writes=[ps2])
                evac(kb, tb + 1, iwo, iwo[:, tb, :], ps2, ps2[:, 0:16])
            kb.act.dma(O["iw"].rearrange("(b p) h -> p b h", p=128), iwo[:], reads=[iwo])
            kb.barrier()
    with ExitStack() as s3:
        stg = Ring([kb.sb([128, 1024], F32, "stg", s3) for _ in range(2)])
        wq = kb.sb([128, 3, 1024], BF16, "wq", s3)
        wkv = kb.sb([128, 2, 1024], BF16, "wkv", s3)
        for kc in range(3):
            load_cast(kb, wq, wq[:, kc, :], W["wq_all"][kc * 128:(kc + 1) * 128, :], stg, lambda s: s[:])
        for kc in range(2):
            load_cast(kb, wkv, wkv[:, kc, :], W["wkv_all"][kc * 128:(kc + 1) * 128, :], stg, lambda s: s[:])
        vecs = kb.sb([128, 8], F32, "vecs", s3)
        kb.sp.dma(vecs[:, 0:3], W["q_norm"][:, :], writes=[vecs])
        kb.sp.dma(vecs[:, 3:5], W["kv_norm"][:, :], writes=[vecs])
        tmp = stat_tmps(kb, s3)
        ar = Ring([kb.sb([128, 8, 512], F32, "hc", s3) for _ in range(1)])
        sig = kb.sb([128, 4, 512], F32, "sig", s3)
        zb = kb.sb([128, 4, 512], BF16, "zb", s3)
        cqs = kb.sb([128, 3, 512], F32, "cqs", s3)
        cqn = kb.sb([128, 3, 512], BF16, "cqn", s3)
        cks = kb.sb([128, 2, 512], F32, "cks", s3)
        ckn = kb.sb([128, 2, 512], BF16, "ckn", s3)
        cc = kb.sb([64, 512], F32, "cc", s3)
        ss = kb.sb([64, 512], F32, "ss", s3)
        krr = kb.sb([64, 2, 512], F32, "krr", s3)
        t1r = Ring([kb.sb([128, 512], F32, "t1", s3) for _ in range(2)])
        t2r = Ring([kb.sb([128, 512], F32, "t2", s3) for _ in range(2)])
        obr = Ring([kb.sb([128, 512], BF16, "ob", s3) for _ in range(3)])
        psr = Ring([P[2], P[3], P[4], P[5]])
        ei = 0
        for tt in range(4):
            ts = slice(tt * 512, (tt + 1) * 512)
            hc = ar.next()
            kb.sp.dma(hc[:], S["hcT"].rearrange("(k p) n -> p k n", p=128)[:, :, ts], writes=[hc])
            kb.act.op(lambda: nc.scalar.activation(out=sig[:], in_=hc[:, 4:8, :], func=AF.Sigmoid), reads=[hc],
                      writes=[sig])
            kb.dve.op(lambda: nc.vector.tensor_tensor(out=zb[:], in0=hc[:, 0:4, :], in1=sig[:], op=ALU.mult),
                      reads=[hc, sig], writes=[zb])
            kb.act.dma(O["zT"].rearrange("(k p) n -> p k n", p=128)[:, :, ts], zb[:], reads=[zb])
            kb.sp.dma(cc[:], W["ropec"][:, ts], writes=[cc])
            kb.sp.dma(ss[:], W["ropes"][:, ts], writes=[ss])
            kb.sp.dma(cqs[:], S["cqT"].rearrange("(k p) n -> p k n", p=128)[:, :, ts], writes=[cqs])
            rstd, _ = col_stats(kb, cx, [(cqs, cqs[:, k, :]) for k in range(3)], 384, False, tmp)
            for k in range(3):
                t1 = t1r.next()
                kb.dve.op(lambda: nc.vector.tensor_tensor(out=t1[:], in0=cqs[:, k, :], in1=rstd[:], op=ALU.mult),
                          reads=[cqs, rstd], writes=[t1])
                kb.act.op(lambda: nc.scalar.activation(out=cqn[:, k, :], in_=t1[:], func=AF.Copy,
                                                       scale=vecs[:, k:k + 1]), reads=[t1, vecs], writes=[cqn])
            for h in range(4):
                ps = psr.next()
                for k in range(3):
                    kb.pe.op(lambda: nc.tensor.matmul(ps[:], lhsT=wq[:, k, h * 256:h * 256 + 128], rhs=cqn[:, k, :],
                                                      start=(k == 0), stop=(k == 2)), reads=[wq, cqn], writes=[ps])
                ob = obr.next()
                evac(kb, ei, ob, ob[:], ps, ps[:]); ei += 1
                kb.act.dma(O["qnT"][h * 128:(h + 1) * 128, ts], ob[:], reads=[ob])
                ps = psr.next()
                ps2 = psr.next()
                for k in range(3):
                    kb.pe.op(lambda: nc.tensor.matmul(ps[:64, :], lhsT=wq[:, k, h * 256 + 128:h * 256 + 192],
                                                      rhs=cqn[:, k, :], start=(k == 0), stop=(k == 2)),
                             reads=[wq, cqn], writes=[ps])
                for k in range(3):
                    kb.pe.op(lambda: nc.tensor.matmul(ps2[:64, :], lhsT=wq[:, k, h * 256 + 192:h * 256 + 256],
                                                      rhs=cqn[:, k, :], start=(k == 0), stop=(k == 2)),
                             reads=[wq, cqn], writes=[ps2])
                t1 = t1r.next(); t2 = t2r.next(); ob = obr.next()
                kb.dve.op(lambda: nc.vector.tensor_tensor(out=t1[:64, :], in0=ps[:64, :], in1=cc[:], op=ALU.mult),
                          reads=[ps, cc], writes=[t1])
                kb.dve.op(lambda: nc.vector.tensor_tensor(out=t2[:64, :], in0=ps2[:64, :], in1=ss[:], op=ALU.mult),
                          reads=[ps2, ss], writes=[t2])
                kb.pool.op(lambda: nc.gpsimd.tensor_tensor(out=ob[:64, :], in0=t1[:64, :], in1=t2[:64, :], op=ALU.add),
                           reads=[t1, t2], writes=[ob])
                kb.act.dma(O["qrT"][h * 64:(h + 1) * 64, ts], ob[:64, :], reads=[ob])
            kb.sp.dma(cks[:], S["ckvT"].rearrange("(k p) n -> p k n", p=128)[:, :, ts], writes=[cks])
            rstd, _ = col_stats(kb, cx, [(cks, cks[:, k, :]) for k in range(2)], 256, False, tmp)
            for k in range(2):
                t1 = t1r.next()
                kb.dve.op(lambda: nc.vector.tensor_tensor(out=t1[:], in0=cks[:, k, :], in1=rstd[:], op=ALU.mult),
                          reads=[cks, rstd], writes=[t1])
                kb.act.op(lambda: nc.scalar.activation(out=ckn[:, k, :], in_=t1[:], func=AF.Copy,
                                                       scale=vecs[:, 3 + k:4 + k]), reads=[t1, vecs], writes=[ckn])
            for h in range(4):
                ps = psr.next()
                for k in range(2):
                    kb.pe.op(lambda: nc.tensor.matmul(ps[:], lhsT=wkv[:, k, h * 128:(h + 1) * 128], rhs=ckn[:, k, :],
                                                      start=(k == 0), stop=(k == 1)), reads=[wkv, ckn], writes=[ps])
                ob = obr.next()
                evac(kb, ei, ob, ob[:], ps, ps[:]); ei += 1
                kb.act.dma(O["knT"][h * 128:(h + 1) * 128, ts], ob[:], reads=[ob])
            for tb in range(4):
                ps = psr.next()
                for k in range(2):
                    kb.pe.op(lambda: nc.tensor.matmul(ps[:], lhsT=ckn[:, k, tb * 128:(tb + 1) * 128],
                                                      rhs=wkv[:, k, 512:1024], start=(k == 0), stop=(k == 1)),
                             reads=[wkv, ckn], writes=[ps])
                ob = obr.next()
                evac(kb, ei, ob, ob[:], ps, ps[:]); ei += 1
                kb.act.dma(O["mV"][tt * 512 + tb * 128:tt * 512 + (tb + 1) * 128, :], ob[:], reads=[ob])
            kb.sp.dma(krr[:, 0, :], S["krraw"][:, ts], writes=[krr])
            kb.sp.dma(krr[:, 1, :], S["krsraw"][:, ts], writes=[krr])
            t1 = t1r.next(); t2 = t2r.next(); ob = obr.next()
            kb.dve.op(lambda: nc.vector.tensor_tensor(out=t1[:64, :], in0=krr[:, 0, :], in1=cc[:], op=ALU.mult),
                      reads=[krr, cc], writes=[t1])
            kb.dve.op(lambda: nc.vector.tensor_tensor(out=t2[:64, :], in0=krr[:, 1, :], in1=ss[:], op=ALU.mult),
                      reads=[krr, ss], writes=[t2])
            kb.pool.op(lambda: nc.gpsimd.tensor_tensor(out=ob[:64, :], in0=t1[:64, :], in1=t2[:64, :], op=ALU.add),
                       reads=[t1, t2], writes=[ob])
            kb.act.dma(O["krT"][:, ts], ob[:64, :], reads=[ob])
        kb.barrier()


A_OUT = {"uT": ([D, TOK], "bf"), "hpT": ([512, TOK], "f32"), "dqT": ([512, TOK], "bf"), "dkT": ([512, TOK], "bf"),
         "dV": ([TOK, 512], "bf"), "iqT": ([1024, TOK], "bf"), "ikT": ([64, TOK], "bf"), "iw": ([TOK, 16], "f32"),
         "qnT": ([512, TOK], "bf"), "qrT": ([256, TOK], "bf"), "knT": ([512, TOK], "bf"), "mV": ([TOK, 512], "bf"),
         "krT": ([64, TOK], "bf"), "zT": ([512, TOK], "bf")}
A_SCR = {"cqT": [384, TOK], "ckvT": [256, TOK], "krraw": [64, TOK], "krsraw": [64, TOK], "hcT": [1024, TOK]}
A_W = {"win_ct": [NCT, 128, KD, 128], "wdv_ct": [4, 128, KD, 128], "wiw_ct": [1, 128, KD, 128],
       "wq_all": [384, 1024], "wkv_all": [256, 1024], "q_norm": [128, 3], "kv_norm": [128, 2],
       "ropec": [64, TOK], "ropes": [64, TOK]}


def ct_layout(w, c0, ncols):
    out = np.zeros((128, KD, 128), np.float32)
    out[:, :, :ncols] = w[:, c0:c0 + ncols].reshape(KD, 128, ncols).transpose(1, 0, 2)
    return out


def host_A_weights(inputs, l, h):
    w_in = inputs["w_in"][l]
    cts = []
    for seg, c0, ncols in CTS:
        if seg == "krs":
            base = SEG_OFF["kr"]
            wsw = np.concatenate([w_in[:, base + 32:base + 64], w_in[:, base:base + 32]], axis=1)
            cts.append(ct_layout(wsw, 0, 64))
        else:
            cts.append(ct_layout(w_in, SEG_OFF[seg] + c0, ncols))
    out = {"win_ct": np.stack(cts)}
    out["wdv_ct"] = np.stack([ct_layout(w_in, SEG_OFF["dv"] + j * 128, 128) for j in range(4)])
    out["wiw_ct"] = np.stack([ct_layout(w_in, SEG_OFF["iw"], 16)])
    wq = inputs["w_q_up"][l]
    parts = []
    for hh in range(4):
        b = hh * 192
        parts += [wq[:, b:b + 128], wq[:, b + 128:b + 192], wq[:, b + 160:b + 192], wq[:, b + 128:b + 160]]
    out["wq_all"] = np.ascontiguousarray(np.concatenate(parts, axis=1))
    wkv = inputs["w_kv_up"][l]
    out["wkv_all"] = np.ascontiguousarray(np.concatenate(
        [wkv[:, hh * 256:hh * 256 + 128] for hh in range(4)] + [wkv[:, hh * 256 + 128:hh * 256 + 256] for hh in range(4)],
        axis=1))
    out["q_norm"] = pk(inputs["q_norm"][l])
    out["kv_norm"] = pk(inputs["kv_norm"][l])
    pos = np.arange(h * TOK, (h + 1) * TOK, dtype=np.float32)
    inv_freq = (np.float32(10000.0) ** (-np.arange(0, 64, 2, dtype=np.float32) / np.float32(64))).astype(np.float32)
    ang = pos[None, :] * inv_freq[:, None]
    cos, sin = np.cos(ang).astype(np.float32), np.sin(ang).astype(np.float32)
    out["ropec"] = np.ascontiguousarray(np.concatenate([cos, cos], axis=0))
    out["ropes"] = np.ascontiguousarray(np.concatenate([-sin, sin], axis=0))
    return out


def build_A(l):
    kb = KB()
    xT = kb.dram("xT", [D, TOK], F32, kind="ExternalInput")
    modT = kb.dram("modT", [128, DEPTH * 96], F32, kind="ExternalInput")
    W = {k: kb.dram(k, v, F32, kind="ExternalInput") for k, v in A_W.items()}
    O = {k: kb.dram(k, v[0], dts(v[1]), kind="ExternalOutput") for k, v in A_OUT.items()}
    S = {k: kb.dram(k, v, F32) for k, v in A_SCR.items()}
    cx = setup_common(kb, modT)
    stage_A(kb, cx, l, xT, W, O, S)
    kb.finish()
    return kb.nc


SLOPES = [2.0 ** (-8.0 * (h + 1) / 4) for h in range(4)]
NBIS = 16
NEGM = -30000.0


def key_tiles(i, with_prev=True):
    tl = [(j * 512, 512, 0) for j in range(4)] if with_prev else []
    for j in range(i // 4):
        tl.append((2048 + j * 512, 512, 1))
    tl.append((2048 + (i // 4) * 512, (i % 4 + 1) * 128, 2))
    return tl


def host_B_consts(h, skip_prev=False):
    c = {}
    q = np.arange(128)[:, None]
    s = np.arange(512)[None, :]
    c["AB"] = np.stack([SLOPES[hh] * (s - q) for hh in range(4)], axis=1).astype(np.float32)
    s1 = np.arange(128)[None, :]
    cm = np.where((s1 // 64) <= (q // 64), 0.0, 1.0)
    c["ABD"] = np.stack([-SLOPES[hh] * np.abs(q - s1) + SLOPES[hh] * 128.0 * wv for hh in range(4) for wv in range(4)],
                        axis=1).astype(np.float32)
    c["CM"] = (cm * NEGM).astype(np.float32)
    c["CMB"] = (cm * -1e6).astype(np.float32)
    c["ident"] = np.eye(128, dtype=np.float32)
    prevb = 0.0 if h == 1 else NEGM
    c["prevb"] = np.full((128, 2), prevb, np.float32)
    c["prevs"] = np.full((128, 2), 0.0 if h == 1 else -1e6, np.float32)
    cb = np.zeros((128, 16 * 4 * 8), np.float32)
    for i in range(16):
        tq0 = 2048 + 128 * i
        for hh in range(4):
            for ti, (s0, w, kind) in enumerate(key_tiles(i, not (skip_prev and h == 0))):
                v = -SLOPES[hh] * (tq0 - s0)
                if kind == 0:
                    v += prevb
                cb[:, (i * 4 + hh) * 8 + ti] = v
    c["cb"] = cb
    c["pw"] = np.tile((2.0 ** -(np.arange(NBIS + 1) + 1.0))[None, :], (128, 1)).astype(np.float32)
    ic = np.zeros((128, 4, 16), np.float32)
    for g, w in enumerate((2, 4, 8, 16)):
        t = np.arange(16) + h * TOK
        ic[:, g, :] = 1.0 / np.minimum(t + 1, w)
    c["invc"] = ic
    return c


B_C = {"AB": [128, 4, 512], "ABD": [128, 16, 128], "CM": [128, 128], "CMB": [128, 128], "ident": [128, 128],
       "prevb": [128, 2], "prevs": [128, 2], "cb": [128, 512], "pw": [128, NBIS + 1], "invc": [128, 4, 16]}
B_W = {"w_pool": [4, 128, 128], "pool_scale": [128, 4], "w_dwT": [128, 4, 31], "b_dw": [128, 4],
       "cln_g": [128, 4], "cln_b": [128, 4]}
B_PREV = {"pdkT": ([512, TOK], "bf"), "pdV": ([TOK, 512], "bf"), "pikT": ([64, TOK], "bf"), "pknT": ([512, TOK], "bf"),
          "pmV": ([TOK, 512], "bf"), "pkrT": ([64, TOK], "bf"), "pz": ([512, 32], "bf"), "php": ([512, 16], "f32")}


def host_B_weights(inputs, l):
    out = {"w_pool": np.ascontiguousarray(inputs["w_pool"][l]), "pool_scale": pk(inputs["pool_scale"][l]),
           "b_dw": pk(inputs["b_dw"][l]), "cln_g": pk(inputs["conv_ln_g"][l]), "cln_b": pk(inputs["conv_ln_b"][l])}
    wd = inputs["w_dw"][l]
    out["w_dwT"] = np.ascontiguousarray(wd.reshape(31, 4, 128).transpose(2, 1, 0))
    return out


PARTS = {"B1", "B2", "B3", "B4"}
DBG = {"ni": 16, "tail": True, "fin": True, "nbis": NBIS, "att": True, "idx": True}


def sect(tag):
    if tag in PARTS:
        with ExitStack() as st:
            yield st


def attn_tail(kb, cx, T, Pb, W, s_chunk0, Vb, h, po, first, last):
    nc = kb.nc
    nb = W // 128
    pt = T["ptps"].next()
    for sb in range(nb):
        kb.pe.op(lambda: nc.tensor.transpose(out=pt[:, sb * 128:(sb + 1) * 128], in_=Pb[:, sb * 128:(sb + 1) * 128],
                                             identity=T["identb"][:]), reads=[Pb, T["identb"]], writes=[pt])
    pts = T["pts"].next()
    evac(kb, T["ei"][0], pts, pts[:, :W], pt, pt[:, :W])
    T["ei"][0] += 1
    for sb in range(nb):
        kb.pe.op(lambda: nc.tensor.matmul(po[:, 0:128], lhsT=pts[:, sb * 128:(sb + 1) * 128],
                                          rhs=Vb[:, s_chunk0 + sb, h * 128:(h + 1) * 128],
                                          start=(first and sb == 0), stop=(last and sb == nb - 1)),
                 reads=[pts, Vb], writes=[po])


def attn_finish(kb, cx, T, po, rs, npieces, yb, h, i):
    nc = kb.nc
    rsum, rinv, on = T["rsum"], T["rinv"], T["on"].next()
    kb.dve.op(lambda: nc.vector.tensor_reduce(out=rsum[:], in_=rs[:, 0:npieces], axis=AX.X, op=ALU.add), reads=[rs], writes=[rsum])
    kb.dve.op(lambda: nc.vector.reciprocal(out=rinv[:], in_=rsum[:]), reads=[rsum], writes=[rinv])
    kb.act.op(lambda: nc.scalar.activation(out=on[:], in_=po[:, 0:128], func=AF.Copy, scale=rinv[:, 0:1]),
              reads=[po, rinv], writes=[on])
    pt = T["ptps"].next()
    kb.pe.op(lambda: nc.tensor.transpose(out=pt[:, 0:128], in_=on[:], identity=T["identb"][:]),
             reads=[on, T["identb"]], writes=[pt])
    kb.dve.op(lambda: nc.vector.tensor_copy(out=yb[:, h, i * 128:(i + 1) * 128], in_=pt[:, 0:128]), reads=[pt],
              writes=[yb])


def stage_B(kb, cx, l, A, PV, W, C, yT, with_prev=True):
    nc = kb.nc
    P, PB = cx.P, cx.PB
    yTv = yT.rearrange("(b k p) n -> b p k n", b=4, p=128)
    with ExitStack() as sc_:
        ident = kb.sb([128, 128], F32, "ident", sc_)
        identb = kb.sb([128, 128], BF16, "identb", sc_)
        kb.sp.dma(ident[:], C["ident"][:, :], writes=[ident])
        kb.pool.op(lambda: nc.gpsimd.tensor_copy(out=identb[:], in_=ident[:]), reads=[ident], writes=[identb])

        for st in sect("B1"):
            hh = kb.sb([128, 16 + TOK], F32, "hh", st)
            sA = kb.sb([128, 16 + TOK], F32, "sA", st)
            sB = kb.sb([128, 16 + TOK], F32, "sB", st)
            pl = kb.sb([128, TOK], BF16, "pl", st)
            t16 = kb.sb([128, 16], F32, "t16", st)
            invc = kb.sb([128, 4, 16], F32, "invc", st)
            wps = kb.sb([128, 4, 128], F32, "wps", st)
            wpb = kb.sb([128, 4, 128], BF16, "wpb", st)
            psc = kb.sb([128, 4], F32, "psc", st)
            ya = kb.sb([128, 4, TOK], BF16, "ya", st)
            kb.sp.dma(invc[:], C["invc"][:, :, :], writes=[invc])
            kb.sp.dma(wps[:], W["w_pool"].rearrange("g c d -> c g d"), writes=[wps])
            kb.pool.op(lambda: nc.gpsimd.tensor_copy(out=wpb[:], in_=wps[:]), reads=[wps], writes=[wpb])
            kb.sp.dma(psc[:], W["pool_scale"][:, :], writes=[psc])
            psr = Ring([P[2], P[3], P[4], P[5]])
            for g in range(4):
                w = 2 ** (g + 1)
                kb.sp.dma(hh[:, 0:16], PV["php"][g * 128:(g + 1) * 128, :], writes=[hh])
                kb.sp.dma(hh[:, 16:], A["hpT"][g * 128:(g + 1) * 128, :], writes=[hh])
                src, d, o = hh, 1, 1
                bufs = [sA, sB]
                for step in range(g + 1):
                    dst = bufs[step % 2]
                    kb.dve.op(lambda: nc.vector.tensor_tensor(out=dst[:, o:], in0=src[:, o:], in1=src[:, o - d:16 + TOK - d],
                                                              op=ALU.add), reads=[src], writes=[dst])
                    src = dst
                    d *= 2
                    o += d
                kb.dve.op(lambda: nc.vector.scalar_tensor_tensor(out=pl[:], in0=src[:, 16:], scalar=1.0 / w, in1=hh[:, 16:],
                                                                 op0=ALU.mult, op1=ALU.subtract),
                          reads=[src, hh], writes=[pl])
                kb.dve.op(lambda: nc.vector.tensor_tensor(out=t16[:], in0=src[:, 16:32], in1=invc[:, g, :], op=ALU.mult),
                          reads=[src, invc], writes=[t16])
                kb.dve.op(lambda: nc.vector.tensor_tensor(out=pl[:, 0:16], in0=t16[:], in1=hh[:, 16:32], op=ALU.subtract),
                          reads=[t16, hh], writes=[pl])
                for tt in range(4):
                    ps = psr.next()
                    kb.pe.op(lambda: nc.tensor.matmul(ps[:], lhsT=wpb[:, g, :], rhs=pl[:, tt * 512:(tt + 1) * 512],
                                                      start=True, stop=True), reads=[wpb, pl], writes=[ps])
                    kb.act.op(lambda: nc.scalar.activation(out=ya[:, g, tt * 512:(tt + 1) * 512], in_=ps[:], func=AF.Copy,
                                                           scale=psc[:, g:g + 1]), reads=[ps, psc], writes=[ya])
            kb.act.dma(yTv[0], ya[:], reads=[ya])
            kb.barrier()

        for st in sect("B2"):
            zb = kb.sb([128, 4, 32 + TOK], BF16, "zb", st)
            wdw = kb.sb([128, 4, 31], F32, "wdw", st)
            dg = kb.sb([128, 4 * 31, 128], BF16, "dg", st)
            vecs = kb.sb([128, 12], F32, "cvecs", st)
            v = kb.sb([128, 4, 512], F32, "cv", st)
            yd = kb.sb([128, 4, TOK], BF16, "yd", st)
            tmp = stat_tmps(kb, st)
            t1r = Ring([kb.sb([128, 512], F32, "t1", st) for _ in range(2)])
            t2r = Ring([kb.sb([128, 512], F32, "t2", st) for _ in range(2)])
            kb.sp.dma(zb[:, :, 0:32], PV["pz"].rearrange("(k p) n -> p k n", p=128), writes=[zb])
            kb.sp.dma(zb[:, :, 32:], A["zT"].rearrange("(k p) n -> p k n", p=128), writes=[zb])
            kb.sp.dma(wdw[:], W["w_dwT"][:, :, :], writes=[wdw])
            kb.sp.dma(vecs[:, 0:4], W["b_dw"][:, :], writes=[vecs])
            kb.sp.dma(vecs[:, 4:8], W["cln_g"][:, :], writes=[vecs])
            kb.sp.dma(vecs[:, 8:12], W["cln_b"][:, :], writes=[vecs])
            for c in range(4):
                for j in range(31):
                    kb.pool.op(lambda: nc.gpsimd.tensor_scalar(out=dg[:, c * 31 + j, :], in0=ident[:],
                                                               scalar1=wdw[:, c, j:j + 1], scalar2=None, op0=ALU.mult),
                               reads=[ident, wdw], writes=[dg])
            psr = Ring([P[2], P[3], P[4], P[5]])
            for tt in range(4):
                for c in range(4):
                    ps = psr.next()
                    for j in range(31):
                        o = 32 + tt * 512 - 30 + j
                        kb.pe.op(lambda: nc.tensor.matmul(ps[:], lhsT=dg[:, c * 31 + j, :], rhs=zb[:, c, o:o + 512],
                                                          start=(j == 0), stop=(j == 30)), reads=[dg, zb], writes=[ps])
                    kb.act.op(lambda: nc.scalar.activation(out=v[:, c, :], in_=ps[:], func=AF.Identity,
                                                           bias=vecs[:, c:c + 1], scale=1.0), reads=[ps, vecs], writes=[v])
                rstd, nmr = col_stats(kb, cx, [(v, v[:, c, :]) for c in range(4)], 512, True, tmp)
                for c in range(4):
                    t1 = t1r.next(); t2 = t2r.next()
                    kb.dve.op(lambda: nc.vector.tensor_tensor(out=t1[:], in0=v[:, c, :], in1=rstd[:], op=ALU.mult),
                              reads=[v, rstd], writes=[t1])
                    kb.pool.op(lambda: nc.gpsimd.tensor_tensor(out=t2[:], in0=t1[:], in1=nmr[:], op=ALU.add),
                               reads=[t1, nmr], writes=[t2])
                    kb.act.op(lambda: nc.scalar.activation(out=yd[:, c, tt * 512:(tt + 1) * 512], in_=t2[:], func=AF.Silu,
                                                           scale=vecs[:, 4 + c:5 + c], bias=vecs[:, 8 + c:9 + c]),
                              reads=[t2, vecs], writes=[yd])
            kb.act.dma(yTv[3], yd[:], reads=[yd])
            kb.barrier()

        def mk_T(st):
            T = {"identb": identb, "ei": [0]}
            T["ptps"] = Ring([PB[0], PB[1], PB[2], PB[3]])
            T["pts"] = Ring([kb.sb([128, 512], BF16, "pts", st) for _ in range(3)])
            T["Pb"] = Ring([kb.sb([128, 512], BF16, "Pb", st) for _ in range(3)])
            T["tmp"] = Ring([kb.sb([128, 512], F32, "atmp", st) for _ in range(3)])
            T["on"] = Ring([kb.sb([128, 128], BF16, "on", st) for _ in range(2)])
            T["rs"] = Ring([kb.sb([128, 16], F32, "rs", st) for _ in range(2)])
            T["rsum"] = kb.sb([128, 1], F32, "rsum", st)
            T["rinv"] = kb.sb([128, 1], F32, "rinv", st)
            return T

        def load_kv(st, K_own, K_prev, V_own, V_prev):
            Kb = kb.sb([128, 4, 2 * TOK], BF16, "Kb", st)
            Vb = kb.sb([128, 32, 512], BF16, "Vb", st)
            if with_prev:
                kb.sp.dma(Kb[:, :, 0:TOK], K_prev.rearrange("(k p) n -> p k n", p=128), writes=[Kb])
            kb.sp.dma(Kb[:, :, TOK:], K_own.rearrange("(k p) n -> p k n", p=128), writes=[Kb])
            if with_prev:
                kb.sp.dma(Vb[:, 0:16, :], V_prev.rearrange("(c p) d -> p c d", p=128), writes=[Vb])
            kb.sp.dma(Vb[:, 16:32, :], V_own.rearrange("(c p) d -> p c d", p=128), writes=[Vb])
            return Kb, Vb

        for st in sect("B3"):
            T = mk_T(st)
            Kb, Vb = load_kv(st, A["knT"], PV["pknT"], A["mV"], PV["pmV"])
            Qb = kb.sb([128, 4, TOK], BF16, "Qb", st)
            Qr = kb.sb([128, 4, TOK], BF16, "Qr", st)
            Kr = kb.sb([128, 2 * TOK], BF16, "Kr", st)
            kb.pool.op(lambda: nc.gpsimd.memset(Qr[:], 0.0), writes=[Qr])
            kb.pool.op(lambda: nc.gpsimd.memset(Kr[:], 0.0), writes=[Kr])
            CM = kb.sb([128, 128], F32, "CM", st)
            prevb = kb.sb([128, 2], F32, "prevb", st)
            yc = kb.sb([128, 4, TOK], BF16, "yc", st)
            kb.sp.dma(Qb[:], A["qnT"].rearrange("(k p) n -> p k n", p=128), writes=[Qb])
            kb.sp.dma(Qr[0:64], A["qrT"].rearrange("(k p) n -> p k n", p=64), writes=[Qr])
            kb.sp.dma(Kr[0:64, 0:TOK], PV["pkrT"][:, :], writes=[Kr])
            kb.sp.dma(Kr[0:64, TOK:], A["krT"][:, :], writes=[Kr])
            kb.sp.dma(CM[:], C["CM"][:, :], writes=[CM])
            kb.sp.dma(prevb[:], C["prevb"][:, :], writes=[prevb])
            scale = 192.0 ** -0.5
            psS = Ring([P[2], P[3]])
            psO = Ring([P[4], P[5]])
            for i in range(DBG["ni"]):
                qs = slice(i * 128, (i + 1) * 128)
                tl = key_tiles(i, with_prev)
                for h in range(4):
                    po = psO.next()
                    rs = T["rs"].next()
                    npc = 0
                    for ti, (s0, Wd, kind) in enumerate(tl):
                        ps = psS.next()
                        kb.pe.op(lambda: nc.tensor.matmul(ps[:, :Wd], lhsT=Qb[:, h, qs], rhs=Kb[:, h, s0:s0 + Wd],
                                                          start=True, stop=False), reads=[Qb, Kb], writes=[ps])
                        kb.pe.op(lambda: nc.tensor.matmul(ps[:, :Wd], lhsT=Qr[:, h, qs], rhs=Kr[:, s0:s0 + Wd],
                                                          start=False, stop=True), reads=[Qr, Kr], writes=[ps])
                        Pb = T["Pb"].next()
                        if kind == 2:
                            tm = T["tmp"].next()
                            if Wd > 128:
                                kb.dve.op(lambda: nc.vector.tensor_scalar(out=tm[:, :Wd - 128], in0=ps[:, :Wd - 128],
                                                                          scalar1=scale, scalar2=None, op0=ALU.mult),
                                          reads=[ps], writes=[tm])
                            kb.dve.op(lambda: nc.vector.scalar_tensor_tensor(out=tm[:, Wd - 128:Wd], in0=ps[:, Wd - 128:Wd],
                                                                             scalar=scale, in1=CM[:], op0=ALU.mult,
                                                                             op1=ALU.add), reads=[ps, CM], writes=[tm])
                            kb.act.op(lambda: nc.scalar.activation(out=Pb[:, :Wd], in_=tm[:, :Wd], func=AF.Exp,
                                                                   accum_out=rs[:, npc:npc + 1]),
                                      reads=[tm], writes=[Pb, rs])
                            npc += 1
                        else:
                            if kind == 0:
                                kb.act.op(lambda: nc.scalar.activation(out=Pb[:, :Wd], in_=ps[:, :Wd], func=AF.Exp,
                                                                       scale=scale, bias=prevb[:, 0:1],
                                                                       accum_out=rs[:, npc:npc + 1]),
                                          reads=[ps, prevb], writes=[Pb, rs])
                            else:
                                kb.act.op(lambda: nc.scalar.activation(out=Pb[:, :Wd], in_=ps[:, :Wd], func=AF.Exp,
                                                                       scale=scale, accum_out=rs[:, npc:npc + 1]),
                                          reads=[ps], writes=[Pb, rs])
                            npc += 1
                        if DBG["tail"]:
                            attn_tail(kb, cx, T, Pb, Wd, s0 // 128, Vb, h, po, ti == 0, ti == len(tl) - 1)
                    if DBG["fin"]:
                        attn_finish(kb, cx, T, po, rs, npc, yc, h, i)
            kb.act.dma(yTv[2], yc[:], reads=[yc])
            kb.barrier()

        for st in sect("B4"):
            T = mk_T(st)
            Kb, Vb = load_kv(st, A["dkT"], PV["pdkT"], A["dV"], PV["pdV"])
            Qb = kb.sb([128, 4, TOK], BF16, "Qb", st)
            Ik = kb.sb([128, 2 * TOK], BF16, "Ik", st)
            iqr = Ring([kb.sb([128, 16, 128], BF16, "iq", st) for _ in range(2)])
            kb.pool.op(lambda: nc.gpsimd.memset(Ik[:], 0.0), writes=[Ik])
            for b_ in iqr.bufs:
                kb.pool.op(lambda: nc.gpsimd.memset(b_[:], 0.0), writes=[b_])
            iwr = Ring([kb.sb([128, 16], F32, "iwt", st) for _ in range(2)])
            iwa = kb.sb([128, 16], F32, "iwa", st)
            iws = kb.sb([128, 16], F32, "iws", st)
            dsg = kb.sb([128, 16, 128], BF16, "dsg", st)
            Rr = Ring([kb.sb([128, 512], BF16, "R", st) for _ in range(3)])
            scb = kb.sb([128, 2 * TOK], F32, "scb", st)
            junk = kb.sb([128, 2 * TOK], BF16, "junk", st)
            AB = kb.sb([128, 4, 512], F32, "AB", st)
            ABD = kb.sb([128, 16, 128], F32, "ABD", st)
            CMB = kb.sb([128, 128], F32, "CMB", st)
            prevs = kb.sb([128, 2], F32, "prevs", st)
            cbt = kb.sb([128, 512], F32, "cbt", st)
            pw = kb.sb([128, NBIS + 1], F32, "pw", st)
            am = kb.sb([128, 8], F32, "am", st)
            sm = {n: kb.sb([128, 1], F32, n, st) for n in ("M", "lo", "W0", "mid", "cnt", "stp")}
            wst_ = kb.sb([128, NBIS + 1], F32, "wsteps", st)
            yb = kb.sb([128, 4, TOK], BF16, "yb", st)
            kb.sp.dma(Qb[:], A["dqT"].rearrange("(k p) n -> p k n", p=128), writes=[Qb])
            kb.sp.dma(Ik[0:64, 0:TOK], PV["pikT"][:, :], writes=[Ik])
            kb.sp.dma(Ik[0:64, TOK:], A["ikT"][:, :], writes=[Ik])
            for nm, t_ in (("AB", AB), ("ABD", ABD)):
                kb.sp.dma(t_[:], C[nm][:, :, :], writes=[t_])
            for nm, t_ in (("CMB", CMB), ("prevs", prevs), ("cb", cbt), ("pw", pw)):
                kb.sp.dma(t_[:], C[nm][:, :], writes=[t_])
            scale = 128.0 ** -0.5
            psS = Ring([P[2], P[3]])
            psO = Ring([P[4], P[5]])
            iqv = A["iqT"].rearrange("(h d) n -> d h n", d=64)
            iwv = A["iw"].rearrange("(b p) h -> b p h", p=128)
            for i in range(DBG["ni"]):
                qs = slice(i * 128, (i + 1) * 128)
                tl = key_tiles(i, with_prev)
                Ntot = tl[-1][0] + tl[-1][1]
                N0 = tl[0][0]
                iq = iqr.next()
                iwt = iwr.next()
                kb.sp.dma(iq[0:64], iqv[:, :, qs], writes=[iq])
                kb.sp.dma(iwt[:], iwv[i], writes=[iwt])
                kb.act.op(lambda: nc.scalar.activation(out=iwa[:], in_=iwt[:], func=AF.Abs), reads=[iwt], writes=[iwa])
                kb.act.op(lambda: nc.scalar.activation(out=iws[:], in_=iwt[:], func=AF.Sign), reads=[iwt], writes=[iws])
                for hh in range(16):
                    kb.pool.op(lambda: nc.gpsimd.tensor_scalar(out=dsg[:, hh, :], in0=ident[:], scalar1=iws[:, hh:hh + 1],
                                                               scalar2=None, op0=ALU.mult),
                               reads=[ident, iws], writes=[dsg])
                for ti, (s0, Wd, kind) in enumerate(tl if DBG["idx"] else []):
                    pss = P[0] if ti % 2 == 0 else P[1]
                    Rs = {}
                    for hh in range(17):
                        if hh < 16:
                            ps = psS.next()
                            kb.pe.op(lambda: nc.tensor.matmul(ps[:, :Wd], lhsT=iq[:, hh, :], rhs=Ik[:, s0:s0 + Wd],
                                                              start=True, stop=True), reads=[iq, Ik], writes=[ps])
                            R = Rr.next()
                            kb.act.op(lambda: nc.scalar.activation(out=R[:, :Wd], in_=ps[:, :Wd], func=AF.Relu,
                                                                   scale=iwa[:, hh:hh + 1]), reads=[ps, iwa], writes=[R])
                            Rs[hh] = R
                        if hh >= 1 and DBG.get("acc", True):
                            g_ = hh - 1
                            R_ = Rs.pop(g_)
                            kb.pe.op(lambda: nc.tensor.matmul(pss[:, :Wd], lhsT=dsg[:, g_, :], rhs=R_[:, :Wd],
                                                              start=(g_ == 0), stop=(g_ == 15)), reads=[dsg, R_],
                                     writes=[pss])
                    if not DBG.get("post", True):
                        continue
                    kb.dve.op(lambda: nc.vector.tensor_reduce(out=am[:, ti:ti + 1], in_=pss[:, :Wd], axis=AX.X, op=ALU.max,
                                                              apply_absolute_value=True), reads=[pss], writes=[am])
                    if DBG.get("post", 2) == 1:
                        continue
                    if kind == 0:
                        kb.dve.op(lambda: nc.vector.tensor_scalar(out=scb[:, s0:s0 + Wd], in0=pss[:, :Wd],
                                                                  scalar1=prevs[:, 0:1], scalar2=None, op0=ALU.add),
                                  reads=[pss, prevs], writes=[scb])
                    else:
                        if kind == 1 or Wd > 128:
                            We = Wd if kind == 1 else Wd - 128
                            kb.dve.op(lambda: nc.vector.tensor_copy(out=scb[:, s0:s0 + We], in_=pss[:, :We]), reads=[pss],
                                      writes=[scb])
                        if kind == 2:
                            kb.dve.op(lambda: nc.vector.tensor_tensor(out=scb[:, s0 + Wd - 128:s0 + Wd],
                                                                      in0=pss[:, Wd - 128:Wd], in1=CMB[:], op=ALU.add),
                                      reads=[pss, CMB], writes=[scb])
                nt = len(tl)
                M, lo, W0, mid, cnt, stp = (sm[n] for n in ("M", "lo", "W0", "mid", "cnt", "stp"))
                kb.dve.op(lambda: nc.vector.tensor_reduce(out=M[:], in_=am[:, 0:nt], axis=AX.X, op=ALU.max), reads=[am], writes=[M])
                kb.dve.op(lambda: nc.vector.tensor_scalar(out=lo[:], in0=M[:], scalar1=-1.0, scalar2=-1.0, op0=ALU.mult,
                                                          op1=ALU.add), reads=[M], writes=[lo])
                kb.dve.op(lambda: nc.vector.tensor_scalar(out=W0[:], in0=M[:], scalar1=2.0, scalar2=2.0, op0=ALU.mult,
                                                          op1=ALU.add), reads=[M], writes=[W0])
                kb.dve.op(lambda: nc.vector.tensor_scalar(out=wst_[:], in0=pw[:], scalar1=W0[:, 0:1], scalar2=None,
                                                          op0=ALU.mult), reads=[pw, W0], writes=[wst_])
                kb.dve.op(lambda: nc.vector.tensor_tensor(out=mid[:], in0=lo[:], in1=wst_[:, 0:1], op=ALU.add),
                          reads=[lo, wst_], writes=[mid])
                for it in range(DBG["nbis"]):
                    kb.dve.op(lambda: nc.vector.tensor_scalar(out=junk[:, N0:Ntot], in0=scb[:, N0:Ntot], scalar1=mid[:, 0:1],
                                                              scalar2=None, op0=ALU.is_ge, op1=ALU.add, accum_out=cnt[:]),
                              reads=[scb, mid], writes=[junk, cnt])
                    kb.dve.op(lambda: nc.vector.tensor_scalar(out=stp[:], in0=cnt[:], scalar1=255.5,
                                                              scalar2=wst_[:, it:it + 1], op0=ALU.is_ge, op1=ALU.mult),
                              reads=[cnt, wst_], writes=[stp])
                    kb.dve.op(lambda: nc.vector.tensor_tensor(out=lo[:], in0=lo[:], in1=stp[:], op=ALU.add),
                              reads=[lo, stp], writes=[lo])
                    kb.dve.op(lambda: nc.vector.tensor_tensor(out=mid[:], in0=lo[:], in1=wst_[:, it + 1:it + 2],
                                                              op=ALU.add), reads=[lo, wst_], writes=[mid])
                kb.dve.op(lambda: nc.vector.tensor_scalar(out=junk[:, N0:Ntot], in0=scb[:, N0:Ntot], scalar1=lo[:, 0:1],
                                                          scalar2=NEGM, op0=ALU.is_lt, op1=ALU.mult),
                          reads=[scb, lo], writes=[junk])
                rss = [T["rs"].next() for _ in range(2)]
                for h in range(4 if DBG["att"] else 0):
                    po = psO.next()
                    rs = rss[h % 2]
                    npc = 0
                    for ti, (s0, Wd, kind) in enumerate(tl):
                        ps = psS.next()
                        kb.pe.op(lambda: nc.tensor.matmul(ps[:, :Wd], lhsT=Qb[:, h, qs], rhs=Kb[:, h, s0:s0 + Wd],
                                                          start=True, stop=False), reads=[Qb, Kb], writes=[ps])
                        kb.pe.op(lambda: nc.tensor.matmul(ps[:, :Wd], lhsT=identb[:], rhs=junk[:, s0:s0 + Wd], start=False,
                                                          stop=True), reads=[identb, junk], writes=[ps])
                        tm = T["tmp"].next()
                        Pb = T["Pb"].next()
                        cbc = (i * 4 + h) * 8 + ti
                        if kind == 2:
                            wv_ = Wd // 128 - 1
                            if Wd > 128:
                                kb.dve.op(lambda: nc.vector.scalar_tensor_tensor(out=tm[:, :Wd - 128], in0=ps[:, :Wd - 128],
                                                                                 scalar=scale, in1=AB[:, h, :Wd - 128],
                                                                                 op0=ALU.mult, op1=ALU.add),
                                          reads=[ps, AB], writes=[tm])
                            kb.dve.op(lambda: nc.vector.scalar_tensor_tensor(out=tm[:, Wd - 128:Wd], in0=ps[:, Wd - 128:Wd],
                                                                             scalar=scale, in1=ABD[:, h * 4 + wv_, :],
                                                                             op0=ALU.mult, op1=ALU.add),
                                      reads=[ps, ABD], writes=[tm])
                            kb.act.op(lambda: nc.scalar.activation(out=Pb[:, :Wd], in_=tm[:, :Wd], func=AF.Exp,
                                                                   bias=cbt[:, cbc:cbc + 1], accum_out=rs[:, npc:npc + 1]),
                                      reads=[tm, cbt], writes=[Pb, rs])
                            npc += 1
                        else:
                            kb.dve.op(lambda: nc.vector.scalar_tensor_tensor(out=tm[:, :Wd], in0=ps[:, :Wd], scalar=scale,
                                                                             in1=AB[:, h, :Wd], op0=ALU.mult, op1=ALU.add),
                                      reads=[ps, AB], writes=[tm])
                            kb.act.op(lambda: nc.scalar.activation(out=Pb[:, :Wd], in_=tm[:, :Wd], func=AF.Exp,
                                                                   bias=cbt[:, cbc:cbc + 1], accum_out=rs[:, npc:npc + 1]),
                                      reads=[tm, cbt], writes=[Pb, rs])
                            npc += 1
                        attn_tail(kb, cx, T, Pb, Wd, s0 // 128, Vb, h, po, ti == 0, ti == len(tl) - 1)
                    attn_finish(kb, cx, T, po, rs, npc, yb, h, i)
            kb.act.dma(yTv[1], yb[:], reads=[yb])
            kb.barrier()


C_W = {"wg_ct": [4 * 16, 128, KD, 128], "wb_ct": [4 * 16, 128, 4, 128], "wo_ct": [16, 128, KD, 128],
       "wfi_ct": [88, 128, KD, 128], "wfo_ct": [16, 128, 44, 128], "b_gate": [128, 64], "ln1_g": [128, 16],
       "ln1_b": [128, 16], "ln2_g": [128, 16], "ln2_b": [128, 16]}


def ctl(w, kchunks):
    K_, C_ = w.shape
    return np.ascontiguousarray(w.reshape(kchunks, 128, C_ // 128, 128).transpose(2, 1, 0, 3))


def host_C_weights(inputs, l):
    out = {}
    out["wg_ct"] = np.concatenate([ctl(inputs["w_gate"][l, i], KD) for i in range(4)], axis=0)
    out["wb_ct"] = np.concatenate([ctl(inputs["w_branch"][l, i], 4) for i in range(4)], axis=0)
    out["wo_ct"] = ctl(inputs["w_o"][l], KD)
    out["wfi_ct"] = ctl(inputs["w_ffn_in"][l], KD)
    out["wfo_ct"] = ctl(inputs["w_ffn_out"][l], 44)
    out["b_gate"] = np.ascontiguousarray(np.concatenate([pk(inputs["b_gate"][l, i]) for i in range(4)], axis=1))
    for n in ("ln1_g", "ln1_b", "ln2_g", "ln2_b"):
        out[n] = pk(inputs[n][l])
    return out


PRECAST = False


def precast_weights(kb, cx, W, tag):
    nc = kb.nc
    out = dict(W)
    with ExitStack() as st:
        stg = Ring([kb.sb([128, 16, 128], F32, "pcs", st) for _ in range(3)])
        bfr = Ring([kb.sb([128, 16, 128], BF16, "pcb", st) for _ in range(3)])
        cnt = 0
        for name, n, nk in (("wg_ct", 64, 16), ("wb_ct", 64, 4), ("wo_ct", 16, 16), ("wfi_ct", 88, 16), ("wfo_ct", 16, 44)):
            dst = kb.dram(f"bf_{tag}_{name}", [n, 128, nk, 128], BF16)
            out[name] = dst
            for t in range(n):
                for k0 in range(0, nk, 16):
                    kk = min(16, nk - k0)
                    s_ = stg.next()
                    b_ = bfr.next()
                    kb.sp.dma(s_[:, :kk, :], W[name][t][:, k0:k0 + kk, :], writes=[s_])
                    if cnt % 2 == 0:
                        kb.pool.op(lambda: nc.gpsimd.tensor_copy(out=b_[:, :kk, :], in_=s_[:, :kk, :]), reads=[s_],
                                   writes=[b_])
                    else:
                        kb.dve.op(lambda: nc.vector.tensor_copy(out=b_[:, :kk, :], in_=s_[:, :kk, :]), reads=[s_],
                                  writes=[b_])
                    cnt += 1
                    kb.act.dma(dst[t][:, k0:k0 + kk, :], b_[:, :kk, :], reads=[b_])
        kb.barrier()
    return out


def stage_C(kb, cx, l, xT, uT_d, yT_d, W, S, xoT):
    nc = kb.nc
    P = cx.P
    v3 = lambda ap: ap.rearrange("(k p) n -> p k n", p=128)
    with ExitStack() as st:
        vec = kb.sb([128, 128], F32, "cvec", st)
        kb.sp.dma(vec[:, 0:64], W["b_gate"][:, :], writes=[vec])
        for j, n in enumerate(("ln1_g", "ln1_b", "ln2_g", "ln2_b")):
            kb.sp.dma(vec[:, 64 + 16 * j:80 + 16 * j], W[n][:, :], writes=[vec])
        wst = Ring([kb.sb([128, 16, 128], F32, "cwst", st) for _ in range(2)])
        wbf = Ring([kb.sb([128, 16, 128], BF16, "cwbf", st) for _ in range(3)])
        wfo = kb.sb([128, 44, 128], BF16, "cwfo", st)
        tmp = stat_tmps(kb, st)
        t1r = Ring([kb.sb([128, 512], F32, "t1", st) for _ in range(2)])
        t2r = Ring([kb.sb([128, 512], F32, "t2", st) for _ in range(2)])
        sgr = Ring([kb.sb([128, 512], F32, "sg", st) for _ in range(2)])
        u = kb.sb([128, KD, 512], BF16, "cu", st)
        z = kb.sb([128, KD, 512], F32, "cz", st)
        hb = kb.sb([128, 44, 512], BF16, "chb", st)
        mg = BufK(hb.h, "cmg", 0, 16)
        y = BufK(hb.h, "cy", 16, 16)
        acc = kb.sb([128, 512], F32, "cacc", st)
        xt = Ring([kb.sb([128, 512], F32, "cxt", st) for _ in range(2)])
        psr = Ring([P[2], P[3], P[4], P[5]])

        def wload(src, nk):
            wb = wbf.next()
            if PRECAST:
                kb.sp.dma(wb[:, :nk, :], src, writes=[wb])
                return wb
            s = wst.next()
            kb.sp.dma(s[:, :nk, :], src, writes=[s])
            kb.pool.op(lambda: nc.gpsimd.tensor_copy(out=wb[:, :nk, :], in_=s[:, :nk, :]), reads=[s], writes=[wb])
            return wb

        def mm(ps, wb, nk, rhs_buf, rhs_fn):
            for k in range(nk):
                kb.pe.op(lambda: nc.tensor.matmul(ps[:], lhsT=wb[:, k, :], rhs=rhs_fn(k), start=(k == 0), stop=(k == nk - 1)),
                         reads=[wb, rhs_buf], writes=[ps])

        def layer_norm_to(src, gcol, bcol, dst_fn, dst_buf, also=None):
            rstd, nmr = col_stats(kb, cx, [(src, src[:, k, :]) for k in range(KD)], D, True, tmp)
            for k in range(KD):
                t1 = t1r.next(); t2 = t2r.next()
                kb.dve.op(lambda: nc.vector.tensor_tensor(out=t1[:], in0=src[:, k, :], in1=rstd[:], op=ALU.mult),
                          reads=[src, rstd], writes=[t1])
                kb.pool.op(lambda: nc.gpsimd.tensor_tensor(out=t2[:], in0=t1[:], in1=nmr[:], op=ALU.add),
                           reads=[t1, nmr], writes=[t2])
                kb.act.op(lambda: nc.scalar.activation(out=dst_fn(k), in_=t2[:], func=AF.Identity,
                                                       scale=vec[:, gcol + k:gcol + k + 1], bias=vec[:, bcol + k:bcol + k + 1]),
                          reads=[t2, vec], writes=[dst_buf])

        def adaln_to(src, l_, isc, ish, dst):
            rstd, nmr = col_stats(kb, cx, [(src, src[:, k, :]) for k in range(KD)], D, True, tmp)
            for k in range(KD):
                t1 = t1r.next(); t2 = t2r.next()
                kb.dve.op(lambda: nc.vector.tensor_tensor(out=t1[:], in0=src[:, k, :], in1=rstd[:], op=ALU.mult),
                          reads=[src, rstd], writes=[t1])
                kb.pool.op(lambda: nc.gpsimd.tensor_tensor(out=t2[:], in0=t1[:], in1=nmr[:], op=ALU.add),
                           reads=[t1, nmr], writes=[t2])
                kb.act.op(lambda: nc.scalar.activation(out=dst[:, k, :], in_=t2[:], func=AF.Identity,
                                                       scale=cx.onep[:, mcol(l_, isc, k):mcol(l_, isc, k) + 1],
                                                       bias=cx.mod[:, mcol(l_, ish, k):mcol(l_, ish, k) + 1]),
                          reads=[t2, cx.onep, cx.mod], writes=[dst])

        for tt in range(4):
            ts = slice(tt * 512, (tt + 1) * 512)
            kb.barrier()
            kb.sp.dma(u[:], v3(uT_d)[:, :, ts], writes=[u])
            kb.sp.dma(y[:], v3(yT_d)[:, :, ts], writes=[y])
            for j in range(16):
                for i in range(4):
                    wg = wload(W["wg_ct"][i * 16 + j], KD)
                    wbr = wload(W["wb_ct"][i * 16 + j], 4)
                    pg = psr.next(); pb = psr.next()
                    mm(pg, wg, KD, u, lambda k: u[:, k, :])
                    mm(pb, wbr, 4, y, lambda k: y[:, i * 4 + k, :])
                    sg = sgr.next()
                    kb.act.op(lambda: nc.scalar.activation(out=sg[:], in_=pg[:], func=AF.Sigmoid,
                                                           bias=vec[:, i * 16 + j:i * 16 + j + 1], scale=1.0),
                              reads=[pg, vec], writes=[sg])
                    if i == 0:
                        kb.dve.op(lambda: nc.vector.tensor_tensor(out=acc[:], in0=pb[:], in1=sg[:], op=ALU.mult),
                                  reads=[pb, sg], writes=[acc])
                    else:
                        t1 = t1r.next()
                        kb.dve.op(lambda: nc.vector.tensor_tensor(out=t1[:], in0=pb[:], in1=sg[:], op=ALU.mult),
                                  reads=[pb, sg], writes=[t1])
                        if i < 3:
                            kb.pool.op(lambda: nc.gpsimd.tensor_tensor(out=acc[:], in0=acc[:], in1=t1[:], op=ALU.add),
                                       reads=[acc, t1], writes=[acc])
                        else:
                            kb.pool.op(lambda: nc.gpsimd.tensor_tensor(out=mg[:, j, :], in0=acc[:], in1=t1[:], op=ALU.add),
                                       reads=[acc, t1], writes=[mg])
            for j in range(16):
                wo = wload(W["wo_ct"][j], KD)
                ps = psr.next()
                mm(ps, wo, KD, mg, lambda k: mg[:, k, :])
                x_ = xt.next()
                kb.sp.dma(x_[:], xT[j * 128:(j + 1) * 128, ts], writes=[x_])
                t1 = t1r.next()
                kb.act.op(lambda: nc.scalar.activation(out=t1[:], in_=ps[:], func=AF.Copy,
                                                       scale=cx.onep[:, mcol(l, 2, j):mcol(l, 2, j) + 1]),
                          reads=[ps, cx.onep], writes=[t1])
                kb.dve.op(lambda: nc.vector.scalar_tensor_tensor(out=z[:, j, :], in0=x_[:], scalar=ALPHA, in1=t1[:],
                                                                 op0=ALU.mult, op1=ALU.add), reads=[x_, t1], writes=[z])
            layer_norm_to(z, 64, 80, lambda k: z[:, k, :], z)
            adaln_to(z, l, 4, 3, u)
            for j in range(44):
                wa = wload(W["wfi_ct"][j], KD)
                wg_ = wload(W["wfi_ct"][44 + j], KD)
                pa = psr.next(); pg = psr.next()
                mm(pa, wa, KD, u, lambda k: u[:, k, :])
                mm(pg, wg_, KD, u, lambda k: u[:, k, :])
                sg = sgr.next()
                kb.act.op(lambda: nc.scalar.activation(out=sg[:], in_=pa[:], func=AF.Silu), reads=[pa], writes=[sg])
                kb.dve.op(lambda: nc.vector.tensor_tensor(out=hb[:, j, :], in0=pg[:], in1=sg[:], op=ALU.mult),
                          reads=[pg, sg], writes=[hb])
            for j in range(16):
                if PRECAST:
                    kb.sp.dma(wfo[:], W["wfo_ct"][j], writes=[wfo])
                for (k0_, nk_) in (() if PRECAST else ((0, 16), (16, 16), (32, 12))):
                    s_ = wst.next()
                    kb.sp.dma(s_[:, :nk_, :], W["wfo_ct"][j][:, k0_:k0_ + nk_, :], writes=[s_])
                    kb.pool.op(lambda: nc.gpsimd.tensor_copy(out=wfo[:, k0_:k0_ + nk_, :], in_=s_[:, :nk_, :]),
                               reads=[s_], writes=[wfo])
                ps = psr.next()
                mm(ps, wfo, 44, hb, lambda k: hb[:, k, :])
                t1 = t1r.next()
                kb.act.op(lambda: nc.scalar.activation(out=t1[:], in_=ps[:], func=AF.Copy,
                                                       scale=cx.onep[:, mcol(l, 5, j):mcol(l, 5, j) + 1]),
                          reads=[ps, cx.onep], writes=[t1])
                kb.dve.op(lambda: nc.vector.scalar_tensor_tensor(out=z[:, j, :], in0=z[:, j, :], scalar=ALPHA, in1=t1[:],
                                                                 op0=ALU.mult, op1=ALU.add), reads=[z, t1], writes=[z])
            layer_norm_to(z, 96, 112, lambda k: z[:, k, :], z)
            kb.act.dma(v3(xoT)[:, :, ts], z[:], reads=[z])
        kb.barrier()


def _launch(nc, in_maps):
    res = run_bass_kernel_spmd(nc, in_maps, core_ids=list(range(8)))
    return res.results


def build_B(l):
    kb = KB()
    modT = kb.dram("modT", [128, DEPTH * 96], F32, kind="ExternalInput")
    A = {k: kb.dram(k, v[0], dts(v[1]), kind="ExternalInput") for k, v in A_OUT.items() if k != "uT"}
    PV = {k: kb.dram(k, v[0], dts(v[1]), kind="ExternalInput") for k, v in B_PREV.items()}
    W = {k: kb.dram(k, v, F32, kind="ExternalInput") for k, v in B_W.items()}
    C = {k: kb.dram(k, v, F32, kind="ExternalInput") for k, v in B_C.items()}
    yT = kb.dram("yT", [2048, TOK], BF16, kind="ExternalOutput")
    cx = setup_common(kb, modT)
    stage_B(kb, cx, l, A, PV, W, C, yT)
    kb.finish()
    return kb.nc


def build_C(l):
    kb = KB()
    modT = kb.dram("modT", [128, DEPTH * 96], F32, kind="ExternalInput")
    xT = kb.dram("xT", [D, TOK], F32, kind="ExternalInput")
    uT = kb.dram("uT", [D, TOK], BF16, kind="ExternalInput")
    yT = kb.dram("yT", [2048, TOK], BF16, kind="ExternalInput")
    W = {k: kb.dram(k, v, F32, kind="ExternalInput") for k, v in C_W.items()}
    xo = kb.dram("xo", [D, TOK], F32, kind="ExternalOutput")
    cx = setup_common(kb, modT)
    stage_C(kb, cx, l, xT, uT, yT, W, {}, xo)
    kb.finish()
    return kb.nc


def kernel_unfused(**inputs):
    inputs = {k: np.asarray(v) for k, v in inputs.items()}
    x = inputs["x"]
    mods = run_mods(inputs)
    xT = []
    for core in range(8):
        b, h = core // 2, core % 2
        xT.append(np.ascontiguousarray(x[b, h * TOK:(h + 1) * TOK, :].T))
    for l in range(DEPTH):
        wA = [host_A_weights(inputs, l, h) for h in range(2)]
        in_maps = []
        for core in range(8):
            b, h = core // 2, core % 2
            m = {"xT": xT[core], "modT": mods[b]}
            m.update(wA[h])
            in_maps.append(m)
        ra = _launch(build_A(l), in_maps)
        wB = host_B_weights(inputs, l)
        cB = [host_B_consts(h) for h in range(2)]
        in_maps = []
        pm = {"pdkT": "dkT", "pdV": "dV", "pikT": "ikT", "pknT": "knT", "pmV": "mV", "pkrT": "krT"}
        for core in range(8):
            b, h = core // 2, core % 2
            own = ra[core]
            m = {"modT": mods[b]}
            for k in A_OUT:
                if k != "uT":
                    m[k] = np.asarray(own[k])
            if h == 1:
                prev = ra[core - 1]
                for k, v in pm.items():
                    m[k] = np.asarray(prev[v])
                m["pz"] = np.ascontiguousarray(np.asarray(prev["zT"])[:, -32:])
                m["php"] = np.ascontiguousarray(np.asarray(prev["hpT"])[:, -16:])
            else:
                for k, v in pm.items():
                    m[k] = np.zeros_like(np.asarray(own[v]))
                m["pz"] = np.zeros_like(np.asarray(own["zT"])[:, -32:])
                m["php"] = np.zeros((512, 16), np.float32)
            m.update(wB)
            m.update(cB[h])
            in_maps.append(m)
        rb = _launch(build_B(l), in_maps)
        wC = host_C_weights(inputs, l)
        in_maps = []
        for core in range(8):
            b = core // 2
            m = {"modT": mods[b], "xT": xT[core], "uT": np.asarray(ra[core]["uT"]), "yT": np.asarray(rb[core]["yT"])}
            m.update(wC)
            in_maps.append(m)
        rc = _launch(build_C(l), in_maps)
        xT = [np.asarray(rc[core]["xo"]) for core in range(8)]
    out = np.empty((NB, SEQ, D), np.float32)
    for core in range(8):
        b, h = core // 2, core % 2
        out[b, h * TOK:(h + 1) * TOK, :] = xT[core].T
    return out


def stage_M(kb, cx, cT, wadas, baT):
    nc = kb.nc
    with ExitStack() as st:
        csb = kb.sb([128, KD, 4], F32, "csb", st)
        cact = kb.sb([128, KD, 4], F32, "cact", st)
        basb = kb.sb([128, DEPTH * 96], F32, "basb", st)
        wr = Ring([kb.sb([128, KD, 512], F32, "wst", st) for _ in range(2)])
        pr = Ring([cx.P[2], cx.P[3]])
        kb.sp.dma(csb[:], cT[:, :, :], writes=[csb])
        kb.sp.dma(basb[:], baT[:, :], writes=[basb])
        kb.act.op(lambda: nc.scalar.activation(out=cact[:], in_=csb[:], func=AF.Silu), reads=[csb], writes=[cact])
        for l in range(DEPTH):
            wav = wadas[l].rearrange("(k p) n -> p k n", p=128)
            for g in range(24):
                w = wr.next()
                kb.sp.dma(w[:], wav[:, :, g * 512:(g + 1) * 512], writes=[w])
                for j in range(4):
                    t = l * 96 + g * 4 + j
                    p = pr.next()
                    for k in range(KD):
                        kb.pe.op(lambda: nc.tensor.matmul(p[:, 0:4], lhsT=w[:, k, j * 128:(j + 1) * 128], rhs=cact[:, k, :],
                                                          start=(k == 0), stop=(k == KD - 1)),
                                 reads=[w, cact], writes=[p])
                    kb.dve.op(lambda: nc.vector.tensor_scalar(out=cx.mod[:, t:t + 1], in0=p[:, 0:1],
                                                              scalar1=basb[:, t:t + 1], scalar2=None, op0=ALU.add),
                              reads=[p, basb], writes=[cx.mod])
        kb.dve.op(lambda: nc.vector.tensor_scalar(out=cx.onep[:], in0=cx.mod[:], scalar1=1.0, scalar2=None, op0=ALU.add),
                  reads=[cx.mod], writes=[cx.onep])
        kb.barrier()


A_WS = {k: v for k, v in A_W.items() if k not in ("ropec", "ropes")}
ROPE = {"ropec": [64, TOK], "ropes": [64, TOK]}


def build_fused():
    global PRECAST
    PRECAST = True
    kb = KB()
    ext = lambda n, shp, dt=F32: kb.dram(n, shp, dt, kind="ExternalInput")
    xT = ext("xT", [D, SEQ])
    cT = ext("cT", [128, KD, 4])
    wadas = [ext(f"w_ada{l}", [D, 6 * D]) for l in range(DEPTH)]
    baT = ext("baT", [128, DEPTH * 96])
    z32 = ext("z32", [512, 32], BF16)
    z16 = ext("z16", [512, 16])
    rope = [{k: ext(f"{k}_h{h}", v) for k, v in ROPE.items()} for h in range(2)]
    BC = [{k: ext(f"{k}_h{h}", v) for k, v in B_C.items()} for h in range(2)]
    WA = [{k: ext(f"L{l}_{k}", v) for k, v in A_WS.items()} for l in range(DEPTH)]
    WB = [{k: ext(f"L{l}_{k}", v) for k, v in B_W.items()} for l in range(DEPTH)]
    WC = [{k: ext(f"L{l}_{k}", v) for k, v in C_W.items()} for l in range(DEPTH)]
    xo = kb.dram("xo", [D, SEQ], F32, kind="ExternalOutput")
    x1 = kb.dram("x1", [D, SEQ], F32)
    cx = setup_common(kb, None)
    stage_M(kb, cx, cT, wadas, baT)
    S = {k: kb.dram(f"S_{k}", v, F32) for k, v in A_SCR.items()}
    for l in range(DEPTH):
        xin = xT if l == 0 else x1
        xout = x1 if l == 0 else xo
        AO = [{k: kb.dram(f"A{l}{h}_{k}", v[0], dts(v[1])) for k, v in A_OUT.items()} for h in range(2)]
        yT = [kb.dram(f"y{l}{h}", [2048, TOK], BF16) for h in range(2)]
        for h in range(2):
            W = dict(WA[l])
            W.update(rope[h])
            stage_A(kb, cx, l, xin[:, h * TOK:(h + 1) * TOK], W, AO[h], S)
        for h in range(2):
            pm = {"pdkT": "dkT", "pdV": "dV", "pikT": "ikT", "pknT": "knT", "pmV": "mV", "pkrT": "krT"}
            PV = {k: AO[0][v] for k, v in pm.items()}
            if h == 1:
                PV["pz"] = AO[0]["zT"][:, TOK - 32:TOK]
                PV["php"] = AO[0]["hpT"][:, TOK - 16:TOK]
            else:
                PV["pz"] = z32
                PV["php"] = z16
            stage_B(kb, cx, l, AO[h], PV, WB[l], BC[h], yT[h], with_prev=(h == 1))
        WCb = precast_weights(kb, cx, WC[l], f"L{l}")
        for h in range(2):
            stage_C(kb, cx, l, xin[:, h * TOK:(h + 1) * TOK], AO[h]["uT"], yT[h], WCb, {},
                    xout[:, h * TOK:(h + 1) * TOK])
    kb.finish()
    return kb.nc


def kernel(**inputs):
    import ml_dtypes
    inputs = {k: np.asarray(v) for k, v in inputs.items()}
    x = inputs["x"]
    c = inputs["c"]
    shared = {"z32": np.zeros((512, 32), ml_dtypes.bfloat16), "z16": np.zeros((512, 16), np.float32)}
    for l in range(DEPTH):
        shared[f"w_ada{l}"] = np.ascontiguousarray(inputs["w_ada"][l])
    shared["baT"] = np.ascontiguousarray(np.concatenate([pk(inputs["b_ada"][l]) for l in range(DEPTH)], axis=1))
    for h in range(2):
        for k, v in host_B_consts(h, skip_prev=True).items():
            shared[f"{k}_h{h}"] = v
    for l in range(DEPTH):
        wa = [host_A_weights(inputs, l, h) for h in range(2)]
        for k in A_WS:
            shared[f"L{l}_{k}"] = wa[0][k]
        if l == 0:
            for h in range(2):
                for k in ROPE:
                    shared[f"{k}_h{h}"] = wa[h][k]
        for k, v in host_B_weights(inputs, l).items():
            shared[f"L{l}_{k}"] = v
        for k, v in host_C_weights(inputs, l).items():
            shared[f"L{l}_{k}"] = v
    in_maps = []
    for core in range(8):
        b = core // 2
        m = dict(shared)
        m["xT"] = np.ascontiguousarray(x[b].T)
        m["cT"] = np.ascontiguousarray(np.repeat(c[b].reshape(KD, 128).T[:, :, None], 4, axis=2))
        in_maps.append(m)
    res = run_bass_kernel_spmd(build_fused(), in_maps, core_ids=list(range(8)))
    out = np.empty((NB, SEQ, D), np.float32)
    for b in range(NB):
        out[b] = np.asarray(res.results[2 * b]["xo"]).T
    return out
```

```python
import numpy as np
from contextlib import ExitStack
import concourse.bass as bass
import concourse.mybir as mybir
from concourse.bass_utils import run_bass_kernel_spmd

F32 = mybir.dt.float32
BF16 = mybir.dt.bfloat16
AF = mybir.ActivationFunctionType
ALU = mybir.AluOpType
AX = mybir.AxisListType

SAME_ENGINE_SYNC = True


class Buf:
    def __init__(self, handle, name):
        self.h = handle
        self.name = name
        self.w = {}
        self.r = {}

    def __getitem__(self, idx):
        return self.h[idx]


class BufV(Buf):
    def __init__(self, handle, name, off, width):
        super().__init__(handle, name)
        self.off = off
        self.width = width

    def __getitem__(self, idx):
        if not isinstance(idx, tuple):
            idx = (idx, slice(None))
        p, c = idx
        a = 0 if c.start is None else c.start
        b = self.width if c.stop is None else c.stop
        return self.h[p, self.off + a:self.off + b]


class BufK(Buf):
    def __init__(self, handle, name, k0, nk):
        super().__init__(handle, name)
        self.k0 = k0
        self.nk = nk

    def __getitem__(self, idx):
        if not isinstance(idx, tuple):
            return self.h[idx, self.k0:self.k0 + self.nk, :]
        p, k = idx[0], idx[1]
        n = idx[2] if len(idx) > 2 else slice(None)
        if isinstance(k, slice):
            a = 0 if k.start is None else k.start
            b = self.nk if k.stop is None else k.stop
            return self.h[p, self.k0 + a:self.k0 + b, n]
        return self.h[p, self.k0 + k, n]


class Eng:
    def __init__(self, kb, name, eng, ndma=0):
        self.kb = kb
        self.name = name
        self.e = eng
        self.sem = kb.newsem("c_" + name)
        self.count = 0
        self.seen = {}
        self.dsems = [kb.newsem(f"d_{name}{i}") for i in range(ndma)]
        self.dcount = 0

    def wait(self, sem, val):
        if val <= 0:
            return
        if sem is self.sem and (not SAME_ENGINE_SYNC or self.name == "pe"):
            return
        if self.seen.get(id(sem), 0) >= val:
            return
        self.e.wait_ge(sem, val)
        self.seen[id(sem)] = val

    def _deps(self, reads, writes):
        for b in reads:
            for sem, val in b.w.values():
                self.wait(sem, val)
        for b in writes:
            for sem, val in b.w.values():
                if sem is not self.sem:
                    self.wait(sem, val)
            for sem, val in b.r.values():
                if sem is not self.sem:
                    self.wait(sem, val)

    def _mark(self, reads, writes, ev):
        for b in reads:
            b.r[id(ev[0])] = ev
        for b in writes:
            b.w = {id(ev[0]): ev}
            b.r = {}

    def op(self, ins_fn, reads=(), writes=(), signal=True):
        signal = True
        self._deps(reads, writes)
        ins = ins_fn()
        if signal:
            self.count += 1
            ins.then_inc(self.sem, 1)
        ev = (self.sem, self.count if signal else self.count + 1)
        self._mark(reads, writes, ev)
        return ins

    def mark_only(self, reads, writes):
        ev = (self.sem, self.count + 1)
        self._mark(reads, writes, ev)

    def dma(self, out, in_, reads=(), writes=(), **kw):
        n = len(self.dsems)
        i = self.dcount
        j = i % n
        sem = self.dsems[j]
        if i >= n:
            self.wait(sem, 16 * (i // n))
        self._deps(reads, writes)
        ins = self.e.dma_start(out=out, in_=in_, **kw)
        ins.then_inc(sem, 16)
        self.dcount += 1
        ev = (sem, 16 * (i // n + 1))
        self._mark(reads, writes, ev)
        return ev


class KB:
    def __init__(self):
        self.nc = bass.Bass("TRN2", target_bir_lowering=False)
        self.es = ExitStack()
        self.sems = []
        nc = self.nc
        self.pe = Eng(self, "pe", nc.tensor)
        self.act = Eng(self, "act", nc.scalar, ndma=4)
        self.dve = Eng(self, "dve", nc.vector)
        self.pool = Eng(self, "pool", nc.gpsimd, ndma=4)
        self.sp = Eng(self, "sp", nc.sync, ndma=8)
        self.engs = [self.pe, self.act, self.dve, self.pool, self.sp]
        self.uid = 0

    def newsem(self, name):
        s = self.es.enter_context(self.nc.semaphore(name))
        self.sems.append(s)
        return s

    def dram(self, name, shape, dt, kind="Internal"):
        return self.nc.dram_tensor(name, list(shape), dt, kind=kind).ap()

    def sb(self, shape, dt, name=None, stack=None):
        self.uid += 1
        name = f"{name or 't'}_{self.uid}"
        h = (stack or self.es).enter_context(self.nc.sbuf_tensor(name, list(shape), dt))
        return Buf(h, name)

    def ps(self, shape, dt=F32, name=None, stack=None):
        self.uid += 1
        name = f"{name or 'p'}_{self.uid}"
        h = (stack or self.es).enter_context(self.nc.psum_tensor(name, list(shape), dt))
        return Buf(h, name)

    def barrier(self):
        evs = []
        for g in self.engs:
            if g.count > 0:
                evs.append((g.sem, g.count))
            n = len(g.dsems)
            for j in range(n):
                cnt = (g.dcount - j + n - 1) // n if g.dcount > j else 0
                if cnt > 0:
                    evs.append((g.dsems[j], 16 * cnt))
        for g in self.engs:
            for sem, val in evs:
                g.wait(sem, val)

    def finish(self):
        self.barrier()
        self.es.close()


class Ring:
    def __init__(self, bufs):
        self.bufs = bufs
        self.i = 0

    def next(self):
        b = self.bufs[self.i % len(self.bufs)]
        self.i += 1
        return b


D = 2048
KD = 16
SEQ = 4096
NB = 4
TOK = 2048
DEPTH = 2
FFN = 5632
EPS = 1e-5
ALPHA = (2 * DEPTH) ** 0.25
SEGS = [("hp", 512), ("dq", 512), ("dk", 512), ("dv", 512), ("iq", 1024), ("ik", 64), ("iw", 16),
        ("cq", 384), ("ckv", 256), ("kr", 64), ("hc", 1024)]
SEG_OFF = {}
_o = 0
for _n, _s in SEGS:
    SEG_OFF[_n] = _o
    _o += _s
FM_SEGS = ["hp", "dq", "dk", "iq", "ik", "cq", "ckv", "kr", "krs", "hc"]
FM_DT = {"hp": "f32", "dq": "bf", "dk": "bf", "iq": "bf", "ik": "bf", "cq": "f32", "ckv": "f32", "kr": "f32",
         "krs": "f32", "hc": "f32"}
FM_ROWS = {"hp": 512, "dq": 512, "dk": 512, "iq": 1024, "ik": 64, "cq": 384, "ckv": 256, "kr": 64, "krs": 64,
           "hc": 1024}
CTS = []
for _n in FM_SEGS:
    _r = FM_ROWS[_n]
    for _c in range(0, _r, 128):
        CTS.append((_n, _c, min(128, _r - _c)))
NCT = len(CTS)


def pk(v):
    v = np.asarray(v)
    return np.ascontiguousarray(v.reshape(-1, 128).T)


def dts(s):
    return F32 if s == "f32" else BF16


def build_mods():
    kb = KB()
    nc = kb.nc
    NCOL = 2 * 6 * D // 8
    NT = NCOL // 128
    wa = kb.dram("wa", [D, NCOL], F32, kind="ExternalInput")
    ba = kb.dram("ba", [128, NT], F32, kind="ExternalInput")
    cT = kb.dram("cT", [128, KD, NB], F32, kind="ExternalInput")
    modT = kb.dram("modT", [128, NT, NB], F32, kind="ExternalOutput")
    csb = kb.sb([128, KD, NB], F32, "csb")
    cact = kb.sb([128, KD, NB], F32, "cact")
    basb = kb.sb([128, NT], F32, "basb")
    msb = kb.sb([128, NT, NB], F32, "msb")
    wr = Ring([kb.sb([128, KD, 512], F32, "wst") for _ in range(2)])
    pr = Ring([kb.ps([128, 512], F32, "ps") for _ in range(2)])
    kb.sp.dma(csb[:], cT[:, :, :], writes=[csb])
    kb.sp.dma(basb[:], ba[:, :], writes=[basb])
    kb.act.op(lambda: nc.scalar.activation(out=cact[:], in_=csb[:], func=AF.Silu), reads=[csb], writes=[cact])
    wav = wa.rearrange("(k p) n -> p k n", p=128)
    for g in range(NCOL // 512):
        w = wr.next()
        kb.sp.dma(w[:], wav[:, :, g * 512:(g + 1) * 512], writes=[w])
        for j in range(4):
            t = g * 4 + j
            p = pr.next()
            for k in range(KD):
                kb.pe.op(lambda: nc.tensor.matmul(p[:, 0:NB], lhsT=w[:, k, j * 128:(j + 1) * 128], rhs=cact[:, k, :],
                                                  start=(k == 0), stop=(k == KD - 1)),
                         reads=[w, cact], writes=[p], signal=(k == KD - 1))
            kb.dve.op(lambda: nc.vector.tensor_scalar(out=msb[:, t, :], in0=p[:, 0:NB], scalar1=basb[:, t:t + 1],
                                                      scalar2=None, op0=ALU.add),
                      reads=[p, basb], writes=[msb])
    kb.sp.dma(modT[:, :, :], msb[:], reads=[msb])
    kb.finish()
    return nc


def run_mods(inputs):
    w_ada = inputs["w_ada"]
    b_ada = inputs["b_ada"]
    c = inputs["c"]
    wcat = np.concatenate([w_ada[0], w_ada[1]], axis=1)
    bcat = np.concatenate([b_ada[0], b_ada[1]], axis=0)
    cT = np.ascontiguousarray(c.reshape(NB, KD, 128).transpose(2, 1, 0))
    NCOL = 3072
    in_maps = []
    for core in range(8):
        sl = slice(core * NCOL, (core + 1) * NCOL)
        in_maps.append({"wa": np.ascontiguousarray(wcat[:, sl]), "ba": pk(bcat[sl]), "cT": cT})
    nc = build_mods()
    res = run_bass_kernel_spmd(nc, in_maps, core_ids=list(range(8)))
    mt = np.concatenate([r["modT"] for r in res.results], axis=1)
    return [np.ascontiguousarray(mt[:, :, b]) for b in range(NB)]


class Ctx:
    pass


def setup_common(kb, modT_d):
    nc = kb.nc
    cx = Ctx()
    cx.P = [kb.ps([128, 512], F32, f"P{i}") for i in range(6)]
    cx.PB = []
    for i in range(2):
        big = kb.ps([128, 1024], BF16, f"PBB{i}")
        cx.PB += [BufV(big.h, f"PB{2 * i}", 0, 512), BufV(big.h, f"PB{2 * i + 1}", 512, 512)]
    cx.ones_f = kb.sb([128, 128], F32, "ones_f")
    cx.ones_b = kb.sb([128, 128], BF16, "ones_b")
    cx.eps = kb.sb([128, 1], F32, "eps")
    cx.mod = kb.sb([128, DEPTH * 96], F32, "mod")
    cx.onep = kb.sb([128, DEPTH * 96], F32, "onep")
    kb.pool.op(lambda: nc.gpsimd.memset(cx.ones_f[:], 1.0), writes=[cx.ones_f])
    kb.pool.op(lambda: nc.gpsimd.memset(cx.ones_b[:], 1.0), writes=[cx.ones_b])
    kb.pool.op(lambda: nc.gpsimd.memset(cx.eps[:], EPS), writes=[cx.eps])
    if modT_d is not None:
        kb.sp.dma(cx.mod[:], modT_d[:, :], writes=[cx.mod])
        kb.dve.op(lambda: nc.vector.tensor_scalar(out=cx.onep[:], in0=cx.mod[:], scalar1=1.0, scalar2=None, op0=ALU.add),
                  reads=[cx.mod], writes=[cx.onep])
    return cx


def mcol(l, i, k):
    return (l * 6 + i) * 16 + k


def col_stats(kb, cx, chunks, nfeat, want_mean, tmp, N=512):
    nc = kb.nc
    ps_s, ps_q = cx.P[0], cx.P[1]
    n = len(chunks)
    for k, (b, ap) in enumerate(chunks):
        rows = ap.shape[0]
        sq = tmp["sq"].next()
        kb.act.op(lambda: nc.scalar.activation(out=sq[:rows, :N], in_=ap, func=AF.Square), reads=[b], writes=[sq])
        kb.pe.op(lambda: nc.tensor.matmul(ps_q[:, :N], lhsT=cx.ones_f[:rows, :], rhs=sq[:rows, :N], start=(k == 0),
                                          stop=(k == n - 1)), reads=[sq, cx.ones_f], writes=[ps_q])
        if want_mean:
            kb.pe.op(lambda: nc.tensor.matmul(ps_s[:, :N], lhsT=cx.ones_f[:rows, :], rhs=ap, start=(k == 0),
                                              stop=(k == n - 1)), reads=[b, cx.ones_f], writes=[ps_s])
    inv = 1.0 / nfeat
    var, rstd = tmp["var"], tmp["rstd"]
    if want_mean:
        mean, msq, nmr = tmp["mean"], tmp["msq"], tmp["nmr"]
        kb.dve.op(lambda: nc.vector.tensor_scalar(out=mean[:, :N], in0=ps_s[:, :N], scalar1=inv, scalar2=None,
                                                  op0=ALU.mult), reads=[ps_s], writes=[mean])
        kb.dve.op(lambda: nc.vector.tensor_tensor(out=msq[:, :N], in0=mean[:, :N], in1=mean[:, :N], op=ALU.mult),
                  reads=[mean], writes=[msq])
        kb.dve.op(lambda: nc.vector.scalar_tensor_tensor(out=var[:, :N], in0=ps_q[:, :N], scalar=inv, in1=msq[:, :N],
                                                         op0=ALU.mult, op1=ALU.subtract),
                  reads=[ps_q, msq], writes=[var])
        kb.act.op(lambda: nc.scalar.activation(out=var[:, :N], in_=var[:, :N], func=AF.Sqrt, bias=cx.eps[:, 0:1],
                                               scale=1.0), reads=[var, cx.eps], writes=[var])
    else:
        kb.act.op(lambda: nc.scalar.activation(out=var[:, :N], in_=ps_q[:, :N], func=AF.Sqrt, bias=cx.eps[:, 0:1],
                                               scale=inv), reads=[ps_q, cx.eps], writes=[var])
    kb.dve.op(lambda: nc.vector.reciprocal(out=rstd[:, :N], in_=var[:, :N]), reads=[var], writes=[rstd])
    if want_mean:
        kb.dve.op(lambda: nc.vector.scalar_tensor_tensor(out=nmr[:, :N], in0=mean[:, :N], scalar=-1.0,
                                                         in1=rstd[:, :N], op0=ALU.mult, op1=ALU.mult),
                  reads=[mean, rstd], writes=[nmr])
        return rstd, nmr
    return rstd, None


def stat_tmps(kb, st, N=512):
    t = {"sq": Ring([kb.sb([128, N], F32, "sq", st) for _ in range(2)])}
    for nm in ("mean", "msq", "var", "rstd", "nmr"):
        t[nm] = kb.sb([128, N], F32, nm, st)
    return t


def load_cast(kb, dst, dst_ap, src_ap, stg_ring, stg_view):
    nc = kb.nc
    s = stg_ring.next()
    kb.sp.dma(stg_view(s), src_ap, writes=[s])
    kb.pool.op(lambda: nc.gpsimd.tensor_copy(out=dst_ap, in_=stg_view(s)), reads=[s], writes=[dst])


def evac(kb, i, out_buf, out_ap, ps_buf, ps_ap):
    nc = kb.nc
    if i % 2 == 0:
        kb.act.op(lambda: nc.scalar.copy(out=out_ap, in_=ps_ap), reads=[ps_buf], writes=[out_buf])
    else:
        kb.dve.op(lambda: nc.vector.tensor_copy(out=out_ap, in_=ps_ap), reads=[ps_buf], writes=[out_buf])


def stage_A(kb, cx, l, xT, W, O, S):
    nc = kb.nc
    P = cx.P
    xTv = xT.rearrange("(k p) n -> p k n", p=128)
    with ExitStack() as st:
        uT = [kb.sb([128, KD, 512], BF16, f"uT{t}", st) for t in range(4)]
        with ExitStack() as s1:
            xr = Ring([kb.sb([128, KD, 512], F32, "xt", s1) for _ in range(2)])
            tmp = stat_tmps(kb, s1)
            t1r = Ring([kb.sb([128, 512], F32, "t1", s1) for _ in range(2)])
            t2r = Ring([kb.sb([128, 512], F32, "t2", s1) for _ in range(2)])
            for tt in range(4):
                xt = xr.next()
                kb.sp.dma(xt[:], xTv[:, :, tt * 512:(tt + 1) * 512], writes=[xt])
                rstd, nmr = col_stats(kb, cx, [(xt, xt[:, k, :]) for k in range(KD)], D, True, tmp)
                for k in range(KD):
                    t1 = t1r.next()
                    t2 = t2r.next()
                    kb.dve.op(lambda: nc.vector.tensor_tensor(out=t1[:], in0=xt[:, k, :], in1=rstd[:], op=ALU.mult),
                              reads=[xt, rstd], writes=[t1])
                    kb.pool.op(lambda: nc.gpsimd.tensor_tensor(out=t2[:], in0=t1[:], in1=nmr[:], op=ALU.add),
                               reads=[t1, nmr], writes=[t2])
                    kb.act.op(lambda: nc.scalar.activation(out=uT[tt][:, k, :], in_=t2[:], func=AF.Identity,
                                                           scale=cx.onep[:, mcol(l, 1, k):mcol(l, 1, k) + 1],
                                                           bias=cx.mod[:, mcol(l, 0, k):mcol(l, 0, k) + 1]),
                              reads=[t2, cx.onep, cx.mod], writes=[uT[tt]])
                kb.act.dma(O["uT"].rearrange("(k p) n -> p k n", p=128)[:, :, tt * 512:(tt + 1) * 512], uT[tt][:],
                           reads=[uT[tt]])
            kb.barrier()
        with ExitStack() as s2:
            wst = Ring([kb.sb([128, KD, 128], F32, "wst", s2) for _ in range(2)])
            wbf = Ring([kb.sb([128, KD, 128], BF16, "wbf", s2) for _ in range(2)])
            obf = Ring([kb.sb([128, TOK], BF16, "obf", s2) for _ in range(2)])
            of32 = Ring([kb.sb([128, TOK], F32, "of32", s2) for _ in range(2)])
            psr = Ring([P[2], P[3], P[4], P[5]])
            dest = {"hp": O["hpT"], "dq": O["dqT"], "dk": O["dkT"], "iq": O["iqT"], "ik": O["ikT"], "cq": S["cqT"],
                    "ckv": S["ckvT"], "kr": S["krraw"], "krs": S["krsraw"], "hc": S["hcT"]}
            ei = 0
            for ct, (seg, c0, ncols) in enumerate(CTS):
                wb = wbf.next()
                load_cast(kb, wb, wb[:], W["win_ct"][ct], wst, lambda s: s[:])
                ob = (of32 if FM_DT[seg] == "f32" else obf).next()
                for tt in range(4):
                    ps = psr.next()
                    for k in range(KD):
                        kb.pe.op(lambda: nc.tensor.matmul(ps[:], lhsT=wb[:, k, :], rhs=uT[tt][:, k, :],
                                                          start=(k == 0), stop=(k == KD - 1)),
                                 reads=[wb, uT[tt]], writes=[ps])
                    evac(kb, ei, ob, ob[:ncols, tt * 512:(tt + 1) * 512], ps, ps[:ncols, :])
                    ei += 1
                kb.act.dma(dest[seg][c0:c0 + ncols, :], ob[:ncols, :], reads=[ob])
            wv = kb.sb([128, KD, 512], BF16, "wv", s2)
            wiw = kb.sb([128, KD, 128], BF16, "wiw", s2)
            for j in range(4):
                load_cast(kb, wv, wv[:, :, j * 128:(j + 1) * 128], W["wdv_ct"][j], wst, lambda s: s[:])
            load_cast(kb, wiw, wiw[:], W["wiw_ct"][0], wst, lambda s: s[:])
            vob = Ring([kb.sb([128, 512], BF16, "vob", s2) for _ in range(2)])
            iwo = kb.sb([128, 16, 16], F32, "iwo", s2)
            for tb in range(16):
                tt, off = tb // 4, (tb % 4) * 128
                ps = psr.next()
                for k in range(KD):
                    kb.pe.op(lambda: nc.tensor.matmul(ps[:], lhsT=uT[tt][:, k, off:off + 128], rhs=wv[:, k, :],
                                                      start=(k == 0), stop=(k == KD - 1)),
                             reads=[wv, uT[tt]], writes=[ps])
                vo = vob.next()
                evac(kb, tb, vo, vo[:], ps, ps[:])
                kb.act.dma(O["dV"][tb * 128:(tb + 1) * 128, :], vo[:], reads=[vo])
                ps2 = psr.next()
                for k in range(KD):
                    kb.pe.op(lambda: nc.tensor.matmul(ps2[:, 0:16], lhsT=uT[tt][:, k, off:off + 128], rhs=wiw[:, k, 0:16],
                                                      start=(k == 0), stop=(k == KD - 1)),
                             reads=[wiw, uT[tt]], writes=[ps2])
                evac(kb, tb + 1, iwo, iwo[:, tb, :], ps2, ps2[:, 0:16])
            kb.act.dma(O["iw"].rearrange("(b p) h -> p b h", p=128), iwo[:], reads=[iwo])
            kb.barrier()
    with ExitStack() as s3:
        stg = Ring([kb.sb([128, 1024], F32, "stg", s3) for _ in range(2)])
        wq = kb.sb([128, 3, 1024], BF16, "wq", s3)
        wkv = kb.sb([128, 2, 1024], BF16, "wkv", s3)
        for kc in range(3):
            load_cast(kb, wq, wq[:, kc, :], W["wq_all"][kc * 128:(kc + 1) * 128, :], stg, lambda s: s[:])
        for kc in range(2):
            load_cast(kb, wkv, wkv[:, kc, :], W["wkv_all"][kc * 128:(kc + 1) * 128, :], stg, lambda s: s[:])
        vecs = kb.sb([128, 8], F32, "vecs", s3)
        kb.sp.dma(vecs[:, 0:3], W["q_norm"][:, :], writes=[vecs])
        kb.sp.dma(vecs[:, 3:5], W["kv_norm"][:, :], writes=[vecs])
        tmp = stat_tmps(kb, s3)
        ar = Ring([kb.sb([128, 8, 512], F32, "hc", s3) for _ in range(1)])
        sig = kb.sb([128, 4, 512], F32, "sig", s3)
        zb = kb.sb([128, 4, 512], BF16, "zb", s3)
        cqs = kb.sb([128, 3, 512], F32, "cqs", s3)
        cqn = kb.sb([128, 3, 512], BF16, "cqn", s3)
        cks = kb.sb([128, 2, 512], F32, "cks", s3)
        ckn = kb.sb([128, 2, 512], BF16, "ckn", s3)
        cc = kb.sb([64, 512], F32, "cc", s3)
        ss = kb.sb([64, 512], F32, "ss", s3)
        krr = kb.sb([64, 2, 512], F32, "krr", s3)
        t1r = Ring([kb.sb([128, 512], F32, "t1", s3) for _ in range(2)])
        t2r = Ring([kb.sb([128, 512], F32, "t2", s3) for _ in range(2)])
        obr = Ring([kb.sb([128, 512], BF16, "ob", s3) for _ in range(3)])
        psr = Ring([P[2], P[3], P[4], P[5]])
        ei = 0
        for tt in range(4):
            ts = slice(tt * 512, (tt + 1) * 512)
            hc = ar.next()
            kb.sp.dma(hc[:], S["hcT"].rearrange("(k p) n -> p k n", p=128)[:, :, ts], writes=[hc])
            kb.act.op(lambda: nc.scalar.activation(out=sig[:], in_=hc[:, 4:8, :], func=AF.Sigmoid), reads=[hc],
                      writes=[sig])
            kb.dve.op(lambda: nc.vector.tensor_tensor(out=zb[:], in0=hc[:, 0:4, :], in1=sig[:], op=ALU.mult),
                      reads=[hc, sig], writes=[zb])
            kb.act.dma(O["zT"].rearrange("(k p) n -> p k n", p=128)[:, :, ts], zb[:], reads=[zb])
            kb.sp.dma(cc[:], W["ropec"][:, ts], writes=[cc])
            kb.sp.dma(ss[:], W["ropes"][:, ts], writes=[ss])
            kb.sp.dma(cqs[:], S["cqT"].rearrange("(k p) n -> p k n", p=128)[:, :, ts], writes=[cqs])
            rstd, _ = col_stats(kb, cx, [(cqs, cqs[:, k, :]) for k in range(3)], 384, False, tmp)
            for k in range(3):
                t1 = t1r.next()
                kb.dve.op(lambda: nc.vector.tensor_tensor(out=t1[:], in0=cqs[:, k, :], in1=rstd[:], op=ALU.mult),
                          reads=[cqs, rstd], writes=[t1])
                kb.act.op(lambda: nc.scalar.activation(out=cqn[:, k, :], in_=t1[:], func=AF.Copy,
                                                       scale=vecs[:, k:k + 1]), reads=[t1, vecs], writes=[cqn])
            for h in range(4):
                ps = psr.next()
                for k in range(3):
                    kb.pe.op(lambda: nc.tensor.matmul(ps[:], lhsT=wq[:, k, h * 256:h * 256 + 128], rhs=cqn[:, k, :],
                                                      start=(k == 0), stop=(k == 2)), reads=[wq, cqn], writes=[ps])
                ob = obr.next()
                evac(kb, ei, ob, ob[:], ps, ps[:]); ei += 1
                kb.act.dma(O["qnT"][h * 128:(h + 1) * 128, ts], ob[:], reads=[ob])
                ps = psr.next()
                ps2 = psr.next()
                for k in range(3):
                    kb.pe.op(lambda: nc.tensor.matmul(ps[:64, :], lhsT=wq[:, k, h * 256 + 128:h * 256 + 192],
                                                      rhs=cqn[:, k, :], start=(k == 0), stop=(k == 2)),
                             reads=[wq, cqn], writes=[ps])
                for k in range(3):
                    kb.pe.op(lambda: nc.tensor.matmul(ps2[:64, :], lhsT=wq[:, k, h * 256 + 192:h * 256 + 256],
                                                      rhs=cqn[:, k, :], start=(k == 0), stop=(k == 2)),
                             reads=[wq, cqn], writes=[ps2])
                t1 = t1r.next(); t2 = t2r.next(); ob = obr.next()
                kb.dve.op(lambda: nc.vector.tensor_tensor(out=t1[:64, :], in0=ps[:64, :], in1=cc[:], op=ALU.mult),
                          reads=[ps, cc], writes=[t1])
                kb.dve.op(lambda: nc.vector.tensor_tensor(out=t2[:64, :], in0=ps2[:64, :], in1=ss[:], op=ALU.mult),
                          reads=[ps2, ss], writes=[t2])
                kb.pool.op(lambda: nc.gpsimd.tensor_tensor(out=ob[:64, :], in0=t1[:64, :], in1=t2[:64, :], op=ALU.add),
                           reads=[t1, t2], writes=[ob])
                kb.act.dma(O["qrT"][h * 64:(h + 1) * 64, ts], ob[:64, :], reads=[ob])
            kb.sp.dma(cks[:], S["ckvT"].rearrange("(k p) n -> p k n", p=128)[:, :, ts], writes=[cks])
            rstd, _ = col_stats(kb, cx, [(cks, cks[:, k, :]) for k in range(2)], 256, False, tmp)
            for k in range(2):
                t1 = t1r.next()
                kb.dve.op(lambda: nc.vector.tensor_tensor(out=t1[:], in0=cks[:, k, :], in1=rstd[:], op=ALU.mult),
                          reads=[cks, rstd], writes=[t1])
                kb.act.op(lambda: nc.scalar.activation(out=ckn[:, k, :], in_=t1[:], func=AF.Copy,
                                                       scale=vecs[:, 3 + k:4 + k]), reads=[t1, vecs], writes=[ckn])
            for h in range(4):
                ps = psr.next()
                for k in range(2):
                    kb.pe.op(lambda: nc.tensor.matmul(ps[:], lhsT=wkv[:, k, h * 128:(h + 1) * 128], rhs=ckn[:, k, :],
                                                      start=(k == 0), stop=(k == 1)), reads=[wkv, ckn], writes=[ps])
                ob = obr.next()
                evac(kb, ei, ob, ob[:], ps, ps[:]); ei += 1
                kb.act.dma(O["knT"][h * 128:(h + 1) * 128, ts], ob[:], reads=[ob])
            for tb in range(4):
                ps = psr.next()
                for k in range(2):
                    kb.pe.op(lambda: nc.tensor.matmul(ps[:], lhsT=ckn[:, k, tb * 128:(tb + 1) * 128],
                                                      rhs=wkv[:, k, 512:1024], start=(k == 0), stop=(k == 1)),
                             reads=[wkv, ckn], writes=[ps])
                ob = obr.next()
                evac(kb, ei, ob, ob[:], ps, ps[:]); ei += 1
                kb.act.dma(O["mV"][tt * 512 + tb * 128:tt * 512 + (tb + 1) * 128, :], ob[:], reads=[ob])
            kb.sp.dma(krr[:, 0, :], S["krraw"][:, ts], writes=[krr])
            kb.sp.dma(krr[:, 1, :], S["krsraw"][:, ts], writes=[krr])
            t1 = t1r.next(); t2 = t2r.next(); ob = obr.next()
            kb.dve.op(lambda: nc.vector.tensor_tensor(out=t1[:64, :], in0=krr[:, 0, :], in1=cc[:], op=ALU.mult),
                      reads=[krr, cc], writes=[t1])
            kb.dve.op(lambda: nc.vector.tensor_tensor(out=t2[:64, :], in0=krr[:, 1, :], in1=ss[:], op=ALU.mult),
                      reads=[krr, ss], writes=[t2])
            kb.pool.op(lambda: nc.gpsimd.tensor_tensor(out=ob[:64, :], in0=t1[:64, :], in1=t2[:64, :], op=ALU.add),
                       reads=[t1, t2], writes=[ob])
            kb.act.dma(O["krT"][:, ts], ob[:64, :], reads=[ob])
        kb.barrier()


A_OUT = {"uT": ([D, TOK], "bf"), "hpT": ([512, TOK], "f32"), "dqT": ([512, TOK], "bf"), "dkT": ([512, TOK], "bf"),
         "dV": ([TOK, 512], "bf"), "iqT": ([1024, TOK], "bf"), "ikT": ([64, TOK], "bf"), "iw": ([TOK, 16], "f32"),
         "qnT": ([512, TOK], "bf"), "qrT": ([256, TOK], "bf"), "knT": ([512, TOK], "bf"), "mV": ([TOK, 512], "bf"),
         "krT": ([64, TOK], "bf"), "zT": ([512, TOK], "bf")}
A_SCR = {"cqT": [384, TOK], "ckvT": [256, TOK], "krraw": [64, TOK], "krsraw": [64, TOK], "hcT": [1024, TOK]}
A_W = {"win_ct": [NCT, 128, KD, 128], "wdv_ct": [4, 128, KD, 128], "wiw_ct": [1, 128, KD, 128],
       "wq_all": [384, 1024], "wkv_all": [256, 1024], "q_norm": [128, 3], "kv_norm": [128, 2],
       "ropec": [64, TOK], "ropes": [64, TOK]}


def ct_layout(w, c0, ncols):
    out = np.zeros((128, KD, 128), np.float32)
    out[:, :, :ncols] = w[:, c0:c0 + ncols].reshape(KD, 128, ncols).transpose(1, 0, 2)
    return out


def host_A_weights(inputs, l, h):
    w_in = inputs["w_in"][l]
    cts = []
    for seg, c0, ncols in CTS:
        if seg == "krs":
            base = SEG_OFF["kr"]
            wsw = np.concatenate([w_in[:, base + 32:base + 64], w_in[:, base:base + 32]], axis=1)
            cts.append(ct_layout(wsw, 0, 64))
        else:
            cts.append(ct_layout(w_in, SEG_OFF[seg] + c0, ncols))
    out = {"win_ct": np.stack(cts)}
    out["wdv_ct"] = np.stack([ct_layout(w_in, SEG_OFF["dv"] + j * 128, 128) for j in range(4)])
    out["wiw_ct"] = np.stack([ct_layout(w_in, SEG_OFF["iw"], 16)])
    wq = inputs["w_q_up"][l]
    parts = []
    for hh in range(4):
        b = hh * 192
        parts += [wq[:, b:b + 128], wq[:, b + 128:b + 192], wq[:, b + 160:b + 192], wq[:, b + 128:b + 160]]
    out["wq_all"] = np.ascontiguousarray(np.concatenate(parts, axis=1))
    wkv = inputs["w_kv_up"][l]
    out["wkv_all"] = np.ascontiguousarray(np.concatenate(
        [wkv[:, hh * 256:hh * 256 + 128] for hh in range(4)] + [wkv[:, hh * 256 + 128:hh * 256 + 256] for hh in range(4)],
        axis=1))
    out["q_norm"] = pk(inputs["q_norm"][l])
    out["kv_norm"] = pk(inputs["kv_norm"][l])
    pos = np.arange(h * TOK, (h + 1) * TOK, dtype=np.float32)
    inv_freq = (np.float32(10000.0) ** (-np.arange(0, 64, 2, dtype=np.float32) / np.float32(64))).astype(np.float32)
    ang = pos[None, :] * inv_freq[:, None]
    cos, sin = np.cos(ang).astype(np.float32), np.sin(ang).astype(np.float32)
    out["ropec"] = np.ascontiguousarray(np.concatenate([cos, cos], axis=0))
    out["ropes"] = np.ascontiguousarray(np.concatenate([-sin, sin], axis=0))
    return out


def build_A(l):
    kb = KB()
    xT = kb.dram("xT", [D, TOK], F32, kind="ExternalInput")
    modT = kb.dram("modT", [128, DEPTH * 96], F32, kind="ExternalInput")
    W = {k: kb.dram(k, v, F32, kind="ExternalInput") for k, v in A_W.items()}
    O = {k: kb.dram(k, v[0], dts(v[1]), kind="ExternalOutput") for k, v in A_OUT.items()}
    S = {k: kb.dram(k, v, F32) for k, v in A_SCR.items()}
    cx = setup_common(kb, modT)
    stage_A(kb, cx, l, xT, W, O, S)
    kb.finish()
    return kb.nc


SLOPES = [2.0 ** (-8.0 * (h + 1) / 4) for h in range(4)]
NBIS = 16
NEGM = -30000.0


def key_tiles(i, with_prev=True):
    tl = [(j * 512, 512, 0) for j in range(4)] if with_prev else []
    for j in range(i // 4):
        tl.append((2048 + j * 512, 512, 1))
    tl.append((2048 + (i // 4) * 512, (i % 4 + 1) * 128, 2))
    return tl


def host_B_consts(h, skip_prev=False):
    c = {}
    q = np.arange(128)[:, None]
    s = np.arange(512)[None, :]
    c["AB"] = np.stack([SLOPES[hh] * (s - q) for hh in range(4)], axis=1).astype(np.float32)
    s1 = np.arange(128)[None, :]
    cm = np.where((s1 // 64) <= (q // 64), 0.0, 1.0)
    c["ABD"] = np.stack([-SLOPES[hh] * np.abs(q - s1) + SLOPES[hh] * 128.0 * wv for hh in range(4) for wv in range(4)],
                        axis=1).astype(np.float32)
    c["CM"] = (cm * NEGM).astype(np.float32)
    c["CMB"] = (cm * -1e6).astype(np.float32)
    c["ident"] = np.eye(128, dtype=np.float32)
    prevb = 0.0 if h == 1 else NEGM
    c["prevb"] = np.full((128, 2), prevb, np.float32)
    c["prevs"] = np.full((128, 2), 0.0 if h == 1 else -1e6, np.float32)
    cb = np.zeros((128, 16 * 4 * 8), np.float32)
    for i in range(16):
        tq0 = 2048 + 128 * i
        for hh in range(4):
            for ti, (s0, w, kind) in enumerate(key_tiles(i, not (skip_prev and h == 0))):
                v = -SLOPES[hh] * (tq0 - s0)
                if kind == 0:
                    v += prevb
                cb[:, (i * 4 + hh) * 8 + ti] = v
    c["cb"] = cb
    c["pw"] = np.tile((2.0 ** -(np.arange(NBIS + 1) + 1.0))[None, :], (128, 1)).astype(np.float32)
    ic = np.zeros((128, 4, 16), np.float32)
    for g, w in enumerate((2, 4, 8, 16)):
        t = np.arange(16) + h * TOK
        ic[:, g, :] = 1.0 / np.minimum(t + 1, w)
    c["invc"] = ic
    return c


B_C = {"AB": [128, 4, 512], "ABD": [128, 16, 128], "CM": [128, 128], "CMB": [128, 128], "ident": [128, 128],
       "prevb": [128, 2], "prevs": [128, 2], "cb": [128, 512], "pw": [128, NBIS + 1], "invc": [128, 4, 16]}
B_W = {"w_pool": [4, 128, 128], "pool_scale": [128, 4], "w_dwT": [128, 4, 31], "b_dw": [128, 4],
       "cln_g": [128, 4], "cln_b": [128, 4]}
B_PREV = {"pdkT": ([512, TOK], "bf"), "pdV": ([TOK, 512], "bf"), "pikT": ([64, TOK], "bf"), "pknT": ([512, TOK], "bf"),
          "pmV": ([TOK, 512], "bf"), "pkrT": ([64, TOK], "bf"), "pz": ([512, 32], "bf"), "php": ([512, 16], "f32")}


def host_B_weights(inputs, l):
    out = {"w_pool": np.ascontiguousarray(inputs["w_pool"][l]), "pool_scale": pk(inputs["pool_scale"][l]),
           "b_dw": pk(inputs["b_dw"][l]), "cln_g": pk(inputs["conv_ln_g"][l]), "cln_b": pk(inputs["conv_ln_b"][l])}
    wd = inputs["w_dw"][l]
    out["w_dwT"] = np.ascontiguousarray(wd.reshape(31, 4, 128).transpose(2, 1, 0))
    return out


PARTS = {"B1", "B2", "B3", "B4"}
DBG = {"ni": 16, "tail": True, "fin": True, "nbis": NBIS, "att": True, "idx": True}


def sect(tag):
    if tag in PARTS:
        with ExitStack() as st:
            yield st


def attn_tail(kb, cx, T, Pb, W, s_chunk0, Vb, h, po, first, last):
    nc = kb.nc
    nb = W // 128
    pt = T["ptps"].next()
    for sb in range(nb):
        kb.pe.op(lambda: nc.tensor.transpose(out=pt[:, sb * 128:(sb + 1) * 128], in_=Pb[:, sb * 128:(sb + 1) * 128],
                                             identity=T["identb"][:]), reads=[Pb, T["identb"]], writes=[pt])
    pts = T["pts"].next()
    evac(kb, T["ei"][0], pts, pts[:, :W], pt, pt[:, :W])
    T["ei"][0] += 1
    for sb in range(nb):
        kb.pe.op(lambda: nc.tensor.matmul(po[:, 0:128], lhsT=pts[:, sb * 128:(sb + 1) * 128],
                                          rhs=Vb[:, s_chunk0 + sb, h * 128:(h + 1) * 128],
                                          start=(first and sb == 0), stop=(last and sb == nb - 1)),
                 reads=[pts, Vb], writes=[po])


def attn_finish(kb, cx, T, po, rs, npieces, yb, h, i):
    nc = kb.nc
    rsum, rinv, on = T["rsum"], T["rinv"], T["on"].next()
    kb.dve.op(lambda: nc.vector.tensor_reduce(out=rsum[:], in_=rs[:, 0:npieces], axis=AX.X, op=ALU.add), reads=[rs], writes=[rsum])
    kb.dve.op(lambda: nc.vector.reciprocal(out=rinv[:], in_=rsum[:]), reads=[rsum], writes=[rinv])
    kb.act.op(lambda: nc.scalar.activation(out=on[:], in_=po[:, 0:128], func=AF.Copy, scale=rinv[:, 0:1]),
              reads=[po, rinv], writes=[on])
    pt = T["ptps"].next()
    kb.pe.op(lambda: nc.tensor.transpose(out=pt[:, 0:128], in_=on[:], identity=T["identb"][:]),
             reads=[on, T["identb"]], writes=[pt])
    kb.dve.op(lambda: nc.vector.tensor_copy(out=yb[:, h, i * 128:(i + 1) * 128], in_=pt[:, 0:128]), reads=[pt],
              writes=[yb])


def stage_B(kb, cx, l, A, PV, W, C, yT, with_prev=True):
    nc = kb.nc
    P, PB = cx.P, cx.PB
    yTv = yT.rearrange("(b k p) n -> b p k n", b=4, p=128)
    with ExitStack() as sc_:
        ident = kb.sb([128, 128], F32, "ident", sc_)
        identb = kb.sb([128, 128], BF16, "identb", sc_)
        kb.sp.dma(ident[:], C["ident"][:, :], writes=[ident])
        kb.pool.op(lambda: nc.gpsimd.tensor_copy(out=identb[:], in_=ident[:]), reads=[ident], writes=[identb])

        for st in sect("B1"):
            hh = kb.sb([128, 16 + TOK], F32, "hh", st)
            sA = kb.sb([128, 16 + TOK], F32, "sA", st)
            sB = kb.sb([128, 16 + TOK], F32, "sB", st)
            pl = kb.sb([128, TOK], BF16, "pl", st)
            t16 = kb.sb([128, 16], F32, "t16", st)
            invc = kb.sb([128, 4, 16], F32, "invc", st)
            wps = kb.sb([128, 4, 128], F32, "wps", st)
            wpb = kb.sb([128, 4, 128], BF16, "wpb", st)
            psc = kb.sb([128, 4], F32, "psc", st)
            ya = kb.sb([128, 4, TOK], BF16, "ya", st)
            kb.sp.dma(invc[:], C["invc"][:, :, :], writes=[invc])
            kb.sp.dma(wps[:], W["w_pool"].rearrange("g c d -> c g d"), writes=[wps])
            kb.pool.op(lambda: nc.gpsimd.tensor_copy(out=wpb[:], in_=wps[:]), reads=[wps], writes=[wpb])
            kb.sp.dma(psc[:], W["pool_scale"][:, :], writes=[psc])
            psr = Ring([P[2], P[3], P[4], P[5]])
            for g in range(4):
                w = 2 ** (g + 1)
                kb.sp.dma(hh[:, 0:16], PV["php"][g * 128:(g + 1) * 128, :], writes=[hh])
                kb.sp.dma(hh[:, 16:], A["hpT"][g * 128:(g + 1) * 128, :], writes=[hh])
                src, d, o = hh, 1, 1
                bufs = [sA, sB]
                for step in range(g + 1):
                    dst = bufs[step % 2]
                    kb.dve.op(lambda: nc.vector.tensor_tensor(out=dst[:, o:], in0=src[:, o:], in1=src[:, o - d:16 + TOK - d],
                                                              op=ALU.add), reads=[src], writes=[dst])
                    src = dst
                    d *= 2
                    o += d
                kb.dve.op(lambda: nc.vector.scalar_tensor_tensor(out=pl[:], in0=src[:, 16:], scalar=1.0 / w, in1=hh[:, 16:],
                                                                 op0=ALU.mult, op1=ALU.subtract),
                          reads=[src, hh], writes=[pl])
                kb.dve.op(lambda: nc.vector.tensor_tensor(out=t16[:], in0=src[:, 16:32], in1=invc[:, g, :], op=ALU.mult),
                          reads=[src, invc], writes=[t16])
                kb.dve.op(lambda: nc.vector.tensor_tensor(out=pl[:, 0:16], in0=t16[:], in1=hh[:, 16:32], op=ALU.subtract),
                          reads=[t16, hh], writes=[pl])
                for tt in range(4):
                    ps = psr.next()
                    kb.pe.op(lambda: nc.tensor.matmul(ps[:], lhsT=wpb[:, g, :], rhs=pl[:, tt * 512:(tt + 1) * 512],
                                                      start=True, stop=True), reads=[wpb, pl], writes=[ps])
                    kb.act.op(lambda: nc.scalar.activation(out=ya[:, g, tt * 512:(tt + 1) * 512], in_=ps[:], func=AF.Copy,
                                                           scale=psc[:, g:g + 1]), reads=[ps, psc], writes=[ya])
            kb.act.dma(yTv[0], ya[:], reads=[ya])
            kb.barrier()

        for st in sect("B2"):
            zb = kb.sb([128, 4, 32 + TOK], BF16, "zb", st)
            wdw = kb.sb([128, 4, 31], F32, "wdw", st)
            dg = kb.sb([128, 4 * 31, 128], BF16, "dg", st)
            vecs = kb.sb([128, 12], F32, "cvecs", st)
            v = kb.sb([128, 4, 512], F32, "cv", st)
            yd = kb.sb([128, 4, TOK], BF16, "yd", st)
            tmp = stat_tmps(kb, st)
            t1r = Ring([kb.sb([128, 512], F32, "t1", st) for _ in range(2)])
            t2r = Ring([kb.sb([128, 512], F32, "t2", st) for _ in range(2)])
            kb.sp.dma(zb[:, :, 0:32], PV["pz"].rearrange("(k p) n -> p k n", p=128), writes=[zb])
            kb.sp.dma(zb[:, :, 32:], A["zT"].rearrange("(k p) n -> p k n", p=128), writes=[zb])
            kb.sp.dma(wdw[:], W["w_dwT"][:, :, :], writes=[wdw])
            kb.sp.dma(vecs[:, 0:4], W["b_dw"][:, :], writes=[vecs])
            kb.sp.dma(vecs[:, 4:8], W["cln_g"][:, :], writes=[vecs])
            kb.sp.dma(vecs[:, 8:12], W["cln_b"][:, :], writes=[vecs])
            for c in range(4):
                for j in range(31):
                    kb.pool.op(lambda: nc.gpsimd.tensor_scalar(out=dg[:, c * 31 + j, :], in0=ident[:],
                                                               scalar1=wdw[:, c, j:j + 1], scalar2=None, op0=ALU.mult),
                               reads=[ident, wdw], writes=[dg])
            psr = Ring([P[2], P[3], P[4], P[5]])
            for tt in range(4):
                for c in range(4):
                    ps = psr.next()
                    for j in range(31):
                        o = 32 + tt * 512 - 30 + j
                        kb.pe.op(lambda: nc.tensor.matmul(ps[:], lhsT=dg[:, c * 31 + j, :], rhs=zb[:, c, o:o + 512],
                                                          start=(j == 0), stop=(j == 30)), reads=[dg, zb], writes=[ps])
                    kb.act.op(lambda: nc.scalar.activation(out=v[:, c, :], in_=ps[:], func=AF.Identity,
                                                           bias=vecs[:, c:c + 1], scale=1.0), reads=[ps, vecs], writes=[v])
                rstd, nmr = col_stats(kb, cx, [(v, v[:, c, :]) for c in range(4)], 512, True, tmp)
                for c in range(4):
                    t1 = t1r.next(); t2 = t2r.next()
                    kb.dve.op(lambda: nc.vector.tensor_tensor(out=t1[:], in0=v[:, c, :], in1=rstd[:], op=ALU.mult),
                              reads=[v, rstd], writes=[t1])
                    kb.pool.op(lambda: nc.gpsimd.tensor_tensor(out=t2[:], in0=t1[:], in1=nmr[:], op=ALU.add),
                               reads=[t1, nmr], writes=[t2])
                    kb.act.op(lambda: nc.scalar.activation(out=yd[:, c, tt * 512:(tt + 1) * 512], in_=t2[:], func=AF.Silu,
                                                           scale=vecs[:, 4 + c:5 + c], bias=vecs[:, 8 + c:9 + c]),
                              reads=[t2, vecs], writes=[yd])
            kb.act.dma(yTv[3], yd[:], reads=[yd])
            kb.barrier()

        def mk_T(st):
            T = {"identb": identb, "ei": [0]}
            T["ptps"] = Ring([PB[0], PB[1], PB[2], PB[3]])
            T["pts"] = Ring([kb.sb([128, 512], BF16, "pts", st) for _ in range(3)])
            T["Pb"] = Ring([kb.sb([128, 512], BF16, "Pb", st) for _ in range(3)])
            T["tmp"] = Ring([kb.sb([128, 512], F32, "atmp", st) for _ in range(3)])
            T["on"] = Ring([kb.sb([128, 128], BF16, "on", st) for _ in range(2)])
            T["rs"] = Ring([kb.sb([128, 16], F32, "rs", st) for _ in range(2)])
            T["rsum"] = kb.sb([128, 1], F32, "rsum", st)
            T["rinv"] = kb.sb([128, 1], F32, "rinv", st)
            return T

        def load_kv(st, K_own, K_prev, V_own, V_prev):
            Kb = kb.sb([128, 4, 2 * TOK], BF16, "Kb", st)
            Vb = kb.sb([128, 32, 512], BF16, "Vb", st)
            if with_prev:
                kb.sp.dma(Kb[:, :, 0:TOK], K_prev.rearrange("(k p) n -> p k n", p=128), writes=[Kb])
            kb.sp.dma(Kb[:, :, TOK:], K_own.rearrange("(k p) n -> p k n", p=128), writes=[Kb])
            if with_prev:
                kb.sp.dma(Vb[:, 0:16, :], V_prev.rearrange("(c p) d -> p c d", p=128), writes=[Vb])
            kb.sp.dma(Vb[:, 16:32, :], V_own.rearrange("(c p) d -> p c d", p=128), writes=[Vb])
            return Kb, Vb

        for st in sect("B3"):
            T = mk_T(st)
            Kb, Vb = load_kv(st, A["knT"], PV["pknT"], A["mV"], PV["pmV"])
            Qb = kb.sb([128, 4, TOK], BF16, "Qb", st)
            Qr = kb.sb([128, 4, TOK], BF16, "Qr", st)
            Kr = kb.sb([128, 2 * TOK], BF16, "Kr", st)
            kb.pool.op(lambda: nc.gpsimd.memset(Qr[:], 0.0), writes=[Qr])
            kb.pool.op(lambda: nc.gpsimd.memset(Kr[:], 0.0), writes=[Kr])
            CM = kb.sb([128, 128], F32, "CM", st)
            prevb = kb.sb([128, 2], F32, "prevb", st)
            yc = kb.sb([128, 4, TOK], BF16, "yc", st)
            kb.sp.dma(Qb[:], A["qnT"].rearrange("(k p) n -> p k n", p=128), writes=[Qb])
            kb.sp.dma(Qr[0:64], A["qrT"].rearrange("(k p) n -> p k n", p=64), writes=[Qr])
            kb.sp.dma(Kr[0:64, 0:TOK], PV["pkrT"][:, :], writes=[Kr])
            kb.sp.dma(Kr[0:64, TOK:], A["krT"][:, :], writes=[Kr])
            kb.sp.dma(CM[:], C["CM"][:, :], writes=[CM])
            kb.sp.dma(prevb[:], C["prevb"][:, :], writes=[prevb])
            scale = 192.0 ** -0.5
            psS = Ring([P[2], P[3]])
            psO = Ring([P[4], P[5]])
            for i in range(DBG["ni"]):
                qs = slice(i * 128, (i + 1) * 128)
                tl = key_tiles(i, with_prev)
                for h in range(4):
                    po = psO.next()
                    rs = T["rs"].next()
                    npc = 0
                    for ti, (s0, Wd, kind) in enumerate(tl):
                        ps = psS.next()
                        kb.pe.op(lambda: nc.tensor.matmul(ps[:, :Wd], lhsT=Qb[:, h, qs], rhs=Kb[:, h, s0:s0 + Wd],
                                                          start=True, stop=False), reads=[Qb, Kb], writes=[ps])
                        kb.pe.op(lambda: nc.tensor.matmul(ps[:, :Wd], lhsT=Qr[:, h, qs], rhs=Kr[:, s0:s0 + Wd],
                                                          start=False, stop=True), reads=[Qr, Kr], writes=[ps])
                        Pb = T["Pb"].next()
                        if kind == 2:
                            tm = T["tmp"].next()
                            if Wd > 128:
                                kb.dve.op(lambda: nc.vector.tensor_scalar(out=tm[:, :Wd - 128], in0=ps[:, :Wd - 128],
                                                                          scalar1=scale, scalar2=None, op0=ALU.mult),
                                          reads=[ps], writes=[tm])
                            kb.dve.op(lambda: nc.vector.scalar_tensor_tensor(out=tm[:, Wd - 128:Wd], in0=ps[:, Wd - 128:Wd],
                                                                             scalar=scale, in1=CM[:], op0=ALU.mult,
                                                                             op1=ALU.add), reads=[ps, CM], writes=[tm])
                            kb.act.op(lambda: nc.scalar.activation(out=Pb[:, :Wd], in_=tm[:, :Wd], func=AF.Exp,
                                                                   accum_out=rs[:, npc:npc + 1]),
                                      reads=[tm], writes=[Pb, rs])
                            npc += 1
                        else:
                            if kind == 0:
                                kb.act.op(lambda: nc.scalar.activation(out=Pb[:, :Wd], in_=ps[:, :Wd], func=AF.Exp,
                                                                       scale=scale, bias=prevb[:, 0:1],
                                                                       accum_out=rs[:, npc:npc + 1]),
                                          reads=[ps, prevb], writes=[Pb, rs])
                            else:
                                kb.act.op(lambda: nc.scalar.activation(out=Pb[:, :Wd], in_=ps[:, :Wd], func=AF.Exp,
                                                                       scale=scale, accum_out=rs[:, npc:npc + 1]),
                                          reads=[ps], writes=[Pb, rs])
                            npc += 1
                        if DBG["tail"]:
                            attn_tail(kb, cx, T, Pb, Wd, s0 // 128, Vb, h, po, ti == 0, ti == len(tl) - 1)
                    if DBG["fin"]:
                        attn_finish(kb, cx, T, po, rs, npc, yc, h, i)
            kb.act.dma(yTv[2], yc[:], reads=[yc])
            kb.barrier()

        for st in sect("B4"):
            T = mk_T(st)
            Kb, Vb = load_kv(st, A["dkT"], PV["pdkT"], A["dV"], PV["pdV"])
            Qb = kb.sb([128, 4, TOK], BF16, "Qb", st)
            Ik = kb.sb([128, 2 * TOK], BF16, "Ik", st)
            iqr = Ring([kb.sb([128, 16, 128], BF16, "iq", st) for _ in range(2)])
            kb.pool.op(lambda: nc.gpsimd.memset(Ik[:], 0.0), writes=[Ik])
            for b_ in iqr.bufs:
                kb.pool.op(lambda: nc.gpsimd.memset(b_[:], 0.0), writes=[b_])
            iwr = Ring([kb.sb([128, 16], F32, "iwt", st) for _ in range(2)])
            iwa = kb.sb([128, 16], F32, "iwa", st)
            iws = kb.sb([128, 16], F32, "iws", st)
            dsg = kb.sb([128, 16, 128], BF16, "dsg", st)
            Rr = Ring([kb.sb([128, 512], BF16, "R", st) for _ in range(3)])
            scb = kb.sb([128, 2 * TOK], F32, "scb", st)
            junk = kb.sb([128, 2 * TOK], BF16, "junk", st)
            AB = kb.sb([128, 4, 512], F32, "AB", st)
            ABD = kb.sb([128, 16, 128], F32, "ABD", st)
            CMB = kb.sb([128, 128], F32, "CMB", st)
            prevs = kb.sb([128, 2], F32, "prevs", st)
            cbt = kb.sb([128, 512], F32, "cbt", st)
            pw = kb.sb([128, NBIS + 1], F32, "pw", st)
            am = kb.sb([128, 8], F32, "am", st)
            sm = {n: kb.sb([128, 1], F32, n, st) for n in ("M", "lo", "W0", "mid", "cnt", "stp")}
            wst_ = kb.sb([128, NBIS + 1], F32, "wsteps", st)
            yb = kb.sb([128, 4, TOK], BF16, "yb", st)
            kb.sp.dma(Qb[:], A["dqT"].rearrange("(k p) n -> p k n", p=128), writes=[Qb])
            kb.sp.dma(Ik[0:64, 0:TOK], PV["pikT"][:, :], writes=[Ik])
            kb.sp.dma(Ik[0:64, TOK:], A["ikT"][:, :], writes=[Ik])
            for nm, t_ in (("AB", AB), ("ABD", ABD)):
                kb.sp.dma(t_[:], C[nm][:, :, :], writes=[t_])
            for nm, t_ in (("CMB", CMB), ("prevs", prevs), ("cb", cbt), ("pw", pw)):
                kb.sp.dma(t_[:], C[nm][:, :], writes=[t_])
            scale = 128.0 ** -0.5
            psS = Ring([P[2], P[3]])
            psO = Ring([P[4], P[5]])
            iqv = A["iqT"].rearrange("(h d) n -> d h n", d=64)
            iwv = A["iw"].rearrange("(b p) h -> b p h", p=128)
            for i in range(DBG["ni"]):
                qs = slice(i * 128, (i + 1) * 128)
                tl = key_tiles(i, with_prev)
                Ntot = tl[-1][0] + tl[-1][1]
                N0 = tl[0][0]
                iq = iqr.next()
                iwt = iwr.next()
                kb.sp.dma(iq[0:64], iqv[:, :, qs], writes=[iq])
                kb.sp.dma(iwt[:], iwv[i], writes=[iwt])
                kb.act.op(lambda: nc.scalar.activation(out=iwa[:], in_=iwt[:], func=AF.Abs), reads=[iwt], writes=[iwa])
                kb.act.op(lambda: nc.scalar.activation(out=iws[:], in_=iwt[:], func=AF.Sign), reads=[iwt], writes=[iws])
                for hh in range(16):
                    kb.pool.op(lambda: nc.gpsimd.tensor_scalar(out=dsg[:, hh, :], in0=ident[:], scalar1=iws[:, hh:hh + 1],
                                                               scalar2=None, op0=ALU.mult),
                               reads=[ident, iws], writes=[dsg])
                for ti, (s0, Wd, kind) in enumerate(tl if DBG["idx"] else []):
                    pss = P[0] if ti % 2 == 0 else P[1]
                    Rs = {}
                    for hh in range(17):
                        if hh < 16:
                            ps = psS.next()
                            kb.pe.op(lambda: nc.tensor.matmul(ps[:, :Wd], lhsT=iq[:, hh, :], rhs=Ik[:, s0:s0 + Wd],
                                                              start=True, stop=True), reads=[iq, Ik], writes=[ps])
                            R = Rr.next()
                            kb.act.op(lambda: nc.scalar.activation(out=R[:, :Wd], in_=ps[:, :Wd], func=AF.Relu,
                                                                   scale=iwa[:, hh:hh + 1]), reads=[ps, iwa], writes=[R])
                            Rs[hh] = R
                        if hh >= 1 and DBG.get("acc", True):
                            g_ = hh - 1
                            R_ = Rs.pop(g_)
                            kb.pe.op(lambda: nc.tensor.matmul(pss[:, :Wd], lhsT=dsg[:, g_, :], rhs=R_[:, :Wd],
                                                              start=(g_ == 0), stop=(g_ == 15)), reads=[dsg, R_],
                                     writes=[pss])
                    if not DBG.get("post", True):
                        continue
                    kb.dve.op(lambda: nc.vector.tensor_reduce(out=am[:, ti:ti + 1], in_=pss[:, :Wd], axis=AX.X, op=ALU.max,
                                                              apply_absolute_value=True), reads=[pss], writes=[am])
                    if DBG.get("post", 2) == 1:
                        continue
                    if kind == 0:
                        kb.dve.op(lambda: nc.vector.tensor_scalar(out=scb[:, s0:s0 + Wd], in0=pss[:, :Wd],
                                                                  scalar1=prevs[:, 0:1], scalar2=None, op0=ALU.add),
                                  reads=[pss, prevs], writes=[scb])
                    else:
                        if kind == 1 or Wd > 128:
                            We = Wd if kind == 1 else Wd - 128
                            kb.dve.op(lambda: nc.vector.tensor_copy(out=scb[:, s0:s0 + We], in_=pss[:, :We]), reads=[pss],
                                      writes=[scb])
                        if kind == 2:
                            kb.dve.op(lambda: nc.vector.tensor_tensor(out=scb[:, s0 + Wd - 128:s0 + Wd],
                                                                      in0=pss[:, Wd - 128:Wd], in1=CMB[:], op=ALU.add),
                                      reads=[pss, CMB], writes=[scb])
                nt = len(tl)
                M, lo, W0, mid, cnt, stp = (sm[n] for n in ("M", "lo", "W0", "mid", "cnt", "stp"))
                kb.dve.op(lambda: nc.vector.tensor_reduce(out=M[:], in_=am[:, 0:nt], axis=AX.X, op=ALU.max), reads=[am], writes=[M])
                kb.dve.op(lambda: nc.vector.tensor_scalar(out=lo[:], in0=M[:], scalar1=-1.0, scalar2=-1.0, op0=ALU.mult,
                                                          op1=ALU.add), reads=[M], writes=[lo])
                kb.dve.op(lambda: nc.vector.tensor_scalar(out=W0[:], in0=M[:], scalar1=2.0, scalar2=2.0, op0=ALU.mult,
                                                          op1=ALU.add), reads=[M], writes=[W0])
                kb.dve.op(lambda: nc.vector.tensor_scalar(out=wst_[:], in0=pw[:], scalar1=W0[:, 0:1], scalar2=None,
                                                          op0=ALU.mult), reads=[pw, W0], writes=[wst_])
                kb.dve.op(lambda: nc.vector.tensor_tensor(out=mid[:], in0=lo[:], in1=wst_[:, 0:1], op=ALU.add),
                          reads=[lo, wst_], writes=[mid])
                for it in range(DBG["nbis"]):
                    kb.dve.op(lambda: nc.vector.tensor_scalar(out=junk[:, N0:Ntot], in0=scb[:, N0:Ntot], scalar1=mid[:, 0:1],
                                                              scalar2=None, op0=ALU.is_ge, op1=ALU.add, accum_out=cnt[:]),
                              reads=[scb, mid], writes=[junk, cnt])
                    kb.dve.op(lambda: nc.vector.tensor_scalar(out=stp[:], in0=cnt[:], scalar1=255.5,
                                                              scalar2=wst_[:, it:it + 1], op0=ALU.is_ge, op1=ALU.mult),
                              reads=[cnt, wst_], writes=[stp])
                    kb.dve.op(lambda: nc.vector.tensor_tensor(out=lo[:], in0=lo[:], in1=stp[:], op=ALU.add),
                              reads=[lo, stp], writes=[lo])
                    kb.dve.op(lambda: nc.vector.tensor_tensor(out=mid[:], in0=lo[:], in1=wst_[:, it + 1:it + 2],
                                                              op=ALU.add), reads=[lo, wst_], writes=[mid])
                kb.dve.op(lambda: nc.vector.tensor_scalar(out=junk[:, N0:Ntot], in0=scb[:, N0:Ntot], scalar1=lo[:, 0:1],
                                                          scalar2=NEGM, op0=ALU.is_lt, op1=ALU.mult),
                          reads=[scb, lo], writes=[junk])
                rss = [T["rs"].next() for _ in range(2)]
                for h in range(4 if DBG["att"] else 0):
                    po = psO.next()
                    rs = rss[h % 2]
                    npc = 0
                    for ti, (s0, Wd, kind) in enumerate(tl):
                        ps = psS.next()
                        kb.pe.op(lambda: nc.tensor.matmul(ps[:, :Wd], lhsT=Qb[:, h, qs], rhs=Kb[:, h, s0:s0 + Wd],
                                                          start=True, stop=False), reads=[Qb, Kb], writes=[ps])
                        kb.pe.op(lambda: nc.tensor.matmul(ps[:, :Wd], lhsT=identb[:], rhs=junk[:, s0:s0 + Wd], start=False,
                                                          stop=True), reads=[identb, junk], writes=[ps])
                        tm = T["tmp"].next()
                        Pb = T["Pb"].next()
                        cbc = (i * 4 + h) * 8 + ti
                        if kind == 2:
                            wv_ = Wd // 128 - 1
                            if Wd > 128:
                                kb.dve.op(lambda: nc.vector.scalar_tensor_tensor(out=tm[:, :Wd - 128], in0=ps[:, :Wd - 128],
                                                                                 scalar=scale, in1=AB[:, h, :Wd - 128],
                                                                                 op0=ALU.mult, op1=ALU.add),
                                          reads=[ps, AB], writes=[tm])
                            kb.dve.op(lambda: nc.vector.scalar_tensor_tensor(out=tm[:, Wd - 128:Wd], in0=ps[:, Wd - 128:Wd],
                                                                             scalar=scale, in1=ABD[:, h * 4 + wv_, :],
                                                                             op0=ALU.mult, op1=ALU.add),
                                      reads=[ps, ABD], writes=[tm])
                            kb.act.op(lambda: nc.scalar.activation(out=Pb[:, :Wd], in_=tm[:, :Wd], func=AF.Exp,
                                                                   bias=cbt[:, cbc:cbc + 1], accum_out=rs[:, npc:npc + 1]),
                                      reads=[tm, cbt], writes=[Pb, rs])
                            npc += 1
                        else:
                            kb.dve.op(lambda: nc.vector.scalar_tensor_tensor(out=tm[:, :Wd], in0=ps[:, :Wd], scalar=scale,
                                                                             in1=AB[:, h, :Wd], op0=ALU.mult, op1=ALU.add),
                                      reads=[ps, AB], writes=[tm])
                            kb.act.op(lambda: nc.scalar.activation(out=Pb[:, :Wd], in_=tm[:, :Wd], func=AF.Exp,
                                                                   bias=cbt[:, cbc:cbc + 1], accum_out=rs[:, npc:npc + 1]),
                                      reads=[tm, cbt], writes=[Pb, rs])
                            npc += 1
                        attn_tail(kb, cx, T, Pb, Wd, s0 // 128, Vb, h, po, ti == 0, ti == len(tl) - 1)
                    attn_finish(kb, cx, T, po, rs, npc, yb, h, i)
            kb.act.dma(yTv[1], yb[:], reads=[yb])
            kb.barrier()


C_W = {"wg_ct": [4 * 16, 128, KD, 128], "wb_ct": [4 * 16, 128, 4, 128], "wo_ct": [16, 128, KD, 128],
       "wfi_ct": [88, 128, KD, 128], "wfo_ct": [16, 128, 44, 128], "b_gate": [128, 64], "ln1_g": [128, 16],
       "ln1_b": [128, 16], "ln2_g": [128, 16], "ln2_b": [128, 16]}


def ctl(w, kchunks):
    K_, C_ = w.shape
    return np.ascontiguousarray(w.reshape(kchunks, 128, C_ // 128, 128).transpose(2, 1, 0, 3))


def host_C_weights(inputs, l):
    out = {}
    out["wg_ct"] = np.concatenate([ctl(inputs["w_gate"][l, i], KD) for i in range(4)], axis=0)
    out["wb_ct"] = np.concatenate([ctl(inputs["w_branch"][l, i], 4) for i in range(4)], axis=0)
    out["wo_ct"] = ctl(inputs["w_o"][l], KD)
    out["wfi_ct"] = ctl(inputs["w_ffn_in"][l], KD)
    out["wfo_ct"] = ctl(inputs["w_ffn_out"][l], 44)
    out["b_gate"] = np.ascontiguousarray(np.concatenate([pk(inputs["b_gate"][l, i]) for i in range(4)], axis=1))
    for n in ("ln1_g", "ln1_b", "ln2_g", "ln2_b"):
        out[n] = pk(inputs[n][l])
    return out


PRECAST = False


def precast_weights(kb, cx, W, tag):
    nc = kb.nc
    out = dict(W)
    with ExitStack() as st:
        stg = Ring([kb.sb([128, 16, 128], F32, "pcs", st) for _ in range(3)])
        bfr = Ring([kb.sb([128, 16, 128], BF16, "pcb", st) for _ in range(3)])
        cnt = 0
        for name, n, nk in (("wg_ct", 64, 16), ("wb_ct", 64, 4), ("wo_ct", 16, 16), ("wfi_ct", 88, 16), ("wfo_ct", 16, 44)):
            dst = kb.dram(f"bf_{tag}_{name}", [n, 128, nk, 128], BF16)
            out[name] = dst
            for t in range(n):
                for k0 in range(0, nk, 16):
                    kk = min(16, nk - k0)
                    s_ = stg.next()
                    b_ = bfr.next()
                    kb.sp.dma(s_[:, :kk, :], W[name][t][:, k0:k0 + kk, :], writes=[s_])
                    if cnt % 2 == 0:
                        kb.pool.op(lambda: nc.gpsimd.tensor_copy(out=b_[:, :kk, :], in_=s_[:, :kk, :]), reads=[s_],
                                   writes=[b_])
                    else:
                        kb.dve.op(lambda: nc.vector.tensor_copy(out=b_[:, :kk, :], in_=s_[:, :kk, :]), reads=[s_],
                                  writes=[b_])
                    cnt += 1
                    kb.act.dma(dst[t][:, k0:k0 + kk, :], b_[:, :kk, :], reads=[b_])
        kb.barrier()
    return out


def stage_C(kb, cx, l, xT, uT_d, yT_d, W, S, xoT):
    nc = kb.nc
    P = cx.P
    v3 = lambda ap: ap.rearrange("(k p) n -> p k n", p=128)
    with ExitStack() as st:
        vec = kb.sb([128, 128], F32, "cvec", st)
        kb.sp.dma(vec[:, 0:64], W["b_gate"][:, :], writes=[vec])
        for j, n in enumerate(("ln1_g", "ln1_b", "ln2_g", "ln2_b")):
            kb.sp.dma(vec[:, 64 + 16 * j:80 + 16 * j], W[n][:, :], writes=[vec])
        wst = Ring([kb.sb([128, 16, 128], F32, "cwst", st) for _ in range(2)])
        wbf = Ring([kb.sb([128, 16, 128], BF16, "cwbf", st) for _ in range(3)])
        wfo = kb.sb([128, 44, 128], BF16, "cwfo", st)
        tmp = stat_tmps(kb, st)
        t1r = Ring([kb.sb([128, 512], F32, "t1", st) for _ in range(2)])
        t2r = Ring([kb.sb([128, 512], F32, "t2", st) for _ in range(2)])
        sgr = Ring([kb.sb([128, 512], F32, "sg", st) for _ in range(2)])
        u = kb.sb([128, KD, 512], BF16, "cu", st)
        z = kb.sb([128, KD, 512], F32, "cz", st)
        hb = kb.sb([128, 44, 512], BF16, "chb", st)
        mg = BufK(hb.h, "cmg", 0, 16)
        y = BufK(hb.h, "cy", 16, 16)
        acc = kb.sb([128, 512], F32, "cacc", st)
        xt = Ring([kb.sb([128, 512], F32, "cxt", st) for _ in range(2)])
        psr = Ring([P[2], P[3], P[4], P[5]])

        def wload(src, nk):
            wb = wbf.next()
            if PRECAST:
                kb.sp.dma(wb[:, :nk, :], src, writes=[wb])
                return wb
            s = wst.next()
            kb.sp.dma(s[:, :nk, :], src, writes=[s])
            kb.pool.op(lambda: nc.gpsimd.tensor_copy(out=wb[:, :nk, :], in_=s[:, :nk, :]), reads=[s], writes=[wb])
            return wb

        def mm(ps, wb, nk, rhs_buf, rhs_fn):
            for k in range(nk):
                kb.pe.op(lambda: nc.tensor.matmul(ps[:], lhsT=wb[:, k, :], rhs=rhs_fn(k), start=(k == 0), stop=(k == nk - 1)),
                         reads=[wb, rhs_buf], writes=[ps])

        def layer_norm_to(src, gcol, bcol, dst_fn, dst_buf, also=None):
            rstd, nmr = col_stats(kb, cx, [(src, src[:, k, :]) for k in range(KD)], D, True, tmp)
            for k in range(KD):
                t1 = t1r.next(); t2 = t2r.next()
                kb.dve.op(lambda: nc.vector.tensor_tensor(out=t1[:], in0=src[:, k, :], in1=rstd[:], op=ALU.mult),
                          reads=[src, rstd], writes=[t1])
                kb.pool.op(lambda: nc.gpsimd.tensor_tensor(out=t2[:], in0=t1[:], in1=nmr[:], op=ALU.add),
                           reads=[t1, nmr], writes=[t2])
                kb.act.op(lambda: nc.scalar.activation(out=dst_fn(k), in_=t2[:], func=AF.Identity,
                                                       scale=vec[:, gcol + k:gcol + k + 1], bias=vec[:, bcol + k:bcol + k + 1]),
                          reads=[t2, vec], writes=[dst_buf])

        def adaln_to(src, l_, isc, ish, dst):
            rstd, nmr = col_stats(kb, cx, [(src, src[:, k, :]) for k in range(KD)], D, True, tmp)
            for k in range(KD):
                t1 = t1r.next(); t2 = t2r.next()
                kb.dve.op(lambda: nc.vector.tensor_tensor(out=t1[:], in0=src[:, k, :], in1=rstd[:], op=ALU.mult),
                          reads=[src, rstd], writes=[t1])
                kb.pool.op(lambda: nc.gpsimd.tensor_tensor(out=t2[:], in0=t1[:], in1=nmr[:], op=ALU.add),
                           reads=[t1, nmr], writes=[t2])
                kb.act.op(lambda: nc.scalar.activation(out=dst[:, k, :], in_=t2[:], func=AF.Identity,
                                                       scale=cx.onep[:, mcol(l_, isc, k):mcol(l_, isc, k) + 1],
                                                       bias=cx.mod[:, mcol(l_, ish, k):mcol(l_, ish, k) + 1]),
                          reads=[t2, cx.onep, cx.mod], writes=[dst])

        for tt in range(4):
            ts = slice(tt * 512, (tt + 1) * 512)
            kb.barrier()
            kb.sp.dma(u[:], v3(uT_d)[:, :, ts], writes=[u])
            kb.sp.dma(y[:], v3(yT_d)[:, :, ts], writes=[y])
            for j in range(16):
                for i in range(4):
                    wg = wload(W["wg_ct"][i * 16 + j], KD)
                    wbr = wload(W["wb_ct"][i * 16 + j], 4)
                    pg = psr.next(); pb = psr.next()
                    mm(pg, wg, KD, u, lambda k: u[:, k, :])
                    mm(pb, wbr, 4, y, lambda k: y[:, i * 4 + k, :])
                    sg = sgr.next()
                    kb.act.op(lambda: nc.scalar.activation(out=sg[:], in_=pg[:], func=AF.Sigmoid,
                                                           bias=vec[:, i * 16 + j:i * 16 + j + 1], scale=1.0),
                              reads=[pg, vec], writes=[sg])
                    if i == 0:
                        kb.dve.op(lambda: nc.vector.tensor_tensor(out=acc[:], in0=pb[:], in1=sg[:], op=ALU.mult),
                                  reads=[pb, sg], writes=[acc])
                    else:
                        t1 = t1r.next()
                        kb.dve.op(lambda: nc.vector.tensor_tensor(out=t1[:], in0=pb[:], in1=sg[:], op=ALU.mult),
                                  reads=[pb, sg], writes=[t1])
                        if i < 3:
                            kb.pool.op(lambda: nc.gpsimd.tensor_tensor(out=acc[:], in0=acc[:], in1=t1[:], op=ALU.add),
                                       reads=[acc, t1], writes=[acc])
                        else:
                            kb.pool.op(lambda: nc.gpsimd.tensor_tensor(out=mg[:, j, :], in0=acc[:], in1=t1[:], op=ALU.add),
                                       reads=[acc, t1], writes=[mg])
            for j in range(16):
                wo = wload(W["wo_ct"][j], KD)
                ps = psr.next()
                mm(ps, wo, KD, mg, lambda k: mg[:, k, :])
                x_ = xt.next()
                kb.sp.dma(x_[:], xT[j * 128:(j + 1) * 128, ts], writes=[x_])
                t1 = t1r.next()
                kb.act.op(lambda: nc.scalar.activation(out=t1[:], in_=ps[:], func=AF.Copy,
                                                       scale=cx.onep[:, mcol(l, 2, j):mcol(l, 2, j) + 1]),
                          reads=[ps, cx.onep], writes=[t1])
                kb.dve.op(lambda: nc.vector.scalar_tensor_tensor(out=z[:, j, :], in0=x_[:], scalar=ALPHA, in1=t1[:],
                                                                 op0=ALU.mult, op1=ALU.add), reads=[x_, t1], writes=[z])
            layer_norm_to(z, 64, 80, lambda k: z[:, k, :], z)
            adaln_to(z, l, 4, 3, u)
            for j in range(44):
                wa = wload(W["wfi_ct"][j], KD)
                wg_ = wload(W["wfi_ct"][44 + j], KD)
                pa = psr.next(); pg = psr.next()
                mm(pa, wa, KD, u, lambda k: u[:, k, :])
                mm(pg, wg_, KD, u, lambda k: u[:, k, :])
                sg = sgr.next()
                kb.act.op(lambda: nc.scalar.activation(out=sg[:], in_=pa[:], func=AF.Silu), reads=[pa], writes=[sg])
                kb.dve.op(lambda: nc.vector.tensor_tensor(out=hb[:, j, :], in0=pg[:], in1=sg[:], op=ALU.mult),
                          reads=[pg, sg], writes=[hb])
            for j in range(16):
                if PRECAST:
                    kb.sp.dma(wfo[:], W["wfo_ct"][j], writes=[wfo])
                for (k0_, nk_) in (() if PRECAST else ((0, 16), (16, 16), (32, 12))):
                    s_ = wst.next()
                    kb.sp.dma(s_[:, :nk_, :], W["wfo_ct"][j][:, k0_:k0_ + nk_, :], writes=[s_])
                    kb.pool.op(lambda: nc.gpsimd.tensor_copy(out=wfo[:, k0_:k0_ + nk_, :], in_=s_[:, :nk_, :]),
                               reads=[s_], writes=[wfo])
                ps = psr.next()
                mm(ps, wfo, 44, hb, lambda k: hb[:, k, :])
                t1 = t1r.next()
                kb.act.op(lambda: nc.scalar.activation(out=t1[:], in_=ps[:], func=AF.Copy,
                                                       scale=cx.onep[:, mcol(l, 5, j):mcol(l, 5, j) + 1]),
                          reads=[ps, cx.onep], writes=[t1])
                kb.dve.op(lambda: nc.vector.scalar_tensor_tensor(out=z[:, j, :], in0=z[:, j, :], scalar=ALPHA, in1=t1[:],
                                                                 op0=ALU.mult, op1=ALU.add), reads=[z, t1], writes=[z])
            layer_norm_to(z, 96, 112, lambda k: z[:, k, :], z)
            kb.act.dma(v3(xoT)[:, :, ts], z[:], reads=[z])
        kb.barrier()


def _launch(nc, in_maps):
    res = run_bass_kernel_spmd(nc, in_maps, core_ids=list(range(8)))
    return res.results


def build_B(l):
    kb = KB()
    modT = kb.dram("modT", [128, DEPTH * 96], F32, kind="ExternalInput")
    A = {k: kb.dram(k, v[0], dts(v[1]), kind="ExternalInput") for k, v in A_OUT.items() if k != "uT"}
    PV = {k: kb.dram(k, v[0], dts(v[1]), kind="ExternalInput") for k, v in B_PREV.items()}
    W = {k: kb.dram(k, v, F32, kind="ExternalInput") for k, v in B_W.items()}
    C = {k: kb.dram(k, v, F32, kind="ExternalInput") for k, v in B_C.items()}
    yT = kb.dram("yT", [2048, TOK], BF16, kind="ExternalOutput")
    cx = setup_common(kb, modT)
    stage_B(kb, cx, l, A, PV, W, C, yT)
    kb.finish()
    return kb.nc


def build_C(l):
    kb = KB()
    modT = kb.dram("modT", [128, DEPTH * 96], F32, kind="ExternalInput")
    xT = kb.dram("xT", [D, TOK], F32, kind="ExternalInput")
    uT = kb.dram("uT", [D, TOK], BF16, kind="ExternalInput")
    yT = kb.dram("yT", [2048, TOK], BF16, kind="ExternalInput")
    W = {k: kb.dram(k, v, F32, kind="ExternalInput") for k, v in C_W.items()}
    xo = kb.dram("xo", [D, TOK], F32, kind="ExternalOutput")
    cx = setup_common(kb, modT)
    stage_C(kb, cx, l, xT, uT, yT, W, {}, xo)
    kb.finish()
    return kb.nc


def kernel_unfused(**inputs):
    inputs = {k: np.asarray(v) for k, v in inputs.items()}
    x = inputs["x"]
    mods = run_mods(inputs)
    xT = []
    for core in range(8):
        b, h = core // 2, core % 2
        xT.append(np.ascontiguousarray(x[b, h * TOK:(h + 1) * TOK, :].T))
    for l in range(DEPTH):
        wA = [host_A_weights(inputs, l, h) for h in range(2)]
        in_maps = []
        for core in range(8):
            b, h = core // 2, core % 2
            m = {"xT": xT[core], "modT": mods[b]}
            m.update(wA[h])
            in_maps.append(m)
        ra = _launch(build_A(l), in_maps)
        wB = host_B_weights(inputs, l)
        cB = [host_B_consts(h) for h in range(2)]
        in_maps = []
        pm = {"pdkT": "dkT", "pdV": "dV", "pikT": "ikT", "pknT": "knT", "pmV": "mV", "pkrT": "krT"}
        for core in range(8):
            b, h = core // 2, core % 2
            own = ra[core]
            m = {"modT": mods[b]}
            for k in A_OUT:
                if k != "uT":
                    m[k] = np.asarray(own[k])
            if h == 1:
                prev = ra[core - 1]
                for k, v in pm.items():
                    m[k] = np.asarray(prev[v])
                m["pz"] = np.ascontiguousarray(np.asarray(prev["zT"])[:, -32:])
                m["php"] = np.ascontiguousarray(np.asarray(prev["hpT"])[:, -16:])
            else:
                for k, v in pm.items():
                    m[k] = np.zeros_like(np.asarray(own[v]))
                m["pz"] = np.zeros_like(np.asarray(own["zT"])[:, -32:])
                m["php"] = np.zeros((512, 16), np.float32)
            m.update(wB)
            m.update(cB[h])
            in_maps.append(m)
        rb = _launch(build_B(l), in_maps)
        wC = host_C_weights(inputs, l)
        in_maps = []
        for core in range(8):
            b = core // 2
            m = {"modT": mods[b], "xT": xT[core], "uT": np.asarray(ra[core]["uT"]), "yT": np.asarray(rb[core]["yT"])}
            m.update(wC)
            in_maps.append(m)
        rc = _launch(build_C(l), in_maps)
        xT = [np.asarray(rc[core]["xo"]) for core in range(8)]
    out = np.empty((NB, SEQ, D), np.float32)
    for core in range(8):
        b, h = core // 2, core % 2
        out[b, h * TOK:(h + 1) * TOK, :] = xT[core].T
    return out


def stage_M(kb, cx, cT, wadas, baT):
    nc = kb.nc
    with ExitStack() as st:
        csb = kb.sb([128, KD, 4], F32, "csb", st)
        cact = kb.sb([128, KD, 4], F32, "cact", st)
        basb = kb.sb([128, DEPTH * 96], F32, "basb", st)
        wr = Ring([kb.sb([128, KD, 512], F32, "wst", st) for _ in range(2)])
        pr = Ring([cx.P[2], cx.P[3]])
        kb.sp.dma(csb[:], cT[:, :, :], writes=[csb])
        kb.sp.dma(basb[:], baT[:, :], writes=[basb])
        kb.act.op(lambda: nc.scalar.activation(out=cact[:], in_=csb[:], func=AF.Silu), reads=[csb], writes=[cact])
        for l in range(DEPTH):
            wav = wadas[l].rearrange("(k p) n -> p k n", p=128)
            for g in range(24):
                w = wr.next()
                kb.sp.dma(w[:], wav[:, :, g * 512:(g + 1) * 512], writes=[w])
                for j in range(4):
                    t = l * 96 + g * 4 + j
                    p = pr.next()
                    for k in range(KD):
                        kb.pe.op(lambda: nc.tensor.matmul(p[:, 0:4], lhsT=w[:, k, j * 128:(j + 1) * 128], rhs=cact[:, k, :],
                                                          start=(k == 0), stop=(k == KD - 1)),
                                 reads=[w, cact], writes=[p])
                    kb.dve.op(lambda: nc.vector.tensor_scalar(out=cx.mod[:, t:t + 1], in0=p[:, 0:1],
                                                              scalar1=basb[:, t:t + 1], scalar2=None, op0=ALU.add),
                              reads=[p, basb], writes=[cx.mod])
        kb.dve.op(lambda: nc.vector.tensor_scalar(out=cx.onep[:], in0=cx.mod[:], scalar1=1.0, scalar2=None, op0=ALU.add),
                  reads=[cx.mod], writes=[cx.onep])
        kb.barrier()


A_WS = {k: v for k, v in A_W.items() if k not in ("ropec", "ropes")}
ROPE = {"ropec": [64, TOK], "ropes": [64, TOK]}


def build_fused():
    global PRECAST
    PRECAST = True
    kb = KB()
    ext = lambda n, shp, dt=F32: kb.dram(n, shp, dt, kind="ExternalInput")
    xT = ext("xT", [D, SEQ])
    cT = ext("cT", [128, KD, 4])
    wadas = [ext(f"w_ada{l}", [D, 6 * D]) for l in range(DEPTH)]
    baT = ext("baT", [128, DEPTH * 96])
    z32 = ext("z32", [512, 32], BF16)
    z16 = ext("z16", [512, 16])
    rope = [{k: ext(f"{k}_h{h}", v) for k, v in ROPE.items()} for h in range(2)]
    BC = [{k: ext(f"{k}_h{h}", v) for k, v in B_C.items()} for h in range(2)]
    WA = [{k: ext(f"L{l}_{k}", v) for k, v in A_WS.items()} for l in range(DEPTH)]
    WB = [{k: ext(f"L{l}_{k}", v) for k, v in B_W.items()} for l in range(DEPTH)]
    WC = [{k: ext(f"L{l}_{k}", v) for k, v in C_W.items()} for l in range(DEPTH)]
    xo = kb.dram("xo", [D, SEQ], F32, kind="ExternalOutput")
    x1 = kb.dram("x1", [D, SEQ], F32)
    cx = setup_common(kb, None)
    stage_M(kb, cx, cT, wadas, baT)
    S = {k: kb.dram(f"S_{k}", v, F32) for k, v in A_SCR.items()}
    for l in range(DEPTH):
        xin = xT if l == 0 else x1
        xout = x1 if l == 0 else xo
        AO = [{k: kb.dram(f"A{l}{h}_{k}", v[0], dts(v[1])) for k, v in A_OUT.items()} for h in range(2)]
        yT = [kb.dram(f"y{l}{h}", [2048, TOK], BF16) for h in range(2)]
        for h in range(2):
            W = dict(WA[l])
            W.update(rope[h])
            stage_A(kb, cx, l, xin[:, h * TOK:(h + 1) * TOK], W, AO[h], S)
        for h in range(2):
            pm = {"pdkT": "dkT", "pdV": "dV", "pikT": "ikT", "pknT": "knT", "pmV": "mV", "pkrT": "krT"}
            PV = {k: AO[0][v] for k, v in pm.items()}
            if h == 1:
                PV["pz"] = AO[0]["zT"][:, TOK - 32:TOK]
                PV["php"] = AO[0]["hpT"][:, TOK - 16:TOK]
            else:
                PV["pz"] = z32
                PV["php"] = z16
            stage_B(kb, cx, l, AO[h], PV, WB[l], BC[h], yT[h], with_prev=(h == 1))
        WCb = precast_weights(kb, cx, WC[l], f"L{l}")
        for h in range(2):
            stage_C(kb, cx, l, xin[:, h * TOK:(h + 1) * TOK], AO[h]["uT"], yT[h], WCb, {},
                    xout[:, h * TOK:(h + 1) * TOK])
    kb.finish()
    return kb.nc


def kernel(**inputs):
    import ml_dtypes
    inputs = {k: np.asarray(v) for k, v in inputs.items()}
    x = inputs["x"]
    c = inputs["c"]
    shared = {"z32": np.zeros((512, 32), ml_dtypes.bfloat16), "z16": np.zeros((512, 16), np.float32)}
    for l in range(DEPTH):
        shared[f"w_ada{l}"] = np.ascontiguousarray(inputs["w_ada"][l])
    shared["baT"] = np.ascontiguousarray(np.concatenate([pk(inputs["b_ada"][l]) for l in range(DEPTH)], axis=1))
    for h in range(2):
        for k, v in host_B_consts(h, skip_prev=True).items():
            shared[f"{k}_h{h}"] = v
    for l in range(DEPTH):
        wa = [host_A_weights(inputs, l, h) for h in range(2)]
        for k in A_WS:
            shared[f"L{l}_{k}"] = wa[0][k]
        if l == 0:
            for h in range(2):
                for k in ROPE:
                    shared[f"{k}_h{h}"] = wa[h][k]
        for k, v in host_B_weights(inputs, l).items():
            shared[f"L{l}_{k}"] = v
        for k, v in host_C_weights(inputs, l).items():
            shared[f"L{l}_{k}"] = v
    in_maps = []
    for core in range(8):
        b = core // 2
        m = dict(shared)
        m["xT"] = np.ascontiguousarray(x[b].T)
        m["cT"] = np.ascontiguousarray(np.repeat(c[b].reshape(KD, 128).T[:, :, None], 4, axis=2))
        in_maps.append(m)
    res = run_bass_kernel_spmd(build_fused(), in_maps, core_ids=list(range(8)))
    out = np.empty((NB, SEQ, D), np.float32)
    for b in range(NB):
        out[b] = np.asarray(res.results[2 * b]["xo"]).T
    return out
```
